# Optimizing a Trainium2 kernel written in Bass

```python
import jax
import jax.numpy as jnp
from jax import lax
import numpy as np

D_MODEL = 2048
BATCH = 2
SEQ = 8192
DEPTH = 2

CTX_LEN = 256
GRID_W = 64

A_HEADS = 8
A_DV = D_MODEL // A_HEADS
A_DK = A_DV // 2
A_CHUNK = 64
F_BIAS = 3.0
B_WIDTH = D_MODEL
CONV_W = 3
C_HEADS = 16
C_DH = D_MODEL // C_HEADS
WIN_ROWS = 8
WIN_COLS = 16
PEER_HEADS = 8
PEER_NKEYS = 128
PEER_N = PEER_NKEYS * PEER_NKEYS
PEER_QDIM = 256
PEER_HALF = PEER_QDIM // 2
PEER_TOPK = 16
PEER_BLOCK = 128
N_BRANCH = 3
ROPE_BASE = 10000.0
EPS = 1e-6

PROJ_LAYOUT = (
    ('a_k', A_HEADS * A_DK),
    ('a_v', A_HEADS * A_DV),
    ('a_gates', 4 * A_HEADS),
    ('c_k', C_HEADS * C_DH),
    ('c_v', C_HEADS * C_DH),
    ('a_q', A_HEADS * A_DK),
    ('a_o', A_HEADS * A_DV),
    ('b_b', B_WIDTH),
    ('b_c', B_WIDTH),
    ('b_x', B_WIDTH),
    ('c_q', C_HEADS * C_DH),
    ('gate', N_BRANCH * D_MODEL),
)
KV_WIDTH = A_HEADS * A_DK + A_HEADS * A_DV + 4 * A_HEADS + 2 * C_HEADS * C_DH
PROJ_WIDTH = KV_WIDTH + A_HEADS * A_DK + A_HEADS * A_DV + 3 * B_WIDTH + C_HEADS * C_DH + N_BRANCH * D_MODEL

kernel_name = 'hybrid_mlstm_shortconv_natten_peer'


def split_cols(p):
    out, off = {}, 0
    for name, w in PROJ_LAYOUT:
        if off + w > p.shape[-1]:
            break
        out[name] = p[..., off:off + w]
        off += w
    return out


def rms_norm(t, g):
    tf = t.astype(jnp.float32)
    y = tf * lax.rsqrt(jnp.mean(tf * tf, axis=-1, keepdims=True) + EPS)
    return (y * g.astype(jnp.float32)).astype(t.dtype)


def modulate(t, shift, scale):
    return t * (1 + scale) + shift


def to_heads(t, n):
    b, s, _ = t.shape
    return t.reshape(b, s, n, -1).transpose(0, 2, 1, 3)


def to_c_heads(t):
    return t.reshape(t.shape[0], t.shape[1], C_HEADS, C_DH)


def flip_seq(t):
    return jnp.flip(t, axis=2)


def rope2d(t):
    s = t.shape[2]
    half = t.shape[-1] // 2
    nf = half // 2
    inv = ROPE_BASE ** (-jnp.arange(nf, dtype=jnp.float32) / nf)
    pos = jnp.arange(s)

    def rot(u, p):
        ang = p.astype(jnp.float32)[:, None] * inv
        cos, sin = jnp.cos(ang), jnp.sin(ang)
        u1, u2 = u[..., :nf], u[..., nf:]
        return jnp.concatenate([u1 * cos - u2 * sin, u2 * cos + u1 * sin], axis=-1)

    tf = t.astype(jnp.float32)
    out = jnp.concatenate([rot(tf[..., :half], pos // GRID_W), rot(tf[..., half:], pos % GRID_W)], axis=-1)
    return out.astype(t.dtype)


def mlstm_gates(gp, bias):
    b, s, _ = gp.shape
    g = gp.astype(jnp.float32).reshape(b, s, 4, A_HEADS) + bias.astype(jnp.float32)
    g = g.transpose(2, 0, 3, 1)
    return (g[0], jax.nn.log_sigmoid(g[1]), g[2], jax.nn.log_sigmoid(g[3]))


def mlstm_final_state(k, v, li, lf):
    kf, vf = k.astype(jnp.float32), v.astype(jnp.float32)
    cs = jnp.cumsum(lf, axis=-1)
    w = cs[..., -1:] - cs + li
    m = jnp.max(w, axis=-1)
    ke = kf * jnp.exp(w - m[..., None])[..., None]
    big_c = jnp.einsum('bhsd,bhse->bhde', ke, vf)
    n = jnp.sum(ke, axis=2)
    return (big_c, n, m)


def zero_state(bsz):
    return (jnp.zeros((bsz, A_HEADS, A_DK, A_DV), jnp.float32),
            jnp.zeros((bsz, A_HEADS, A_DK), jnp.float32),
            jnp.zeros((bsz, A_HEADS), jnp.float32))


def mlstm_chunkwise(q, k, v, li, lf, state):
    b, h, s, _ = q.shape
    nc = s // A_CHUNK

    def chunks(t):
        t = t.reshape(b, h, nc, A_CHUNK, *t.shape[3:])
        return jnp.moveaxis(t, 2, 0)

    tril = jnp.tril(jnp.ones((A_CHUNK, A_CHUNK), dtype=bool))

    def step(carry, xs):
        big_c, n, m = carry
        qc, kc, vc, lic, lfc = xs
        bc = jnp.cumsum(lfc, axis=-1)
        dlog = jnp.where(tril, bc[..., :, None] - bc[..., None, :] + lic[..., None, :], -jnp.inf)
        inter = bc + m[..., None]
        m_row = jnp.maximum(inter, jnp.max(dlog, axis=-1))
        w_inter = jnp.exp(inter - m_row)
        sc = jnp.einsum('bhtd,bhsd->bhts', qc, kc) * jnp.exp(dlog - m_row[..., None])
        num = w_inter[..., None] * jnp.einsum('bhtd,bhde->bhte', qc, big_c) + jnp.einsum('bhts,bhse->bhte', sc, vc)
        den = w_inter * jnp.einsum('bhtd,bhd->bht', qc, n) + jnp.sum(sc, axis=-1)
        hc = num / jnp.maximum(jnp.abs(den), jnp.exp(-m_row))[..., None]
        b_last = bc[..., -1]
        w_s = b_last[..., None] - bc + lic
        m_new = jnp.maximum(b_last + m, jnp.max(w_s, axis=-1))
        decay = jnp.exp(b_last + m - m_new)
        ke = kc * jnp.exp(w_s - m_new[..., None])[..., None]
        c_new = decay[..., None, None] * big_c + jnp.einsum('bhsd,bhse->bhde', ke, vc)
        n_new = decay[..., None] * n + jnp.sum(ke, axis=2)
        return (c_new, n_new, m_new), hc

    state, hs = lax.scan(step, state, (chunks(q), chunks(k), chunks(v), chunks(li), chunks(lf)))
    return jnp.moveaxis(hs, 0, 2).reshape(b, h, s, -1), state


def mlstm_mixer(q, k, v, o, gates, state_f, state_b, hnorm_g):
    q = q.astype(jnp.float32) * (A_DK ** -0.5)
    k = k.astype(jnp.float32)
    v = v.astype(jnp.float32)
    li_f, lf_f, li_b, lf_b = gates
    h_f, _ = mlstm_chunkwise(q, k, v, li_f, lf_f, state_f)
    h_b, _ = mlstm_chunkwise(flip_seq(q), flip_seq(k), flip_seq(v), flip_seq(li_b), flip_seq(lf_b), state_b)
    hs = h_f + flip_seq(h_b)
    hs = hs * lax.rsqrt(jnp.mean(hs * hs, axis=-1, keepdims=True) + EPS) * hnorm_g.astype(jnp.float32).reshape(A_HEADS, 1, A_DV)
    bsz, _, s, _ = hs.shape
    hs = hs.transpose(0, 2, 1, 3).reshape(bsz, s, A_HEADS * A_DV)
    return (jax.nn.sigmoid(o.astype(jnp.float32)) * hs).astype(o.dtype)


def short_conv_mixer(bg, cg, xin, w):
    u = cg * xin
    s = u.shape[1]
    pad = CONV_W // 2
    up = jnp.pad(u, ((0, 0), (pad, pad), (0, 0)))
    y = up[:, 0:s] * w[0]
    for j in range(1, CONV_W):
        y = y + up[:, j:j + s] * w[j]
    return bg * y


def neighbourhood_attention(q, k, v, k_ctx, v_ctx, rpb):
    bsz, s, nh, dh = q.shape
    rows = s // GRID_W
    wr = min(WIN_ROWS, rows)
    nloc = wr * WIN_COLS
    qg = (q.reshape(bsz, rows, GRID_W, nh, dh) * (dh ** -0.5)).transpose(1, 0, 2, 3, 4)
    kg = k.reshape(bsz, rows, GRID_W, nh, dh)
    vg = v.reshape(bsz, rows, GRID_W, nh, dh)
    col = np.arange(GRID_W)
    col0 = np.clip(col - WIN_COLS // 2, 0, GRID_W - WIN_COLS)
    col_idx = col0[:, None] + np.arange(WIN_COLS)
    dc_idx = col_idx - col[:, None] + (WIN_COLS - 1)

    def row_step(args):
        r, q_row = args
        r0 = jnp.clip(r - wr // 2, 0, rows - wr)
        k_win = lax.dynamic_slice_in_dim(kg, r0, wr, axis=1)[:, :, col_idx]
        v_win = lax.dynamic_slice_in_dim(vg, r0, wr, axis=1)[:, :, col_idx]
        dr_idx = r0 + jnp.arange(wr) - r + (WIN_ROWS - 1)
        bias = rpb[:, dr_idx[None, :, None], dc_idx[:, None, :]]
        s_loc = jnp.einsum('bqhd,brqwhd->bhqrw', q_row, k_win).astype(jnp.float32) + bias.astype(jnp.float32)
        s_ctx = jnp.einsum('bqhd,bchd->bhqc', q_row, k_ctx).astype(jnp.float32)
        sc = jnp.concatenate([s_loc.reshape(bsz, nh, GRID_W, nloc), s_ctx], axis=-1)
        p = jax.nn.softmax(sc, axis=-1).astype(v.dtype)
        p_loc = p[..., :nloc].reshape(bsz, nh, GRID_W, wr, WIN_COLS)
        p_ctx = p[..., nloc:]
        return jnp.einsum('bhqrw,brqwhd->bqhd', p_loc, v_win) + jnp.einsum('bhqc,bchd->bqhd', p_ctx, v_ctx)

    out = lax.map(row_step, (jnp.arange(rows), qg))
    return out.transpose(1, 0, 2, 3, 4).reshape(bsz, s, nh * dh)


def context_attention(q, k, v):
    sc = jnp.einsum('bqhd,bkhd->bhqk', q, k).astype(jnp.float32) * (q.shape[-1] ** -0.5)
    p = jax.nn.softmax(sc, axis=-1).astype(v.dtype)
    return jnp.einsum('bhqk,bkhd->bqhd', p, v).reshape(q.shape[0], q.shape[1], -1)


def merge_branches(gate_pre, ya, yb, yc, w_a, w_b, w_c, w_o):
    b, s, _ = gate_pre.shape
    g = jax.nn.sigmoid(gate_pre.astype(jnp.float32)).astype(ya.dtype).reshape(b, s, N_BRANCH, D_MODEL)
    y = g[:, :, 0] * (ya @ w_a) + g[:, :, 1] * (yb @ w_b) + g[:, :, 2] * (yc @ w_c)
    return y @ w_o


def peer_ffn(xn, wq, sub_keys, u, v):
    b, t, d = xn.shape
    xb = xn.reshape(-1, PEER_BLOCK, d)

    def block(xt):
        q = (xt @ wq).reshape(PEER_BLOCK, PEER_HEADS, 2, PEER_HALF)
        s = jnp.einsum('thpd,hpkd->thpk', q, sub_keys).astype(jnp.float32)
        s_top, i_top = lax.top_k(s, PEER_TOPK)
        cand = (s_top[:, :, 0, :, None] + s_top[:, :, 1, None, :]).reshape(PEER_BLOCK, PEER_HEADS, -1)
        cand_idx = (i_top[:, :, 0, :, None] * PEER_NKEYS + i_top[:, :, 1, None, :]).reshape(PEER_BLOCK, PEER_HEADS, -1)
        best, pos = lax.top_k(cand, PEER_TOPK)
        idx = jnp.take_along_axis(cand_idx, pos, axis=-1)
        g = jax.nn.softmax(best, axis=-1)
        act = jax.nn.gelu(jnp.einsum('td,thkd->thk', xt, u[idx]).astype(jnp.float32), approximate=False)
        return jnp.einsum('thk,thkd->td', (g * act).astype(xt.dtype), v[idx])

    return lax.map(block, xb).reshape(b, t, d)


def setup_inputs(seed: int = 0) -> dict:
    key = jax.random.key(seed)
    ks = jax.random.split(key, 21)
    nl, d = DEPTH, D_MODEL

    def nrm(k, shape, scale):
        return scale * jax.random.normal(k, shape, jnp.float32)

    gate_off = jnp.array([0.0, F_BIAS, 0.0, F_BIAS], jnp.float32)[None, :, None]
    return {
        'x': nrm(ks[0], (BATCH, SEQ, d), 1.0),
        'c': nrm(ks[1], (BATCH, d), 1.0),
        'ctx': nrm(ks[2], (BATCH, CTX_LEN, d), 1.0),
        'c_ctx': nrm(ks[3], (d,), 1.0),
        'w_ada': nrm(ks[4], (nl, d, 6 * d), 0.5 * d ** -0.5),
        'b_ada': nrm(ks[5], (nl, 6 * d), 0.02),
        'norm_g': 1.0 + nrm(ks[6], (nl, 2, d), 0.05),
        'w_in': nrm(ks[7], (nl, d, PROJ_WIDTH), d ** -0.5),
        'a_gate_b': gate_off + nrm(ks[8], (nl, 4, A_HEADS), 0.1),
        'a_hnorm_g': 1.0 + nrm(ks[9], (nl, A_HEADS * A_DV), 0.05),
        'b_conv': nrm(ks[10], (nl, CONV_W, B_WIDTH), CONV_W ** -0.5),
        'c_qk_g': 1.0 + nrm(ks[11], (nl, 2, C_DH), 0.05),
        'c_rpb': nrm(ks[12], (nl, C_HEADS, 2 * WIN_ROWS - 1, 2 * WIN_COLS - 1), 0.1),
        'w_a_out': nrm(ks[13], (nl, A_HEADS * A_DV, d), (A_HEADS * A_DV) ** -0.5),
        'w_b_out': nrm(ks[14], (nl, B_WIDTH, d), B_WIDTH ** -0.5),
        'w_c_out': nrm(ks[15], (nl, C_HEADS * C_DH, d), (C_HEADS * C_DH) ** -0.5),
        'w_out': nrm(ks[16], (nl, d, d), d ** -0.5),
        'peer_wq': nrm(ks[17], (nl, d, PEER_HEADS * PEER_QDIM), d ** -0.5),
        'peer_keys': nrm(ks[18], (nl, PEER_HEADS, 2, PEER_NKEYS, PEER_HALF), PEER_HALF ** -0.5),
        'peer_u': nrm(ks[19], (nl, PEER_N, d), d ** -0.5),
        'peer_v': nrm(ks[20], (nl, PEER_N, d), PEER_HEADS ** -0.5),
    }


def reference(x, c, ctx, c_ctx, w_ada, b_ada, norm_g, w_in, a_gate_b, a_hnorm_g, b_conv, c_qk_g, c_rpb,
              w_a_out, w_b_out, w_c_out, w_out, peer_wq, peer_keys, peer_u, peer_v):
    bsz = x.shape[0]
    h_ctx = ctx
    for l in range(DEPTH):
        last = l == DEPTH - 1
        mod_x = jnp.split((jax.nn.silu(c) @ w_ada[l] + b_ada[l])[:, None, :], 6, axis=-1)
        mod_c = jnp.split(jax.nn.silu(c_ctx) @ w_ada[l] + b_ada[l], 6, axis=-1)

        cn = modulate(rms_norm(h_ctx, norm_g[l, 0]), mod_c[0], mod_c[1])
        pc = split_cols(cn @ (w_in[l, :, :KV_WIDTH] if last else w_in[l]))
        ka_c = to_heads(pc['a_k'], A_HEADS)
        va_c = to_heads(pc['a_v'], A_HEADS)
        gates_c = mlstm_gates(pc['a_gates'], a_gate_b[l])
        state_f = mlstm_final_state(ka_c, va_c, gates_c[0], gates_c[1])
        state_b = mlstm_final_state(flip_seq(ka_c), flip_seq(va_c), flip_seq(gates_c[2]), flip_seq(gates_c[3]))
        kc_c = rms_norm(to_c_heads(pc['c_k']), c_qk_g[l, 1])
        vc_c = to_c_heads(pc['c_v'])

        xn = modulate(rms_norm(x, norm_g[l, 0]), mod_x[0], mod_x[1])
        p = split_cols(xn @ w_in[l])
        ya = mlstm_mixer(rope2d(to_heads(p['a_q'], A_HEADS)), rope2d(to_heads(p['a_k'], A_HEADS)),
                         to_heads(p['a_v'], A_HEADS), p['a_o'], mlstm_gates(p['a_gates'], a_gate_b[l]),
                         state_f, state_b, a_hnorm_g[l])
        yb = short_conv_mixer(p['b_b'], p['b_c'], p['b_x'], b_conv[l])
        yc = neighbourhood_attention(rms_norm(to_c_heads(p['c_q']), c_qk_g[l, 0]),
                                     rms_norm(to_c_heads(p['c_k']), c_qk_g[l, 1]),
                                     to_c_heads(p['c_v']), kc_c, vc_c, c_rpb[l])
        mix = merge_branches(p['gate'], ya, yb, yc, w_a_out[l], w_b_out[l], w_c_out[l], w_out[l])
        x_new = x + mod_x[2] * mix
        xn2 = modulate(rms_norm(x_new, norm_g[l, 1]), mod_x[3], mod_x[4])
        x_new = x_new + mod_x[5] * peer_ffn(xn2, peer_wq[l], peer_keys[l], peer_u[l], peer_v[l])

        if not last:
            ya_c = mlstm_mixer(to_heads(pc['a_q'], A_HEADS), ka_c, va_c, pc['a_o'], gates_c,
                               zero_state(bsz), zero_state(bsz), a_hnorm_g[l])
            yb_c = short_conv_mixer(pc['b_b'], pc['b_c'], pc['b_x'], b_conv[l])
            yc_c = context_attention(rms_norm(to_c_heads(pc['c_q']), c_qk_g[l, 0]), kc_c, vc_c)
            mix_c = merge_branches(pc['gate'], ya_c, yb_c, yc_c, w_a_out[l], w_b_out[l], w_c_out[l], w_out[l])
            hc = h_ctx + mod_c[2] * mix_c
            hcn = modulate(rms_norm(hc, norm_g[l, 1]), mod_c[3], mod_c[4])
            h_ctx = hc + mod_c[5] * peer_ffn(hcn, peer_wq[l], peer_keys[l], peer_u[l], peer_v[l])
        x = x_new
    return x
```

```python
import numpy as np
from contextlib import ExitStack
import concourse.bass as bass
import concourse.mybir as mybir
from concourse.bass_utils import run_bass_kernel_spmd

F32 = mybir.dt.float32
BF16 = mybir.dt.bfloat16
ALU = mybir.AluOpType
AF = mybir.ActivationFunctionType
AX = mybir.AxisListType

D = 2048
KC = 16
PW = 24608
EPS = 1e-6
ENGS = ["pe", "act", "dve", "pool", "sp"]
NDS = 24

O_AK, O_AV, O_AG, O_CK, O_CV, O_AQ, O_AO, O_BB, O_BC, O_BX, O_CQ, O_GT = (
    0, 1024, 3072, 3104, 5152, 7200, 8224, 10272, 12320, 14368, 16416, 18464)


class PSplit:
    BOUNDS = [0, 7200, 14368, 18464, PW]

    def __init__(self, aps):
        self.aps = aps

    def __getitem__(self, key):
        rows, cols = key
        for gi in range(4):
            lo, hi = self.BOUNDS[gi], self.BOUNDS[gi + 1]
            if cols.start >= lo and cols.stop <= hi:
                return self.aps[gi][rows, cols.start - lo:cols.stop - lo]
        raise ValueError("column range straddles projection groups: %s" % (cols,))


class G:
    def __init__(self, nc, es):
        self.nc = nc
        self.sem = {e: es.enter_context(nc.semaphore("s_" + e)) for e in ENGS}
        self.dsem = [es.enter_context(nc.semaphore("d%d" % i)) for i in range(NDS)]
        self.cnt = {e: 0 for e in ENGS}
        self.dcnt = [0] * NDS
        self.dnext = 0
        self.known = {e: {} for e in ENGS}
        self.nstage = 0
        self.ntens = 0

    def dram(self, name, shape, dt):
        return self.nc.dram_tensor(name, list(shape), dt)


class Stage:
    def __init__(self, g, name=None):
        self.g = g
        g.nstage += 1
        self.name = name or ("st%d" % g.nstage)
        self.ops = {e: [] for e in ENGS}
        self.lastw = {}
        self.readers = {}
        self.es = ExitStack()
        self.start_cnt = dict(g.cnt)
        self.start_d = list(g.dcnt)

    def sb(self, shape, dt=F32, name=None):
        self.g.ntens += 1
        return self.es.enter_context(self.g.nc.sbuf_tensor("%s_t%d" % (name or "sb", self.g.ntens), list(shape), dt))

    def ps(self, shape, dt=F32, name=None):
        self.g.ntens += 1
        return self.es.enter_context(self.g.nc.psum_tensor("%s_p%d" % (name or "ps", self.g.ntens), list(shape), dt))

    def add(self, eng, fn, rd=(), wr=(), dma=False):
        g = self.g
        deps = []
        for k in rd:
            t = self.lastw.get(k)
            if t is not None:
                deps.append(t)
        for k in wr:
            t = self.lastw.get(k)
            cands = ([t] if t is not None else []) + self.readers.get(k, [])
            for c in cands:
                if (not dma) and c[0] == "c" and c[1] == eng:
                    continue
                deps.append(c)
        if dma:
            j = g.dnext
            g.dnext = (j + 1) % NDS
            g.dcnt[j] += 1
            tok = ("d", j, g.dcnt[j])
            if g.dcnt[j] > 1:
                deps.append(("d", j, g.dcnt[j] - 1))
        else:
            g.cnt[eng] += 1
            tok = ("c", eng, g.cnt[eng])
        for k in wr:
            self.lastw[k] = tok
            self.readers[k] = []
        for k in rd:
            lst = self.readers.setdefault(k, [])
            if tok[0] == "c":
                lst[:] = [x for x in lst if not (x[0] == "c" and x[1] == tok[1])]
            lst.append(tok)
        self.ops[eng].append((fn, deps, tok))
        return tok

    def op(self, eng, meth, rd=(), wr=(), **kw):
        return self.add(eng, lambda e: getattr(e, meth)(**kw), rd, wr)

    def dma(self, eng, out, in_, rd=(), wr=(), **kw):
        return self.add(eng, lambda e: e.dma_start(out=out, in_=in_, **kw), rd, wr, dma=True)

    def emit(self):
        g = self.g
        nc = g.nc

        def run(ename, e):
            known = g.known[ename]

            def wait(tok):
                if tok[0] == "c":
                    sem, val, key = g.sem[tok[1]], tok[2], tok[1]
                else:
                    sem, val, key = g.dsem[tok[1]], 16 * tok[2], "d%d" % tok[1]
                if known.get(key, 0) < val:
                    e.wait_ge(sem, val)
                    known[key] = val

            for o in ENGS:
                if o != ename and self.start_cnt[o] > 0:
                    wait(("c", o, self.start_cnt[o]))
            for j in range(NDS):
                if self.start_d[j] > 0:
                    wait(("d", j, self.start_d[j]))
            for fn, deps, tok in self.ops[ename]:
                for d in deps:
                    wait(d)
                ins = fn(e)
                if tok[0] == "c":
                    ins.then_inc(g.sem[tok[1]], 1)
                else:
                    ins.then_inc(g.dsem[tok[1]], 16)

        with nc.Block() as block:
            if self.ops["pe"]:
                block.tensor(lambda e: run("pe", e))
            if self.ops["act"]:
                block.scalar(lambda e: run("act", e))
            if self.ops["dve"]:
                block.vector(lambda e: run("dve", e))
            if self.ops["pool"]:
                block.gpsimd(lambda e: run("pool", e))
            if self.ops["sp"]:
                block.sync(lambda e: run("sp", e))
        self.es.close()


def final_wait(g):
    st = Stage(g, "final")
    nc = g.nc

    def run(e):
        for o in ENGS:
            if o != "sp" and g.cnt[o] > 0:
                e.wait_ge(g.sem[o], g.cnt[o])
        for j in range(NDS):
            if g.dcnt[j] > 0:
                e.wait_ge(g.dsem[j], 16 * g.dcnt[j])

    with nc.Block() as block:
        block.sync(run)
    st.es.close()


def bcast_row(ap_row, nparts=128):
    return ap_row.partition_broadcast(nparts)


def stage_mod(g, cc, w_ada_l, b_ada_l, norm_g_l, modv):
    st = Stage(g, "mod")
    ccT = st.sb([128, 2, KC], F32, "ccT")
    sil = st.sb([128, 2, KC], F32, "sil")
    sig = st.sb([128, 2, KC], F32, "sig")
    NCH = 6 * D // 512
    wst = [st.sb([128, KC, 512], F32, "wada") for _ in range(2)]
    bb = [st.sb([2, 512], F32, "bada") for _ in range(2)]
    ng = [st.sb([2, 512], F32, "ng") for _ in range(2)]
    res = [st.sb([2, 512], F32, "res") for _ in range(2)]
    pst = [st.ps([128, 512], F32, "pmod") for _ in range(2)]
    for r in range(2):
        st.dma("sp", ccT[:, r, :], cc[r, :].rearrange("(k p) -> p k", p=128), wr=["ccT"], allow_slow_non_contiguous=True)
    st.add("act", lambda e: e.activation(out=sig[:], in_=ccT[:], func=AF.Sigmoid), rd=["ccT"], wr=["sig"])
    st.add("dve", lambda e: e.tensor_tensor(out=sil[:], in0=ccT[:], in1=sig[:], op=ALU.mult), rd=["ccT", "sig"], wr=["sil"])
    segmap = {0: 1, 1: 0, 2: 2, 3: 4, 4: 3, 5: 5}
    for n in range(NCH):
        i = n % 2
        w = wst[i]
        seg, cs = n // 4, (n % 4) * 512
        st.dma("sp", w[:], w_ada_l[:, n * 512:(n + 1) * 512].rearrange("(k p) c -> p k c", p=128), wr=["w%d" % i])
        for r in range(2):
            st.dma("sp", bb[i][r:r + 1, :], b_ada_l[:, n * 512:(n + 1) * 512], wr=["bb%d" % i])
            if seg in (1, 4):
                j = 0 if seg == 1 else 1
                st.dma("sp", ng[i][r:r + 1, :], norm_g_l[:, j * D + cs:j * D + cs + 512], wr=["ng%d" % i])
        for k in range(KC):
            st.add("pe", lambda e, w=w, k=k, i=i: e.matmul(pst[i][0:2, :], lhsT=sil[:, :, k], rhs=w[:, k, :],
                                                           start=(k == 0), stop=(k == KC - 1)),
                   rd=["sil", "w%d" % i], wr=["p%d" % i])
        st.add("dve", lambda e, i=i: e.tensor_tensor(out=res[i][:], in0=pst[i][0:2, :], in1=bb[i][:], op=ALU.add),
               rd=["p%d" % i, "bb%d" % i], wr=["res%d" % i])
        if seg in (1, 4):
            st.add("dve", lambda e, i=i: e.scalar_tensor_tensor(out=res[i][:], in0=res[i][:], scalar=1.0, in1=ng[i][:],
                                                                op0=ALU.add, op1=ALU.mult),
                   rd=["res%d" % i, "ng%d" % i], wr=["res%d" % i])
        o0 = segmap[seg] * D + cs
        st.dma("sp", modv[:, o0:o0 + 512], res[i][:], rd=["res%d" % i])
    st.emit()


def stage_norm(g, src, modv, jn, xnT, tiles, ident_bf):
    st = Stage(g, "norm")
    A = [st.sb([128, D], F32, "A") for _ in range(2)]
    B = [st.sb([128, D], F32, "B") for _ in range(2)]
    ident = st.sb([128, 128], BF16, "ident")
    st.dma("sp", ident[:], ident_bf, wr=["ident"])
    for r in range(2):
        st.dma("sp", A[r][:], bcast_row(modv[r:r + 1, (3 * jn) * D:(3 * jn + 1) * D]), wr=["A%d" % r])
        st.dma("sp", B[r][:], bcast_row(modv[r:r + 1, (3 * jn + 1) * D:(3 * jn + 2) * D]), wr=["B%d" % r])
    NB = 2
    xt = [st.sb([128, D], F32, "x") for _ in range(NB)]
    junk = [st.sb([128, D], F32, "junk") for _ in range(NB)]
    tmp = [st.sb([128, D], F32, "tmp") for _ in range(NB)]
    xb = [st.sb([128, D], BF16, "xb") for _ in range(NB)]
    xT = [st.sb([128, D], BF16, "xT") for _ in range(NB)]
    ss = [st.sb([128, 1], F32, "ss") for _ in range(NB)]
    rs = [st.sb([128, 1], F32, "rs") for _ in range(NB)]
    pt = [st.ps([128, D], BF16, "pT") for _ in range(NB)]
    for i, t in enumerate(tiles):
        b = i % NB
        r = 1 if t < 2 else 0
        st.dma("sp", xt[b][:], src[t * 128:(t + 1) * 128, :], wr=["x%d" % b])
        st.add("act", lambda e, b=b: e.activation(out=junk[b][:], in_=xt[b][:], func=AF.Square, accum_out=ss[b][:]),
               rd=["x%d" % b], wr=["junk%d" % b, "ss%d" % b])
        st.add("act", lambda e, b=b: e.activation(out=rs[b][:], in_=ss[b][:], func=AF.Sqrt, scale=1.0 / D, bias=EPS),
               rd=["ss%d" % b], wr=["rs%d" % b])
        st.add("dve", lambda e, b=b: e.reciprocal(out=rs[b][:], in_=rs[b][:]),
               rd=["rs%d" % b], wr=["rs%d" % b])
        st.add("dve", lambda e, b=b, r=r: e.scalar_tensor_tensor(out=tmp[b][:], in0=xt[b][:], scalar=rs[b][:], in1=A[r][:],
                                                                 op0=ALU.mult, op1=ALU.mult),
               rd=["x%d" % b, "rs%d" % b, "A%d" % r], wr=["tmp%d" % b])
        st.add("pool", lambda e, b=b, r=r: e.tensor_tensor(out=xb[b][:], in0=tmp[b][:], in1=B[r][:], op=ALU.add),
               rd=["tmp%d" % b, "B%d" % r], wr=["xb%d" % b])
        for k in range(KC):
            st.add("pe", lambda e, b=b, k=k: e.transpose(out=pt[b][:, k * 128:(k + 1) * 128], in_=xb[b][:, k * 128:(k + 1) * 128], identity=ident[:]),
                   rd=["xb%d" % b, "ident"], wr=["pt%d" % b])
        st.add("act", lambda e, b=b: e.copy(out=xT[b][:, 0:1024], in_=pt[b][:, 0:1024]), rd=["pt%d" % b], wr=["xTa%d" % b])
        st.add("dve", lambda e, b=b: e.tensor_copy(out=xT[b][:, 1024:2048], in_=pt[b][:, 1024:2048]), rd=["pt%d" % b], wr=["xTb%d" % b])
        st.dma("pool", xnT[t], xT[b][:], rd=["xTa%d" % b, "xTb%d" % b])
    st.emit()


def stage_linear(g, xT, tiles, W, N, out, oc0=0, w_bf=False, slab=1024):
    st = Stage(g, "lin")
    wst = [st.sb([128, KC, 512], F32, "wst") for _ in range(2)] if not w_bf else None
    wsl = [st.sb([128, KC, slab], BF16, "wsl") for _ in range(2)]
    NX = 3
    xt = [st.sb([128, D], BF16, "xt") for _ in range(NX)]
    ob = [st.sb([128, slab], F32, "ob") for _ in range(2)]
    pp = [st.ps([128, 512], F32, "pl") for _ in range(4)]
    nsl = (N + slab - 1) // slab
    cnt = 0
    hcnt = 0
    pcnt = 0
    for s in range(nsl):
        c0 = s * slab
        ns = min(slab, N - c0)
        w = wsl[s % 2]
        wk = "wsl%d" % (s % 2)
        for h0 in range(0, ns, 512):
            hw = min(512, ns - h0)
            src = W[:, c0 + h0:c0 + h0 + hw].rearrange("(k p) c -> p k c", p=128)
            if w_bf:
                st.dma("sp", w[:, :, h0:h0 + hw], src, wr=[wk + "_%d" % h0])
            else:
                ws = wst[hcnt % 2]
                wsk = "wst%d" % (hcnt % 2)
                st.dma("sp", ws[:, :, 0:hw], src, wr=[wsk])
                eng = "pool" if hcnt % 2 == 0 else "dve"
                st.add(eng, lambda e, w=w, ws=ws, h0=h0, hw=hw: e.tensor_copy(out=w[:, :, h0:h0 + hw], in_=ws[:, :, 0:hw]),
                       rd=[wsk], wr=[wk + "_%d" % h0])
                hcnt += 1
        for t in tiles:
            xb = cnt % NX
            o = ob[cnt % 2]
            ok = "ob%d" % (cnt % 2)
            st.dma("sp", xt[xb][:], xT[t], wr=["xt%d" % xb])
            for h0 in range(0, ns, 512):
                hw = min(512, ns - h0)
                p = pp[pcnt % 4]
                pk = "pp%d" % (pcnt % 4)
                for k in range(KC):
                    st.add("pe", lambda e, p=p, xb=xb, k=k, w=w, h0=h0, hw=hw: e.matmul(
                        p[:, 0:hw], lhsT=xt[xb][:, k * 128:(k + 1) * 128], rhs=w[:, k, h0:h0 + hw], start=(k == 0), stop=(k == KC - 1)),
                        rd=["xt%d" % xb, wk + "_%d" % h0], wr=[pk])
                if pcnt % 2 == 0:
                    st.add("act", lambda e, o=o, p=p, h0=h0, hw=hw: e.copy(out=o[:, h0:h0 + hw], in_=p[:, 0:hw]), rd=[pk], wr=[ok + "_%d" % h0])
                else:
                    st.add("dve", lambda e, o=o, p=p, h0=h0, hw=hw: e.tensor_copy(out=o[:, h0:h0 + hw], in_=p[:, 0:hw]), rd=[pk], wr=[ok + "_%d" % h0])
                pcnt += 1
            st.dma("pool", out[t * 128:(t + 1) * 128, oc0 + c0:oc0 + c0 + ns], o[:, 0:ns],
                   rd=[ok + "_%d" % h0 for h0 in range(0, ns, 512)])
            cnt += 1
    st.emit()


def bc3(ap2d, n):
    return ap2d.unsqueeze(2).broadcast_to([ap2d.shape[0], ap2d.shape[1], n])


def bcmid(ap2d, n):
    return ap2d.unsqueeze(1).broadcast_to([ap2d.shape[0], n, ap2d.shape[1]])


class TrOut:
    def __init__(self, st, ident, nb=2):
        self.st = st
        self.ident = ident
        self.nb = nb
        self.pt = [st.ps([128, D], BF16, "trp") for _ in range(nb)]
        self.xT = [st.sb([128, D], BF16, "trx") for _ in range(nb)]
        self.i = 0

    def go(self, src, srckeys, dst):
        st = self.st
        b = self.i % self.nb
        self.i += 1
        pt, xT, ident = self.pt[b], self.xT[b], self.ident
        for k in range(KC):
            st.add("pe", lambda e, k=k: e.transpose(out=pt[:, k * 128:(k + 1) * 128], in_=src[:, k * 128:(k + 1) * 128], identity=ident[:]),
                   rd=list(srckeys) + ["ident"], wr=["trp%d" % b])
        st.add("act", lambda e: e.copy(out=xT[:, 0:1024], in_=pt[:, 0:1024]), rd=["trp%d" % b], wr=["trxa%d" % b])
        st.add("dve", lambda e: e.tensor_copy(out=xT[:, 1024:2048], in_=pt[:, 1024:2048]), rd=["trp%d" % b], wr=["trxb%d" % b])
        st.dma("pool", dst, xT[:], rd=["trxa%d" % b, "trxb%d" % b])


def stage_mlstm_prep(g, p, tiles, cst, ident_bf, gate_b_l, rope, qT_s, kT_s, kb_s, v2f_s, v2b_s, gpk_s):
    st = Stage(g, "mprep")
    SC = 128.0 ** -0.5
    cs = st.sb([128, 512], F32, "cst")
    ident = st.sb([128, 128], BF16, "ident")
    gbias = st.sb([128, 32], F32, "gbias")
    st.dma("sp", cs[:], cst, wr=["cst"])
    st.dma("sp", ident[:], ident_bf, wr=["ident"])
    st.dma("sp", gbias[:], bcast_row(gate_b_l), wr=["gbias"])
    MF, MB, ONES = cs[:, 0:128], cs[:, 128:256], cs[:, 256:384]
    NB = 2
    q = [st.sb([128, 1024], F32, "q") for _ in range(NB)]
    k = [st.sb([128, 1024], F32, "k") for _ in range(NB)]
    v = [st.sb([128, 2048], F32, "v") for _ in range(NB)]
    gp = [st.sb([128, 32], F32, "gp") for _ in range(NB)]
    cq = [st.sb([128, 128], F32, "cq") for _ in range(NB)]
    sq = [st.sb([128, 128], F32, "sq") for _ in range(NB)]
    ck = [st.sb([128, 128], F32, "ck") for _ in range(NB)]
    sk = [st.sb([128, 128], F32, "sk") for _ in range(NB)]
    t1d = {n: [st.sb([128, 1024], F32, "t1" + n) for _ in range(NB)] for n in ("q", "k")}
    t2d = {n: [st.sb([128, 1024], F32, "t2" + n) for _ in range(NB)] for n in ("q", "k")}
    qb = [st.sb([128, 1024], BF16, "qb") for _ in range(NB)]
    kb = [st.sb([128, 1024], BF16, "kb") for _ in range(NB)]
    qTs = [st.sb([128, 1024], BF16, "qTs") for _ in range(NB)]
    kTs = [st.sb([128, 1024], BF16, "kTs") for _ in range(NB)]
    v2f = [st.sb([128, 8, 257], BF16, "v2f") for _ in range(NB)]
    v2b = [st.sb([128, 8, 257], BF16, "v2b") for _ in range(NB)]
    lf = [st.sb([128, 16], F32, "lf") for _ in range(NB)]
    gk = [st.sb([128, 48], F32, "gk") for _ in range(NB)]
    tmp = [st.sb([128, 16], F32, "tmpg") for _ in range(NB)]
    pq = [st.ps([128, 1024], BF16, "pq") for _ in range(NB)]
    pk = [st.ps([128, 1024], BF16, "pk") for _ in range(NB)]
    pg = [st.ps([128, 32], F32, "pg") for _ in range(NB)]
    cosq, sinq, cosk, sink = rope
    for i, t in enumerate(tiles):
        b = i % NB
        B = "%d" % b
        rows = slice(t * 128, (t + 1) * 128)
        st.dma("sp", q[b][:], p[rows, O_AQ:O_AQ + 1024], wr=["q" + B])
        st.dma("sp", k[b][:], p[rows, O_AK:O_AK + 1024], wr=["k" + B])
        st.dma("sp", v[b][:], p[rows, O_AV:O_AV + 2048], wr=["v" + B])
        st.dma("sp", gp[b][:], p[rows, O_AG:O_AG + 32], wr=["gp" + B])
        if t >= 2:
            lr = slice((t - 2) * 128, (t - 1) * 128)
            st.dma("sp", cq[b][:], cosq[lr, :], wr=["cq" + B])
            st.dma("sp", sq[b][:], sinq[lr, :], wr=["sq" + B])
            st.dma("sp", ck[b][:], cosk[lr, :], wr=["ck" + B])
            st.dma("sp", sk[b][:], sink[lr, :], wr=["sk" + B])
            for (src, cc_, ss_, dst, nm, eng) in ((q, cq, sq, qb, "q", "dve"), (k, ck, sk, kb, "k", "pool")):
                s4 = src[b][:].rearrange("p (h f u j) -> p h f u j", h=8, f=2, u=2, j=32)
                t1v, t2v = t1d[nm][b], t2d[nm][b]
                d4 = t2v[:].rearrange("p (h f u j) -> p h f u j", h=8, f=2, u=2, j=32)
                nsn = ss_[b][:, 0:64].rearrange("p (f j) -> p f j", f=2).unsqueeze(1).broadcast_to([128, 8, 2, 32])
                psn = ss_[b][:, 64:128].rearrange("p (f j) -> p f j", f=2).unsqueeze(1).broadcast_to([128, 8, 2, 32])
                tk1, tk2 = "t1" + nm + B, "t2" + nm + B
                st.op(eng, "tensor_tensor", rd=[nm + B, "c" + nm + B], wr=[tk1],
                      out=t1v[:].rearrange("p (h d) -> p h d", h=8), in0=src[b][:].rearrange("p (h d) -> p h d", h=8),
                      in1=bcmid(cc_[b][:], 8), op=ALU.mult)
                st.op(eng, "tensor_tensor", rd=[nm + B, "s" + nm + B], wr=[tk2],
                      out=d4[:, :, :, 0, :], in0=s4[:, :, :, 1, :], in1=nsn, op=ALU.mult)
                st.op(eng, "tensor_tensor", rd=[nm + B, "s" + nm + B], wr=[tk2],
                      out=d4[:, :, :, 1, :], in0=s4[:, :, :, 0, :], in1=psn, op=ALU.mult)
                st.op(eng, "tensor_tensor", rd=[tk1, tk2], wr=[nm + "b" + B], out=dst[b][:], in0=t1v[:], in1=t2v[:], op=ALU.add)
        else:
            st.op("act", "mul", rd=["q" + B], wr=["qb" + B], out=qb[b][:], in_=q[b][:], mul=SC)
            st.op("dve", "tensor_copy", rd=["k" + B], wr=["kb" + B], out=kb[b][:], in_=k[b][:])
        for h in range(8):
            st.op("pe", "transpose", rd=["qb" + B, "ident"], wr=["pq" + B],
                  out=pq[b][:, h * 128:(h + 1) * 128], in_=qb[b][:, h * 128:(h + 1) * 128], identity=ident[:])
        st.op("act", "copy", rd=["pq" + B], wr=["qTs" + B], out=qTs[b][:], in_=pq[b][:])
        for h in range(8):
            st.op("pe", "transpose", rd=["kb" + B, "ident"], wr=["pk" + B],
                  out=pk[b][:, h * 128:(h + 1) * 128], in_=kb[b][:, h * 128:(h + 1) * 128], identity=ident[:])
        st.op("dve", "tensor_copy", rd=["pk" + B], wr=["kTs" + B], out=kTs[b][:], in_=pk[b][:])
        st.dma("pool", qT_s[t], qTs[b][:], rd=["qTs" + B])
        st.dma("pool", kT_s[t], kTs[b][:], rd=["kTs" + B])
        st.dma("pool", kb_s[rows, :], kb[b][:], rd=["kb" + B])
        st.op("dve", "tensor_tensor", rd=["gp" + B, "gbias"], wr=["gp" + B], out=gp[b][:], in0=gp[b][:], in1=gbias[:], op=ALU.add)
        st.op("act", "activation", rd=["gp" + B], wr=["lf" + B], out=lf[b][:, 0:8], in_=gp[b][:, 8:16], func=AF.Exp, scale=-1.0)
        st.op("act", "activation", rd=["gp" + B], wr=["lf" + B], out=lf[b][:, 8:16], in_=gp[b][:, 24:32], func=AF.Exp, scale=-1.0)
        st.op("act", "activation", rd=["lf" + B], wr=["lf" + B], out=lf[b][:], in_=lf[b][:], func=AF.Ln, bias=1.0)
        st.op("dve", "tensor_scalar", rd=["lf" + B], wr=["lf" + B], out=lf[b][:], in0=lf[b][:], scalar1=-1.0, scalar2=None, op0=ALU.mult)
        st.op("pe", "matmul", rd=["lf" + B, "cst"], wr=["pg" + B], out=pg[b][:, 0:8], lhsT=MF, rhs=lf[b][:, 0:8], start=True, stop=True)
        st.op("pe", "matmul", rd=["lf" + B, "cst"], wr=["pg" + B], out=pg[b][:, 8:16], lhsT=MB, rhs=lf[b][:, 8:16], start=True, stop=True)
        st.op("pe", "matmul", rd=["lf" + B, "cst"], wr=["pg" + B], out=pg[b][:, 16:32], lhsT=ONES, rhs=lf[b][:, 0:16], start=True, stop=True)
        st.op("dve", "tensor_tensor", rd=["gp" + B, "pg" + B], wr=["tmp" + B], out=tmp[b][:, 0:8], in0=gp[b][:, 0:8], in1=pg[b][:, 0:8], op=ALU.subtract)
        st.op("dve", "tensor_tensor", rd=["gp" + B, "pg" + B], wr=["tmp" + B], out=tmp[b][:, 8:16], in0=gp[b][:, 16:24], in1=pg[b][:, 8:16], op=ALU.subtract)
        st.op("act", "activation", rd=["tmp" + B], wr=["gk" + B], out=gk[b][:, 0:8], in_=tmp[b][:, 0:8], func=AF.Exp)
        st.op("act", "activation", rd=["tmp" + B], wr=["gk" + B], out=gk[b][:, 16:24], in_=tmp[b][:, 8:16], func=AF.Exp)
        st.op("act", "activation", rd=["pg" + B], wr=["gk" + B], out=gk[b][:, 8:16], in_=pg[b][:, 0:8], func=AF.Exp)
        st.op("act", "activation", rd=["pg" + B], wr=["gk" + B], out=gk[b][:, 24:32], in_=pg[b][:, 8:16], func=AF.Exp)
        st.op("act", "activation", rd=["pg" + B], wr=["gk" + B], out=gk[b][:, 32:48], in_=pg[b][:, 16:32], func=AF.Exp)
        st.dma("pool", gpk_s[rows, :], gk[b][:], rd=["gk" + B])
        v3 = v[b][:].rearrange("p (h d) -> p h d", h=8)
        st.op("dve", "tensor_tensor", rd=["v" + B, "gk" + B], wr=["v2f" + B], out=v2f[b][:, :, 0:256], in0=v3, in1=bc3(gk[b][:, 0:8], 256), op=ALU.mult)
        st.op("pool", "tensor_tensor", rd=["v" + B, "gk" + B], wr=["v2b" + B], out=v2b[b][:, :, 0:256], in0=v3, in1=bc3(gk[b][:, 16:24], 256), op=ALU.mult)
        st.op("dve", "tensor_copy", rd=["gk" + B], wr=["v2f" + B], out=v2f[b][:, :, 256], in_=gk[b][:, 0:8])
        st.op("pool", "tensor_copy", rd=["gk" + B], wr=["v2b" + B], out=v2b[b][:, :, 256], in_=gk[b][:, 16:24])
        st.dma("pool", v2f_s[rows, :], v2f[b][:].rearrange("p h d -> p (h d)"), rd=["v2f" + B])
        st.dma("pool", v2b_s[rows, :], v2b[b][:].rearrange("p h d -> p (h d)"), rd=["v2b" + B])
    st.emit()


def stage_mlstm_scan(g, bwd, order, cst, ident_bf, qT_s, kT_s, kb_s, v2_s, gpk_s, hf_s, p=None, hnorm_l=None, yaT=None, skip_out=()):
    st = Stage(g, "mscan")
    cs = st.sb([128, 512], F32, "cst")
    st.dma("sp", cs[:], cst, wr=["cst"])
    MASK = cs[:, 128:256] if bwd else cs[:, 0:128]
    o_ebc = 24 if bwd else 8
    o_eB = 40 if bwd else 32
    C32 = st.sb([128, 8, 257], F32, "C32")
    Cb = st.sb([128, 8, 257], BF16, "Cb")
    st.op("dve", "memset", wr=["C32_%d" % h for h in range(8)], ap=C32[:], constant=0.0)
    st.op("pool", "memset", wr=["Cb_%d" % h for h in range(8)], ap=Cb[:], constant=0.0)
    NB = 2
    qT = [st.sb([128, 1024], BF16, "qT") for _ in range(NB)]
    kT = [st.sb([128, 1024], BF16, "kT") for _ in range(NB)]
    kb = [st.sb([128, 1024], BF16, "kb") for _ in range(NB)]
    v2 = [st.sb([128, 8, 257], BF16, "v2") for _ in range(NB)]
    gk = [st.sb([128, 48], F32, "gk") for _ in range(NB)]
    hb = [st.sb([128, 2048], F32, "hb") for _ in range(NB)]
    sm = [st.sb([128, 128], BF16, "sm") for _ in range(2)]
    dd = [st.sb([128, 4], F32, "dd") for _ in range(2)]
    ps_s = [st.ps([128, 512], F32, "ps_s") for _ in range(2)]
    ps_o = [st.ps([128, 512], F32, "ps_o") for _ in range(2)]
    ps_c = [st.ps([128, 512], F32, "ps_c") for _ in range(2)]
    if bwd:
        ident = st.sb([128, 128], BF16, "ident")
        st.dma("sp", ident[:], ident_bf, wr=["ident"])
        HN = st.sb([128, 2048], F32, "HN")
        st.dma("sp", HN[:], bcast_row(hnorm_l), wr=["HN"])
        hf = st.sb([128, 2048], F32, "hf")
        og = st.sb([128, 2048], F32, "og")
        sq = st.sb([128, 2048], F32, "sq")
        yab = st.sb([128, 2048], BF16, "yab")
        ms = st.sb([128, 8], F32, "ms")
        tr = TrOut(st, ident, nb=1)
    hc = 0
    for i, t in enumerate(order):
        b = i % NB
        B = "%d" % b
        rows = slice(t * 128, (t + 1) * 128)
        st.dma("sp", qT[b][:], qT_s[t], wr=["qT" + B])
        st.dma("sp", kT[b][:], kT_s[t], wr=["kT" + B])
        st.dma("sp", kb[b][:], kb_s[rows, :], wr=["kb" + B])
        st.dma("sp", v2[b][:].rearrange("p h d -> p (h d)"), v2_s[rows, :], wr=["v2" + B])
        st.dma("sp", gk[b][:], gpk_s[rows, :], wr=["gk" + B])
        for h in range(8):
            j = hc % 2
            J = "%d" % j
            hc += 1
            hs = slice(h * 128, (h + 1) * 128)
            st.op("pe", "matmul", rd=["kT" + B, "qT" + B], wr=["ps_s" + J], out=ps_s[j][:, 0:128], lhsT=kT[b][:, hs], rhs=qT[b][:, hs], start=True, stop=True)
            st.op("dve", "tensor_tensor", rd=["ps_s" + J, "cst"], wr=["sm" + J], out=sm[j][:], in0=ps_s[j][:, 0:128], in1=MASK, op=ALU.mult)
            st.op("pe", "matmul", rd=["qT" + B, "Cb_%d" % h], wr=["ps_o" + J], out=ps_o[j][:, 0:257], lhsT=qT[b][:, hs], rhs=Cb[:, h, :], start=True, stop=False)
            st.op("pe", "matmul", rd=["sm" + J, "v2" + B], wr=["ps_o" + J], out=ps_o[j][:, 0:257], lhsT=sm[j][:], rhs=v2[b][:, h, :], start=False, stop=True)
            ebc = gk[b][:, o_ebc + h:o_ebc + h + 1]
            st.op("dve", "tensor_scalar", rd=["ps_o" + J, "gk" + B], wr=["dd" + J], out=dd[j][:, 3:4], in0=ps_o[j][:, 256:257], scalar1=ebc, scalar2=-1.0,
                  op0=ALU.mult, op1=ALU.mult)
            st.op("dve", "tensor_scalar", rd=["ps_o" + J, "gk" + B], wr=["dd" + J], out=dd[j][:, 0:1], in0=ps_o[j][:, 256:257], scalar1=ebc, scalar2=1.0,
                  op0=ALU.mult, op1=ALU.max)
            st.op("dve", "tensor_tensor", rd=["dd" + J], wr=["dd" + J], out=dd[j][:, 0:1], in0=dd[j][:, 0:1], in1=dd[j][:, 3:4], op=ALU.max)
            st.op("dve", "reciprocal", rd=["dd" + J], wr=["dd" + J], out=dd[j][:, 1:2], in_=dd[j][:, 0:1])
            st.op("dve", "tensor_tensor", rd=["dd" + J, "gk" + B], wr=["dd" + J], out=dd[j][:, 2:3], in0=dd[j][:, 1:2], in1=ebc, op=ALU.mult)
            st.op("act", "activation", rd=["ps_o" + J, "dd" + J], wr=["hb" + B], out=hb[b][:, h * 256:(h + 1) * 256], in_=ps_o[j][:, 0:256],
                  func=AF.Identity, scale=dd[j][:, 2:3])
            st.op("pe", "matmul", rd=["kb" + B, "v2" + B], wr=["ps_c" + J], out=ps_c[j][:, 0:257], lhsT=kb[b][:, hs], rhs=v2[b][:, h, :], start=True, stop=True)
            st.op("dve", "tensor_tensor", rd=["ps_c" + J, "C32_%d" % h], wr=["C32_%d" % h], out=C32[:, h, :], in0=ps_c[j][:, 0:257], in1=C32[:, h, :], op=ALU.add)
            st.op("pool", "tensor_scalar", rd=["C32_%d" % h, "gk" + B], wr=["C32_%d" % h], out=C32[:, h, :], in0=C32[:, h, :],
                  scalar1=gk[b][:, o_eB + h:o_eB + h + 1], scalar2=None, op0=ALU.mult)
            st.op("act", "copy", rd=["C32_%d" % h], wr=["Cb_%d" % h], out=Cb[:, h, :], in_=C32[:, h, :])
        if not bwd:
            st.dma("pool", hf_s[rows, :], hb[b][:], rd=["hb" + B])
        elif t not in skip_out:
            st.dma("sp", hf[:], hf_s[rows, :], wr=["hf"])
            st.dma("sp", og[:], p[rows, O_AO:O_AO + 2048], wr=["og"])
            st.op("dve", "tensor_tensor", rd=["hb" + B, "hf"], wr=["hf"], out=hf[:], in0=hb[b][:], in1=hf[:], op=ALU.add)
            st.op("act", "activation", rd=["hf"], wr=["sq"], out=sq[:], in_=hf[:], func=AF.Square)
            st.op("dve", "tensor_reduce", rd=["sq"], wr=["ms"], out=ms[:], in_=sq[:].rearrange("p (h d) -> p h d", h=8), axis=AX.X, op=ALU.add)
            st.op("act", "activation", rd=["ms"], wr=["ms"], out=ms[:], in_=ms[:], func=AF.Sqrt, scale=1.0 / 256, bias=EPS)
            st.op("dve", "reciprocal", rd=["ms"], wr=["ms"], out=ms[:], in_=ms[:])
            st.op("pool", "tensor_tensor", rd=["hf", "ms"], wr=["hf"], out=hf[:].rearrange("p (h d) -> p h d", h=8),
                  in0=hf[:].rearrange("p (h d) -> p h d", h=8), in1=bc3(ms[:], 256), op=ALU.mult)
            st.op("act", "activation", rd=["og"], wr=["og"], out=og[:], in_=og[:], func=AF.Sigmoid)
            st.op("dve", "tensor_tensor", rd=["hf", "HN"], wr=["hf"], out=hf[:], in0=hf[:], in1=HN[:], op=ALU.mult)
            st.op("pool", "tensor_tensor", rd=["hf", "og"], wr=["yab"], out=yab[:], in0=hf[:], in1=og[:], op=ALU.mult)
            tr.go(yab, ["yab"], yaT[t])
    st.emit()


def host_consts(n_lat_tokens):
    import ml_dtypes
    s = np.arange(128)
    MF = (s[:, None] <= s[None, :]).astype(np.float32)
    MB = (s[:, None] >= s[None, :]).astype(np.float32)
    cst = np.concatenate([MF, MB, np.ones((128, 128), np.float32), np.eye(128, dtype=np.float32)], axis=1)
    ident_bf = np.eye(128).astype(ml_dtypes.bfloat16)
    nf = 32
    inv = (10000.0 ** (-np.arange(nf, dtype=np.float32) / nf)).astype(np.float32)
    pos = np.arange(n_lat_tokens)
    ang_r = (pos // 64).astype(np.float32)[:, None] * inv
    ang_c = (pos % 64).astype(np.float32)[:, None] * inv
    cr, sr, cc_, sc_ = np.cos(ang_r), np.sin(ang_r), np.cos(ang_c), np.sin(ang_c)
    cosf = np.concatenate([cr, cr, cc_, cc_], axis=1).astype(np.float32)
    sinf = np.concatenate([-sr, -sc_, sr, sc_], axis=1).astype(np.float32)
    SC = np.float32(128.0 ** -0.5)
    return dict(cst=cst, ident_bf=ident_bf, cosq=cosf * SC, sinq=sinf * SC, cosk=cosf, sink=sinf)


def stage_cast_T(g, src, tiles, ident_bf, dstT):
    st = Stage(g, "castT")
    ident = st.sb([128, 128], BF16, "ident")
    st.dma("sp", ident[:], ident_bf, wr=["ident"])
    x = [st.sb([128, D], F32, "x") for _ in range(2)]
    xb = [st.sb([128, D], BF16, "xb") for _ in range(2)]
    tr = TrOut(st, ident, nb=2)
    for i, t in enumerate(tiles):
        b = i % 2
        B = "%d" % b
        st.dma("sp", x[b][:], src[t * 128:(t + 1) * 128, :], wr=["x" + B])
        st.op("pool" if b else "dve", "tensor_copy", rd=["x" + B], wr=["xb" + B], out=xb[b][:], in_=x[b][:])
        tr.go(xb[b], ["xb" + B], dstT[t])
    st.emit()


def stage_conv(g, p, tiles, NT, conv_l, ident_bf, ybT):
    st = Stage(g, "conv")
    ident = st.sb([128, 128], BF16, "ident")
    st.dma("sp", ident[:], ident_bf, wr=["ident"])
    W = [st.sb([128, D], F32, "cw") for _ in range(3)]
    for j in range(3):
        st.dma("sp", W[j][:], bcast_row(conv_l[j:j + 1, :]), wr=["W%d" % j])
    NB = 2
    names = ["c0", "x0", "bb", "cm", "xm", "cp", "xp"]
    buf = {n: [st.sb([128, D], F32, n) for _ in range(NB)] for n in names}
    yb = [st.sb([128, D], BF16, "yb") for _ in range(NB)]
    tr = TrOut(st, ident, nb=2)
    for i, t in enumerate(tiles):
        b = i % NB
        B = "%d" % b
        r0 = t * 128
        first = t in (0, 2)
        last = t in (1, NT - 1)
        T = {n: buf[n][b] for n in names}
        K = {n: n + B for n in names}
        st.dma("sp", T["c0"][:], p[r0:r0 + 128, O_BC:O_BC + D], wr=[K["c0"]])
        st.dma("sp", T["x0"][:], p[r0:r0 + 128, O_BX:O_BX + D], wr=[K["x0"]])
        st.dma("sp", T["bb"][:], p[r0:r0 + 128, O_BB:O_BB + D], wr=[K["bb"]])
        for (cn, xn, off, edge) in (("cm", "xm", -1, first), ("cp", "xp", 1, last)):
            for (n, col) in ((cn, O_BC), (xn, O_BX)):
                if not edge:
                    st.dma("sp", T[n][:], p[r0 + off:r0 + off + 128, col:col + D], wr=[K[n]])
                else:
                    st.op("pool", "memset", wr=[K[n]], ap=T[n][:], constant=0.0)
                    if off < 0:
                        st.dma("sp", T[n][1:128, :], p[r0:r0 + 127, col:col + D], wr=[K[n]])
                    else:
                        st.dma("sp", T[n][0:127, :], p[r0 + 1:r0 + 128, col:col + D], wr=[K[n]])
        tt = lambda eng, o, a, bb_, op, rd, wr: st.op(eng, "tensor_tensor", rd=rd, wr=wr, out=o, in0=a, in1=bb_, op=op)
        tt("dve", T["x0"][:], T["c0"][:], T["x0"][:], ALU.mult, [K["c0"], K["x0"]], [K["x0"]])
        tt("pool", T["xm"][:], T["cm"][:], T["xm"][:], ALU.mult, [K["cm"], K["xm"]], [K["xm"]])
        tt("pool", T["xp"][:], T["cp"][:], T["xp"][:], ALU.mult, [K["cp"], K["xp"]], [K["xp"]])
        tt("dve", T["xm"][:], T["xm"][:], W[0][:], ALU.mult, [K["xm"], "W0"], [K["xm"]])
        tt("pool", T["x0"][:], T["x0"][:], W[1][:], ALU.mult, [K["x0"], "W1"], [K["x0"]])
        tt("dve", T["xp"][:], T["xp"][:], W[2][:], ALU.mult, [K["xp"], "W2"], [K["xp"]])
        tt("pool", T["x0"][:], T["x0"][:], T["xm"][:], ALU.add, [K["x0"], K["xm"]], [K["x0"]])
        tt("dve", T["x0"][:], T["x0"][:], T["xp"][:], ALU.add, [K["x0"], K["xp"]], [K["x0"]])
        tt("dve", yb[b][:], T["x0"][:], T["bb"][:], ALU.mult, [K["x0"], K["bb"]], ["yb" + B])
        tr.go(yb[b], ["yb" + B], ybT[t])
    st.emit()


def na_plan(ROWS):
    kbs, cls, keys = [], [], {}
    for i in range(ROWS // 2):
        r = 2 * i
        kb = min(max(r - 4, 0), ROWS - 9)
        r0a = min(max(r - 4, 0), ROWS - 8)
        r0b = min(max(r + 1 - 4, 0), ROWS - 8)
        key = (r - kb, r0a - kb, r0b - kb)
        if key not in keys:
            keys[key] = len(keys)
        kbs.append(kb)
        cls.append(keys[key])
    return kbs, cls, list(keys.keys())


def na_bias_host(rpb_l, ROWS):
    _, _, keys = na_plan(ROWS)
    qrow = np.arange(128) // 64
    qc = np.arange(128) % 64
    j = np.arange(576) // 64
    kc = np.arange(576) % 64
    col0 = np.clip(qc - 8, 0, 48)
    colok = (kc[None, :] >= col0[:, None]) & (kc[None, :] < col0[:, None] + 16)
    dc = np.clip(kc[None, :] - qc[:, None] + 15, 0, 30)
    out = np.full((16, len(keys), 128, 5, 128), -30000.0, np.float32)
    for ci, (rel, a, b) in enumerate(keys):
        lo = np.where(qrow == 0, a, b)
        rowok = (j[None, :] >= lo[:, None]) & (j[None, :] < lo[:, None] + 8)
        dr = np.clip(j[None, :] - rel - qrow[:, None] + 7, 0, 14)
        ok = rowok & colok
        Tm = np.where(ok[None], rpb_l[:, dr, dc], np.float32(-30000.0))
        TT = np.full((16, 640, 128), -30000.0, np.float32)
        TT[:, :576, :] = Tm.transpose(0, 2, 1)
        out[:, ci] = TT.reshape(16, 5, 128, 128).transpose(0, 2, 1, 3)
    return out.reshape(16, len(keys), 128, 640)


def stage_na_prep(g, p, tiles, qkg_l, ident_bf, cqT_s, ckT_s, cv1_s):
    st = Stage(g, "naprep")
    ident = st.sb([128, 128], BF16, "ident")
    st.dma("sp", ident[:], ident_bf, wr=["ident"])
    GQ = st.sb([128, D], F32, "GQ")
    GK = st.sb([128, D], F32, "GK")
    for h in range(16):
        st.dma("sp", GQ[:, h * 128:(h + 1) * 128], bcast_row(qkg_l[0:1, :]), wr=["GQ"])
        st.dma("sp", GK[:, h * 128:(h + 1) * 128], bcast_row(qkg_l[1:2, :]), wr=["GK"])
    st.op("dve", "tensor_scalar", rd=["GQ"], wr=["GQ"], out=GQ[:], in0=GQ[:], scalar1=128.0 ** -0.5, scalar2=None, op0=ALU.mult)
    NB = 2
    x = {n: [st.sb([128, D], F32, n) for _ in range(NB)] for n in ("q", "k", "v")}
    sq = [st.sb([128, D], F32, "sq") for _ in range(NB)]
    ss = {n: [st.sb([128, 16], F32, "ss" + n) for _ in range(NB)] for n in ("q", "k")}
    xb = {n: [st.sb([128, D], BF16, "xb" + n) for _ in range(NB)] for n in ("q", "k")}
    v1 = [st.sb([128, 16, 129], BF16, "v1") for _ in range(NB)]
    for b in range(NB):
        st.op("pool", "memset", wr=["v1%d" % b], ap=v1[b][:], constant=1.0)
    tr = TrOut(st, ident, nb=2)
    for i, t in enumerate(tiles):
        b = i % NB
        B = "%d" % b
        rows = slice(t * 128, (t + 1) * 128)
        st.dma("sp", x["q"][b][:], p[rows, O_CQ:O_CQ + D], wr=["q" + B])
        st.dma("sp", x["k"][b][:], p[rows, O_CK:O_CK + D], wr=["k" + B])
        st.dma("sp", x["v"][b][:], p[rows, O_CV:O_CV + D], wr=["v" + B])
        for n, Gt, gk_, dst in (("q", GQ, "GQ", cqT_s), ("k", GK, "GK", ckT_s)):
            xt = x[n][b]
            s_ = ss[n][b]
            st.op("act", "activation", rd=[n + B], wr=["sq" + B], out=sq[b][:], in_=xt[:], func=AF.Square)
            st.op("dve", "tensor_reduce", rd=["sq" + B], wr=["ss" + n + B], out=s_[:], in_=sq[b][:].rearrange("p (h d) -> p h d", h=16), axis=AX.X, op=ALU.add)
            st.op("act", "activation", rd=["ss" + n + B], wr=["ss" + n + B], out=s_[:], in_=s_[:], func=AF.Sqrt, scale=1.0 / 128, bias=EPS)
            st.op("dve", "reciprocal", rd=["ss" + n + B], wr=["ss" + n + B], out=s_[:], in_=s_[:])
            st.op("pool", "tensor_tensor", rd=[n + B, "ss" + n + B], wr=[n + B], out=xt[:].rearrange("p (h d) -> p h d", h=16),
                  in0=xt[:].rearrange("p (h d) -> p h d", h=16), in1=bc3(s_[:], 128), op=ALU.mult)
            st.op("dve", "tensor_tensor", rd=[n + B, gk_], wr=["xb" + n + B], out=xb[n][b][:], in0=xt[:], in1=Gt[:], op=ALU.mult)
            tr.go(xb[n][b], ["xb" + n + B], dst[t])
        st.op("pool", "tensor_copy", rd=["v" + B], wr=["v1" + B], out=v1[b][:, :, 0:128], in_=x["v"][b][:].rearrange("p (h d) -> p h d", h=16))
        st.dma("pool", cv1_s[rows, :], v1[b][:].rearrange("p h d -> p (h d)"), rd=["v1" + B])
    st.emit()


def stage_na(g, NL, need_ctx, nab_l, ident_bf, cqT_s, ckT_s, cv1_s, yc_s):
    st = Stage(g, "na")
    NT = NL + 2
    ROWS = 2 * NL
    kbs, cls, keys = na_plan(ROWS)
    NC = len(keys)
    ident = st.sb([128, 128], BF16, "ident")
    st.dma("sp", ident[:], ident_bf, wr=["ident"])
    nm8 = st.sb([128, 1], F32, "nm8")
    st.op("dve", "memset", wr=["nm8"], ap=nm8[:], constant=-8.0)
    kTh = st.sb([128, NT * 128], BF16, "kTh")
    qTh = st.sb([128, NT * 128], BF16, "qTh")
    va = st.sb([128, NT, 129], BF16, "va")
    vb = st.sb([128, NL, 129], BF16, "vb")
    bst = st.sb([128, NC, 640], F32, "bst")
    bT = st.sb([128, NC, 640], BF16, "bT")
    E = [st.sb([128, 1024], BF16, "E") for _ in range(2)]
    ob = [st.sb([128, 128], F32, "ob") for _ in range(2)]
    rr = [st.sb([128, 1], F32, "rr") for _ in range(2)]
    ps = [st.ps([128, 1024], F32, "nps") for _ in range(2)]
    po = [st.ps([128, 512], F32, "npo") for _ in range(2)]
    cv3 = cv1_s.rearrange("(t p) (h c) -> p t h c", p=128, h=16)
    cnt = 0
    for h in range(16):
        hs = slice(h * 128, (h + 1) * 128)
        st.dma("sp", kTh[:].rearrange("p (t c) -> p t c", t=NT), ckT_s[:, :, hs].rearrange("t p c -> p t c"), wr=["kTh"])
        st.dma("sp", qTh[:].rearrange("p (t c) -> p t c", t=NT), cqT_s[:, :, hs].rearrange("t p c -> p t c"), wr=["qTh"])
        st.dma("sp", va[:], cv3[:, :, h, :], wr=["va"])
        st.dma("sp", vb[:, 0:NL - 1, :], cv1_s[256 + 64:256 + 64 + (NL - 1) * 128, h * 129:(h + 1) * 129].rearrange("(t p) c -> p t c", p=128), wr=["vb"])
        st.dma("sp", vb[0:64, NL - 1, :], cv1_s[256 + 64 + (NL - 1) * 128:256 + NL * 128, h * 129:(h + 1) * 129], wr=["vb"])
        st.dma("sp", bst[:], nab_l[h].rearrange("c p x -> p c x"), wr=["bst"])
        st.op("pool", "tensor_copy", rd=["bst"], wr=["bT"], out=bT[:], in_=bst[:])
        qtiles = ([0, 1] if need_ctx else []) + list(range(2, NT))
        for t in qtiles:
            j = cnt % 2
            J = "%d" % j
            cnt += 1
            q_ = qTh[:, t * 128:(t + 1) * 128]
            pj = ps[j]
            if t >= 2:
                i = t - 2
                kb, c_ = kbs[i], cls[i]
                tok0 = 256 + kb * 64
                for c in range(5):
                    kn = 128 if c < 4 else 64
                    st.op("pe", "matmul", rd=["kTh", "qTh"], wr=["ps" + J], out=pj[0:kn, c * 128:(c + 1) * 128],
                          lhsT=kTh[:, tok0 + c * 128:tok0 + c * 128 + kn], rhs=q_, start=True, stop=False)
                    st.op("pe", "matmul", rd=["ident", "bT"], wr=["ps" + J], out=pj[0:kn, c * 128:(c + 1) * 128],
                          lhsT=ident[0:kn, 0:kn], rhs=bT[0:kn, c_, c * 128:(c + 1) * 128], start=False, stop=True)
            for c in range(2):
                st.op("pe", "matmul", rd=["kTh", "qTh"], wr=["ps" + J], out=pj[:, (5 + c) * 128:(6 + c) * 128],
                      lhsT=kTh[:, c * 128:(c + 1) * 128], rhs=q_, start=True, stop=True)
            if t >= 2:
                st.op("act", "activation", rd=["ps" + J, "nm8"], wr=["E" + J], out=E[j][:, 0:512], in_=pj[:, 0:512], func=AF.Exp, bias=nm8[:])
                st.op("act", "activation", rd=["ps" + J, "nm8"], wr=["E" + J], out=E[j][0:64, 512:640], in_=pj[0:64, 512:640], func=AF.Exp, bias=nm8[0:64, :])
            st.op("act", "activation", rd=["ps" + J, "nm8"], wr=["E" + J], out=E[j][:, 640:896], in_=pj[:, 640:896], func=AF.Exp, bias=nm8[:])
            chunks = []
            if t >= 2:
                for c in range(5):
                    kn = 128 if c < 4 else 64
                    if kb % 2 == 0:
                        vv = va[0:kn, 2 + kb // 2 + c, :]
                    else:
                        vv = vb[0:kn, (kb - 1) // 2 + c, :]
                    chunks.append((E[j][0:kn, c * 128:(c + 1) * 128], vv))
            for c in range(2):
                chunks.append((E[j][:, (5 + c) * 128:(6 + c) * 128], va[:, c, :]))
            for ci, (l_, r_) in enumerate(chunks):
                st.op("pe", "matmul", rd=["E" + J, "va", "vb"], wr=["po" + J], out=po[j][:, 0:129], lhsT=l_, rhs=r_,
                      start=(ci == 0), stop=(ci == len(chunks) - 1))
            st.op("dve", "reciprocal", rd=["po" + J], wr=["rr" + J], out=rr[j][:], in_=po[j][:, 128:129])
            st.op("dve", "tensor_scalar", rd=["po" + J, "rr" + J], wr=["ob" + J], out=ob[j][:], in0=po[j][:, 0:128], scalar1=rr[j][:], scalar2=None, op0=ALU.mult)
            st.dma("pool", yc_s[t * 128:(t + 1) * 128, hs], ob[j][:], rd=["ob" + J])
    st.emit()


def stage_gate(g, p, tiles, brs, ident_bf, yT):
    st = Stage(g, "gate")
    ident = st.sb([128, 128], BF16, "ident")
    st.dma("sp", ident[:], ident_bf, wr=["ident"])
    NB = 2
    gt = [[st.sb([128, D], F32, "gt") for _ in range(3)] for _ in range(NB)]
    br = [[st.sb([128, D], F32, "br") for _ in range(3)] for _ in range(NB)]
    yb = [st.sb([128, D], BF16, "yb") for _ in range(NB)]
    tr = TrOut(st, ident, nb=2)
    for i, t in enumerate(tiles):
        b = i % NB
        B = "%d" % b
        rows = slice(t * 128, (t + 1) * 128)
        for j in range(3):
            st.dma("sp", gt[b][j][:], p[rows, O_GT + j * D:O_GT + (j + 1) * D], wr=["gt%d" % j + B])
            st.dma("sp", br[b][j][:], brs[j][rows, :], wr=["br%d" % j + B])
            st.op("act", "activation", rd=["gt%d" % j + B], wr=["gt%d" % j + B], out=gt[b][j][:], in_=gt[b][j][:], func=AF.Sigmoid)
            st.op("pool" if j == 1 else "dve", "tensor_tensor", rd=["gt%d" % j + B, "br%d" % j + B], wr=["br%d" % j + B],
                  out=br[b][j][:], in0=br[b][j][:], in1=gt[b][j][:], op=ALU.mult)
        st.op("pool", "tensor_tensor", rd=["br0" + B, "br1" + B], wr=["br0" + B], out=br[b][0][:], in0=br[b][0][:], in1=br[b][1][:], op=ALU.add)
        st.op("dve", "tensor_tensor", rd=["br0" + B, "br2" + B], wr=["yb" + B], out=yb[b][:], in0=br[b][0][:], in1=br[b][2][:], op=ALU.add)
        tr.go(yb[b], ["yb" + B], yT[t])
    st.emit()


def stage_resid(g, xsrc, ysrc, tiles, modv, seg, dst, dst_row0=0):
    st = Stage(g, "resid")
    Gv = [st.sb([128, D], F32, "Gv") for _ in range(2)]
    for r in range(2):
        st.dma("sp", Gv[r][:], bcast_row(modv[r:r + 1, seg * D:(seg + 1) * D]), wr=["G%d" % r])
    NB = 2
    x = [st.sb([128, D], F32, "x") for _ in range(NB)]
    y = [st.sb([128, D], F32, "y") for _ in range(NB)]
    for i, t in enumerate(tiles):
        b = i % NB
        B = "%d" % b
        r = 1 if t < 2 else 0
        rows = slice(t * 128, (t + 1) * 128)
        st.dma("sp", x[b][:], xsrc[rows, :], wr=["x" + B])
        st.dma("sp", y[b][:], ysrc[rows, :], wr=["y" + B])
        st.op("pool" if b else "dve", "tensor_tensor", rd=["y" + B, "G%d" % r], wr=["y" + B], out=y[b][:], in0=y[b][:], in1=Gv[r][:], op=ALU.mult)
        st.op("dve", "tensor_tensor", rd=["x" + B, "y" + B], wr=["y" + B], out=y[b][:], in0=y[b][:], in1=x[b][:], op=ALU.add)
        st.dma("pool", dst[t * 128 - dst_row0:(t + 1) * 128 - dst_row0, :], y[b][:], rd=["y" + B])
    st.emit()


def stage_uT(g, u_l, cst, uT_s):
    st = Stage(g, "uT")
    cs = st.sb([128, 512], F32, "cst")
    st.dma("sp", cs[:], cst, wr=["cst"])
    IDF = cs[:, 384:512]
    ub = [st.sb([128, D], F32, "ub") for _ in range(2)]
    ob = [st.sb([128, KC, 128], BF16, "uo") for _ in range(2)]
    pu = [st.ps([128, D], F32, "pu") for _ in range(2)]
    for eb in range(128):
        b = eb % 2
        B = "%d" % b
        st.dma("sp", ub[b][:], u_l[eb * 128:(eb + 1) * 128, :], wr=["ub" + B])
        for k in range(KC):
            st.op("pe", "transpose", rd=["ub" + B, "cst"], wr=["pu" + B], out=pu[b][:, k * 128:(k + 1) * 128], in_=ub[b][:, k * 128:(k + 1) * 128], identity=IDF)
        for q4 in range(4):
            st.op("act" if q4 % 2 == 0 else "dve", "copy" if q4 % 2 == 0 else "tensor_copy", rd=["pu" + B], wr=["uo%d" % q4 + B],
                  out=ob[b][:, q4 * 4:(q4 + 1) * 4, :], in_=pu[b][:, q4 * 512:(q4 + 1) * 512].rearrange("p (k e) -> p k e", k=4))
        st.dma("pool", uT_s[:, eb * 128:(eb + 1) * 128].rearrange("(k p) e -> p k e", p=128), ob[b][:], rd=["uo%d" % q4 + B for q4 in range(4)])
    st.emit()


def stage_peer_scores(g, xT, tiles, wq_l, keys_l, cst, sc_s, selp_s):
    st = Stage(g, "pscore")
    cs = st.sb([128, 512], F32, "cst")
    st.dma("sp", cs[:], cst, wr=["cst"])
    IDF = cs[:, 384:512]
    wq = st.sb([128, KC, D], BF16, "wq")
    wst = [st.sb([128, KC, 512], F32, "wst") for _ in range(2)]
    for n in range(4):
        st.dma("sp", wst[n % 2][:], wq_l[:, n * 512:(n + 1) * 512].rearrange("(k p) c -> p k c", p=128), wr=["wst%d" % (n % 2)])
        st.op("pool" if n % 2 else "dve", "tensor_copy", rd=["wst%d" % (n % 2)], wr=["wq"], out=wq[:, :, n * 512:(n + 1) * 512], in_=wst[n % 2][:])
    keysT = st.sb([128, 16, 128], BF16, "keysT")
    kst = [st.sb([128, 128], F32, "kst") for _ in range(2)]
    pq = [st.ps([128, 512], F32, "pq") for _ in range(2)]
    kl = keys_l.rearrange("h p k d -> (h p) k d")
    for blk in range(16):
        j = blk % 2
        st.dma("sp", kst[j][:], kl[blk], wr=["kst%d" % j])
        st.op("pe", "transpose", rd=["kst%d" % j, "cst"], wr=["pq%d" % j], out=pq[j][:, 0:128], in_=kst[j][:], identity=IDF)
        st.op("dve", "tensor_copy", rd=["pq%d" % j], wr=["keysT"], out=keysT[:, blk, :], in_=pq[j][:, 0:128])
    pss = st.ps([128, D], F32, "pss")
    NB = 2
    xt = [st.sb([128, D], BF16, "xt") for _ in range(NB)]
    qTb = [st.sb([128, 128], BF16, "qTb") for _ in range(2)]
    sc = [st.sb([128, D], F32, "sc") for _ in range(NB)]
    wk = st.sb([128, 256], F32, "wk")
    t16 = st.sb([128, 16, 16], F32, "t16")
    cand = st.sb([128, 16, 16], F32, "cand")
    ex = st.sb([128, 256], F32, "ex")
    junk = st.sb([128, 256], F32, "junk")
    c8 = st.sb([128, 16], F32, "c8")
    sm_ = st.sb([128, 32], F32, "small")
    selp = [st.sb([128, 16], F32, "selp") for _ in range(NB)]
    qc = 0
    for i, t in enumerate(tiles):
        b = i % NB
        B = "%d" % b
        rows = slice(t * 128, (t + 1) * 128)
        st.dma("sp", xt[b][:], xT[t], wr=["xt" + B])
        for blk in range(16):
            j = qc % 2
            J = "%d" % j
            qc += 1
            for k in range(KC):
                st.op("pe", "matmul", rd=["wq", "xt" + B], wr=["pq" + J], out=pq[j][:, 0:128], lhsT=wq[:, k, blk * 128:(blk + 1) * 128],
                      rhs=xt[b][:, k * 128:(k + 1) * 128], start=(k == 0), stop=(k == KC - 1))
            if j == 0:
                st.op("act", "copy", rd=["pq" + J], wr=["qTb" + J], out=qTb[j][:], in_=pq[j][:, 0:128])
            else:
                st.op("dve", "tensor_copy", rd=["pq" + J], wr=["qTb" + J], out=qTb[j][:], in_=pq[j][:, 0:128])
            st.op("pe", "matmul", rd=["qTb" + J, "keysT"], wr=["pss"], out=pss[:, blk * 128:(blk + 1) * 128], lhsT=qTb[j][:], rhs=keysT[:, blk, :], start=True, stop=True)
        for q4 in range(4):
            st.op("act" if q4 % 2 == 0 else "dve", "copy" if q4 % 2 == 0 else "tensor_copy", rd=["pss"], wr=["sc%d" % q4 + B],
                  out=sc[b][:, q4 * 512:(q4 + 1) * 512], in_=pss[:, q4 * 512:(q4 + 1) * 512])
        SK = ["sc%d" % q4 + B for q4 in range(4)]
        st.dma("pool", sc_s[rows, :], sc[b][:], rd=SK)
        for blk in range(16):
            sl = sc[b][:, blk * 128:(blk + 1) * 128]
            st.op("dve", "max", rd=SK, wr=["t16"], out=t16[:, blk, 0:8], in_=sl)
            st.op("dve", "match_replace", rd=SK + ["t16"], wr=["wk"], out=wk[:, 0:128], in_to_replace=t16[:, blk, 0:8], in_values=sl, imm_value=-1e30)
            st.op("dve", "max", rd=["wk"], wr=["t16"], out=t16[:, blk, 8:16], in_=wk[:, 0:128])
        for h in range(8):
            st.op("dve", "tensor_tensor", rd=["t16"], wr=["cand"], out=cand[:], in0=bc3(t16[:, 2 * h, :], 16), in1=bcmid(t16[:, 2 * h + 1, :], 16), op=ALU.add)
            cf = cand[:].rearrange("p a b -> p (a b)")
            st.op("dve", "max", rd=["cand"], wr=["c8"], out=c8[:, 0:8], in_=cf)
            st.op("dve", "match_replace", rd=["cand", "c8"], wr=["wk"], out=wk[:], in_to_replace=c8[:, 0:8], in_values=cf, imm_value=-1e30)
            st.op("dve", "max", rd=["wk"], wr=["c8"], out=c8[:, 8:16], in_=wk[:])
            st.op("dve", "tensor_scalar", rd=["c8"], wr=["small"], out=sm_[:, 0:1], in0=c8[:, 0:1], scalar1=-1.0, scalar2=None, op0=ALU.mult)
            st.op("act", "activation", rd=["cand", "small"], wr=["ex"], out=ex[:], in_=cf, func=AF.Exp, bias=sm_[:, 0:1])
            st.op("dve", "scalar_tensor_tensor", rd=["cand", "c8", "ex"], wr=["junk", "small"], out=junk[:], in0=cf, scalar=c8[:, 15:16], in1=ex[:],
                  op0=ALU.is_ge, op1=ALU.mult, accum_out=sm_[:, 1:2])
            st.op("act", "activation", rd=["small"], wr=["small"], out=sm_[:, 2:3], in_=sm_[:, 1:2], func=AF.Ln)
            st.op("dve", "tensor_copy", rd=["c8"], wr=["selp" + B], out=selp[b][:, 2 * h:2 * h + 1], in_=c8[:, 15:16])
            st.op("dve", "tensor_tensor", rd=["small"], wr=["selp" + B], out=selp[b][:, 2 * h + 1:2 * h + 2], in0=sm_[:, 0:1], in1=sm_[:, 2:3], op=ALU.subtract)
        st.dma("pool", selp_s[rows, :], selp[b][:], rd=["selp" + B])
    st.emit()


def stage_peer_experts(g, xT, tiles, uT_s, v_l, ident_bf, sc_s, selp_s, po_s, NG=16):
    st = Stage(g, "pexp")
    ident = st.sb([128, 128], BF16, "ident")
    st.dma("sp", ident[:], ident_bf, wr=["ident"])
    uTg = st.sb([128, KC, 1024], BF16, "uTg")
    vg = st.sb([128, 8, D], BF16, "vg")
    vst = [st.sb([128, D], F32, "vst") for _ in range(2)]
    NB = 2
    xt = [st.sb([128, D], BF16, "xt") for _ in range(NB)]
    sc = [st.sb([128, D], F32, "sc") for _ in range(NB)]
    selp = [st.sb([128, 16], F32, "selp") for _ in range(NB)]
    sm = [st.sb([128, 1024], F32, "sm") for _ in range(2)]
    ex = [st.sb([128, 1024], F32, "ex") for _ in range(2)]
    mk = [st.sb([128, 1024], BF16, "mk") for _ in range(8)]
    gl = st.sb([128, 1024], F32, "gl")
    WT = st.sb([128, 1024], BF16, "WT")
    ob = [st.sb([128, D], F32, "ob") for _ in range(2)]
    ps_h = st.ps([128, 1024], F32, "ps_h")
    ps_g = st.ps([128, 1024], F32, "ps_g")
    ps_o = [st.ps([128, 512], F32, "ps_o") for _ in range(2)]
    it = 0
    hc = 0
    oc = 0
    for gi in range(NG):
        st.dma("sp", uTg[:], uT_s[:, gi * 1024:(gi + 1) * 1024].rearrange("(k p) e -> p k e", p=128), wr=["uTg"])
        for c in range(8):
            j = c % 2
            st.dma("sp", vst[j][:], v_l[gi * 1024 + c * 128:gi * 1024 + (c + 1) * 128, :], wr=["vst%d" % j])
            st.op("pool" if j else "dve", "tensor_copy", rd=["vst%d" % j], wr=["vg"], out=vg[:, c, :], in_=vst[j][:])
        for t in tiles:
            b = it % NB
            B = "%d" % b
            it += 1
            rows = slice(t * 128, (t + 1) * 128)
            st.dma("sp", xt[b][:], xT[t], wr=["xt" + B])
            st.dma("sp", sc[b][:], sc_s[rows, :], wr=["sc" + B])
            st.dma("sp", selp[b][:], selp_s[rows, :], wr=["selp" + B])
            for c in range(8):
                for k in range(KC):
                    st.op("pe", "matmul", rd=["uTg", "xt" + B], wr=["ps_h"], out=ps_h[:, c * 128:(c + 1) * 128], lhsT=uTg[:, k, c * 128:(c + 1) * 128],
                          rhs=xt[b][:, k * 128:(k + 1) * 128], start=(k == 0), stop=(k == KC - 1))
            for q2 in range(2):
                st.op("act", "activation", rd=["ps_h"], wr=["gl%d" % q2], out=gl[:, q2 * 512:(q2 + 1) * 512], in_=ps_h[:, q2 * 512:(q2 + 1) * 512], func=AF.Gelu)
            for h in range(8):
                j = hc % 2
                J = "%d" % j
                hc += 1
                s1 = sc[b][:, (2 * h) * 128 + gi * 8:(2 * h) * 128 + gi * 8 + 8]
                s2 = sc[b][:, (2 * h + 1) * 128:(2 * h + 2) * 128]
                sm3 = sm[j][:].rearrange("p (a b) -> p a b", a=8)
                st.op("pool", "tensor_tensor", rd=["sc" + B], wr=["sm" + J], out=sm3, in0=bc3(s1, 128), in1=bcmid(s2, 8), op=ALU.add)
                st.op("act", "activation", rd=["sm" + J, "selp" + B], wr=["ex" + J], out=ex[j][:], in_=sm[j][:], func=AF.Exp, bias=selp[b][:, 2 * h + 1:2 * h + 2])
                st.op("dve", "scalar_tensor_tensor", rd=["sm" + J, "ex" + J, "selp" + B], wr=["mk%d" % h], out=mk[h][:], in0=sm[j][:], scalar=selp[b][:, 2 * h:2 * h + 1],
                      in1=ex[j][:], op0=ALU.is_ge, op1=ALU.mult)
            for c in range(8):
                for h in range(8):
                    st.op("pe", "matmul", rd=["mk%d" % h, "ident"], wr=["ps_g"], out=ps_g[:, c * 128:(c + 1) * 128], lhsT=mk[h][:, c * 128:(c + 1) * 128],
                          rhs=ident[:], start=(h == 0), stop=(h == 7))
            for q2 in range(2):
                st.op("dve", "tensor_tensor", rd=["ps_g", "gl%d" % q2], wr=["WT"], out=WT[:, q2 * 512:(q2 + 1) * 512], in0=ps_g[:, q2 * 512:(q2 + 1) * 512],
                      in1=gl[:, q2 * 512:(q2 + 1) * 512], op=ALU.mult)
            o = ob[b]
            for cb in range(4):
                j = oc % 2
                J = "%d" % j
                oc += 1
                for c in range(8):
                    st.op("pe", "matmul", rd=["WT", "vg"], wr=["ps_o" + J], out=ps_o[j][:], lhsT=WT[:, c * 128:(c + 1) * 128], rhs=vg[:, c, cb * 512:(cb + 1) * 512],
                          start=(c == 0), stop=(c == 7))
                if j == 0:
                    st.op("act", "copy", rd=["ps_o" + J], wr=["ob%d_%d" % (b, cb)], out=o[:, cb * 512:(cb + 1) * 512], in_=ps_o[j][:])
                else:
                    st.op("dve", "tensor_copy", rd=["ps_o" + J], wr=["ob%d_%d" % (b, cb)], out=o[:, cb * 512:(cb + 1) * 512], in_=ps_o[j][:])
            okeys = ["ob%d_%d" % (b, cb) for cb in range(4)]
            if gi == 0:
                st.dma("pool", po_s[rows, :], o[:], rd=okeys, wr=[("po", t)])
            else:
                st.dma("pool", po_s[rows, :], o[:], rd=okeys, wr=[("po", t)], accum_op=ALU.add)
    st.emit()


def build_program(NL, depth=2):
    NT = NL + 2
    ROWS = 2 * NL
    NCLS = len(na_plan(ROWS)[2])
    nc = bass.Bass("TRN2", target_bir_lowering=False)

    def inp(name, shape, dt=F32):
        return nc.dram_tensor(name, list(shape), dt, kind="ExternalInput").ap()

    xcat = inp("xcat", [NT * 128, D])
    cc = inp("cc", [2, D])
    w_ada = inp("w_ada", [depth, D, 6 * D])
    b_ada = inp("b_ada", [depth, 6 * D])
    norm_g = inp("norm_g", [depth, 2 * D])
    w_in = inp("w_in", [depth, D, PW])
    gate_b = inp("a_gate_b", [depth, 32])
    hnorm = inp("a_hnorm_g", [depth, D])
    b_conv = inp("b_conv", [depth, 3, D])
    qkg = inp("c_qk_g", [depth, 2, 128])
    nab = inp("nab", [depth, 16, NCLS, 128, 640])
    w_a = inp("w_a_out", [depth, D, D])
    w_b = inp("w_b_out", [depth, D, D])
    w_c = inp("w_c_out", [depth, D, D])
    w_o = inp("w_out", [depth, D, D])
    wq = inp("peer_wq", [depth, D, D])
    pkeys = inp("peer_keys", [depth, 8, 2, 128, 128])
    pu = inp("peer_u", [depth, 16384, D])
    pv = inp("peer_v", [depth, 16384, D])
    cst = inp("cst", [128, 512])
    ident = inp("ident_bf", [128, 128], BF16)
    rope = [inp(n, [NL * 128, 128]) for n in ("cosq", "sinq", "cosk", "sink")]
    out = nc.dram_tensor("out", [NL * 128, D], F32, kind="ExternalOutput").ap()

    with ExitStack() as es:
        es.enter_context(nc.allow_low_precision("bf16 matmul operands, fp32 accumulation"))
        g = G(nc, es)
        R = NT * 128

        def dr(name, shape, dt=F32):
            return g.dram(name, shape, dt).ap()

        modv = dr("modv", [2, 6 * D])
        xnT = dr("xnT", [NT, 128, D], BF16)
        pparts = [dr("p%d" % gi, [R, PSplit.BOUNDS[gi + 1] - PSplit.BOUNDS[gi]]) for gi in range(4)]
        p = PSplit(pparts)
        qT_s = dr("qT_s", [NT, 128, 1024], BF16)
        kT_s = dr("kT_s", [NT, 128, 1024], BF16)
        kb_s = dr("kb_s", [R, 1024], BF16)
        v2f_s = dr("v2f_s", [R, 8 * 257], BF16)
        v2b_s = dr("v2b_s", [R, 8 * 257], BF16)
        gpk_s = dr("gpk_s", [R, 48])
        hf_s = dr("hf_s", [R, D])
        yaT = dr("yaT", [NT, 128, D], BF16)
        ybT = dr("ybT", [NT, 128, D], BF16)
        ycT = dr("ycT", [NT, 128, D], BF16)
        yT = dr("yT", [NT, 128, D], BF16)
        cqT_s = dr("cqT_s", [NT, 128, D], BF16)
        ckT_s = dr("ckT_s", [NT, 128, D], BF16)
        cv1_s = dr("cv1_s", [R, 16 * 129], BF16)
        yc_s = dr("yc_s", [R, D])
        brs = [dr("br%d" % j, [R, D]) for j in range(3)]
        mix = dr("mix", [R, D])
        x1 = dr("x1", [R, D])
        xmid = dr("xmid", [R, D])
        uT_s = dr("uT_s", [D, 16384], BF16)
        sc_s = dr("sc_s", [R, D])
        selp_s = dr("selp_s", [R, 16])
        po_s = dr("po_s", [R, D])

        allt = list(range(NT))
        lat = list(range(2, NT))
        for l in range(depth):
            last = l == depth - 1
            xin = xcat if l == 0 else xmid
            T = lat if last else allt
            stage_mod(g, cc, w_ada[l], b_ada[l:l + 1, :], norm_g[l:l + 1, :], modv)
            stage_norm(g, xin, modv, 0, xnT, allt, ident)
            for gi in range(4):
                lo, hi = PSplit.BOUNDS[gi], PSplit.BOUNDS[gi + 1]
                stage_linear(g, xnT, allt, w_in[l][:, lo:hi], hi - lo, pparts[gi])
            stage_mlstm_prep(g, p, allt, cst, ident, gate_b[l:l + 1, :], rope, qT_s, kT_s, kb_s, v2f_s, v2b_s, gpk_s)
            stage_mlstm_scan(g, False, [0, 1] + lat, cst, ident, qT_s, kT_s, kb_s, v2f_s, gpk_s, hf_s)
            stage_mlstm_scan(g, True, [1, 0] + lat[::-1], cst, ident, qT_s, kT_s, kb_s, v2b_s, gpk_s, hf_s, p=p, hnorm_l=hnorm[l:l + 1, :], yaT=yaT,
                             skip_out=((0, 1) if last else ()))
            stage_conv(g, p, T, NT, b_conv[l], ident, ybT)
            stage_na_prep(g, p, allt, qkg[l], ident, cqT_s, ckT_s, cv1_s)
            stage_na(g, NL, not last, nab[l], ident, cqT_s, ckT_s, cv1_s, yc_s)
            stage_cast_T(g, yc_s, T, ident, ycT)
            stage_linear(g, yaT, T, w_a[l], D, brs[0])
            stage_linear(g, ybT, T, w_b[l], D, brs[1])
            stage_linear(g, ycT, T, w_c[l], D, brs[2])
            stage_gate(g, p, T, brs, ident, yT)
            stage_linear(g, yT, T, w_o[l], D, mix)
            stage_resid(g, xin, mix, T, modv, 2, x1)
            stage_norm(g, x1, modv, 1, xnT, T, ident)
            stage_uT(g, pu[l], cst, uT_s)
            stage_peer_scores(g, xnT, T, wq[l], pkeys[l], cst, sc_s, selp_s)
            stage_peer_experts(g, xnT, T, uT_s, pv[l], ident, sc_s, selp_s, po_s)
            if last:
                stage_resid(g, x1, po_s, T, modv, 5, out, dst_row0=256)
            else:
                stage_resid(g, x1, po_s, T, modv, 5, xmid)
        final_wait(g)
    return nc


_PROG = {}


def kernel(x, c, ctx, c_ctx, w_ada, b_ada, norm_g, w_in, a_gate_b, a_hnorm_g, b_conv, c_qk_g, c_rpb,
           w_a_out, w_b_out, w_c_out, w_out, peer_wq, peer_keys, peer_u, peer_v):
    f = lambda a: np.ascontiguousarray(np.asarray(a, dtype=np.float32))
    x, ctx = f(x), f(ctx)
    B, S, _ = x.shape
    NL = S // 128
    depth = w_ada.shape[0]
    hc = host_consts(S)
    nab = np.stack([na_bias_host(f(c_rpb)[l], 2 * NL) for l in range(depth)], 0)
    shared = dict(w_ada=f(w_ada), b_ada=f(b_ada), norm_g=f(norm_g).reshape(depth, 2 * D), w_in=f(w_in),
                  a_gate_b=f(a_gate_b).reshape(depth, 32), a_hnorm_g=f(a_hnorm_g), b_conv=f(b_conv), c_qk_g=f(c_qk_g), nab=nab,
                  w_a_out=f(w_a_out), w_b_out=f(w_b_out), w_c_out=f(w_c_out), w_out=f(w_out), peer_wq=f(peer_wq),
                  peer_keys=f(peer_keys), peer_u=f(peer_u), peer_v=f(peer_v), **hc)
    in_maps = []
    for b in range(B):
        m = dict(shared)
        m["xcat"] = np.concatenate([ctx[b], x[b]], axis=0)
        m["cc"] = np.stack([f(c)[b], f(c_ctx)], axis=0)
        in_maps.append(m)
    key = (NL, depth)
    if key not in _PROG:
        _PROG[key] = build_program(NL, depth)
    res = run_bass_kernel_spmd(_PROG[key], in_maps, core_ids=list(range(B)))
    return np.stack([res.results[b]["out"] for b in range(B)], axis=0)
```

```python
import numpy as np
from contextlib import ExitStack
import concourse.bass as bass
import concourse.mybir as mybir
from concourse.bass_utils import run_bass_kernel_spmd

F32 = mybir.dt.float32
BF16 = mybir.dt.bfloat16
ALU = mybir.AluOpType
AF = mybir.ActivationFunctionType
AX = mybir.AxisListType

D = 2048
KC = 16
PW = 24608
EPS = 1e-6
ENGS = ["pe", "act", "dve", "pool", "sp"]
NDS = 24

O_AK, O_AV, O_AG, O_CK, O_CV, O_AQ, O_AO, O_BB, O_BC, O_BX, O_CQ, O_GT = (
    0, 1024, 3072, 3104, 5152, 7200, 8224, 10272, 12320, 14368, 16416, 18464)


class PSplit:
    BOUNDS = [0, 7200, 14368, 18464, PW]

    def __init__(self, aps):
        self.aps = aps

    def __getitem__(self, key):
        rows, cols = key
        for gi in range(4):
            lo, hi = self.BOUNDS[gi], self.BOUNDS[gi + 1]
            if cols.start >= lo and cols.stop <= hi:
                return self.aps[gi][rows, cols.start - lo:cols.stop - lo]
        raise ValueError("column range straddles projection groups: %s" % (cols,))


class G:
    def __init__(self, nc, es):
        self.nc = nc
        self.sem = {e: es.enter_context(nc.semaphore("s_" + e)) for e in ENGS}
        self.dsem = [es.enter_context(nc.semaphore("d%d" % i)) for i in range(NDS)]
        self.cnt = {e: 0 for e in ENGS}
        self.dcnt = [0] * NDS
        self.dnext = 0
        self.known = {e: {} for e in ENGS}
        self.nstage = 0
        self.ntens = 0

    def dram(self, name, shape, dt):
        return self.nc.dram_tensor(name, list(shape), dt)


class Stage:
    def __init__(self, g, name=None):
        self.g = g
        g.nstage += 1
        self.name = name or ("st%d" % g.nstage)
        self.ops = {e: [] for e in ENGS}
        self.lastw = {}
        self.readers = {}
        self.es = ExitStack()
        self.start_cnt = dict(g.cnt)
        self.start_d = list(g.dcnt)

    def sb(self, shape, dt=F32, name=None):
        self.g.ntens += 1
        return self.es.enter_context(self.g.nc.sbuf_tensor("%s_t%d" % (name or "sb", self.g.ntens), list(shape), dt))

    def ps(self, shape, dt=F32, name=None):
        self.g.ntens += 1
        return self.es.enter_context(self.g.nc.psum_tensor("%s_p%d" % (name or "ps", self.g.ntens), list(shape), dt))

    def add(self, eng, fn, rd=(), wr=(), dma=False):
        g = self.g
        deps = []
        for k in rd:
            t = self.lastw.get(k)
            if t is not None:
                deps.append(t)
        for k in wr:
            t = self.lastw.get(k)
            cands = ([t] if t is not None else []) + self.readers.get(k, [])
            for c in cands:
                if (not dma) and c[0] == "c" and c[1] == eng:
                    continue
                deps.append(c)
        if dma:
            j = g.dnext
            g.dnext = (j + 1) % NDS
            g.dcnt[j] += 1
            tok = ("d", j, g.dcnt[j])
            if g.dcnt[j] > 1:
                deps.append(("d", j, g.dcnt[j] - 1))
        else:
            g.cnt[eng] += 1
            tok = ("c", eng, g.cnt[eng])
        for k in wr:
            self.lastw[k] = tok
            self.readers[k] = []
        for k in rd:
            lst = self.readers.setdefault(k, [])
            if tok[0] == "c":
                lst[:] = [x for x in lst if not (x[0] == "c" and x[1] == tok[1])]
            lst.append(tok)
        self.ops[eng].append((fn, deps, tok))
        return tok

    def op(self, eng, meth, rd=(), wr=(), **kw):
        return self.add(eng, lambda e: getattr(e, meth)(**kw), rd, wr)

    def dma(self, eng, out, in_, rd=(), wr=(), **kw):
        return self.add(eng, lambda e: e.dma_start(out=out, in_=in_, **kw), rd, wr, dma=True)

    def emit(self):
        g = self.g
        nc = g.nc

        def run(ename, e):
            known = g.known[ename]

            def wait(tok):
                if tok[0] == "c":
                    sem, val, key = g.sem[tok[1]], tok[2], tok[1]
                else:
                    sem, val, key = g.dsem[tok[1]], 16 * tok[2], "d%d" % tok[1]
                if known.get(key, 0) < val:
                    e.wait_ge(sem, val)
                    known[key] = val

            for o in ENGS:
                if o != ename and self.start_cnt[o] > 0:
                    wait(("c", o, self.start_cnt[o]))
            for j in range(NDS):
                if self.start_d[j] > 0:
                    wait(("d", j, self.start_d[j]))
            for fn, deps, tok in self.ops[ename]:
                for d in deps:
                    wait(d)
                ins = fn(e)
                if tok[0] == "c":
                    ins.then_inc(g.sem[tok[1]], 1)
                else:
                    ins.then_inc(g.dsem[tok[1]], 16)

        with nc.Block() as block:
            if self.ops["pe"]:
                block.tensor(lambda e: run("pe", e))
            if self.ops["act"]:
                block.scalar(lambda e: run("act", e))
            if self.ops["dve"]:
                block.vector(lambda e: run("dve", e))
            if self.ops["pool"]:
                block.gpsimd(lambda e: run("pool", e))
            if self.ops["sp"]:
                block.sync(lambda e: run("sp", e))
        self.es.close()


def final_wait(g):
    st = Stage(g, "final")
    nc = g.nc

    def run(e):
        for o in ENGS:
            if o != "sp" and g.cnt[o] > 0:
                e.wait_ge(g.sem[o], g.cnt[o])
        for j in range(NDS):
            if g.dcnt[j] > 0:
                e.wait_ge(g.dsem[j], 16 * g.dcnt[j])

    with nc.Block() as block:
        block.sync(run)
    st.es.close()


def bcast_row(ap_row, nparts=128):
    return ap_row.partition_broadcast(nparts)


def stage_mod(g, cc, w_ada_l, b_ada_l, norm_g_l, modv):
    st = Stage(g, "mod")
    ccT = st.sb([128, 2, KC], F32, "ccT")
    sil = st.sb([128, 2, KC], F32, "sil")
    sig = st.sb([128, 2, KC], F32, "sig")
    NCH = 6 * D // 512
    wst = [st.sb([128, KC, 512], F32, "wada") for _ in range(2)]
    bb = [st.sb([2, 512], F32, "bada") for _ in range(2)]
    ng = [st.sb([2, 512], F32, "ng") for _ in range(2)]
    res = [st.sb([2, 512], F32, "res") for _ in range(2)]
    pst = [st.ps([128, 512], F32, "pmod") for _ in range(2)]
    for r in range(2):
        st.dma("sp", ccT[:, r, :], cc[r, :].rearrange("(k p) -> p k", p=128), wr=["ccT"], allow_slow_non_contiguous=True)
    st.add("act", lambda e: e.activation(out=sig[:], in_=ccT[:], func=AF.Sigmoid), rd=["ccT"], wr=["sig"])
    st.add("dve", lambda e: e.tensor_tensor(out=sil[:], in0=ccT[:], in1=sig[:], op=ALU.mult), rd=["ccT", "sig"], wr=["sil"])
    segmap = {0: 1, 1: 0, 2: 2, 3: 4, 4: 3, 5: 5}
    for n in range(NCH):
        i = n % 2
        w = wst[i]
        seg, cs = n // 4, (n % 4) * 512
        st.dma("sp", w[:], w_ada_l[:, n * 512:(n + 1) * 512].rearrange("(k p) c -> p k c", p=128), wr=["w%d" % i])
        for r in range(2):
            st.dma("sp", bb[i][r:r + 1, :], b_ada_l[:, n * 512:(n + 1) * 512], wr=["bb%d" % i])
            if seg in (1, 4):
                j = 0 if seg == 1 else 1
                st.dma("sp", ng[i][r:r + 1, :], norm_g_l[:, j * D + cs:j * D + cs + 512], wr=["ng%d" % i])
        for k in range(KC):
            st.add("pe", lambda e, w=w, k=k, i=i: e.matmul(pst[i][0:2, :], lhsT=sil[:, :, k], rhs=w[:, k, :],
                                                           start=(k == 0), stop=(k == KC - 1)),
                   rd=["sil", "w%d" % i], wr=["p%d" % i])
        st.add("dve", lambda e, i=i: e.tensor_tensor(out=res[i][:], in0=pst[i][0:2, :], in1=bb[i][:], op=ALU.add),
               rd=["p%d" % i, "bb%d" % i], wr=["res%d" % i])
        if seg in (1, 4):
            st.add("dve", lambda e, i=i: e.scalar_tensor_tensor(out=res[i][:], in0=res[i][:], scalar=1.0, in1=ng[i][:],
                                                                op0=ALU.add, op1=ALU.mult),
                   rd=["res%d" % i, "ng%d" % i], wr=["res%d" % i])
        o0 = segmap[seg] * D + cs
        st.dma("sp", modv[:, o0:o0 + 512], res[i][:], rd=["res%d" % i])
    st.emit()


def stage_norm(g, src, modv, jn, xnT, tiles, ident_bf):
    st = Stage(g, "norm")
    A = [st.sb([128, D], F32, "A") for _ in range(2)]
    B = [st.sb([128, D], F32, "B") for _ in range(2)]
    ident = st.sb([128, 128], BF16, "ident")
    st.dma("sp", ident[:], ident_bf, wr=["ident"])
    for r in range(2):
        st.dma("sp", A[r][:], bcast_row(modv[r:r + 1, (3 * jn) * D:(3 * jn + 1) * D]), wr=["A%d" % r])
        st.dma("sp", B[r][:], bcast_row(modv[r:r + 1, (3 * jn + 1) * D:(3 * jn + 2) * D]), wr=["B%d" % r])
    NB = 2
    xt = [st.sb([128, D], F32, "x") for _ in range(NB)]
    junk = [st.sb([128, D], F32, "junk") for _ in range(NB)]
    tmp = [st.sb([128, D], F32, "tmp") for _ in range(NB)]
    xb = [st.sb([128, D], BF16, "xb") for _ in range(NB)]
    xT = [st.sb([128, D], BF16, "xT") for _ in range(NB)]
    ss = [st.sb([128, 1], F32, "ss") for _ in range(NB)]
    rs = [st.sb([128, 1], F32, "rs") for _ in range(NB)]
    pt = [st.ps([128, D], BF16, "pT") for _ in range(NB)]
    for i, t in enumerate(tiles):
        b = i % NB
        r = 1 if t < 2 else 0
        st.dma("sp", xt[b][:], src[t * 128:(t + 1) * 128, :], wr=["x%d" % b])
        st.add("act", lambda e, b=b: e.activation(out=junk[b][:], in_=xt[b][:], func=AF.Square, accum_out=ss[b][:]),
               rd=["x%d" % b], wr=["junk%d" % b, "ss%d" % b])
        st.add("act", lambda e, b=b: e.activation(out=rs[b][:], in_=ss[b][:], func=AF.Sqrt, scale=1.0 / D, bias=EPS),
               rd=["ss%d" % b], wr=["rs%d" % b])
        st.add("dve", lambda e, b=b: e.reciprocal(out=rs[b][:], in_=rs[b][:]),
               rd=["rs%d" % b], wr=["rs%d" % b])
        st.add("dve", lambda e, b=b, r=r: e.scalar_tensor_tensor(out=tmp[b][:], in0=xt[b][:], scalar=rs[b][:], in1=A[r][:],
                                                                 op0=ALU.mult, op1=ALU.mult),
               rd=["x%d" % b, "rs%d" % b, "A%d" % r], wr=["tmp%d" % b])
        st.add("pool", lambda e, b=b, r=r: e.tensor_tensor(out=xb[b][:], in0=tmp[b][:], in1=B[r][:], op=ALU.add),
               rd=["tmp%d" % b, "B%d" % r], wr=["xb%d" % b])
        for k in range(KC):
            st.add("pe", lambda e, b=b, k=k: e.transpose(out=pt[b][:, k * 128:(k + 1) * 128], in_=xb[b][:, k * 128:(k + 1) * 128], identity=ident[:]),
                   rd=["xb%d" % b, "ident"], wr=["pt%d" % b])
        st.add("act", lambda e, b=b: e.copy(out=xT[b][:, 0:1024], in_=pt[b][:, 0:1024]), rd=["pt%d" % b], wr=["xTa%d" % b])
        st.add("dve", lambda e, b=b: e.tensor_copy(out=xT[b][:, 1024:2048], in_=pt[b][:, 1024:2048]), rd=["pt%d" % b], wr=["xTb%d" % b])
        st.dma("pool", xnT[t], xT[b][:], rd=["xTa%d" % b, "xTb%d" % b])
    st.emit()


def stage_linear(g, xT, tiles, W, N, out, oc0=0, w_bf=False, slab=1024):
    st = Stage(g, "lin")
    wst = [st.sb([128, KC, 512], F32, "wst") for _ in range(2)] if not w_bf else None
    wsl = [st.sb([128, KC, slab], BF16, "wsl") for _ in range(2)]
    NX = 3
    xt = [st.sb([128, D], BF16, "xt") for _ in range(NX)]
    ob = [st.sb([128, slab], F32, "ob") for _ in range(2)]
    pp = [st.ps([128, 512], F32, "pl") for _ in range(4)]
    nsl = (N + slab - 1) // slab
    cnt = 0
    hcnt = 0
    pcnt = 0
    for s in range(nsl):
        c0 = s * slab
        ns = min(slab, N - c0)
        w = wsl[s % 2]
        wk = "wsl%d" % (s % 2)
        for h0 in range(0, ns, 512):
            hw = min(512, ns - h0)
            src = W[:, c0 + h0:c0 + h0 + hw].rearrange("(k p) c -> p k c", p=128)
            if w_bf:
                st.dma("sp", w[:, :, h0:h0 + hw], src, wr=[wk + "_%d" % h0])
            else:
                ws = wst[hcnt % 2]
                wsk = "wst%d" % (hcnt % 2)
                st.dma("sp", ws[:, :, 0:hw], src, wr=[wsk])
                eng = "pool" if hcnt % 2 == 0 else "dve"
                st.add(eng, lambda e, w=w, ws=ws, h0=h0, hw=hw: e.tensor_copy(out=w[:, :, h0:h0 + hw], in_=ws[:, :, 0:hw]),
                       rd=[wsk], wr=[wk + "_%d" % h0])
                hcnt += 1
        for t in tiles:
            xb = cnt % NX
            o = ob[cnt % 2]
            ok = "ob%d" % (cnt % 2)
            st.dma("sp", xt[xb][:], xT[t], wr=["xt%d" % xb])
            for h0 in range(0, ns, 512):
                hw = min(512, ns - h0)
                p = pp[pcnt % 4]
                pk = "pp%d" % (pcnt % 4)
                for k in range(KC):
                    st.add("pe", lambda e, p=p, xb=xb, k=k, w=w, h0=h0, hw=hw: e.matmul(
                        p[:, 0:hw], lhsT=xt[xb][:, k * 128:(k + 1) * 128], rhs=w[:, k, h0:h0 + hw], start=(k == 0), stop=(k == KC - 1)),
                        rd=["xt%d" % xb, wk + "_%d" % h0], wr=[pk])
                if pcnt % 2 == 0:
                    st.add("act", lambda e, o=o, p=p, h0=h0, hw=hw: e.copy(out=o[:, h0:h0 + hw], in_=p[:, 0:hw]), rd=[pk], wr=[ok + "_%d" % h0])
                else:
                    st.add("dve", lambda e, o=o, p=p, h0=h0, hw=hw: e.tensor_copy(out=o[:, h0:h0 + hw], in_=p[:, 0:hw]), rd=[pk], wr=[ok + "_%d" % h0])
                pcnt += 1
            st.dma("pool", out[t * 128:(t + 1) * 128, oc0 + c0:oc0 + c0 + ns], o[:, 0:ns],
                   rd=[ok + "_%d" % h0 for h0 in range(0, ns, 512)])
            cnt += 1
    st.emit()


def bc3(ap2d, n):
    return ap2d.unsqueeze(2).broadcast_to([ap2d.shape[0], ap2d.shape[1], n])


def bcmid(ap2d, n):
    return ap2d.unsqueeze(1).broadcast_to([ap2d.shape[0], n, ap2d.shape[1]])


class TrOut:
    def __init__(self, st, ident, nb=2):
        self.st = st
        self.ident = ident
        self.nb = nb
        self.pt = [st.ps([128, D], BF16, "trp") for _ in range(nb)]
        self.xT = [st.sb([128, D], BF16, "trx") for _ in range(nb)]
        self.i = 0

    def go(self, src, srckeys, dst):
        st = self.st
        b = self.i % self.nb
        self.i += 1
        pt, xT, ident = self.pt[b], self.xT[b], self.ident
        for k in range(KC):
            st.add("pe", lambda e, k=k: e.transpose(out=pt[:, k * 128:(k + 1) * 128], in_=src[:, k * 128:(k + 1) * 128], identity=ident[:]),
                   rd=list(srckeys) + ["ident"], wr=["trp%d" % b])
        st.add("act", lambda e: e.copy(out=xT[:, 0:1024], in_=pt[:, 0:1024]), rd=["trp%d" % b], wr=["trxa%d" % b])
        st.add("dve", lambda e: e.tensor_copy(out=xT[:, 1024:2048], in_=pt[:, 1024:2048]), rd=["trp%d" % b], wr=["trxb%d" % b])
        st.dma("pool", dst, xT[:], rd=["trxa%d" % b, "trxb%d" % b])


def stage_mlstm_prep(g, p, tiles, cst, ident_bf, gate_b_l, rope, qT_s, kT_s, kb_s, v2f_s, v2b_s, gpk_s):
    st = Stage(g, "mprep")
    SC = 128.0 ** -0.5
    cs = st.sb([128, 512], F32, "cst")
    ident = st.sb([128, 128], BF16, "ident")
    gbias = st.sb([128, 32], F32, "gbias")
    st.dma("sp", cs[:], cst, wr=["cst"])
    st.dma("sp", ident[:], ident_bf, wr=["ident"])
    st.dma("sp", gbias[:], bcast_row(gate_b_l), wr=["gbias"])
    MF, MB, ONES = cs[:, 0:128], cs[:, 128:256], cs[:, 256:384]
    NB = 2
    q = [st.sb([128, 1024], F32, "q") for _ in range(NB)]
    k = [st.sb([128, 1024], F32, "k") for _ in range(NB)]
    v = [st.sb([128, 2048], F32, "v") for _ in range(NB)]
    gp = [st.sb([128, 32], F32, "gp") for _ in range(NB)]
    cq = [st.sb([128, 128], F32, "cq") for _ in range(NB)]
    sq = [st.sb([128, 128], F32, "sq") for _ in range(NB)]
    ck = [st.sb([128, 128], F32, "ck") for _ in range(NB)]
    sk = [st.sb([128, 128], F32, "sk") for _ in range(NB)]
    t1d = {n: [st.sb([128, 1024], F32, "t1" + n) for _ in range(NB)] for n in ("q", "k")}
    t2d = {n: [st.sb([128, 1024], F32, "t2" + n) for _ in range(NB)] for n in ("q", "k")}
    qb = [st.sb([128, 1024], BF16, "qb") for _ in range(NB)]
    kb = [st.sb([128, 1024], BF16, "kb") for _ in range(NB)]
    qTs = [st.sb([128, 1024], BF16, "qTs") for _ in range(NB)]
    kTs = [st.sb([128, 1024], BF16, "kTs") for _ in range(NB)]
    v2f = [st.sb([128, 8, 257], BF16, "v2f") for _ in range(NB)]
    v2b = [st.sb([128, 8, 257], BF16, "v2b") for _ in range(NB)]
    lf = [st.sb([128, 16], F32, "lf") for _ in range(NB)]
    gk = [st.sb([128, 48], F32, "gk") for _ in range(NB)]
    tmp = [st.sb([128, 16], F32, "tmpg") for _ in range(NB)]
    pq = [st.ps([128, 1024], BF16, "pq") for _ in range(NB)]
    pk = [st.ps([128, 1024], BF16, "pk") for _ in range(NB)]
    pg = [st.ps([128, 32], F32, "pg") for _ in range(NB)]
    cosq, sinq, cosk, sink = rope
    for i, t in enumerate(tiles):
        b = i % NB
        B = "%d" % b
        rows = slice(t * 128, (t + 1) * 128)
        st.dma("sp", q[b][:], p[rows, O_AQ:O_AQ + 1024], wr=["q" + B])
        st.dma("sp", k[b][:], p[rows, O_AK:O_AK + 1024], wr=["k" + B])
        st.dma("sp", v[b][:], p[rows, O_AV:O_AV + 2048], wr=["v" + B])
        st.dma("sp", gp[b][:], p[rows, O_AG:O_AG + 32], wr=["gp" + B])
        if t >= 2:
            lr = slice((t - 2) * 128, (t - 1) * 128)
            st.dma("sp", cq[b][:], cosq[lr, :], wr=["cq" + B])
            st.dma("sp", sq[b][:], sinq[lr, :], wr=["sq" + B])
            st.dma("sp", ck[b][:], cosk[lr, :], wr=["ck" + B])
            st.dma("sp", sk[b][:], sink[lr, :], wr=["sk" + B])
            for (src, cc_, ss_, dst, nm, eng) in ((q, cq, sq, qb, "q", "dve"), (k, ck, sk, kb, "k", "pool")):
                s4 = src[b][:].rearrange("p (h f u j) -> p h f u j", h=8, f=2, u=2, j=32)
                t1v, t2v = t1d[nm][b], t2d[nm][b]
                d4 = t2v[:].rearrange("p (h f u j) -> p h f u j", h=8, f=2, u=2, j=32)
                nsn = ss_[b][:, 0:64].rearrange("p (f j) -> p f j", f=2).unsqueeze(1).broadcast_to([128, 8, 2, 32])
                psn = ss_[b][:, 64:128].rearrange("p (f j) -> p f j", f=2).unsqueeze(1).broadcast_to([128, 8, 2, 32])
                tk1, tk2 = "t1" + nm + B, "t2" + nm + B
                st.op(eng, "tensor_tensor", rd=[nm + B, "c" + nm + B], wr=[tk1],
                      out=t1v[:].rearrange("p (h d) -> p h d", h=8), in0=src[b][:].rearrange("p (h d) -> p h d", h=8),
                      in1=bcmid(cc_[b][:], 8), op=ALU.mult)
                st.op(eng, "tensor_tensor", rd=[nm + B, "s" + nm + B], wr=[tk2],
                      out=d4[:, :, :, 0, :], in0=s4[:, :, :, 1, :], in1=nsn, op=ALU.mult)
                st.op(eng, "tensor_tensor", rd=[nm + B, "s" + nm + B], wr=[tk2],
                      out=d4[:, :, :, 1, :], in0=s4[:, :, :, 0, :], in1=psn, op=ALU.mult)
                st.op(eng, "tensor_tensor", rd=[tk1, tk2], wr=[nm + "b" + B], out=dst[b][:], in0=t1v[:], in1=t2v[:], op=ALU.add)
        else:
            st.op("act", "mul", rd=["q" + B], wr=["qb" + B], out=qb[b][:], in_=q[b][:], mul=SC)
            st.op("dve", "tensor_copy", rd=["k" + B], wr=["kb" + B], out=kb[b][:], in_=k[b][:])
        for h in range(8):
            st.op("pe", "transpose", rd=["qb" + B, "ident"], wr=["pq" + B],
                  out=pq[b][:, h * 128:(h + 1) * 128], in_=qb[b][:, h * 128:(h + 1) * 128], identity=ident[:])
        st.op("act", "copy", rd=["pq" + B], wr=["qTs" + B], out=qTs[b][:], in_=pq[b][:])
        for h in range(8):
            st.op("pe", "transpose", rd=["kb" + B, "ident"], wr=["pk" + B],
                  out=pk[b][:, h * 128:(h + 1) * 128], in_=kb[b][:, h * 128:(h + 1) * 128], identity=ident[:])
        st.op("dve", "tensor_copy", rd=["pk" + B], wr=["kTs" + B], out=kTs[b][:], in_=pk[b][:])
        st.dma("pool", qT_s[t], qTs[b][:], rd=["qTs" + B])
        st.dma("pool", kT_s[t], kTs[b][:], rd=["kTs" + B])
        st.dma("pool", kb_s[rows, :], kb[b][:], rd=["kb" + B])
        st.op("dve", "tensor_tensor", rd=["gp" + B, "gbias"], wr=["gp" + B], out=gp[b][:], in0=gp[b][:], in1=gbias[:], op=ALU.add)
        st.op("act", "activation", rd=["gp" + B], wr=["lf" + B], out=lf[b][:, 0:8], in_=gp[b][:, 8:16], func=AF.Exp, scale=-1.0)
        st.op("act", "activation", rd=["gp" + B], wr=["lf" + B], out=lf[b][:, 8:16], in_=gp[b][:, 24:32], func=AF.Exp, scale=-1.0)
        st.op("act", "activation", rd=["lf" + B], wr=["lf" + B], out=lf[b][:], in_=lf[b][:], func=AF.Ln, bias=1.0)
        st.op("dve", "tensor_scalar", rd=["lf" + B], wr=["lf" + B], out=lf[b][:], in0=lf[b][:], scalar1=-1.0, scalar2=None, op0=ALU.mult)
        st.op("pe", "matmul", rd=["lf" + B, "cst"], wr=["pg" + B], out=pg[b][:, 0:8], lhsT=MF, rhs=lf[b][:, 0:8], start=True, stop=True)
        st.op("pe", "matmul", rd=["lf" + B, "cst"], wr=["pg" + B], out=pg[b][:, 8:16], lhsT=MB, rhs=lf[b][:, 8:16], start=True, stop=True)
        st.op("pe", "matmul", rd=["lf" + B, "cst"], wr=["pg" + B], out=pg[b][:, 16:32], lhsT=ONES, rhs=lf[b][:, 0:16], start=True, stop=True)
        st.op("dve", "tensor_tensor", rd=["gp" + B, "pg" + B], wr=["tmp" + B], out=tmp[b][:, 0:8], in0=gp[b][:, 0:8], in1=pg[b][:, 0:8], op=ALU.subtract)
        st.op("dve", "tensor_tensor", rd=["gp" + B, "pg" + B], wr=["tmp" + B], out=tmp[b][:, 8:16], in0=gp[b][:, 16:24], in1=pg[b][:, 8:16], op=ALU.subtract)
        st.op("act", "activation", rd=["tmp" + B], wr=["gk" + B], out=gk[b][:, 0:8], in_=tmp[b][:, 0:8], func=AF.Exp)
        st.op("act", "activation", rd=["tmp" + B], wr=["gk" + B], out=gk[b][:, 16:24], in_=tmp[b][:, 8:16], func=AF.Exp)
        st.op("act", "activation", rd=["pg" + B], wr=["gk" + B], out=gk[b][:, 8:16], in_=pg[b][:, 0:8], func=AF.Exp)
        st.op("act", "activation", rd=["pg" + B], wr=["gk" + B], out=gk[b][:, 24:32], in_=pg[b][:, 8:16], func=AF.Exp)
        st.op("act", "activation", rd=["pg" + B], wr=["gk" + B], out=gk[b][:, 32:48], in_=pg[b][:, 16:32], func=AF.Exp)
        st.dma("pool", gpk_s[rows, :], gk[b][:], rd=["gk" + B])
        v3 = v[b][:].rearrange("p (h d) -> p h d", h=8)
        st.op("dve", "tensor_tensor", rd=["v" + B, "gk" + B], wr=["v2f" + B], out=v2f[b][:, :, 0:256], in0=v3, in1=bc3(gk[b][:, 0:8], 256), op=ALU.mult)
        st.op("pool", "tensor_tensor", rd=["v" + B, "gk" + B], wr=["v2b" + B], out=v2b[b][:, :, 0:256], in0=v3, in1=bc3(gk[b][:, 16:24], 256), op=ALU.mult)
        st.op("dve", "tensor_copy", rd=["gk" + B], wr=["v2f" + B], out=v2f[b][:, :, 256], in_=gk[b][:, 0:8])
        st.op("pool", "tensor_copy", rd=["gk" + B], wr=["v2b" + B], out=v2b[b][:, :, 256], in_=gk[b][:, 16:24])
        st.dma("pool", v2f_s[rows, :], v2f[b][:].rearrange("p h d -> p (h d)"), rd=["v2f" + B])
        st.dma("pool", v2b_s[rows, :], v2b[b][:].rearrange("p h d -> p (h d)"), rd=["v2b" + B])
    st.emit()


def stage_mlstm_scan(g, bwd, order, cst, ident_bf, qT_s, kT_s, kb_s, v2_s, gpk_s, hf_s, p=None, hnorm_l=None, yaT=None, skip_out=()):
    st = Stage(g, "mscan")
    cs = st.sb([128, 512], F32, "cst")
    st.dma("sp", cs[:], cst, wr=["cst"])
    MASK = cs[:, 128:256] if bwd else cs[:, 0:128]
    o_ebc = 24 if bwd else 8
    o_eB = 40 if bwd else 32
    C32 = st.sb([128, 8, 257], F32, "C32")
    Cb = st.sb([128, 8, 257], BF16, "Cb")
    st.op("dve", "memset", wr=["C32_%d" % h for h in range(8)], ap=C32[:], constant=0.0)
    st.op("pool", "memset", wr=["Cb_%d" % h for h in range(8)], ap=Cb[:], constant=0.0)
    NB = 2
    qT = [st.sb([128, 1024], BF16, "qT") for _ in range(NB)]
    kT = [st.sb([128, 1024], BF16, "kT") for _ in range(NB)]
    kb = [st.sb([128, 1024], BF16, "kb") for _ in range(NB)]
    v2 = [st.sb([128, 8, 257], BF16, "v2") for _ in range(NB)]
    gk = [st.sb([128, 48], F32, "gk") for _ in range(NB)]
    hb = [st.sb([128, 2048], F32, "hb") for _ in range(NB)]
    sm = [st.sb([128, 128], BF16, "sm") for _ in range(2)]
    dd = [st.sb([128, 4], F32, "dd") for _ in range(2)]
    ps_s = [st.ps([128, 512], F32, "ps_s") for _ in range(2)]
    ps_o = [st.ps([128, 512], F32, "ps_o") for _ in range(2)]
    ps_c = [st.ps([128, 512], F32, "ps_c") for _ in range(2)]
    if bwd:
        ident = st.sb([128, 128], BF16, "ident")
        st.dma("sp", ident[:], ident_bf, wr=["ident"])
        HN = st.sb([128, 2048], F32, "HN")
        st.dma("sp", HN[:], bcast_row(hnorm_l), wr=["HN"])
        hf = st.sb([128, 2048], F32, "hf")
        og = st.sb([128, 2048], F32, "og")
        sq = st.sb([128, 2048], F32, "sq")
        yab = st.sb([128, 2048], BF16, "yab")
        ms = st.sb([128, 8], F32, "ms")
        tr = TrOut(st, ident, nb=1)
    hc = 0
    for i, t in enumerate(order):
        b = i % NB
        B = "%d" % b
        rows = slice(t * 128, (t + 1) * 128)
        st.dma("sp", qT[b][:], qT_s[t], wr=["qT" + B])
        st.dma("sp", kT[b][:], kT_s[t], wr=["kT" + B])
        st.dma("sp", kb[b][:], kb_s[rows, :], wr=["kb" + B])
        st.dma("sp", v2[b][:].rearrange("p h d -> p (h d)"), v2_s[rows, :], wr=["v2" + B])
        st.dma("sp", gk[b][:], gpk_s[rows, :], wr=["gk" + B])
        def smat(h_, j_):
            hs_ = slice(h_ * 128, (h_ + 1) * 128)
            st.op("pe", "matmul", rd=["kT" + B, "qT" + B], wr=["ps_s%d" % j_], out=ps_s[j_][:, 0:128], lhsT=kT[b][:, hs_], rhs=qT[b][:, hs_], start=True, stop=True)

        smat(0, hc % 2)
        for h in range(8):
            j = hc % 2
            J = "%d" % j
            hc += 1
            hs = slice(h * 128, (h + 1) * 128)
            if h + 1 < 8:
                smat(h + 1, hc % 2)
            st.op("dve", "tensor_tensor", rd=["ps_s" + J, "cst"], wr=["sm" + J], out=sm[j][:], in0=ps_s[j][:, 0:128], in1=MASK, op=ALU.mult)
            st.op("pe", "matmul", rd=["qT" + B, "Cb_%d" % h], wr=["ps_o" + J], out=ps_o[j][:, 0:257], lhsT=qT[b][:, hs], rhs=Cb[:, h, :], start=True, stop=False)
            st.op("pe", "matmul", rd=["sm" + J, "v2" + B], wr=["ps_o" + J], out=ps_o[j][:, 0:257], lhsT=sm[j][:], rhs=v2[b][:, h, :], start=False, stop=True)
            ebc = gk[b][:, o_ebc + h:o_ebc + h + 1]
            st.op("dve", "tensor_scalar", rd=["ps_o" + J, "gk" + B], wr=["dd" + J], out=dd[j][:, 3:4], in0=ps_o[j][:, 256:257], scalar1=ebc, scalar2=-1.0,
                  op0=ALU.mult, op1=ALU.mult)
            st.op("dve", "tensor_scalar", rd=["ps_o" + J, "gk" + B], wr=["dd" + J], out=dd[j][:, 0:1], in0=ps_o[j][:, 256:257], scalar1=ebc, scalar2=1.0,
                  op0=ALU.mult, op1=ALU.max)
            st.op("dve", "tensor_tensor", rd=["dd" + J], wr=["dd" + J], out=dd[j][:, 0:1], in0=dd[j][:, 0:1], in1=dd[j][:, 3:4], op=ALU.max)
            st.op("dve", "reciprocal", rd=["dd" + J], wr=["dd" + J], out=dd[j][:, 1:2], in_=dd[j][:, 0:1])
            st.op("dve", "tensor_tensor", rd=["dd" + J, "gk" + B], wr=["dd" + J], out=dd[j][:, 2:3], in0=dd[j][:, 1:2], in1=ebc, op=ALU.mult)
            st.op("act", "activation", rd=["ps_o" + J, "dd" + J], wr=["hb" + B], out=hb[b][:, h * 256:(h + 1) * 256], in_=ps_o[j][:, 0:256],
                  func=AF.Identity, scale=dd[j][:, 2:3])
            st.op("pe", "matmul", rd=["kb" + B, "v2" + B], wr=["ps_c" + J], out=ps_c[j][:, 0:257], lhsT=kb[b][:, hs], rhs=v2[b][:, h, :], start=True, stop=True)
            st.op("dve", "tensor_tensor", rd=["ps_c" + J, "C32_%d" % h], wr=["C32_%d" % h], out=C32[:, h, :], in0=ps_c[j][:, 0:257], in1=C32[:, h, :], op=ALU.add)
            st.op("pool", "tensor_scalar", rd=["C32_%d" % h, "gk" + B], wr=["C32_%d" % h], out=C32[:, h, :], in0=C32[:, h, :],
                  scalar1=gk[b][:, o_eB + h:o_eB + h + 1], scalar2=None, op0=ALU.mult)
            st.op("act", "copy", rd=["C32_%d" % h], wr=["Cb_%d" % h], out=Cb[:, h, :], in_=C32[:, h, :])
        if not bwd:
            st.dma("pool", hf_s[rows, :], hb[b][:], rd=["hb" + B])
        elif t not in skip_out:
            st.dma("sp", hf[:], hf_s[rows, :], wr=["hf"])
            st.dma("sp", og[:], p[rows, O_AO:O_AO + 2048], wr=["og"])
            st.op("dve", "tensor_tensor", rd=["hb" + B, "hf"], wr=["hf"], out=hf[:], in0=hb[b][:], in1=hf[:], op=ALU.add)
            st.op("act", "activation", rd=["hf"], wr=["sq"], out=sq[:], in_=hf[:], func=AF.Square)
            st.op("dve", "tensor_reduce", rd=["sq"], wr=["ms"], out=ms[:], in_=sq[:].rearrange("p (h d) -> p h d", h=8), axis=AX.X, op=ALU.add)
            st.op("act", "activation", rd=["ms"], wr=["ms"], out=ms[:], in_=ms[:], func=AF.Sqrt, scale=1.0 / 256, bias=EPS)
            st.op("dve", "reciprocal", rd=["ms"], wr=["ms"], out=ms[:], in_=ms[:])
            st.op("pool", "tensor_tensor", rd=["hf", "ms"], wr=["hf"], out=hf[:].rearrange("p (h d) -> p h d", h=8),
                  in0=hf[:].rearrange("p (h d) -> p h d", h=8), in1=bc3(ms[:], 256), op=ALU.mult)
            st.op("act", "activation", rd=["og"], wr=["og"], out=og[:], in_=og[:], func=AF.Sigmoid)
            st.op("dve", "tensor_tensor", rd=["hf", "HN"], wr=["hf"], out=hf[:], in0=hf[:], in1=HN[:], op=ALU.mult)
            st.op("pool", "tensor_tensor", rd=["hf", "og"], wr=["yab"], out=yab[:], in0=hf[:], in1=og[:], op=ALU.mult)
            tr.go(yab, ["yab"], yaT[t])
    st.emit()


def host_consts(n_lat_tokens):
    import ml_dtypes
    s = np.arange(128)
    MF = (s[:, None] <= s[None, :]).astype(np.float32)
    MB = (s[:, None] >= s[None, :]).astype(np.float32)
    cst = np.concatenate([MF, MB, np.ones((128, 128), np.float32), np.eye(128, dtype=np.float32)], axis=1)
    ident_bf = np.eye(128).astype(ml_dtypes.bfloat16)
    nf = 32
    inv = (10000.0 ** (-np.arange(nf, dtype=np.float32) / nf)).astype(np.float32)
    pos = np.arange(n_lat_tokens)
    ang_r = (pos // 64).astype(np.float32)[:, None] * inv
    ang_c = (pos % 64).astype(np.float32)[:, None] * inv
    cr, sr, cc_, sc_ = np.cos(ang_r), np.sin(ang_r), np.cos(ang_c), np.sin(ang_c)
    cosf = np.concatenate([cr, cr, cc_, cc_], axis=1).astype(np.float32)
    sinf = np.concatenate([-sr, -sc_, sr, sc_], axis=1).astype(np.float32)
    SC = np.float32(128.0 ** -0.5)
    return dict(cst=cst, ident_bf=ident_bf, cosq=cosf * SC, sinq=sinf * SC, cosk=cosf, sink=sinf)


def stage_cast_T(g, src, tiles, ident_bf, dstT):
    st = Stage(g, "castT")
    ident = st.sb([128, 128], BF16, "ident")
    st.dma("sp", ident[:], ident_bf, wr=["ident"])
    x = [st.sb([128, D], F32, "x") for _ in range(2)]
    xb = [st.sb([128, D], BF16, "xb") for _ in range(2)]
    tr = TrOut(st, ident, nb=2)
    for i, t in enumerate(tiles):
        b = i % 2
        B = "%d" % b
        st.dma("sp", x[b][:], src[t * 128:(t + 1) * 128, :], wr=["x" + B])
        st.op("pool" if b else "dve", "tensor_copy", rd=["x" + B], wr=["xb" + B], out=xb[b][:], in_=x[b][:])
        tr.go(xb[b], ["xb" + B], dstT[t])
    st.emit()


def stage_conv(g, p, tiles, NT, conv_l, ident_bf, ybT):
    st = Stage(g, "conv")
    ident = st.sb([128, 128], BF16, "ident")
    st.dma("sp", ident[:], ident_bf, wr=["ident"])
    W = [st.sb([128, D], F32, "cw") for _ in range(3)]
    for j in range(3):
        st.dma("sp", W[j][:], bcast_row(conv_l[j:j + 1, :]), wr=["W%d" % j])
    NB = 2
    names = ["c0", "x0", "bb", "cm", "xm", "cp", "xp"]
    buf = {n: [st.sb([128, D], F32, n) for _ in range(NB)] for n in names}
    yb = [st.sb([128, D], BF16, "yb") for _ in range(NB)]
    tr = TrOut(st, ident, nb=2)
    for i, t in enumerate(tiles):
        b = i % NB
        B = "%d" % b
        r0 = t * 128
        first = t in (0, 2)
        last = t in (1, NT - 1)
        T = {n: buf[n][b] for n in names}
        K = {n: n + B for n in names}
        st.dma("sp", T["c0"][:], p[r0:r0 + 128, O_BC:O_BC + D], wr=[K["c0"]])
        st.dma("sp", T["x0"][:], p[r0:r0 + 128, O_BX:O_BX + D], wr=[K["x0"]])
        st.dma("sp", T["bb"][:], p[r0:r0 + 128, O_BB:O_BB + D], wr=[K["bb"]])
        for (cn, xn, off, edge) in (("cm", "xm", -1, first), ("cp", "xp", 1, last)):
            for (n, col) in ((cn, O_BC), (xn, O_BX)):
                if not edge:
                    st.dma("sp", T[n][:], p[r0 + off:r0 + off + 128, col:col + D], wr=[K[n]])
                else:
                    st.op("pool", "memset", wr=[K[n]], ap=T[n][:], constant=0.0)
                    if off < 0:
                        st.dma("sp", T[n][1:128, :], p[r0:r0 + 127, col:col + D], wr=[K[n]])
                    else:
                        st.dma("sp", T[n][0:127, :], p[r0 + 1:r0 + 128, col:col + D], wr=[K[n]])
        tt = lambda eng, o, a, bb_, op, rd, wr: st.op(eng, "tensor_tensor", rd=rd, wr=wr, out=o, in0=a, in1=bb_, op=op)
        tt("dve", T["x0"][:], T["c0"][:], T["x0"][:], ALU.mult, [K["c0"], K["x0"]], [K["x0"]])
        tt("pool", T["xm"][:], T["cm"][:], T["xm"][:], ALU.mult, [K["cm"], K["xm"]], [K["xm"]])
        tt("pool", T["xp"][:], T["cp"][:], T["xp"][:], ALU.mult, [K["cp"], K["xp"]], [K["xp"]])
        tt("dve", T["xm"][:], T["xm"][:], W[0][:], ALU.mult, [K["xm"], "W0"], [K["xm"]])
        tt("pool", T["x0"][:], T["x0"][:], W[1][:], ALU.mult, [K["x0"], "W1"], [K["x0"]])
        tt("dve", T["xp"][:], T["xp"][:], W[2][:], ALU.mult, [K["xp"], "W2"], [K["xp"]])
        tt("pool", T["x0"][:], T["x0"][:], T["xm"][:], ALU.add, [K["x0"], K["xm"]], [K["x0"]])
        tt("dve", T["x0"][:], T["x0"][:], T["xp"][:], ALU.add, [K["x0"], K["xp"]], [K["x0"]])
        tt("dve", yb[b][:], T["x0"][:], T["bb"][:], ALU.mult, [K["x0"], K["bb"]], ["yb" + B])
        tr.go(yb[b], ["yb" + B], ybT[t])
    st.emit()


def na_plan(ROWS):
    kbs, cls, keys = [], [], {}
    for i in range(ROWS // 2):
        r = 2 * i
        kb = min(max(r - 4, 0), ROWS - 9)
        r0a = min(max(r - 4, 0), ROWS - 8)
        r0b = min(max(r + 1 - 4, 0), ROWS - 8)
        key = (r - kb, r0a - kb, r0b - kb)
        if key not in keys:
            keys[key] = len(keys)
        kbs.append(kb)
        cls.append(keys[key])
    return kbs, cls, list(keys.keys())


def na_bias_host(rpb_l, ROWS):
    _, _, keys = na_plan(ROWS)
    qrow = np.arange(128) // 64
    qc = np.arange(128) % 64
    j = np.arange(576) // 64
    kc = np.arange(576) % 64
    col0 = np.clip(qc - 8, 0, 48)
    colok = (kc[None, :] >= col0[:, None]) & (kc[None, :] < col0[:, None] + 16)
    dc = np.clip(kc[None, :] - qc[:, None] + 15, 0, 30)
    out = np.full((16, len(keys), 128, 5, 128), -30000.0, np.float32)
    for ci, (rel, a, b) in enumerate(keys):
        lo = np.where(qrow == 0, a, b)
        rowok = (j[None, :] >= lo[:, None]) & (j[None, :] < lo[:, None] + 8)
        dr = np.clip(j[None, :] - rel - qrow[:, None] + 7, 0, 14)
        ok = rowok & colok
        Tm = np.where(ok[None], rpb_l[:, dr, dc], np.float32(-30000.0))
        TT = np.full((16, 640, 128), -30000.0, np.float32)
        TT[:, :576, :] = Tm.transpose(0, 2, 1)
        out[:, ci] = TT.reshape(16, 5, 128, 128).transpose(0, 2, 1, 3)
    return out.reshape(16, len(keys), 128, 640)


def stage_na_prep(g, p, tiles, qkg_l, ident_bf, cqT_s, ckT_s, cv1_s):
    st = Stage(g, "naprep")
    ident = st.sb([128, 128], BF16, "ident")
    st.dma("sp", ident[:], ident_bf, wr=["ident"])
    GQ = st.sb([128, D], F32, "GQ")
    GK = st.sb([128, D], F32, "GK")
    for h in range(16):
        st.dma("sp", GQ[:, h * 128:(h + 1) * 128], bcast_row(qkg_l[0:1, :]), wr=["GQ"])
        st.dma("sp", GK[:, h * 128:(h + 1) * 128], bcast_row(qkg_l[1:2, :]), wr=["GK"])
    st.op("dve", "tensor_scalar", rd=["GQ"], wr=["GQ"], out=GQ[:], in0=GQ[:], scalar1=128.0 ** -0.5, scalar2=None, op0=ALU.mult)
    NB = 2
    x = {n: [st.sb([128, D], F32, n) for _ in range(NB)] for n in ("q", "k", "v")}
    sq = [st.sb([128, D], F32, "sq") for _ in range(NB)]
    ss = {n: [st.sb([128, 16], F32, "ss" + n) for _ in range(NB)] for n in ("q", "k")}
    xb = {n: [st.sb([128, D], BF16, "xb" + n) for _ in range(NB)] for n in ("q", "k")}
    v1 = [st.sb([128, 16, 129], BF16, "v1") for _ in range(NB)]
    for b in range(NB):
        st.op("pool", "memset", wr=["v1%d" % b], ap=v1[b][:], constant=1.0)
    tr = TrOut(st, ident, nb=2)
    for i, t in enumerate(tiles):
        b = i % NB
        B = "%d" % b
        rows = slice(t * 128, (t + 1) * 128)
        st.dma("sp", x["q"][b][:], p[rows, O_CQ:O_CQ + D], wr=["q" + B])
        st.dma("sp", x["k"][b][:], p[rows, O_CK:O_CK + D], wr=["k" + B])
        st.dma("sp", x["v"][b][:], p[rows, O_CV:O_CV + D], wr=["v" + B])
        for n, Gt, gk_, dst in (("q", GQ, "GQ", cqT_s), ("k", GK, "GK", ckT_s)):
            xt = x[n][b]
            s_ = ss[n][b]
            st.op("act", "activation", rd=[n + B], wr=["sq" + B], out=sq[b][:], in_=xt[:], func=AF.Square)
            st.op("dve", "tensor_reduce", rd=["sq" + B], wr=["ss" + n + B], out=s_[:], in_=sq[b][:].rearrange("p (h d) -> p h d", h=16), axis=AX.X, op=ALU.add)
            st.op("act", "activation", rd=["ss" + n + B], wr=["ss" + n + B], out=s_[:], in_=s_[:], func=AF.Sqrt, scale=1.0 / 128, bias=EPS)
            st.op("dve", "reciprocal", rd=["ss" + n + B], wr=["ss" + n + B], out=s_[:], in_=s_[:])
            st.op("pool", "tensor_tensor", rd=[n + B, "ss" + n + B], wr=[n + B], out=xt[:].rearrange("p (h d) -> p h d", h=16),
                  in0=xt[:].rearrange("p (h d) -> p h d", h=16), in1=bc3(s_[:], 128), op=ALU.mult)
            st.op("dve", "tensor_tensor", rd=[n + B, gk_], wr=["xb" + n + B], out=xb[n][b][:], in0=xt[:], in1=Gt[:], op=ALU.mult)
            tr.go(xb[n][b], ["xb" + n + B], dst[t])
        st.op("pool", "tensor_copy", rd=["v" + B], wr=["v1" + B], out=v1[b][:, :, 0:128], in_=x["v"][b][:].rearrange("p (h d) -> p h d", h=16))
        st.dma("pool", cv1_s[rows, :], v1[b][:].rearrange("p h d -> p (h d)"), rd=["v1" + B])
    st.emit()


def stage_na(g, NL, need_ctx, nab_l, ident_bf, cqT_s, ckT_s, cv1_s, yc_s):
    st = Stage(g, "na")
    NT = NL + 2
    ROWS = 2 * NL
    kbs, cls, keys = na_plan(ROWS)
    NC = len(keys)
    ident = st.sb([128, 128], BF16, "ident")
    st.dma("sp", ident[:], ident_bf, wr=["ident"])
    nm8 = st.sb([128, 1], F32, "nm8")
    st.op("dve", "memset", wr=["nm8"], ap=nm8[:], constant=-8.0)
    kTh = st.sb([128, NT * 128], BF16, "kTh")
    qTh = st.sb([128, NT * 128], BF16, "qTh")
    va = st.sb([128, NT, 129], BF16, "va")
    vb = st.sb([128, NL, 129], BF16, "vb")
    bst = st.sb([128, NC, 640], F32, "bst")
    bT = st.sb([128, NC, 640], BF16, "bT")
    E = [st.sb([128, 1024], BF16, "E") for _ in range(2)]
    ob = [st.sb([128, 128], F32, "ob") for _ in range(2)]
    rr = [st.sb([128, 1], F32, "rr") for _ in range(2)]
    ps = [st.ps([128, 1024], F32, "nps") for _ in range(2)]
    po = [st.ps([128, 512], F32, "npo") for _ in range(2)]
    cv3 = cv1_s.rearrange("(t p) (h c) -> p t h c", p=128, h=16)
    cnt = 0
    for h in range(16):
        hs = slice(h * 128, (h + 1) * 128)
        st.dma("sp", kTh[:].rearrange("p (t c) -> p t c", t=NT), ckT_s[:, :, hs].rearrange("t p c -> p t c"), wr=["kTh"])
        st.dma("sp", qTh[:].rearrange("p (t c) -> p t c", t=NT), cqT_s[:, :, hs].rearrange("t p c -> p t c"), wr=["qTh"])
        st.dma("sp", va[:], cv3[:, :, h, :], wr=["va"])
        st.dma("sp", vb[:, 0:NL - 1, :], cv1_s[256 + 64:256 + 64 + (NL - 1) * 128, h * 129:(h + 1) * 129].rearrange("(t p) c -> p t c", p=128), wr=["vb"])
        st.dma("sp", vb[0:64, NL - 1, :], cv1_s[256 + 64 + (NL - 1) * 128:256 + NL * 128, h * 129:(h + 1) * 129], wr=["vb"])
        st.dma("sp", bst[:], nab_l[h].rearrange("c p x -> p c x"), wr=["bst"])
        st.op("pool", "tensor_copy", rd=["bst"], wr=["bT"], out=bT[:], in_=bst[:])
        qtiles = ([0, 1] if need_ctx else []) + list(range(2, NT))

        def qk(t, j):
            J = "%d" % j
            q_ = qTh[:, t * 128:(t + 1) * 128]
            pj = ps[j]
            if t >= 2:
                i = t - 2
                kb, c_ = kbs[i], cls[i]
                tok0 = 256 + kb * 64
                for c in range(5):
                    kn = 128 if c < 4 else 64
                    st.op("pe", "matmul", rd=["kTh", "qTh"], wr=["ps" + J], out=pj[0:kn, c * 128:(c + 1) * 128],
                          lhsT=kTh[:, tok0 + c * 128:tok0 + c * 128 + kn], rhs=q_, start=True, stop=False)
                    st.op("pe", "matmul", rd=["ident", "bT"], wr=["ps" + J], out=pj[0:kn, c * 128:(c + 1) * 128],
                          lhsT=ident[0:kn, 0:kn], rhs=bT[0:kn, c_, c * 128:(c + 1) * 128], start=False, stop=True)
            for c in range(2):
                st.op("pe", "matmul", rd=["kTh", "qTh"], wr=["ps" + J], out=pj[:, (5 + c) * 128:(6 + c) * 128],
                      lhsT=kTh[:, c * 128:(c + 1) * 128], rhs=q_, start=True, stop=True)

        def rest(t, j):
            J = "%d" % j
            pj = ps[j]
            if t >= 2:
                kb = kbs[t - 2]
                st.op("act", "activation", rd=["ps" + J, "nm8"], wr=["E" + J], out=E[j][:, 0:512], in_=pj[:, 0:512], func=AF.Exp, bias=nm8[:])
                st.op("act", "activation", rd=["ps" + J, "nm8"], wr=["E" + J], out=E[j][0:64, 512:640], in_=pj[0:64, 512:640], func=AF.Exp, bias=nm8[0:64, :])
            st.op("act", "activation", rd=["ps" + J, "nm8"], wr=["E" + J], out=E[j][:, 640:896], in_=pj[:, 640:896], func=AF.Exp, bias=nm8[:])
            chunks = []
            if t >= 2:
                for c in range(5):
                    kn = 128 if c < 4 else 64
                    if kb % 2 == 0:
                        vv = va[0:kn, 2 + kb // 2 + c, :]
                    else:
                        vv = vb[0:kn, (kb - 1) // 2 + c, :]
                    chunks.append((E[j][0:kn, c * 128:(c + 1) * 128], vv))
            for c in range(2):
                chunks.append((E[j][:, (5 + c) * 128:(6 + c) * 128], va[:, c, :]))
            for ci, (l_, r_) in enumerate(chunks):
                st.op("pe", "matmul", rd=["E" + J, "va", "vb"], wr=["po" + J], out=po[j][:, 0:129], lhsT=l_, rhs=r_,
                      start=(ci == 0), stop=(ci == len(chunks) - 1))
            st.op("dve", "reciprocal", rd=["po" + J], wr=["rr" + J], out=rr[j][:], in_=po[j][:, 128:129])
            st.op("dve", "tensor_scalar", rd=["po" + J, "rr" + J], wr=["ob" + J], out=ob[j][:], in0=po[j][:, 0:128], scalar1=rr[j][:], scalar2=None, op0=ALU.mult)
            st.dma("pool", yc_s[t * 128:(t + 1) * 128, hs], ob[j][:], rd=["ob" + J])

        qk(qtiles[0], cnt % 2)
        for n, t in enumerate(qtiles):
            if n + 1 < len(qtiles):
                qk(qtiles[n + 1], (cnt + 1) % 2)
            rest(t, cnt % 2)
            cnt += 1
    st.emit()


def stage_gate(g, p, tiles, brs, ident_bf, yT):
    st = Stage(g, "gate")
    ident = st.sb([128, 128], BF16, "ident")
    st.dma("sp", ident[:], ident_bf, wr=["ident"])
    NB = 2
    gt = [[st.sb([128, D], F32, "gt") for _ in range(3)] for _ in range(NB)]
    br = [[st.sb([128, D], F32, "br") for _ in range(3)] for _ in range(NB)]
    yb = [st.sb([128, D], BF16, "yb") for _ in range(NB)]
    tr = TrOut(st, ident, nb=2)
    for i, t in enumerate(tiles):
        b = i % NB
        B = "%d" % b
        rows = slice(t * 128, (t + 1) * 128)
        for j in range(3):
            st.dma("sp", gt[b][j][:], p[rows, O_GT + j * D:O_GT + (j + 1) * D], wr=["gt%d" % j + B])
            st.dma("sp", br[b][j][:], brs[j][rows, :], wr=["br%d" % j + B])
            st.op("act", "activation", rd=["gt%d" % j + B], wr=["gt%d" % j + B], out=gt[b][j][:], in_=gt[b][j][:], func=AF.Sigmoid)
            st.op("pool" if j == 1 else "dve", "tensor_tensor", rd=["gt%d" % j + B, "br%d" % j + B], wr=["br%d" % j + B],
                  out=br[b][j][:], in0=br[b][j][:], in1=gt[b][j][:], op=ALU.mult)
        st.op("pool", "tensor_tensor", rd=["br0" + B, "br1" + B], wr=["br0" + B], out=br[b][0][:], in0=br[b][0][:], in1=br[b][1][:], op=ALU.add)
        st.op("dve", "tensor_tensor", rd=["br0" + B, "br2" + B], wr=["yb" + B], out=yb[b][:], in0=br[b][0][:], in1=br[b][2][:], op=ALU.add)
        tr.go(yb[b], ["yb" + B], yT[t])
    st.emit()


def stage_resid(g, xsrc, ysrc, tiles, modv, seg, dst, dst_row0=0):
    st = Stage(g, "resid")
    Gv = [st.sb([128, D], F32, "Gv") for _ in range(2)]
    for r in range(2):
        st.dma("sp", Gv[r][:], bcast_row(modv[r:r + 1, seg * D:(seg + 1) * D]), wr=["G%d" % r])
    NB = 2
    x = [st.sb([128, D], F32, "x") for _ in range(NB)]
    y = [st.sb([128, D], F32, "y") for _ in range(NB)]
    for i, t in enumerate(tiles):
        b = i % NB
        B = "%d" % b
        r = 1 if t < 2 else 0
        rows = slice(t * 128, (t + 1) * 128)
        st.dma("sp", x[b][:], xsrc[rows, :], wr=["x" + B])
        st.dma("sp", y[b][:], ysrc[rows, :], wr=["y" + B])
        st.op("pool" if b else "dve", "tensor_tensor", rd=["y" + B, "G%d" % r], wr=["y" + B], out=y[b][:], in0=y[b][:], in1=Gv[r][:], op=ALU.mult)
        st.op("dve", "tensor_tensor", rd=["x" + B, "y" + B], wr=["y" + B], out=y[b][:], in0=y[b][:], in1=x[b][:], op=ALU.add)
        st.dma("pool", dst[t * 128 - dst_row0:(t + 1) * 128 - dst_row0, :], y[b][:], rd=["y" + B])
    st.emit()


def stage_uT(g, u_l, cst, uT_s):
    st = Stage(g, "uT")
    cs = st.sb([128, 512], F32, "cst")
    st.dma("sp", cs[:], cst, wr=["cst"])
    IDF = cs[:, 384:512]
    ub = [st.sb([128, D], F32, "ub") for _ in range(2)]
    ob = [st.sb([128, KC, 128], BF16, "uo") for _ in range(2)]
    pu = [st.ps([128, D], F32, "pu") for _ in range(2)]
    for eb in range(128):
        b = eb % 2
        B = "%d" % b
        st.dma("sp", ub[b][:], u_l[eb * 128:(eb + 1) * 128, :], wr=["ub" + B])
        for k in range(KC):
            st.op("pe", "transpose", rd=["ub" + B, "cst"], wr=["pu" + B], out=pu[b][:, k * 128:(k + 1) * 128], in_=ub[b][:, k * 128:(k + 1) * 128], identity=IDF)
        for q4 in range(4):
            st.op("act" if q4 % 2 == 0 else "dve", "copy" if q4 % 2 == 0 else "tensor_copy", rd=["pu" + B], wr=["uo%d" % q4 + B],
                  out=ob[b][:, q4 * 4:(q4 + 1) * 4, :], in_=pu[b][:, q4 * 512:(q4 + 1) * 512].rearrange("p (k e) -> p k e", k=4))
        st.dma("pool", uT_s[:, eb * 128:(eb + 1) * 128].rearrange("(k p) e -> p k e", p=128), ob[b][:], rd=["uo%d" % q4 + B for q4 in range(4)])
    st.emit()


def stage_peer_scores(g, xT, tiles, wq_l, keys_l, cst, sc_s, selp_s):
    st = Stage(g, "pscore")
    cs = st.sb([128, 512], F32, "cst")
    st.dma("sp", cs[:], cst, wr=["cst"])
    IDF = cs[:, 384:512]
    wq = st.sb([128, KC, D], BF16, "wq")
    wst = [st.sb([128, KC, 512], F32, "wst") for _ in range(2)]
    for n in range(4):
        st.dma("sp", wst[n % 2][:], wq_l[:, n * 512:(n + 1) * 512].rearrange("(k p) c -> p k c", p=128), wr=["wst%d" % (n % 2)])
        st.op("pool" if n % 2 else "dve", "tensor_copy", rd=["wst%d" % (n % 2)], wr=["wq"], out=wq[:, :, n * 512:(n + 1) * 512], in_=wst[n % 2][:])
    keysT = st.sb([128, 16, 128], BF16, "keysT")
    kst = [st.sb([128, 128], F32, "kst") for _ in range(2)]
    pq = [st.ps([128, 512], F32, "pq") for _ in range(2)]
    kl = keys_l.rearrange("h p k d -> (h p) k d")
    for blk in range(16):
        j = blk % 2
        st.dma("sp", kst[j][:], kl[blk], wr=["kst%d" % j])
        st.op("pe", "transpose", rd=["kst%d" % j, "cst"], wr=["pq%d" % j], out=pq[j][:, 0:128], in_=kst[j][:], identity=IDF)
        st.op("dve", "tensor_copy", rd=["pq%d" % j], wr=["keysT"], out=keysT[:, blk, :], in_=pq[j][:, 0:128])
    pss = st.ps([128, D], F32, "pss")
    NB = 2
    xt = [st.sb([128, D], BF16, "xt") for _ in range(NB)]
    qTb = [st.sb([128, 128], BF16, "qTb") for _ in range(2)]
    sc = [st.sb([128, D], F32, "sc") for _ in range(NB)]
    wk = st.sb([128, 256], F32, "wk")
    t16 = st.sb([128, 16, 16], F32, "t16")
    cand = st.sb([128, 16, 16], F32, "cand")
    ex = st.sb([128, 256], F32, "ex")
    junk = st.sb([128, 256], F32, "junk")
    c8 = st.sb([128, 16], F32, "c8")
    sm_ = st.sb([128, 32], F32, "small")
    selp = [st.sb([128, 16], F32, "selp") for _ in range(NB)]
    qc = 0
    for i, t in enumerate(tiles):
        b = i % NB
        B = "%d" % b
        rows = slice(t * 128, (t + 1) * 128)
        st.dma("sp", xt[b][:], xT[t], wr=["xt" + B])
        for blk in range(16):
            j = qc % 2
            J = "%d" % j
            qc += 1
            for k in range(KC):
                st.op("pe", "matmul", rd=["wq", "xt" + B], wr=["pq" + J], out=pq[j][:, 0:128], lhsT=wq[:, k, blk * 128:(blk + 1) * 128],
                      rhs=xt[b][:, k * 128:(k + 1) * 128], start=(k == 0), stop=(k == KC - 1))
            if j == 0:
                st.op("act", "copy", rd=["pq" + J], wr=["qTb" + J], out=qTb[j][:], in_=pq[j][:, 0:128])
            else:
                st.op("dve", "tensor_copy", rd=["pq" + J], wr=["qTb" + J], out=qTb[j][:], in_=pq[j][:, 0:128])
            st.op("pe", "matmul", rd=["qTb" + J, "keysT"], wr=["pss"], out=pss[:, blk * 128:(blk + 1) * 128], lhsT=qTb[j][:], rhs=keysT[:, blk, :], start=True, stop=True)
        for q4 in range(4):
            st.op("act" if q4 % 2 == 0 else "dve", "copy" if q4 % 2 == 0 else "tensor_copy", rd=["pss"], wr=["sc%d" % q4 + B],
                  out=sc[b][:, q4 * 512:(q4 + 1) * 512], in_=pss[:, q4 * 512:(q4 + 1) * 512])
        SK = ["sc%d" % q4 + B for q4 in range(4)]
        st.dma("pool", sc_s[rows, :], sc[b][:], rd=SK)
        for blk in range(16):
            sl = sc[b][:, blk * 128:(blk + 1) * 128]
            st.op("dve", "max", rd=SK, wr=["t16"], out=t16[:, blk, 0:8], in_=sl)
            st.op("dve", "match_replace", rd=SK + ["t16"], wr=["wk"], out=wk[:, 0:128], in_to_replace=t16[:, blk, 0:8], in_values=sl, imm_value=-1e30)
            st.op("dve", "max", rd=["wk"], wr=["t16"], out=t16[:, blk, 8:16], in_=wk[:, 0:128])
        for h in range(8):
            st.op("dve", "tensor_tensor", rd=["t16"], wr=["cand"], out=cand[:], in0=bc3(t16[:, 2 * h, :], 16), in1=bcmid(t16[:, 2 * h + 1, :], 16), op=ALU.add)
            cf = cand[:].rearrange("p a b -> p (a b)")
            st.op("dve", "max", rd=["cand"], wr=["c8"], out=c8[:, 0:8], in_=cf)
            st.op("dve", "match_replace", rd=["cand", "c8"], wr=["wk"], out=wk[:], in_to_replace=c8[:, 0:8], in_values=cf, imm_value=-1e30)
            st.op("dve", "max", rd=["wk"], wr=["c8"], out=c8[:, 8:16], in_=wk[:])
            st.op("dve", "tensor_scalar", rd=["c8"], wr=["small"], out=sm_[:, 0:1], in0=c8[:, 0:1], scalar1=-1.0, scalar2=None, op0=ALU.mult)
            st.op("act", "activation", rd=["cand", "small"], wr=["ex"], out=ex[:], in_=cf, func=AF.Exp, bias=sm_[:, 0:1])
            st.op("dve", "scalar_tensor_tensor", rd=["cand", "c8", "ex"], wr=["junk", "small"], out=junk[:], in0=cf, scalar=c8[:, 15:16], in1=ex[:],
                  op0=ALU.is_ge, op1=ALU.mult, accum_out=sm_[:, 1:2])
            st.op("act", "activation", rd=["small"], wr=["small"], out=sm_[:, 2:3], in_=sm_[:, 1:2], func=AF.Ln)
            st.op("dve", "tensor_copy", rd=["c8"], wr=["selp" + B], out=selp[b][:, 2 * h:2 * h + 1], in_=c8[:, 15:16])
            st.op("dve", "tensor_tensor", rd=["small"], wr=["selp" + B], out=selp[b][:, 2 * h + 1:2 * h + 2], in0=sm_[:, 0:1], in1=sm_[:, 2:3], op=ALU.subtract)
        st.dma("pool", selp_s[rows, :], selp[b][:], rd=["selp" + B])
    st.emit()


def stage_peer_experts(g, xT, tiles, uT_s, v_l, ident_bf, sc_s, selp_s, po_s, NG=16):
    st = Stage(g, "pexp")
    ident = st.sb([128, 128], BF16, "ident")
    st.dma("sp", ident[:], ident_bf, wr=["ident"])
    uTg = st.sb([128, KC, 1024], BF16, "uTg")
    vg = st.sb([128, 8, D], BF16, "vg")
    vst = [st.sb([128, D], F32, "vst") for _ in range(2)]
    NB = 2
    xt = [st.sb([128, D], BF16, "xt") for _ in range(NB)]
    sc = [st.sb([128, D], F32, "sc") for _ in range(NB)]
    selp = [st.sb([128, 16], F32, "selp") for _ in range(NB)]
    sm = [st.sb([128, 1024], F32, "sm") for _ in range(2)]
    ex = [st.sb([128, 1024], F32, "ex") for _ in range(2)]
    mk = [[st.sb([128, 1024], BF16, "mk") for _ in range(8)] for _ in range(NB)]
    gl = [st.sb([128, 1024], F32, "gl") for _ in range(NB)]
    WT = [st.sb([128, 1024], BF16, "WT") for _ in range(NB)]
    ob = [st.sb([128, D], F32, "ob") for _ in range(2)]
    ps_h = [st.ps([128, 1024], F32, "ps_h") for _ in range(NB)]
    ps_g = st.ps([128, 1024], F32, "ps_g")
    ps_o = [st.ps([128, 512], F32, "ps_o") for _ in range(2)]
    hc = [0]
    oc = [0]
    iters = [(gi, t) for gi in range(NG) for t in tiles]

    def front(n):
        gi, t = iters[n]
        b = n % NB
        B = "%d" % b
        rows = slice(t * 128, (t + 1) * 128)
        if t == tiles[0]:
            st.dma("sp", uTg[:], uT_s[:, gi * 1024:(gi + 1) * 1024].rearrange("(k p) e -> p k e", p=128), wr=["uTg"])
        st.dma("sp", xt[b][:], xT[t], wr=["xt" + B])
        st.dma("sp", sc[b][:], sc_s[rows, :], wr=["sc" + B])
        st.dma("sp", selp[b][:], selp_s[rows, :], wr=["selp" + B])
        for c in range(8):
            for k in range(KC):
                st.op("pe", "matmul", rd=["uTg", "xt" + B], wr=["ps_h" + B], out=ps_h[b][:, c * 128:(c + 1) * 128], lhsT=uTg[:, k, c * 128:(c + 1) * 128],
                      rhs=xt[b][:, k * 128:(k + 1) * 128], start=(k == 0), stop=(k == KC - 1))
        for q2 in range(2):
            st.op("act", "activation", rd=["ps_h" + B], wr=["gl%d" % q2 + B], out=gl[b][:, q2 * 512:(q2 + 1) * 512], in_=ps_h[b][:, q2 * 512:(q2 + 1) * 512], func=AF.Gelu)
        for h in range(8):
            j = hc[0] % 2
            J = "%d" % j
            hc[0] += 1
            s1 = sc[b][:, (2 * h) * 128 + gi * 8:(2 * h) * 128 + gi * 8 + 8]
            s2 = sc[b][:, (2 * h + 1) * 128:(2 * h + 2) * 128]
            sm3 = sm[j][:].rearrange("p (a b) -> p a b", a=8)
            st.op("pool", "tensor_tensor", rd=["sc" + B], wr=["sm" + J], out=sm3, in0=bc3(s1, 128), in1=bcmid(s2, 8), op=ALU.add)
            st.op("act", "activation", rd=["sm" + J, "selp" + B], wr=["ex" + J], out=ex[j][:], in_=sm[j][:], func=AF.Exp, bias=selp[b][:, 2 * h + 1:2 * h + 2])
            st.op("dve", "scalar_tensor_tensor", rd=["sm" + J, "ex" + J, "selp" + B], wr=["mk%d" % h + B], out=mk[b][h][:], in0=sm[j][:], scalar=selp[b][:, 2 * h:2 * h + 1],
                  in1=ex[j][:], op0=ALU.is_ge, op1=ALU.mult)

    def back(n):
        gi, t = iters[n]
        b = n % NB
        B = "%d" % b
        rows = slice(t * 128, (t + 1) * 128)
        if t == tiles[0]:
            for c in range(8):
                j = c % 2
                st.dma("sp", vst[j][:], v_l[gi * 1024 + c * 128:gi * 1024 + (c + 1) * 128, :], wr=["vst%d" % j])
                st.op("pool" if j else "dve", "tensor_copy", rd=["vst%d" % j], wr=["vg"], out=vg[:, c, :], in_=vst[j][:])
        for c in range(8):
            for h in range(8):
                st.op("pe", "matmul", rd=["mk%d" % h + B, "ident"], wr=["ps_g"], out=ps_g[:, c * 128:(c + 1) * 128], lhsT=mk[b][h][:, c * 128:(c + 1) * 128],
                      rhs=ident[:], start=(h == 0), stop=(h == 7))
        for q2 in range(2):
            st.op("dve", "tensor_tensor", rd=["ps_g", "gl%d" % q2 + B], wr=["WT" + B], out=WT[b][:, q2 * 512:(q2 + 1) * 512], in0=ps_g[:, q2 * 512:(q2 + 1) * 512],
                  in1=gl[b][:, q2 * 512:(q2 + 1) * 512], op=ALU.mult)
        o = ob[b]
        for cb in range(4):
            j = oc[0] % 2
            J = "%d" % j
            oc[0] += 1
            for c in range(8):
                st.op("pe", "matmul", rd=["WT" + B, "vg"], wr=["ps_o" + J], out=ps_o[j][:], lhsT=WT[b][:, c * 128:(c + 1) * 128], rhs=vg[:, c, cb * 512:(cb + 1) * 512],
                      start=(c == 0), stop=(c == 7))
            if j == 0:
                st.op("act", "copy", rd=["ps_o" + J], wr=["ob%d_%d" % (b, cb)], out=o[:, cb * 512:(cb + 1) * 512], in_=ps_o[j][:])
            else:
                st.op("dve", "tensor_copy", rd=["ps_o" + J], wr=["ob%d_%d" % (b, cb)], out=o[:, cb * 512:(cb + 1) * 512], in_=ps_o[j][:])
        okeys = ["ob%d_%d" % (b, cb) for cb in range(4)]
        if gi == 0:
            st.dma("pool", po_s[rows, :], o[:], rd=okeys, wr=[("po", t)])
        else:
            st.dma("pool", po_s[rows, :], o[:], rd=okeys, wr=[("po", t)], accum_op=ALU.add)

    front(0)
    for n in range(len(iters)):
        if n + 1 < len(iters):
            front(n + 1)
        back(n)
    st.emit()


def build_program(NL, depth=2):
    NT = NL + 2
    ROWS = 2 * NL
    NCLS = len(na_plan(ROWS)[2])
    nc = bass.Bass("TRN2", target_bir_lowering=False)

    def inp(name, shape, dt=F32):
        return nc.dram_tensor(name, list(shape), dt, kind="ExternalInput").ap()

    xcat = inp("xcat", [NT * 128, D])
    cc = inp("cc", [2, D])
    w_ada = inp("w_ada", [depth, D, 6 * D])
    b_ada = inp("b_ada", [depth, 6 * D])
    norm_g = inp("norm_g", [depth, 2 * D])
    w_in = inp("w_in", [depth, D, PW])
    gate_b = inp("a_gate_b", [depth, 32])
    hnorm = inp("a_hnorm_g", [depth, D])
    b_conv = inp("b_conv", [depth, 3, D])
    qkg = inp("c_qk_g", [depth, 2, 128])
    nab = inp("nab", [depth, 16, NCLS, 128, 640])
    w_a = inp("w_a_out", [depth, D, D])
    w_b = inp("w_b_out", [depth, D, D])
    w_c = inp("w_c_out", [depth, D, D])
    w_o = inp("w_out", [depth, D, D])
    wq = inp("peer_wq", [depth, D, D])
    pkeys = inp("peer_keys", [depth, 8, 2, 128, 128])
    pu = inp("peer_u", [depth, 16384, D])
    pv = inp("peer_v", [depth, 16384, D])
    cst = inp("cst", [128, 512])
    ident = inp("ident_bf", [128, 128], BF16)
    rope = [inp(n, [NL * 128, 128]) for n in ("cosq", "sinq", "cosk", "sink")]
    out = nc.dram_tensor("out", [NL * 128, D], F32, kind="ExternalOutput").ap()

    with ExitStack() as es:
        es.enter_context(nc.allow_low_precision("bf16 matmul operands, fp32 accumulation"))
        g = G(nc, es)
        R = NT * 128

        def dr(name, shape, dt=F32):
            return g.dram(name, shape, dt).ap()

        modv = dr("modv", [2, 6 * D])
        xnT = dr("xnT", [NT, 128, D], BF16)
        pparts = [dr("p%d" % gi, [R, PSplit.BOUNDS[gi + 1] - PSplit.BOUNDS[gi]]) for gi in range(4)]
        p = PSplit(pparts)
        qT_s = dr("qT_s", [NT, 128, 1024], BF16)
        kT_s = dr("kT_s", [NT, 128, 1024], BF16)
        kb_s = dr("kb_s", [R, 1024], BF16)
        v2f_s = dr("v2f_s", [R, 8 * 257], BF16)
        v2b_s = dr("v2b_s", [R, 8 * 257], BF16)
        gpk_s = dr("gpk_s", [R, 48])
        hf_s = dr("hf_s", [R, D])
        yaT = dr("yaT", [NT, 128, D], BF16)
        ybT = dr("ybT", [NT, 128, D], BF16)
        ycT = dr("ycT", [NT, 128, D], BF16)
        yT = dr("yT", [NT, 128, D], BF16)
        cqT_s = dr("cqT_s", [NT, 128, D], BF16)
        ckT_s = dr("ckT_s", [NT, 128, D], BF16)
        cv1_s = dr("cv1_s", [R, 16 * 129], BF16)
        yc_s = dr("yc_s", [R, D])
        brs = [dr("br%d" % j, [R, D]) for j in range(3)]
        mix = dr("mix", [R, D])
        x1 = dr("x1", [R, D])
        xmid = dr("xmid", [R, D])
        uT_s = dr("uT_s", [D, 16384], BF16)
        sc_s = dr("sc_s", [R, D])
        selp_s = dr("selp_s", [R, 16])
        po_s = dr("po_s", [R, D])

        allt = list(range(NT))
        lat = list(range(2, NT))
        for l in range(depth):
            last = l == depth - 1
            xin = xcat if l == 0 else xmid
            T = lat if last else allt
            stage_mod(g, cc, w_ada[l], b_ada[l:l + 1, :], norm_g[l:l + 1, :], modv)
            stage_norm(g, xin, modv, 0, xnT, allt, ident)
            for gi in range(4):
                lo, hi = PSplit.BOUNDS[gi], PSplit.BOUNDS[gi + 1]
                stage_linear(g, xnT, allt, w_in[l][:, lo:hi], hi - lo, pparts[gi])
            stage_mlstm_prep(g, p, allt, cst, ident, gate_b[l:l + 1, :], rope, qT_s, kT_s, kb_s, v2f_s, v2b_s, gpk_s)
            stage_mlstm_scan(g, False, [0, 1] + lat, cst, ident, qT_s, kT_s, kb_s, v2f_s, gpk_s, hf_s)
            stage_mlstm_scan(g, True, [1, 0] + lat[::-1], cst, ident, qT_s, kT_s, kb_s, v2b_s, gpk_s, hf_s, p=p, hnorm_l=hnorm[l:l + 1, :], yaT=yaT,
                             skip_out=((0, 1) if last else ()))
            stage_conv(g, p, T, NT, b_conv[l], ident, ybT)
            stage_na_prep(g, p, allt, qkg[l], ident, cqT_s, ckT_s, cv1_s)
            stage_na(g, NL, not last, nab[l], ident, cqT_s, ckT_s, cv1_s, yc_s)
            stage_cast_T(g, yc_s, T, ident, ycT)
            stage_linear(g, yaT, T, w_a[l], D, brs[0])
            stage_linear(g, ybT, T, w_b[l], D, brs[1])
            stage_linear(g, ycT, T, w_c[l], D, brs[2])
            stage_gate(g, p, T, brs, ident, yT)
            stage_linear(g, yT, T, w_o[l], D, mix)
            stage_resid(g, xin, mix, T, modv, 2, x1)
            stage_norm(g, x1, modv, 1, xnT, T, ident)
            stage_uT(g, pu[l], cst, uT_s)
            stage_peer_scores(g, xnT, T, wq[l], pkeys[l], cst, sc_s, selp_s)
            stage_peer_experts(g, xnT, T, uT_s, pv[l], ident, sc_s, selp_s, po_s)
            if last:
                stage_resid(g, x1, po_s, T, modv, 5, out, dst_row0=256)
            else:
                stage_resid(g, x1, po_s, T, modv, 5, xmid)
        final_wait(g)
    return nc


_PROG = {}


def kernel(x, c, ctx, c_ctx, w_ada, b_ada, norm_g, w_in, a_gate_b, a_hnorm_g, b_conv, c_qk_g, c_rpb,
           w_a_out, w_b_out, w_c_out, w_out, peer_wq, peer_keys, peer_u, peer_v):
    f = lambda a: np.ascontiguousarray(np.asarray(a, dtype=np.float32))
    x, ctx = f(x), f(ctx)
    B, S, _ = x.shape
    NL = S // 128
    depth = w_ada.shape[0]
    hc = host_consts(S)
    nab = np.stack([na_bias_host(f(c_rpb)[l], 2 * NL) for l in range(depth)], 0)
    shared = dict(w_ada=f(w_ada), b_ada=f(b_ada), norm_g=f(norm_g).reshape(depth, 2 * D), w_in=f(w_in),
                  a_gate_b=f(a_gate_b).reshape(depth, 32), a_hnorm_g=f(a_hnorm_g), b_conv=f(b_conv), c_qk_g=f(c_qk_g), nab=nab,
                  w_a_out=f(w_a_out), w_b_out=f(w_b_out), w_c_out=f(w_c_out), w_out=f(w_out), peer_wq=f(peer_wq),
                  peer_keys=f(peer_keys), peer_u=f(peer_u), peer_v=f(peer_v), **hc)
    in_maps = []
    for b in range(B):
        m = dict(shared)
        m["xcat"] = np.concatenate([ctx[b], x[b]], axis=0)
        m["cc"] = np.stack([f(c)[b], f(c_ctx)], axis=0)
        in_maps.append(m)
    key = (NL, depth)
    if key not in _PROG:
        _PROG[key] = build_program(NL, depth)
    res = run_bass_kernel_spmd(_PROG[key], in_maps, core_ids=list(range(B)))
    return np.stack([res.results[b]["out"] for b in range(B)], axis=0)
```

```python
import numpy as np
from contextlib import ExitStack
import concourse.bass as bass
import concourse.mybir as mybir
from concourse.bass_utils import run_bass_kernel_spmd

F32 = mybir.dt.float32
BF16 = mybir.dt.bfloat16
ALU = mybir.AluOpType
AF = mybir.ActivationFunctionType
AX = mybir.AxisListType

D = 2048
KC = 16
PW = 24608
EPS = 1e-6
ENGS = ["pe", "act", "dve", "pool", "sp"]
NDS = 24

O_AK, O_AV, O_AG, O_CK, O_CV, O_AQ, O_AO, O_BB, O_BC, O_BX, O_CQ, O_GT = (
    0, 1024, 3072, 3104, 5152, 7200, 8224, 10272, 12320, 14368, 16416, 18464)


class PSplit:
    BOUNDS = [0, 7200, 14368, 18464, PW]

    def __init__(self, aps):
        self.aps = aps

    def __getitem__(self, key):
        rows, cols = key
        for gi in range(4):
            lo, hi = self.BOUNDS[gi], self.BOUNDS[gi + 1]
            if cols.start >= lo and cols.stop <= hi:
                return self.aps[gi][rows, cols.start - lo:cols.stop - lo]
        raise ValueError("column range straddles projection groups: %s" % (cols,))


class G:
    def __init__(self, nc, es):
        self.nc = nc
        self.sem = {e: es.enter_context(nc.semaphore("s_" + e)) for e in ENGS}
        self.dsem = [es.enter_context(nc.semaphore("d%d" % i)) for i in range(NDS)]
        self.cnt = {e: 0 for e in ENGS}
        self.dcnt = [0] * NDS
        self.dnext = 0
        self.known = {e: {} for e in ENGS}
        self.nstage = 0
        self.ntens = 0

    def dram(self, name, shape, dt):
        return self.nc.dram_tensor(name, list(shape), dt)


class Stage:
    def __init__(self, g, name=None):
        self.g = g
        g.nstage += 1
        self.name = name or ("st%d" % g.nstage)
        self.ops = {e: [] for e in ENGS}
        self.lastw = {}
        self.readers = {}
        self.es = ExitStack()
        self.start_cnt = dict(g.cnt)
        self.start_d = list(g.dcnt)

    def sb(self, shape, dt=F32, name=None):
        self.g.ntens += 1
        return self.es.enter_context(self.g.nc.sbuf_tensor("%s_t%d" % (name or "sb", self.g.ntens), list(shape), dt))

    def ps(self, shape, dt=F32, name=None):
        self.g.ntens += 1
        return self.es.enter_context(self.g.nc.psum_tensor("%s_p%d" % (name or "ps", self.g.ntens), list(shape), dt))

    def add(self, eng, fn, rd=(), wr=(), dma=False):
        g = self.g
        deps = []
        for k in rd:
            t = self.lastw.get(k)
            if t is not None:
                deps.append(t)
        for k in wr:
            t = self.lastw.get(k)
            cands = ([t] if t is not None else []) + self.readers.get(k, [])
            for c in cands:
                if (not dma) and c[0] == "c" and c[1] == eng:
                    continue
                deps.append(c)
        if dma:
            j = g.dnext
            g.dnext = (j + 1) % NDS
            g.dcnt[j] += 1
            tok = ("d", j, g.dcnt[j])
            if g.dcnt[j] > 1:
                deps.append(("d", j, g.dcnt[j] - 1))
        else:
            g.cnt[eng] += 1
            tok = ("c", eng, g.cnt[eng])
        for k in wr:
            self.lastw[k] = tok
            self.readers[k] = []
        for k in rd:
            lst = self.readers.setdefault(k, [])
            if tok[0] == "c":
                lst[:] = [x for x in lst if not (x[0] == "c" and x[1] == tok[1])]
            lst.append(tok)
        self.ops[eng].append((fn, deps, tok))
        return tok

    def op(self, eng, meth, rd=(), wr=(), **kw):
        return self.add(eng, lambda e: getattr(e, meth)(**kw), rd, wr)

    def dma(self, eng, out, in_, rd=(), wr=(), **kw):
        return self.add(eng, lambda e: e.dma_start(out=out, in_=in_, **kw), rd, wr, dma=True)

    def emit(self):
        g = self.g
        nc = g.nc

        def run(ename, e):
            known = g.known[ename]

            def wait(tok):
                if tok[0] == "c":
                    sem, val, key = g.sem[tok[1]], tok[2], tok[1]
                else:
                    sem, val, key = g.dsem[tok[1]], 16 * tok[2], "d%d" % tok[1]
                if known.get(key, 0) < val:
                    e.wait_ge(sem, val)
                    known[key] = val

            for o in ENGS:
                if o != ename and self.start_cnt[o] > 0:
                    wait(("c", o, self.start_cnt[o]))
            for j in range(NDS):
                if self.start_d[j] > 0:
                    wait(("d", j, self.start_d[j]))
            for fn, deps, tok in self.ops[ename]:
                for d in deps:
                    wait(d)
                ins = fn(e)
                if tok[0] == "c":
                    ins.then_inc(g.sem[tok[1]], 1)
                else:
                    ins.then_inc(g.dsem[tok[1]], 16)

        with nc.Block() as block:
            if self.ops["pe"]:
                block.tensor(lambda e: run("pe", e))
            if self.ops["act"]:
                block.scalar(lambda e: run("act", e))
            if self.ops["dve"]:
                block.vector(lambda e: run("dve", e))
            if self.ops["pool"]:
                block.gpsimd(lambda e: run("pool", e))
            if self.ops["sp"]:
                block.sync(lambda e: run("sp", e))
        self.es.close()


def final_wait(g):
    st = Stage(g, "final")
    nc = g.nc

    def run(e):
        for o in ENGS:
            if o != "sp" and g.cnt[o] > 0:
                e.wait_ge(g.sem[o], g.cnt[o])
        for j in range(NDS):
            if g.dcnt[j] > 0:
                e.wait_ge(g.dsem[j], 16 * g.dcnt[j])

    with nc.Block() as block:
        block.sync(run)
    st.es.close()


def bcast_row(ap_row, nparts=128):
    return ap_row.partition_broadcast(nparts)


def stage_mod(g, cc, w_ada_l, b_ada_l, norm_g_l, modv):
    st = Stage(g, "mod")
    ccT = st.sb([128, 2, KC], F32, "ccT")
    sil = st.sb([128, 2, KC], F32, "sil")
    sig = st.sb([128, 2, KC], F32, "sig")
    NCH = 6 * D // 512
    wst = [st.sb([128, KC, 512], F32, "wada") for _ in range(2)]
    bb = [st.sb([2, 512], F32, "bada") for _ in range(2)]
    ng = [st.sb([2, 512], F32, "ng") for _ in range(2)]
    res = [st.sb([2, 512], F32, "res") for _ in range(2)]
    pst = [st.ps([128, 512], F32, "pmod") for _ in range(2)]
    for r in range(2):
        st.dma("sp", ccT[:, r, :], cc[r, :].rearrange("(k p) -> p k", p=128), wr=["ccT"], allow_slow_non_contiguous=True)
    st.add("act", lambda e: e.activation(out=sig[:], in_=ccT[:], func=AF.Sigmoid), rd=["ccT"], wr=["sig"])
    st.add("dve", lambda e: e.tensor_tensor(out=sil[:], in0=ccT[:], in1=sig[:], op=ALU.mult), rd=["ccT", "sig"], wr=["sil"])
    segmap = {0: 1, 1: 0, 2: 2, 3: 4, 4: 3, 5: 5}
    for n in range(NCH):
        i = n % 2
        w = wst[i]
        seg, cs = n // 4, (n % 4) * 512
        st.dma("sp", w[:], w_ada_l[:, n * 512:(n + 1) * 512].rearrange("(k p) c -> p k c", p=128), wr=["w%d" % i])
        for r in range(2):
            st.dma("sp", bb[i][r:r + 1, :], b_ada_l[:, n * 512:(n + 1) * 512], wr=["bb%d" % i])
            if seg in (1, 4):
                j = 0 if seg == 1 else 1
                st.dma("sp", ng[i][r:r + 1, :], norm_g_l[:, j * D + cs:j * D + cs + 512], wr=["ng%d" % i])
        for k in range(KC):
            st.add("pe", lambda e, w=w, k=k, i=i: e.matmul(pst[i][0:2, :], lhsT=sil[:, :, k], rhs=w[:, k, :],
                                                           start=(k == 0), stop=(k == KC - 1)),
                   rd=["sil", "w%d" % i], wr=["p%d" % i])
        st.add("dve", lambda e, i=i: e.tensor_tensor(out=res[i][:], in0=pst[i][0:2, :], in1=bb[i][:], op=ALU.add),
               rd=["p%d" % i, "bb%d" % i], wr=["res%d" % i])
        if seg in (1, 4):
            st.add("dve", lambda e, i=i: e.scalar_tensor_tensor(out=res[i][:], in0=res[i][:], scalar=1.0, in1=ng[i][:],
                                                                op0=ALU.add, op1=ALU.mult),
                   rd=["res%d" % i, "ng%d" % i], wr=["res%d" % i])
        o0 = segmap[seg] * D + cs
        st.dma("sp", modv[:, o0:o0 + 512], res[i][:], rd=["res%d" % i])
    st.emit()


def stage_norm(g, src, modv, jn, xnT, tiles, ident_bf):
    st = Stage(g, "norm")
    A = [st.sb([128, D], F32, "A") for _ in range(2)]
    B = [st.sb([128, D], F32, "B") for _ in range(2)]
    ident = st.sb([128, 128], BF16, "ident")
    st.dma("sp", ident[:], ident_bf, wr=["ident"])
    for r in range(2):
        st.dma("sp", A[r][:], bcast_row(modv[r:r + 1, (3 * jn) * D:(3 * jn + 1) * D]), wr=["A%d" % r])
        st.dma("sp", B[r][:], bcast_row(modv[r:r + 1, (3 * jn + 1) * D:(3 * jn + 2) * D]), wr=["B%d" % r])
    NB = 2
    xt = [st.sb([128, D], F32, "x") for _ in range(NB)]
    junk = [st.sb([128, D], F32, "junk") for _ in range(NB)]
    tmp = [st.sb([128, D], F32, "tmp") for _ in range(NB)]
    xb = [st.sb([128, D], BF16, "xb") for _ in range(NB)]
    xT = [st.sb([128, D], BF16, "xT") for _ in range(NB)]
    ss = [st.sb([128, 1], F32, "ss") for _ in range(NB)]
    rs = [st.sb([128, 1], F32, "rs") for _ in range(NB)]
    pt = [st.ps([128, D], BF16, "pT") for _ in range(NB)]
    for i, t in enumerate(tiles):
        b = i % NB
        r = 1 if t < 2 else 0
        st.dma("sp", xt[b][:], src[t * 128:(t + 1) * 128, :], wr=["x%d" % b])
        st.add("act", lambda e, b=b: e.activation(out=junk[b][:], in_=xt[b][:], func=AF.Square, accum_out=ss[b][:]),
               rd=["x%d" % b], wr=["junk%d" % b, "ss%d" % b])
        st.add("act", lambda e, b=b: e.activation(out=rs[b][:], in_=ss[b][:], func=AF.Sqrt, scale=1.0 / D, bias=EPS),
               rd=["ss%d" % b], wr=["rs%d" % b])
        st.add("dve", lambda e, b=b: e.reciprocal(out=rs[b][:], in_=rs[b][:]),
               rd=["rs%d" % b], wr=["rs%d" % b])
        st.add("dve", lambda e, b=b, r=r: e.scalar_tensor_tensor(out=tmp[b][:], in0=xt[b][:], scalar=rs[b][:], in1=A[r][:],
                                                                 op0=ALU.mult, op1=ALU.mult),
               rd=["x%d" % b, "rs%d" % b, "A%d" % r], wr=["tmp%d" % b])
        st.add("pool", lambda e, b=b, r=r: e.tensor_tensor(out=xb[b][:], in0=tmp[b][:], in1=B[r][:], op=ALU.add),
               rd=["tmp%d" % b, "B%d" % r], wr=["xb%d" % b])
        for k in range(KC):
            st.add("pe", lambda e, b=b, k=k: e.transpose(out=pt[b][:, k * 128:(k + 1) * 128], in_=xb[b][:, k * 128:(k + 1) * 128], identity=ident[:]),
                   rd=["xb%d" % b, "ident"], wr=["pt%d" % b])
        st.add("act", lambda e, b=b: e.copy(out=xT[b][:, 0:1024], in_=pt[b][:, 0:1024]), rd=["pt%d" % b], wr=["xTa%d" % b])
        st.add("dve", lambda e, b=b: e.tensor_copy(out=xT[b][:, 1024:2048], in_=pt[b][:, 1024:2048]), rd=["pt%d" % b], wr=["xTb%d" % b])
        st.dma("pool", xnT[t], xT[b][:], rd=["xTa%d" % b, "xTb%d" % b])
    st.emit()


def stage_linear(g, xT, tiles, W, N, out, oc0=0, w_bf=False, slab=1024):
    st = Stage(g, "lin")
    wst = [st.sb([128, KC, 512], F32, "wst") for _ in range(2)] if not w_bf else None
    wsl = [st.sb([128, KC, slab], BF16, "wsl") for _ in range(2)]
    NX = 3
    xt = [st.sb([128, D], BF16, "xt") for _ in range(NX)]
    ob = [st.sb([128, slab], F32, "ob") for _ in range(2)]
    pp = [st.ps([128, 512], F32, "pl") for _ in range(4)]
    nsl = (N + slab - 1) // slab
    cnt = 0
    hcnt = 0
    pcnt = 0
    for s in range(nsl):
        c0 = s * slab
        ns = min(slab, N - c0)
        w = wsl[s % 2]
        wk = "wsl%d" % (s % 2)
        for h0 in range(0, ns, 512):
            hw = min(512, ns - h0)
            src = W[:, c0 + h0:c0 + h0 + hw].rearrange("(k p) c -> p k c", p=128)
            if w_bf:
                st.dma("sp", w[:, :, h0:h0 + hw], src, wr=[wk + "_%d" % h0])
            else:
                ws = wst[hcnt % 2]
                wsk = "wst%d" % (hcnt % 2)
                st.dma("sp", ws[:, :, 0:hw], src, wr=[wsk])
                eng = "pool" if hcnt % 2 == 0 else "dve"
                st.add(eng, lambda e, w=w, ws=ws, h0=h0, hw=hw: e.tensor_copy(out=w[:, :, h0:h0 + hw], in_=ws[:, :, 0:hw]),
                       rd=[wsk], wr=[wk + "_%d" % h0])
                hcnt += 1
        for t in tiles:
            xb = cnt % NX
            o = ob[cnt % 2]
            ok = "ob%d" % (cnt % 2)
            st.dma("sp", xt[xb][:], xT[t], wr=["xt%d" % xb])
            for h0 in range(0, ns, 512):
                hw = min(512, ns - h0)
                p = pp[pcnt % 4]
                pk = "pp%d" % (pcnt % 4)
                for k in range(KC):
                    st.add("pe", lambda e, p=p, xb=xb, k=k, w=w, h0=h0, hw=hw: e.matmul(
                        p[:, 0:hw], lhsT=xt[xb][:, k * 128:(k + 1) * 128], rhs=w[:, k, h0:h0 + hw], start=(k == 0), stop=(k == KC - 1)),
                        rd=["xt%d" % xb, wk + "_%d" % h0], wr=[pk])
                if pcnt % 2 == 0:
                    st.add("act", lambda e, o=o, p=p, h0=h0, hw=hw: e.copy(out=o[:, h0:h0 + hw], in_=p[:, 0:hw]), rd=[pk], wr=[ok + "_%d" % h0])
                else:
                    st.add("dve", lambda e, o=o, p=p, h0=h0, hw=hw: e.tensor_copy(out=o[:, h0:h0 + hw], in_=p[:, 0:hw]), rd=[pk], wr=[ok + "_%d" % h0])
                pcnt += 1
            st.dma("pool", out[t * 128:(t + 1) * 128, oc0 + c0:oc0 + c0 + ns], o[:, 0:ns],
                   rd=[ok + "_%d" % h0 for h0 in range(0, ns, 512)])
            cnt += 1
    st.emit()


def bc3(ap2d, n):
    return ap2d.unsqueeze(2).broadcast_to([ap2d.shape[0], ap2d.shape[1], n])


def bcmid(ap2d, n):
    return ap2d.unsqueeze(1).broadcast_to([ap2d.shape[0], n, ap2d.shape[1]])


class TrOut:
    def __init__(self, st, ident, nb=2):
        self.st = st
        self.ident = ident
        self.nb = nb
        self.pt = [st.ps([128, D], BF16, "trp") for _ in range(nb)]
        self.xT = [st.sb([128, D], BF16, "trx") for _ in range(nb)]
        self.i = 0

    def go(self, src, srckeys, dst):
        st = self.st
        b = self.i % self.nb
        self.i += 1
        pt, xT, ident = self.pt[b], self.xT[b], self.ident
        for k in range(KC):
            st.add("pe", lambda e, k=k: e.transpose(out=pt[:, k * 128:(k + 1) * 128], in_=src[:, k * 128:(k + 1) * 128], identity=ident[:]),
                   rd=list(srckeys) + ["ident"], wr=["trp%d" % b])
        st.add("act", lambda e: e.copy(out=xT[:, 0:1024], in_=pt[:, 0:1024]), rd=["trp%d" % b], wr=["trxa%d" % b])
        st.add("dve", lambda e: e.tensor_copy(out=xT[:, 1024:2048], in_=pt[:, 1024:2048]), rd=["trp%d" % b], wr=["trxb%d" % b])
        st.dma("pool", dst, xT[:], rd=["trxa%d" % b, "trxb%d" % b])


def stage_mlstm_prep(g, p, tiles, cst, ident_bf, gate_b_l, rope, qT_s, kT_s, kb_s, v2f_s, v2b_s, gpk_s):
    st = Stage(g, "mprep")
    SC = 128.0 ** -0.5
    cs = st.sb([128, 512], F32, "cst")
    ident = st.sb([128, 128], BF16, "ident")
    gbias = st.sb([128, 32], F32, "gbias")
    st.dma("sp", cs[:], cst, wr=["cst"])
    st.dma("sp", ident[:], ident_bf, wr=["ident"])
    st.dma("sp", gbias[:], bcast_row(gate_b_l), wr=["gbias"])
    MF, MB, ONES = cs[:, 0:128], cs[:, 128:256], cs[:, 256:384]
    NB = 2
    q = [st.sb([128, 1024], F32, "q") for _ in range(NB)]
    k = [st.sb([128, 1024], F32, "k") for _ in range(NB)]
    v = [st.sb([128, 2048], F32, "v") for _ in range(NB)]
    gp = [st.sb([128, 32], F32, "gp") for _ in range(NB)]
    cq = [st.sb([128, 128], F32, "cq") for _ in range(NB)]
    sq = [st.sb([128, 128], F32, "sq") for _ in range(NB)]
    ck = [st.sb([128, 128], F32, "ck") for _ in range(NB)]
    sk = [st.sb([128, 128], F32, "sk") for _ in range(NB)]
    t1d = {n: [st.sb([128, 1024], F32, "t1" + n) for _ in range(NB)] for n in ("q", "k")}
    t2d = {n: [st.sb([128, 1024], F32, "t2" + n) for _ in range(NB)] for n in ("q", "k")}
    qb = [st.sb([128, 1024], BF16, "qb") for _ in range(NB)]
    kb = [st.sb([128, 1024], BF16, "kb") for _ in range(NB)]
    qTs = [st.sb([128, 1024], BF16, "qTs") for _ in range(NB)]
    kTs = [st.sb([128, 1024], BF16, "kTs") for _ in range(NB)]
    v2f = [st.sb([128, 8, 257], BF16, "v2f") for _ in range(NB)]
    v2b = [st.sb([128, 8, 257], BF16, "v2b") for _ in range(NB)]
    lf = [st.sb([128, 16], F32, "lf") for _ in range(NB)]
    gk = [st.sb([128, 48], F32, "gk") for _ in range(NB)]
    tmp = [st.sb([128, 16], F32, "tmpg") for _ in range(NB)]
    pq = [st.ps([128, 1024], BF16, "pq") for _ in range(NB)]
    pk = [st.ps([128, 1024], BF16, "pk") for _ in range(NB)]
    pg = [st.ps([128, 32], F32, "pg") for _ in range(NB)]
    cosq, sinq, cosk, sink = rope
    for i, t in enumerate(tiles):
        b = i % NB
        B = "%d" % b
        rows = slice(t * 128, (t + 1) * 128)
        st.dma("sp", q[b][:], p[rows, O_AQ:O_AQ + 1024], wr=["q" + B])
        st.dma("sp", k[b][:], p[rows, O_AK:O_AK + 1024], wr=["k" + B])
        st.dma("sp", v[b][:], p[rows, O_AV:O_AV + 2048], wr=["v" + B])
        st.dma("sp", gp[b][:], p[rows, O_AG:O_AG + 32], wr=["gp" + B])
        if t >= 2:
            lr = slice((t - 2) * 128, (t - 1) * 128)
            st.dma("sp", cq[b][:], cosq[lr, :], wr=["cq" + B])
            st.dma("sp", sq[b][:], sinq[lr, :], wr=["sq" + B])
            st.dma("sp", ck[b][:], cosk[lr, :], wr=["ck" + B])
            st.dma("sp", sk[b][:], sink[lr, :], wr=["sk" + B])
            for (src, cc_, ss_, dst, nm, eng) in ((q, cq, sq, qb, "q", "dve"), (k, ck, sk, kb, "k", "pool")):
                s4 = src[b][:].rearrange("p (h f u j) -> p h f u j", h=8, f=2, u=2, j=32)
                t1v, t2v = t1d[nm][b], t2d[nm][b]
                d4 = t2v[:].rearrange("p (h f u j) -> p h f u j", h=8, f=2, u=2, j=32)
                nsn = ss_[b][:, 0:64].rearrange("p (f j) -> p f j", f=2).unsqueeze(1).broadcast_to([128, 8, 2, 32])
                psn = ss_[b][:, 64:128].rearrange("p (f j) -> p f j", f=2).unsqueeze(1).broadcast_to([128, 8, 2, 32])
                tk1, tk2 = "t1" + nm + B, "t2" + nm + B
                st.op(eng, "tensor_tensor", rd=[nm + B, "c" + nm + B], wr=[tk1],
                      out=t1v[:].rearrange("p (h d) -> p h d", h=8), in0=src[b][:].rearrange("p (h d) -> p h d", h=8),
                      in1=bcmid(cc_[b][:], 8), op=ALU.mult)
                st.op(eng, "tensor_tensor", rd=[nm + B, "s" + nm + B], wr=[tk2],
                      out=d4[:, :, :, 0, :], in0=s4[:, :, :, 1, :], in1=nsn, op=ALU.mult)
                st.op(eng, "tensor_tensor", rd=[nm + B, "s" + nm + B], wr=[tk2],
                      out=d4[:, :, :, 1, :], in0=s4[:, :, :, 0, :], in1=psn, op=ALU.mult)
                st.op(eng, "tensor_tensor", rd=[tk1, tk2], wr=[nm + "b" + B], out=dst[b][:], in0=t1v[:], in1=t2v[:], op=ALU.add)
        else:
            st.op("act", "mul", rd=["q" + B], wr=["qb" + B], out=qb[b][:], in_=q[b][:], mul=SC)
            st.op("dve", "tensor_copy", rd=["k" + B], wr=["kb" + B], out=kb[b][:], in_=k[b][:])
        for h in range(8):
            st.op("pe", "transpose", rd=["qb" + B, "ident"], wr=["pq" + B],
                  out=pq[b][:, h * 128:(h + 1) * 128], in_=qb[b][:, h * 128:(h + 1) * 128], identity=ident[:])
        st.op("act", "copy", rd=["pq" + B], wr=["qTs" + B], out=qTs[b][:], in_=pq[b][:])
        for h in range(8):
            st.op("pe", "transpose", rd=["kb" + B, "ident"], wr=["pk" + B],
                  out=pk[b][:, h * 128:(h + 1) * 128], in_=kb[b][:, h * 128:(h + 1) * 128], identity=ident[:])
        st.op("dve", "tensor_copy", rd=["pk" + B], wr=["kTs" + B], out=kTs[b][:], in_=pk[b][:])
        st.dma("pool", qT_s[t], qTs[b][:], rd=["qTs" + B])
        st.dma("pool", kT_s[t], kTs[b][:], rd=["kTs" + B])
        st.dma("pool", kb_s[rows, :], kb[b][:], rd=["kb" + B])
        st.op("dve", "tensor_tensor", rd=["gp" + B, "gbias"], wr=["gp" + B], out=gp[b][:], in0=gp[b][:], in1=gbias[:], op=ALU.add)
        st.op("act", "activation", rd=["gp" + B], wr=["lf" + B], out=lf[b][:, 0:8], in_=gp[b][:, 8:16], func=AF.Exp, scale=-1.0)
        st.op("act", "activation", rd=["gp" + B], wr=["lf" + B], out=lf[b][:, 8:16], in_=gp[b][:, 24:32], func=AF.Exp, scale=-1.0)
        st.op("act", "activation", rd=["lf" + B], wr=["lf" + B], out=lf[b][:], in_=lf[b][:], func=AF.Ln, bias=1.0)
        st.op("dve", "tensor_scalar", rd=["lf" + B], wr=["lf" + B], out=lf[b][:], in0=lf[b][:], scalar1=-1.0, scalar2=None, op0=ALU.mult)
        st.op("pe", "matmul", rd=["lf" + B, "cst"], wr=["pg" + B], out=pg[b][:, 0:8], lhsT=MF, rhs=lf[b][:, 0:8], start=True, stop=True)
        st.op("pe", "matmul", rd=["lf" + B, "cst"], wr=["pg" + B], out=pg[b][:, 8:16], lhsT=MB, rhs=lf[b][:, 8:16], start=True, stop=True)
        st.op("pe", "matmul", rd=["lf" + B, "cst"], wr=["pg" + B], out=pg[b][:, 16:32], lhsT=ONES, rhs=lf[b][:, 0:16], start=True, stop=True)
        st.op("dve", "tensor_tensor", rd=["gp" + B, "pg" + B], wr=["tmp" + B], out=tmp[b][:, 0:8], in0=gp[b][:, 0:8], in1=pg[b][:, 0:8], op=ALU.subtract)
        st.op("dve", "tensor_tensor", rd=["gp" + B, "pg" + B], wr=["tmp" + B], out=tmp[b][:, 8:16], in0=gp[b][:, 16:24], in1=pg[b][:, 8:16], op=ALU.subtract)
        st.op("act", "activation", rd=["tmp" + B], wr=["gk" + B], out=gk[b][:, 0:8], in_=tmp[b][:, 0:8], func=AF.Exp)
        st.op("act", "activation", rd=["tmp" + B], wr=["gk" + B], out=gk[b][:, 16:24], in_=tmp[b][:, 8:16], func=AF.Exp)
        st.op("act", "activation", rd=["pg" + B], wr=["gk" + B], out=gk[b][:, 8:16], in_=pg[b][:, 0:8], func=AF.Exp)
        st.op("act", "activation", rd=["pg" + B], wr=["gk" + B], out=gk[b][:, 24:32], in_=pg[b][:, 8:16], func=AF.Exp)
        st.op("act", "activation", rd=["pg" + B], wr=["gk" + B], out=gk[b][:, 32:48], in_=pg[b][:, 16:32], func=AF.Exp)
        st.dma("pool", gpk_s[rows, :], gk[b][:], rd=["gk" + B])
        v3 = v[b][:].rearrange("p (h d) -> p h d", h=8)
        st.op("dve", "tensor_tensor", rd=["v" + B, "gk" + B], wr=["v2f" + B], out=v2f[b][:, :, 0:256], in0=v3, in1=bc3(gk[b][:, 0:8], 256), op=ALU.mult)
        st.op("pool", "tensor_tensor", rd=["v" + B, "gk" + B], wr=["v2b" + B], out=v2b[b][:, :, 0:256], in0=v3, in1=bc3(gk[b][:, 16:24], 256), op=ALU.mult)
        st.op("dve", "tensor_copy", rd=["gk" + B], wr=["v2f" + B], out=v2f[b][:, :, 256], in_=gk[b][:, 0:8])
        st.op("pool", "tensor_copy", rd=["gk" + B], wr=["v2b" + B], out=v2b[b][:, :, 256], in_=gk[b][:, 16:24])
        st.dma("pool", v2f_s[rows, :], v2f[b][:].rearrange("p h d -> p (h d)"), rd=["v2f" + B])
        st.dma("pool", v2b_s[rows, :], v2b[b][:].rearrange("p h d -> p (h d)"), rd=["v2b" + B])
    st.emit()


def stage_mlstm_scan(g, bwd, order, cst, ident_bf, qT_s, kT_s, kb_s, v2_s, gpk_s, hf_s, p=None, hnorm_l=None, yaT=None, skip_out=()):
    st = Stage(g, "mscan")
    cs = st.sb([128, 512], F32, "cst")
    st.dma("sp", cs[:], cst, wr=["cst"])
    MASK = cs[:, 128:256] if bwd else cs[:, 0:128]
    o_ebc = 24 if bwd else 8
    o_eB = 40 if bwd else 32
    C32 = st.sb([128, 8, 257], F32, "C32")
    Cb = st.sb([128, 8, 257], BF16, "Cb")
    st.op("dve", "memset", wr=["C32_%d" % h for h in range(8)], ap=C32[:], constant=0.0)
    st.op("pool", "memset", wr=["Cb_%d" % h for h in range(8)], ap=Cb[:], constant=0.0)
    NB = 2
    qT = [st.sb([128, 1024], BF16, "qT") for _ in range(NB)]
    kT = [st.sb([128, 1024], BF16, "kT") for _ in range(NB)]
    kb = [st.sb([128, 1024], BF16, "kb") for _ in range(NB)]
    v2 = [st.sb([128, 8, 257], BF16, "v2") for _ in range(NB)]
    gk = [st.sb([128, 48], F32, "gk") for _ in range(NB)]
    hb = [st.sb([128, 2048], F32, "hb") for _ in range(NB)]
    sm = [st.sb([128, 128], BF16, "sm") for _ in range(2)]
    dd = [st.sb([128, 4], F32, "dd") for _ in range(2)]
    ps_s = [st.ps([128, 512], F32, "ps_s") for _ in range(2)]
    ps_o = [st.ps([128, 512], F32, "ps_o") for _ in range(2)]
    ps_c = [st.ps([128, 512], F32, "ps_c") for _ in range(2)]
    if bwd:
        ident = st.sb([128, 128], BF16, "ident")
        st.dma("sp", ident[:], ident_bf, wr=["ident"])
        HN = st.sb([128, 2048], F32, "HN")
        st.dma("sp", HN[:], bcast_row(hnorm_l), wr=["HN"])
        hf = st.sb([128, 2048], F32, "hf")
        og = st.sb([128, 2048], F32, "og")
        sq = st.sb([128, 2048], F32, "sq")
        yab = st.sb([128, 2048], BF16, "yab")
        ms = st.sb([128, 8], F32, "ms")
        tr = TrOut(st, ident, nb=1)
    hc = 0
    for i, t in enumerate(order):
        b = i % NB
        B = "%d" % b
        rows = slice(t * 128, (t + 1) * 128)
        st.dma("sp", qT[b][:], qT_s[t], wr=["qT" + B])
        st.dma("sp", kT[b][:], kT_s[t], wr=["kT" + B])
        st.dma("sp", kb[b][:], kb_s[rows, :], wr=["kb" + B])
        st.dma("sp", v2[b][:].rearrange("p h d -> p (h d)"), v2_s[rows, :], wr=["v2" + B])
        st.dma("sp", gk[b][:], gpk_s[rows, :], wr=["gk" + B])
        def smat(h_, j_):
            hs_ = slice(h_ * 128, (h_ + 1) * 128)
            st.op("pe", "matmul", rd=["kT" + B, "qT" + B], wr=["ps_s%d" % j_], out=ps_s[j_][:, 0:128], lhsT=kT[b][:, hs_], rhs=qT[b][:, hs_], start=True, stop=True)

        smat(0, hc % 2)
        for h in range(8):
            j = hc % 2
            J = "%d" % j
            hc += 1
            hs = slice(h * 128, (h + 1) * 128)
            if h + 1 < 8:
                smat(h + 1, hc % 2)
            st.op("dve", "tensor_tensor", rd=["ps_s" + J, "cst"], wr=["sm" + J], out=sm[j][:], in0=ps_s[j][:, 0:128], in1=MASK, op=ALU.mult)
            st.op("pe", "matmul", rd=["qT" + B, "Cb_%d" % h], wr=["ps_o" + J], out=ps_o[j][:, 0:257], lhsT=qT[b][:, hs], rhs=Cb[:, h, :], start=True, stop=False)
            st.op("pe", "matmul", rd=["sm" + J, "v2" + B], wr=["ps_o" + J], out=ps_o[j][:, 0:257], lhsT=sm[j][:], rhs=v2[b][:, h, :], start=False, stop=True)
            ebc = gk[b][:, o_ebc + h:o_ebc + h + 1]
            st.op("dve", "tensor_scalar", rd=["ps_o" + J, "gk" + B], wr=["dd" + J], out=dd[j][:, 3:4], in0=ps_o[j][:, 256:257], scalar1=ebc, scalar2=-1.0,
                  op0=ALU.mult, op1=ALU.mult)
            st.op("dve", "tensor_scalar", rd=["ps_o" + J, "gk" + B], wr=["dd" + J], out=dd[j][:, 0:1], in0=ps_o[j][:, 256:257], scalar1=ebc, scalar2=1.0,
                  op0=ALU.mult, op1=ALU.max)
            st.op("dve", "tensor_tensor", rd=["dd" + J], wr=["dd" + J], out=dd[j][:, 0:1], in0=dd[j][:, 0:1], in1=dd[j][:, 3:4], op=ALU.max)
            st.op("dve", "reciprocal", rd=["dd" + J], wr=["dd" + J], out=dd[j][:, 1:2], in_=dd[j][:, 0:1])
            st.op("dve", "tensor_tensor", rd=["dd" + J, "gk" + B], wr=["dd" + J], out=dd[j][:, 2:3], in0=dd[j][:, 1:2], in1=ebc, op=ALU.mult)
            st.op("act", "activation", rd=["ps_o" + J, "dd" + J], wr=["hb" + B], out=hb[b][:, h * 256:(h + 1) * 256], in_=ps_o[j][:, 0:256],
                  func=AF.Identity, scale=dd[j][:, 2:3])
            st.op("pe", "matmul", rd=["kb" + B, "v2" + B], wr=["ps_c" + J], out=ps_c[j][:, 0:257], lhsT=kb[b][:, hs], rhs=v2[b][:, h, :], start=True, stop=True)
            st.op("dve", "tensor_tensor", rd=["ps_c" + J, "C32_%d" % h], wr=["C32_%d" % h], out=C32[:, h, :], in0=ps_c[j][:, 0:257], in1=C32[:, h, :], op=ALU.add)
            st.op("pool", "tensor_scalar", rd=["C32_%d" % h, "gk" + B], wr=["C32_%d" % h], out=C32[:, h, :], in0=C32[:, h, :],
                  scalar1=gk[b][:, o_eB + h:o_eB + h + 1], scalar2=None, op0=ALU.mult)
            st.op("act", "copy", rd=["C32_%d" % h], wr=["Cb_%d" % h], out=Cb[:, h, :], in_=C32[:, h, :])
        if not bwd:
            st.dma("pool", hf_s[rows, :], hb[b][:], rd=["hb" + B])
        elif t not in skip_out:
            st.dma("sp", hf[:], hf_s[rows, :], wr=["hf"])
            st.dma("sp", og[:], p[rows, O_AO:O_AO + 2048], wr=["og"])
            st.op("dve", "tensor_tensor", rd=["hb" + B, "hf"], wr=["hf"], out=hf[:], in0=hb[b][:], in1=hf[:], op=ALU.add)
            st.op("act", "activation", rd=["hf"], wr=["sq"], out=sq[:], in_=hf[:], func=AF.Square)
            st.op("dve", "tensor_reduce", rd=["sq"], wr=["ms"], out=ms[:], in_=sq[:].rearrange("p (h d) -> p h d", h=8), axis=AX.X, op=ALU.add)
            st.op("act", "activation", rd=["ms"], wr=["ms"], out=ms[:], in_=ms[:], func=AF.Sqrt, scale=1.0 / 256, bias=EPS)
            st.op("dve", "reciprocal", rd=["ms"], wr=["ms"], out=ms[:], in_=ms[:])
            st.op("pool", "tensor_tensor", rd=["hf", "ms"], wr=["hf"], out=hf[:].rearrange("p (h d) -> p h d", h=8),
                  in0=hf[:].rearrange("p (h d) -> p h d", h=8), in1=bc3(ms[:], 256), op=ALU.mult)
            st.op("act", "activation", rd=["og"], wr=["og"], out=og[:], in_=og[:], func=AF.Sigmoid)
            st.op("dve", "tensor_tensor", rd=["hf", "HN"], wr=["hf"], out=hf[:], in0=hf[:], in1=HN[:], op=ALU.mult)
            st.op("pool", "tensor_tensor", rd=["hf", "og"], wr=["yab"], out=yab[:], in0=hf[:], in1=og[:], op=ALU.mult)
            tr.go(yab, ["yab"], yaT[t])
    st.emit()


def host_consts(n_lat_tokens):
    import ml_dtypes
    s = np.arange(128)
    MF = (s[:, None] <= s[None, :]).astype(np.float32)
    MB = (s[:, None] >= s[None, :]).astype(np.float32)
    cst = np.concatenate([MF, MB, np.ones((128, 128), np.float32), np.eye(128, dtype=np.float32)], axis=1)
    ident_bf = np.eye(128).astype(ml_dtypes.bfloat16)
    nf = 32
    inv = (10000.0 ** (-np.arange(nf, dtype=np.float32) / nf)).astype(np.float32)
    pos = np.arange(n_lat_tokens)
    ang_r = (pos // 64).astype(np.float32)[:, None] * inv
    ang_c = (pos % 64).astype(np.float32)[:, None] * inv
    cr, sr, cc_, sc_ = np.cos(ang_r), np.sin(ang_r), np.cos(ang_c), np.sin(ang_c)
    cosf = np.concatenate([cr, cr, cc_, cc_], axis=1).astype(np.float32)
    sinf = np.concatenate([-sr, -sc_, sr, sc_], axis=1).astype(np.float32)
    SC = np.float32(128.0 ** -0.5)
    return dict(cst=cst, ident_bf=ident_bf, cosq=cosf * SC, sinq=sinf * SC, cosk=cosf, sink=sinf)


def stage_cast_T(g, src, tiles, ident_bf, dstT):
    st = Stage(g, "castT")
    ident = st.sb([128, 128], BF16, "ident")
    st.dma("sp", ident[:], ident_bf, wr=["ident"])
    x = [st.sb([128, D], F32, "x") for _ in range(2)]
    xb = [st.sb([128, D], BF16, "xb") for _ in range(2)]
    tr = TrOut(st, ident, nb=2)
    for i, t in enumerate(tiles):
        b = i % 2
        B = "%d" % b
        st.dma("sp", x[b][:], src[t * 128:(t + 1) * 128, :], wr=["x" + B])
        st.op("pool" if b else "dve", "tensor_copy", rd=["x" + B], wr=["xb" + B], out=xb[b][:], in_=x[b][:])
        tr.go(xb[b], ["xb" + B], dstT[t])
    st.emit()


def stage_conv(g, p, tiles, NT, conv_l, ident_bf, ybT):
    st = Stage(g, "conv")
    ident = st.sb([128, 128], BF16, "ident")
    st.dma("sp", ident[:], ident_bf, wr=["ident"])
    W = [st.sb([128, D], F32, "cw") for _ in range(3)]
    for j in range(3):
        st.dma("sp", W[j][:], bcast_row(conv_l[j:j + 1, :]), wr=["W%d" % j])
    NB = 2
    names = ["c0", "x0", "bb", "cm", "xm", "cp", "xp"]
    buf = {n: [st.sb([128, D], F32, n) for _ in range(NB)] for n in names}
    yb = [st.sb([128, D], BF16, "yb") for _ in range(NB)]
    tr = TrOut(st, ident, nb=2)
    for i, t in enumerate(tiles):
        b = i % NB
        B = "%d" % b
        r0 = t * 128
        first = t in (0, 2)
        last = t in (1, NT - 1)
        T = {n: buf[n][b] for n in names}
        K = {n: n + B for n in names}
        st.dma("sp", T["c0"][:], p[r0:r0 + 128, O_BC:O_BC + D], wr=[K["c0"]])
        st.dma("sp", T["x0"][:], p[r0:r0 + 128, O_BX:O_BX + D], wr=[K["x0"]])
        st.dma("sp", T["bb"][:], p[r0:r0 + 128, O_BB:O_BB + D], wr=[K["bb"]])
        for (cn, xn, off, edge) in (("cm", "xm", -1, first), ("cp", "xp", 1, last)):
            for (n, col) in ((cn, O_BC), (xn, O_BX)):
                if not edge:
                    st.dma("sp", T[n][:], p[r0 + off:r0 + off + 128, col:col + D], wr=[K[n]])
                else:
                    st.op("pool", "memset", wr=[K[n]], ap=T[n][:], constant=0.0)
                    if off < 0:
                        st.dma("sp", T[n][1:128, :], p[r0:r0 + 127, col:col + D], wr=[K[n]])
                    else:
                        st.dma("sp", T[n][0:127, :], p[r0 + 1:r0 + 128, col:col + D], wr=[K[n]])
        tt = lambda eng, o, a, bb_, op, rd, wr: st.op(eng, "tensor_tensor", rd=rd, wr=wr, out=o, in0=a, in1=bb_, op=op)
        tt("dve", T["x0"][:], T["c0"][:], T["x0"][:], ALU.mult, [K["c0"], K["x0"]], [K["x0"]])
        tt("pool", T["xm"][:], T["cm"][:], T["xm"][:], ALU.mult, [K["cm"], K["xm"]], [K["xm"]])
        tt("pool", T["xp"][:], T["cp"][:], T["xp"][:], ALU.mult, [K["cp"], K["xp"]], [K["xp"]])
        tt("dve", T["xm"][:], T["xm"][:], W[0][:], ALU.mult, [K["xm"], "W0"], [K["xm"]])
        tt("pool", T["x0"][:], T["x0"][:], W[1][:], ALU.mult, [K["x0"], "W1"], [K["x0"]])
        tt("dve", T["xp"][:], T["xp"][:], W[2][:], ALU.mult, [K["xp"], "W2"], [K["xp"]])
        tt("pool", T["x0"][:], T["x0"][:], T["xm"][:], ALU.add, [K["x0"], K["xm"]], [K["x0"]])
        tt("dve", T["x0"][:], T["x0"][:], T["xp"][:], ALU.add, [K["x0"], K["xp"]], [K["x0"]])
        tt("dve", yb[b][:], T["x0"][:], T["bb"][:], ALU.mult, [K["x0"], K["bb"]], ["yb" + B])
        tr.go(yb[b], ["yb" + B], ybT[t])
    st.emit()


def na_plan(ROWS):
    kbs, cls, keys = [], [], {}
    for i in range(ROWS // 2):
        r = 2 * i
        kb = min(max(r - 4, 0), ROWS - 9)
        r0a = min(max(r - 4, 0), ROWS - 8)
        r0b = min(max(r + 1 - 4, 0), ROWS - 8)
        key = (r - kb, r0a - kb, r0b - kb)
        if key not in keys:
            keys[key] = len(keys)
        kbs.append(kb)
        cls.append(keys[key])
    return kbs, cls, list(keys.keys())


def na_bias_host(rpb_l, ROWS):
    _, _, keys = na_plan(ROWS)
    qrow = np.arange(128) // 64
    qc = np.arange(128) % 64
    j = np.arange(576) // 64
    kc = np.arange(576) % 64
    col0 = np.clip(qc - 8, 0, 48)
    colok = (kc[None, :] >= col0[:, None]) & (kc[None, :] < col0[:, None] + 16)
    dc = np.clip(kc[None, :] - qc[:, None] + 15, 0, 30)
    out = np.full((16, len(keys), 128, 5, 128), -30000.0, np.float32)
    for ci, (rel, a, b) in enumerate(keys):
        lo = np.where(qrow == 0, a, b)
        rowok = (j[None, :] >= lo[:, None]) & (j[None, :] < lo[:, None] + 8)
        dr = np.clip(j[None, :] - rel - qrow[:, None] + 7, 0, 14)
        ok = rowok & colok
        Tm = np.where(ok[None], rpb_l[:, dr, dc], np.float32(-30000.0))
        TT = np.full((16, 640, 128), -30000.0, np.float32)
        TT[:, :576, :] = Tm.transpose(0, 2, 1)
        out[:, ci] = TT.reshape(16, 5, 128, 128).transpose(0, 2, 1, 3)
    return out.reshape(16, len(keys), 128, 640)


def stage_na_prep(g, p, tiles, qkg_l, ident_bf, cqT_s, ckT_s, cv1_s):
    st = Stage(g, "naprep")
    ident = st.sb([128, 128], BF16, "ident")
    st.dma("sp", ident[:], ident_bf, wr=["ident"])
    GQ = st.sb([128, D], F32, "GQ")
    GK = st.sb([128, D], F32, "GK")
    for h in range(16):
        st.dma("sp", GQ[:, h * 128:(h + 1) * 128], bcast_row(qkg_l[0:1, :]), wr=["GQ"])
        st.dma("sp", GK[:, h * 128:(h + 1) * 128], bcast_row(qkg_l[1:2, :]), wr=["GK"])
    st.op("dve", "tensor_scalar", rd=["GQ"], wr=["GQ"], out=GQ[:], in0=GQ[:], scalar1=128.0 ** -0.5, scalar2=None, op0=ALU.mult)
    NB = 2
    x = {n: [st.sb([128, D], F32, n) for _ in range(NB)] for n in ("q", "k", "v")}
    sq = [st.sb([128, D], F32, "sq") for _ in range(NB)]
    ss = {n: [st.sb([128, 16], F32, "ss" + n) for _ in range(NB)] for n in ("q", "k")}
    xb = {n: [st.sb([128, D], BF16, "xb" + n) for _ in range(NB)] for n in ("q", "k")}
    v1 = [st.sb([128, 16, 129], BF16, "v1") for _ in range(NB)]
    for b in range(NB):
        st.op("pool", "memset", wr=["v1%d" % b], ap=v1[b][:], constant=1.0)
    tr = TrOut(st, ident, nb=2)
    for i, t in enumerate(tiles):
        b = i % NB
        B = "%d" % b
        rows = slice(t * 128, (t + 1) * 128)
        st.dma("sp", x["q"][b][:], p[rows, O_CQ:O_CQ + D], wr=["q" + B])
        st.dma("sp", x["k"][b][:], p[rows, O_CK:O_CK + D], wr=["k" + B])
        st.dma("sp", x["v"][b][:], p[rows, O_CV:O_CV + D], wr=["v" + B])
        for n, Gt, gk_, dst in (("q", GQ, "GQ", cqT_s), ("k", GK, "GK", ckT_s)):
            xt = x[n][b]
            s_ = ss[n][b]
            st.op("act", "activation", rd=[n + B], wr=["sq" + B], out=sq[b][:], in_=xt[:], func=AF.Square)
            st.op("dve", "tensor_reduce", rd=["sq" + B], wr=["ss" + n + B], out=s_[:], in_=sq[b][:].rearrange("p (h d) -> p h d", h=16), axis=AX.X, op=ALU.add)
            st.op("act", "activation", rd=["ss" + n + B], wr=["ss" + n + B], out=s_[:], in_=s_[:], func=AF.Sqrt, scale=1.0 / 128, bias=EPS)
            st.op("dve", "reciprocal", rd=["ss" + n + B], wr=["ss" + n + B], out=s_[:], in_=s_[:])
            st.op("pool", "tensor_tensor", rd=[n + B, "ss" + n + B], wr=[n + B], out=xt[:].rearrange("p (h d) -> p h d", h=16),
                  in0=xt[:].rearrange("p (h d) -> p h d", h=16), in1=bc3(s_[:], 128), op=ALU.mult)
            st.op("dve", "tensor_tensor", rd=[n + B, gk_], wr=["xb" + n + B], out=xb[n][b][:], in0=xt[:], in1=Gt[:], op=ALU.mult)
            tr.go(xb[n][b], ["xb" + n + B], dst[t])
        st.op("pool", "tensor_copy", rd=["v" + B], wr=["v1" + B], out=v1[b][:, :, 0:128], in_=x["v"][b][:].rearrange("p (h d) -> p h d", h=16))
        st.dma("pool", cv1_s[rows, :], v1[b][:].rearrange("p h d -> p (h d)"), rd=["v1" + B])
    st.emit()


def stage_na(g, NL, need_ctx, nab_l, ident_bf, cqT_s, ckT_s, cv1_s, yc_s):
    st = Stage(g, "na")
    NT = NL + 2
    ROWS = 2 * NL
    kbs, cls, keys = na_plan(ROWS)
    NC = len(keys)
    ident = st.sb([128, 128], BF16, "ident")
    st.dma("sp", ident[:], ident_bf, wr=["ident"])
    nm8 = st.sb([128, 1], F32, "nm8")
    st.op("dve", "memset", wr=["nm8"], ap=nm8[:], constant=-8.0)
    kTh = st.sb([128, NT * 128], BF16, "kTh")
    qTh = st.sb([128, NT * 128], BF16, "qTh")
    va = st.sb([128, NT, 129], BF16, "va")
    vb = st.sb([128, NL, 129], BF16, "vb")
    bst = st.sb([128, NC, 640], F32, "bst")
    bT = st.sb([128, NC, 640], BF16, "bT")
    E = [st.sb([128, 1024], BF16, "E") for _ in range(2)]
    ob = [st.sb([128, 128], F32, "ob") for _ in range(2)]
    rr = [st.sb([128, 1], F32, "rr") for _ in range(2)]
    ps = [st.ps([128, 1024], F32, "nps") for _ in range(2)]
    po = [st.ps([128, 512], F32, "npo") for _ in range(2)]
    cv3 = cv1_s.rearrange("(t p) (h c) -> p t h c", p=128, h=16)
    cnt = 0
    for h in range(16):
        hs = slice(h * 128, (h + 1) * 128)
        st.dma("sp", kTh[:].rearrange("p (t c) -> p t c", t=NT), ckT_s[:, :, hs].rearrange("t p c -> p t c"), wr=["kTh"])
        st.dma("sp", qTh[:].rearrange("p (t c) -> p t c", t=NT), cqT_s[:, :, hs].rearrange("t p c -> p t c"), wr=["qTh"])
        st.dma("sp", va[:], cv3[:, :, h, :], wr=["va"])
        st.dma("sp", vb[:, 0:NL - 1, :], cv1_s[256 + 64:256 + 64 + (NL - 1) * 128, h * 129:(h + 1) * 129].rearrange("(t p) c -> p t c", p=128), wr=["vb"])
        st.dma("sp", vb[0:64, NL - 1, :], cv1_s[256 + 64 + (NL - 1) * 128:256 + NL * 128, h * 129:(h + 1) * 129], wr=["vb"])
        st.dma("sp", bst[:], nab_l[h].rearrange("c p x -> p c x"), wr=["bst"])
        st.op("pool", "tensor_copy", rd=["bst"], wr=["bT"], out=bT[:], in_=bst[:])
        qtiles = ([0, 1] if need_ctx else []) + list(range(2, NT))

        def qk(t, j):
            J = "%d" % j
            q_ = qTh[:, t * 128:(t + 1) * 128]
            pj = ps[j]
            if t >= 2:
                i = t - 2
                kb, c_ = kbs[i], cls[i]
                tok0 = 256 + kb * 64
                for c in range(5):
                    kn = 128 if c < 4 else 64
                    st.op("pe", "matmul", rd=["kTh", "qTh"], wr=["ps" + J], out=pj[0:kn, c * 128:(c + 1) * 128],
                          lhsT=kTh[:, tok0 + c * 128:tok0 + c * 128 + kn], rhs=q_, start=True, stop=False)
                    st.op("pe", "matmul", rd=["ident", "bT"], wr=["ps" + J], out=pj[0:kn, c * 128:(c + 1) * 128],
                          lhsT=ident[0:kn, 0:kn], rhs=bT[0:kn, c_, c * 128:(c + 1) * 128], start=False, stop=True)
            for c in range(2):
                st.op("pe", "matmul", rd=["kTh", "qTh"], wr=["ps" + J], out=pj[:, (5 + c) * 128:(6 + c) * 128],
                      lhsT=kTh[:, c * 128:(c + 1) * 128], rhs=q_, start=True, stop=True)

        def rest(t, j):
            J = "%d" % j
            pj = ps[j]
            if t >= 2:
                kb = kbs[t - 2]
                st.op("act", "activation", rd=["ps" + J, "nm8"], wr=["E" + J], out=E[j][:, 0:512], in_=pj[:, 0:512], func=AF.Exp, bias=nm8[:])
                st.op("act", "activation", rd=["ps" + J, "nm8"], wr=["E" + J], out=E[j][0:64, 512:640], in_=pj[0:64, 512:640], func=AF.Exp, bias=nm8[0:64, :])
            st.op("act", "activation", rd=["ps" + J, "nm8"], wr=["E" + J], out=E[j][:, 640:896], in_=pj[:, 640:896], func=AF.Exp, bias=nm8[:])
            chunks = []
            if t >= 2:
                for c in range(5):
                    kn = 128 if c < 4 else 64
                    if kb % 2 == 0:
                        vv = va[0:kn, 2 + kb // 2 + c, :]
                    else:
                        vv = vb[0:kn, (kb - 1) // 2 + c, :]
                    chunks.append((E[j][0:kn, c * 128:(c + 1) * 128], vv))
            for c in range(2):
                chunks.append((E[j][:, (5 + c) * 128:(6 + c) * 128], va[:, c, :]))
            for ci, (l_, r_) in enumerate(chunks):
                st.op("pe", "matmul", rd=["E" + J, "va", "vb"], wr=["po" + J], out=po[j][:, 0:129], lhsT=l_, rhs=r_,
                      start=(ci == 0), stop=(ci == len(chunks) - 1))
            st.op("dve", "reciprocal", rd=["po" + J], wr=["rr" + J], out=rr[j][:], in_=po[j][:, 128:129])
            st.op("dve", "tensor_scalar", rd=["po" + J, "rr" + J], wr=["ob" + J], out=ob[j][:], in0=po[j][:, 0:128], scalar1=rr[j][:], scalar2=None, op0=ALU.mult)
            st.dma("pool", yc_s[t * 128:(t + 1) * 128, hs], ob[j][:], rd=["ob" + J])

        qk(qtiles[0], cnt % 2)
        for n, t in enumerate(qtiles):
            if n + 1 < len(qtiles):
                qk(qtiles[n + 1], (cnt + 1) % 2)
            rest(t, cnt % 2)
            cnt += 1
    st.emit()


def stage_gate(g, p, tiles, brs, ident_bf, yT):
    st = Stage(g, "gate")
    ident = st.sb([128, 128], BF16, "ident")
    st.dma("sp", ident[:], ident_bf, wr=["ident"])
    NB = 2
    gt = [[st.sb([128, D], F32, "gt") for _ in range(3)] for _ in range(NB)]
    br = [[st.sb([128, D], F32, "br") for _ in range(3)] for _ in range(NB)]
    yb = [st.sb([128, D], BF16, "yb") for _ in range(NB)]
    tr = TrOut(st, ident, nb=2)
    for i, t in enumerate(tiles):
        b = i % NB
        B = "%d" % b
        rows = slice(t * 128, (t + 1) * 128)
        for j in range(3):
            st.dma("sp", gt[b][j][:], p[rows, O_GT + j * D:O_GT + (j + 1) * D], wr=["gt%d" % j + B])
            st.dma("sp", br[b][j][:], brs[j][rows, :], wr=["br%d" % j + B])
            st.op("act", "activation", rd=["gt%d" % j + B], wr=["gt%d" % j + B], out=gt[b][j][:], in_=gt[b][j][:], func=AF.Sigmoid)
            st.op("pool" if j == 1 else "dve", "tensor_tensor", rd=["gt%d" % j + B, "br%d" % j + B], wr=["br%d" % j + B],
                  out=br[b][j][:], in0=br[b][j][:], in1=gt[b][j][:], op=ALU.mult)
        st.op("pool", "tensor_tensor", rd=["br0" + B, "br1" + B], wr=["br0" + B], out=br[b][0][:], in0=br[b][0][:], in1=br[b][1][:], op=ALU.add)
        st.op("dve", "tensor_tensor", rd=["br0" + B, "br2" + B], wr=["yb" + B], out=yb[b][:], in0=br[b][0][:], in1=br[b][2][:], op=ALU.add)
        tr.go(yb[b], ["yb" + B], yT[t])
    st.emit()


def stage_resid(g, xsrc, ysrc, tiles, modv, seg, dst, dst_row0=0):
    st = Stage(g, "resid")
    Gv = [st.sb([128, D], F32, "Gv") for _ in range(2)]
    for r in range(2):
        st.dma("sp", Gv[r][:], bcast_row(modv[r:r + 1, seg * D:(seg + 1) * D]), wr=["G%d" % r])
    NB = 2
    x = [st.sb([128, D], F32, "x") for _ in range(NB)]
    y = [st.sb([128, D], F32, "y") for _ in range(NB)]
    for i, t in enumerate(tiles):
        b = i % NB
        B = "%d" % b
        r = 1 if t < 2 else 0
        rows = slice(t * 128, (t + 1) * 128)
        st.dma("sp", x[b][:], xsrc[rows, :], wr=["x" + B])
        st.dma("sp", y[b][:], ysrc[rows, :], wr=["y" + B])
        st.op("pool" if b else "dve", "tensor_tensor", rd=["y" + B, "G%d" % r], wr=["y" + B], out=y[b][:], in0=y[b][:], in1=Gv[r][:], op=ALU.mult)
        st.op("dve", "tensor_tensor", rd=["x" + B, "y" + B], wr=["y" + B], out=y[b][:], in0=y[b][:], in1=x[b][:], op=ALU.add)
        st.dma("pool", dst[t * 128 - dst_row0:(t + 1) * 128 - dst_row0, :], y[b][:], rd=["y" + B])
    st.emit()


def stage_uT(g, u_l, cst, uT_s):
    st = Stage(g, "uT")
    cs = st.sb([128, 512], F32, "cst")
    st.dma("sp", cs[:], cst, wr=["cst"])
    IDF = cs[:, 384:512]
    ub = [st.sb([128, D], F32, "ub") for _ in range(2)]
    ob = [st.sb([128, KC, 128], BF16, "uo") for _ in range(2)]
    pu = [st.ps([128, D], F32, "pu") for _ in range(2)]
    for eb in range(128):
        b = eb % 2
        B = "%d" % b
        st.dma("sp", ub[b][:], u_l[eb * 128:(eb + 1) * 128, :], wr=["ub" + B])
        for k in range(KC):
            st.op("pe", "transpose", rd=["ub" + B, "cst"], wr=["pu" + B], out=pu[b][:, k * 128:(k + 1) * 128], in_=ub[b][:, k * 128:(k + 1) * 128], identity=IDF)
        for q4 in range(4):
            st.op("act" if q4 % 2 == 0 else "dve", "copy" if q4 % 2 == 0 else "tensor_copy", rd=["pu" + B], wr=["uo%d" % q4 + B],
                  out=ob[b][:, q4 * 4:(q4 + 1) * 4, :], in_=pu[b][:, q4 * 512:(q4 + 1) * 512].rearrange("p (k e) -> p k e", k=4))
        st.dma("pool", uT_s[:, eb * 128:(eb + 1) * 128].rearrange("(k p) e -> p k e", p=128), ob[b][:], rd=["uo%d" % q4 + B for q4 in range(4)])
    st.emit()


def stage_peer_scores(g, xT, tiles, wq_l, keys_l, cst, sc_s, selp_s):
    st = Stage(g, "pscore")
    cs = st.sb([128, 512], F32, "cst")
    st.dma("sp", cs[:], cst, wr=["cst"])
    IDF = cs[:, 384:512]
    wq = st.sb([128, KC, D], BF16, "wq")
    wst = [st.sb([128, KC, 512], F32, "wst") for _ in range(2)]
    for n in range(4):
        st.dma("sp", wst[n % 2][:], wq_l[:, n * 512:(n + 1) * 512].rearrange("(k p) c -> p k c", p=128), wr=["wst%d" % (n % 2)])
        st.op("pool" if n % 2 else "dve", "tensor_copy", rd=["wst%d" % (n % 2)], wr=["wq"], out=wq[:, :, n * 512:(n + 1) * 512], in_=wst[n % 2][:])
    keysT = st.sb([128, 16, 128], BF16, "keysT")
    kst = [st.sb([128, 128], F32, "kst") for _ in range(2)]
    pq = [st.ps([128, 512], F32, "pq") for _ in range(2)]
    kl = keys_l.rearrange("h p k d -> (h p) k d")
    for blk in range(16):
        j = blk % 2
        st.dma("sp", kst[j][:], kl[blk], wr=["kst%d" % j])
        st.op("pe", "transpose", rd=["kst%d" % j, "cst"], wr=["pq%d" % j], out=pq[j][:, 0:128], in_=kst[j][:], identity=IDF)
        st.op("dve", "tensor_copy", rd=["pq%d" % j], wr=["keysT"], out=keysT[:, blk, :], in_=pq[j][:, 0:128])
    pss = st.ps([128, D], F32, "pss")
    NB = 2
    xt = [st.sb([128, D], BF16, "xt") for _ in range(NB)]
    qTb = [st.sb([128, 128], BF16, "qTb") for _ in range(2)]
    sc = [st.sb([128, D], F32, "sc") for _ in range(NB)]
    wk = st.sb([128, 256], F32, "wk")
    t16 = st.sb([128, 16, 16], F32, "t16")
    cand = st.sb([128, 16, 16], F32, "cand")
    ex = st.sb([128, 256], F32, "ex")
    junk = st.sb([128, 256], F32, "junk")
    c8 = st.sb([128, 16], F32, "c8")
    sm_ = st.sb([128, 32], F32, "small")
    selp = [st.sb([128, 16], F32, "selp") for _ in range(NB)]
    qc = 0
    for i, t in enumerate(tiles):
        b = i % NB
        B = "%d" % b
        rows = slice(t * 128, (t + 1) * 128)
        st.dma("sp", xt[b][:], xT[t], wr=["xt" + B])
        for blk in range(16):
            j = qc % 2
            J = "%d" % j
            qc += 1
            for k in range(KC):
                st.op("pe", "matmul", rd=["wq", "xt" + B], wr=["pq" + J], out=pq[j][:, 0:128], lhsT=wq[:, k, blk * 128:(blk + 1) * 128],
                      rhs=xt[b][:, k * 128:(k + 1) * 128], start=(k == 0), stop=(k == KC - 1))
            if j == 0:
                st.op("act", "copy", rd=["pq" + J], wr=["qTb" + J], out=qTb[j][:], in_=pq[j][:, 0:128])
            else:
                st.op("dve", "tensor_copy", rd=["pq" + J], wr=["qTb" + J], out=qTb[j][:], in_=pq[j][:, 0:128])
            st.op("pe", "matmul", rd=["qTb" + J, "keysT"], wr=["pss"], out=pss[:, blk * 128:(blk + 1) * 128], lhsT=qTb[j][:], rhs=keysT[:, blk, :], start=True, stop=True)
        for q4 in range(4):
            st.op("act" if q4 % 2 == 0 else "dve", "copy" if q4 % 2 == 0 else "tensor_copy", rd=["pss"], wr=["sc%d" % q4 + B],
                  out=sc[b][:, q4 * 512:(q4 + 1) * 512], in_=pss[:, q4 * 512:(q4 + 1) * 512])
        SK = ["sc%d" % q4 + B for q4 in range(4)]
        st.dma("pool", sc_s[rows, :], sc[b][:], rd=SK)
        for blk in range(16):
            sl = sc[b][:, blk * 128:(blk + 1) * 128]
            st.op("dve", "max", rd=SK, wr=["t16"], out=t16[:, blk, 0:8], in_=sl)
            st.op("dve", "match_replace", rd=SK + ["t16"], wr=["wk"], out=wk[:, 0:128], in_to_replace=t16[:, blk, 0:8], in_values=sl, imm_value=-1e30)
            st.op("dve", "max", rd=["wk"], wr=["t16"], out=t16[:, blk, 8:16], in_=wk[:, 0:128])
        for h in range(8):
            st.op("dve", "tensor_tensor", rd=["t16"], wr=["cand"], out=cand[:], in0=bc3(t16[:, 2 * h, :], 16), in1=bcmid(t16[:, 2 * h + 1, :], 16), op=ALU.add)
            cf = cand[:].rearrange("p a b -> p (a b)")
            st.op("dve", "max", rd=["cand"], wr=["c8"], out=c8[:, 0:8], in_=cf)
            st.op("dve", "match_replace", rd=["cand", "c8"], wr=["wk"], out=wk[:], in_to_replace=c8[:, 0:8], in_values=cf, imm_value=-1e30)
            st.op("dve", "max", rd=["wk"], wr=["c8"], out=c8[:, 8:16], in_=wk[:])
            st.op("dve", "tensor_scalar", rd=["c8"], wr=["small"], out=sm_[:, 0:1], in0=c8[:, 0:1], scalar1=-1.0, scalar2=None, op0=ALU.mult)
            st.op("act", "activation", rd=["cand", "small"], wr=["ex"], out=ex[:], in_=cf, func=AF.Exp, bias=sm_[:, 0:1])
            st.op("dve", "scalar_tensor_tensor", rd=["cand", "c8", "ex"], wr=["junk", "small"], out=junk[:], in0=cf, scalar=c8[:, 15:16], in1=ex[:],
                  op0=ALU.is_ge, op1=ALU.mult, accum_out=sm_[:, 1:2])
            st.op("act", "activation", rd=["small"], wr=["small"], out=sm_[:, 2:3], in_=sm_[:, 1:2], func=AF.Ln)
            st.op("dve", "tensor_copy", rd=["c8"], wr=["selp" + B], out=selp[b][:, 2 * h:2 * h + 1], in_=c8[:, 15:16])
            st.op("dve", "tensor_tensor", rd=["small"], wr=["selp" + B], out=selp[b][:, 2 * h + 1:2 * h + 2], in0=sm_[:, 0:1], in1=sm_[:, 2:3], op=ALU.subtract)
        st.dma("pool", selp_s[rows, :], selp[b][:], rd=["selp" + B])
    st.emit()


def stage_peer_experts(g, xT, tiles, uT_s, v_l, ident_bf, sc_s, selp_s, po_s, NG=16):
    st = Stage(g, "pexp")
    ident = st.sb([128, 128], BF16, "ident")
    st.dma("sp", ident[:], ident_bf, wr=["ident"])
    uTg = st.sb([128, KC, 1024], BF16, "uTg")
    vg = st.sb([128, 8, D], BF16, "vg")
    vst = [st.sb([128, D], F32, "vst") for _ in range(2)]
    NB = 2
    xt = [st.sb([128, D], BF16, "xt") for _ in range(NB)]
    sc = [st.sb([128, D], F32, "sc") for _ in range(NB)]
    selp = [st.sb([128, 16], F32, "selp") for _ in range(NB)]
    sm = [st.sb([128, 1024], F32, "sm") for _ in range(2)]
    ex = [st.sb([128, 1024], F32, "ex") for _ in range(2)]
    mk = [[st.sb([128, 1024], BF16, "mk") for _ in range(8)] for _ in range(NB)]
    gl = [st.sb([128, 1024], F32, "gl") for _ in range(NB)]
    WT = [st.sb([128, 1024], BF16, "WT") for _ in range(NB)]
    ob = [st.sb([128, D], F32, "ob") for _ in range(2)]
    ps_h = st.ps([128, 1024], F32, "ps_h")
    ps_g = st.ps([128, 1024], F32, "ps_g")
    ps_o = [st.ps([128, 512], F32, "ps_o") for _ in range(4)]
    hc = [0]
    iters = [(gi, t) for gi in range(NG) for t in tiles]
    NI = len(iters)

    def loads(n):
        gi, t = iters[n]
        B = "%d" % (n % NB)
        b = n % NB
        rows = slice(t * 128, (t + 1) * 128)
        if t == tiles[0]:
            st.dma("sp", uTg[:], uT_s[:, gi * 1024:(gi + 1) * 1024].rearrange("(k p) e -> p k e", p=128), wr=["uTg"])
        st.dma("sp", xt[b][:], xT[t], wr=["xt" + B])
        st.dma("sp", sc[b][:], sc_s[rows, :], wr=["sc" + B])
        st.dma("sp", selp[b][:], selp_s[rows, :], wr=["selp" + B])

    def heads(n, h0, h1):
        gi, t = iters[n]
        b = n % NB
        B = "%d" % b

        def add(h, j):
            s1 = sc[b][:, (2 * h) * 128 + gi * 8:(2 * h) * 128 + gi * 8 + 8]
            s2 = sc[b][:, (2 * h + 1) * 128:(2 * h + 2) * 128]
            sm3 = sm[j][:].rearrange("p (a b) -> p a b", a=8)
            st.op("dve", "tensor_tensor", rd=["sc" + B], wr=["sm%d" % j], out=sm3, in0=bc3(s1, 128), in1=bcmid(s2, 8), op=ALU.add)

        add(h0, hc[0] % 2)
        for h in range(h0, h1):
            j = hc[0] % 2
            J = "%d" % j
            hc[0] += 1
            if h + 1 < h1:
                add(h + 1, hc[0] % 2)
            st.op("act", "activation", rd=["sm" + J, "selp" + B], wr=["ex" + J], out=ex[j][:], in_=sm[j][:], func=AF.Exp, bias=selp[b][:, 2 * h + 1:2 * h + 2])
            st.op("dve", "scalar_tensor_tensor", rd=["sm" + J, "ex" + J, "selp" + B], wr=["mk%d" % h + B], out=mk[b][h][:], in0=sm[j][:], scalar=selp[b][:, 2 * h:2 * h + 1],
                  in1=ex[j][:], op0=ALU.is_ge, op1=ALU.mult)

    def hT(n):
        b = n % NB
        B = "%d" % b
        for c in range(8):
            for k in range(KC):
                st.op("pe", "matmul", rd=["uTg", "xt" + B], wr=["ps_h"], out=ps_h[:, c * 128:(c + 1) * 128], lhsT=uTg[:, k, c * 128:(c + 1) * 128],
                      rhs=xt[b][:, k * 128:(k + 1) * 128], start=(k == 0), stop=(k == KC - 1))

    def gelu(n):
        b = n % NB
        B = "%d" % b
        for q2 in range(2):
            st.op("act", "activation", rd=["ps_h"], wr=["gl%d" % q2 + B], out=gl[b][:, q2 * 512:(q2 + 1) * 512], in_=ps_h[:, q2 * 512:(q2 + 1) * 512], func=AF.Gelu)

    def vload(n):
        gi, t = iters[n]
        if t == tiles[0]:
            for c in range(8):
                j = c % 2
                st.dma("sp", vst[j][:], v_l[gi * 1024 + c * 128:gi * 1024 + (c + 1) * 128, :], wr=["vst%d" % j])
                st.op("pool", "tensor_copy", rd=["vst%d" % j], wr=["vg"], out=vg[:, c, :], in_=vst[j][:])

    def mask(n):
        b = n % NB
        B = "%d" % b
        for c in range(8):
            for h in range(8):
                st.op("pe", "matmul", rd=["mk%d" % h + B, "ident"], wr=["ps_g"], out=ps_g[:, c * 128:(c + 1) * 128], lhsT=mk[b][h][:, c * 128:(c + 1) * 128],
                      rhs=ident[:], start=(h == 0), stop=(h == 7))

    def wt(n):
        b = n % NB
        B = "%d" % b
        for q2 in range(2):
            st.op("dve", "tensor_tensor", rd=["ps_g", "gl%d" % q2 + B], wr=["WT" + B], out=WT[b][:, q2 * 512:(q2 + 1) * 512], in0=ps_g[:, q2 * 512:(q2 + 1) * 512],
                  in1=gl[b][:, q2 * 512:(q2 + 1) * 512], op=ALU.mult)

    def outmm(n):
        b = n % NB
        B = "%d" % b
        for cb in range(4):
            for c in range(8):
                st.op("pe", "matmul", rd=["WT" + B, "vg"], wr=["ps_o%d" % cb], out=ps_o[cb][:], lhsT=WT[b][:, c * 128:(c + 1) * 128], rhs=vg[:, c, cb * 512:(cb + 1) * 512],
                      start=(c == 0), stop=(c == 7))

    def evac_store(n):
        gi, t = iters[n]
        b = n % NB
        rows = slice(t * 128, (t + 1) * 128)
        o = ob[b]
        for cb in range(4):
            if cb % 2 == 0:
                st.op("act", "copy", rd=["ps_o%d" % cb], wr=["ob%d_%d" % (b, cb)], out=o[:, cb * 512:(cb + 1) * 512], in_=ps_o[cb][:])
            else:
                st.op("dve", "tensor_copy", rd=["ps_o%d" % cb], wr=["ob%d_%d" % (b, cb)], out=o[:, cb * 512:(cb + 1) * 512], in_=ps_o[cb][:])
        okeys = ["ob%d_%d" % (b, cb) for cb in range(4)]
        if gi == 0:
            st.dma("pool", po_s[rows, :], o[:], rd=okeys, wr=[("po", t)])
        else:
            st.dma("pool", po_s[rows, :], o[:], rd=okeys, wr=[("po", t)], accum_op=ALU.add)

    loads(0)
    heads(0, 0, 8)
    hT(0)
    gelu(0)
    for n in range(NI):
        nx = n + 1 < NI
        if nx:
            loads(n + 1)
        vload(n)
        mask(n)
        if nx:
            heads(n + 1, 0, 4)
        wt(n)
        if nx:
            hT(n + 1)
        outmm(n)
        if nx:
            heads(n + 1, 4, 8)
            gelu(n + 1)
        evac_store(n)
    st.emit()


def build_program(NL, depth=2):
    NT = NL + 2
    ROWS = 2 * NL
    NCLS = len(na_plan(ROWS)[2])
    nc = bass.Bass("TRN2", target_bir_lowering=False)

    def inp(name, shape, dt=F32):
        return nc.dram_tensor(name, list(shape), dt, kind="ExternalInput").ap()

    xcat = inp("xcat", [NT * 128, D])
    cc = inp("cc", [2, D])
    w_ada = inp("w_ada", [depth, D, 6 * D])
    b_ada = inp("b_ada", [depth, 6 * D])
    norm_g = inp("norm_g", [depth, 2 * D])
    w_in = inp("w_in", [depth, D, PW])
    gate_b = inp("a_gate_b", [depth, 32])
    hnorm = inp("a_hnorm_g", [depth, D])
    b_conv = inp("b_conv", [depth, 3, D])
    qkg = inp("c_qk_g", [depth, 2, 128])
    nab = inp("nab", [depth, 16, NCLS, 128, 640])
    w_a = inp("w_a_out", [depth, D, D])
    w_b = inp("w_b_out", [depth, D, D])
    w_c = inp("w_c_out", [depth, D, D])
    w_o = inp("w_out", [depth, D, D])
    wq = inp("peer_wq", [depth, D, D])
    pkeys = inp("peer_keys", [depth, 8, 2, 128, 128])
    pu = inp("peer_u", [depth, 16384, D])
    pv = inp("peer_v", [depth, 16384, D])
    cst = inp("cst", [128, 512])
    ident = inp("ident_bf", [128, 128], BF16)
    rope = [inp(n, [NL * 128, 128]) for n in ("cosq", "sinq", "cosk", "sink")]
    out = nc.dram_tensor("out", [NL * 128, D], F32, kind="ExternalOutput").ap()

    with ExitStack() as es:
        es.enter_context(nc.allow_low_precision("bf16 matmul operands, fp32 accumulation"))
        g = G(nc, es)
        R = NT * 128

        def dr(name, shape, dt=F32):
            return g.dram(name, shape, dt).ap()

        modv = dr("modv", [2, 6 * D])
        xnT = dr("xnT", [NT, 128, D], BF16)
        pparts = [dr("p%d" % gi, [R, PSplit.BOUNDS[gi + 1] - PSplit.BOUNDS[gi]]) for gi in range(4)]
        p = PSplit(pparts)
        qT_s = dr("qT_s", [NT, 128, 1024], BF16)
        kT_s = dr("kT_s", [NT, 128, 1024], BF16)
        kb_s = dr("kb_s", [R, 1024], BF16)
        v2f_s = dr("v2f_s", [R, 8 * 257], BF16)
        v2b_s = dr("v2b_s", [R, 8 * 257], BF16)
        gpk_s = dr("gpk_s", [R, 48])
        hf_s = dr("hf_s", [R, D])
        yaT = dr("yaT", [NT, 128, D], BF16)
        ybT = dr("ybT", [NT, 128, D], BF16)
        ycT = dr("ycT", [NT, 128, D], BF16)
        yT = dr("yT", [NT, 128, D], BF16)
        cqT_s = dr("cqT_s", [NT, 128, D], BF16)
        ckT_s = dr("ckT_s", [NT, 128, D], BF16)
        cv1_s = dr("cv1_s", [R, 16 * 129], BF16)
        yc_s = dr("yc_s", [R, D])
        brs = [dr("br%d" % j, [R, D]) for j in range(3)]
        mix = dr("mix", [R, D])
        x1 = dr("x1", [R, D])
        xmid = dr("xmid", [R, D])
        uT_s = dr("uT_s", [D, 16384], BF16)
        sc_s = dr("sc_s", [R, D])
        selp_s = dr("selp_s", [R, 16])
        po_s = dr("po_s", [R, D])

        allt = list(range(NT))
        lat = list(range(2, NT))
        for l in range(depth):
            last = l == depth - 1
            xin = xcat if l == 0 else xmid
            T = lat if last else allt
            stage_mod(g, cc, w_ada[l], b_ada[l:l + 1, :], norm_g[l:l + 1, :], modv)
            stage_norm(g, xin, modv, 0, xnT, allt, ident)
            for gi in range(4):
                lo, hi = PSplit.BOUNDS[gi], PSplit.BOUNDS[gi + 1]
                stage_linear(g, xnT, allt, w_in[l][:, lo:hi], hi - lo, pparts[gi])
            stage_mlstm_prep(g, p, allt, cst, ident, gate_b[l:l + 1, :], rope, qT_s, kT_s, kb_s, v2f_s, v2b_s, gpk_s)
            stage_mlstm_scan(g, False, [0, 1] + lat, cst, ident, qT_s, kT_s, kb_s, v2f_s, gpk_s, hf_s)
            stage_mlstm_scan(g, True, [1, 0] + lat[::-1], cst, ident, qT_s, kT_s, kb_s, v2b_s, gpk_s, hf_s, p=p, hnorm_l=hnorm[l:l + 1, :], yaT=yaT,
                             skip_out=((0, 1) if last else ()))
            stage_conv(g, p, T, NT, b_conv[l], ident, ybT)
            stage_na_prep(g, p, allt, qkg[l], ident, cqT_s, ckT_s, cv1_s)
            stage_na(g, NL, not last, nab[l], ident, cqT_s, ckT_s, cv1_s, yc_s)
            stage_cast_T(g, yc_s, T, ident, ycT)
            stage_linear(g, yaT, T, w_a[l], D, brs[0])
            stage_linear(g, ybT, T, w_b[l], D, brs[1])
            stage_linear(g, ycT, T, w_c[l], D, brs[2])
            stage_gate(g, p, T, brs, ident, yT)
            stage_linear(g, yT, T, w_o[l], D, mix)
            stage_resid(g, xin, mix, T, modv, 2, x1)
            stage_norm(g, x1, modv, 1, xnT, T, ident)
            stage_uT(g, pu[l], cst, uT_s)
            stage_peer_scores(g, xnT, T, wq[l], pkeys[l], cst, sc_s, selp_s)
            stage_peer_experts(g, xnT, T, uT_s, pv[l], ident, sc_s, selp_s, po_s)
            if last:
                stage_resid(g, x1, po_s, T, modv, 5, out, dst_row0=256)
            else:
                stage_resid(g, x1, po_s, T, modv, 5, xmid)
        final_wait(g)
    return nc


_PROG = {}


def kernel(x, c, ctx, c_ctx, w_ada, b_ada, norm_g, w_in, a_gate_b, a_hnorm_g, b_conv, c_qk_g, c_rpb,
           w_a_out, w_b_out, w_c_out, w_out, peer_wq, peer_keys, peer_u, peer_v):
    f = lambda a: np.ascontiguousarray(np.asarray(a, dtype=np.float32))
    x, ctx = f(x), f(ctx)
    B, S, _ = x.shape
    NL = S // 128
    depth = w_ada.shape[0]
    hc = host_consts(S)
    nab = np.stack([na_bias_host(f(c_rpb)[l], 2 * NL) for l in range(depth)], 0)
    shared = dict(w_ada=f(w_ada), b_ada=f(b_ada), norm_g=f(norm_g).reshape(depth, 2 * D), w_in=f(w_in),
                  a_gate_b=f(a_gate_b).reshape(depth, 32), a_hnorm_g=f(a_hnorm_g), b_conv=f(b_conv), c_qk_g=f(c_qk_g), nab=nab,
                  w_a_out=f(w_a_out), w_b_out=f(w_b_out), w_c_out=f(w_c_out), w_out=f(w_out), peer_wq=f(peer_wq),
                  peer_keys=f(peer_keys), peer_u=f(peer_u), peer_v=f(peer_v), **hc)
    in_maps = []
    for b in range(B):
        m = dict(shared)
        m["xcat"] = np.concatenate([ctx[b], x[b]], axis=0)
        m["cc"] = np.stack([f(c)[b], f(c_ctx)], axis=0)
        in_maps.append(m)
    key = (NL, depth)
    if key not in _PROG:
        _PROG[key] = build_program(NL, depth)
    res = run_bass_kernel_spmd(_PROG[key], in_maps, core_ids=list(range(B)))
    return np.stack([res.results[b]["out"] for b in range(B)], axis=0)
```

```python
import numpy as np
from contextlib import ExitStack
import concourse.bass as bass
import concourse.mybir as mybir
from concourse.bass_utils import run_bass_kernel_spmd

F32 = mybir.dt.float32
BF16 = mybir.dt.bfloat16
ALU = mybir.AluOpType
AF = mybir.ActivationFunctionType
AX = mybir.AxisListType

D = 2048
KC = 16
PW = 24608
EPS = 1e-6
ENGS = ["pe", "act", "dve", "pool", "sp"]
NDS = 24

O_AK, O_AV, O_AG, O_CK, O_CV, O_AQ, O_AO, O_BB, O_BC, O_BX, O_CQ, O_GT = (
    0, 1024, 3072, 3104, 5152, 7200, 8224, 10272, 12320, 14368, 16416, 18464)


class PSplit:
    BOUNDS = [0, 7200, 14368, 18464, PW]

    def __init__(self, aps):
        self.aps = aps

    def __getitem__(self, key):
        rows, cols = key
        for gi in range(4):
            lo, hi = self.BOUNDS[gi], self.BOUNDS[gi + 1]
            if cols.start >= lo and cols.stop <= hi:
                return self.aps[gi][rows, cols.start - lo:cols.stop - lo]
        raise ValueError("column range straddles projection groups: %s" % (cols,))


class G:
    def __init__(self, nc, es):
        self.nc = nc
        self.sem = {e: es.enter_context(nc.semaphore("s_" + e)) for e in ENGS}
        self.dsem = [es.enter_context(nc.semaphore("d%d" % i)) for i in range(NDS)]
        self.cnt = {e: 0 for e in ENGS}
        self.dcnt = [0] * NDS
        self.dnext = 0
        self.known = {e: {} for e in ENGS}
        self.nstage = 0
        self.ntens = 0

    def dram(self, name, shape, dt):
        return self.nc.dram_tensor(name, list(shape), dt)


class Stage:
    def __init__(self, g, name=None):
        self.g = g
        g.nstage += 1
        self.name = name or ("st%d" % g.nstage)
        self.ops = {e: [] for e in ENGS}
        self.lastw = {}
        self.readers = {}
        self.es = ExitStack()
        self.start_cnt = dict(g.cnt)
        self.start_d = list(g.dcnt)

    def sb(self, shape, dt=F32, name=None):
        self.g.ntens += 1
        return self.es.enter_context(self.g.nc.sbuf_tensor("%s_t%d" % (name or "sb", self.g.ntens), list(shape), dt))

    def ps(self, shape, dt=F32, name=None):
        self.g.ntens += 1
        return self.es.enter_context(self.g.nc.psum_tensor("%s_p%d" % (name or "ps", self.g.ntens), list(shape), dt))

    def add(self, eng, fn, rd=(), wr=(), dma=False):
        g = self.g
        deps = []
        for k in rd:
            t = self.lastw.get(k)
            if t is not None:
                deps.append(t)
        for k in wr:
            t = self.lastw.get(k)
            cands = ([t] if t is not None else []) + self.readers.get(k, [])
            for c in cands:
                if (not dma) and c[0] == "c" and c[1] == eng:
                    continue
                deps.append(c)
        if dma:
            j = g.dnext
            g.dnext = (j + 1) % NDS
            g.dcnt[j] += 1
            tok = ("d", j, g.dcnt[j])
            if g.dcnt[j] > 1:
                deps.append(("d", j, g.dcnt[j] - 1))
        else:
            g.cnt[eng] += 1
            tok = ("c", eng, g.cnt[eng])
        for k in wr:
            self.lastw[k] = tok
            self.readers[k] = []
        for k in rd:
            lst = self.readers.setdefault(k, [])
            if tok[0] == "c":
                lst[:] = [x for x in lst if not (x[0] == "c" and x[1] == tok[1])]
            lst.append(tok)
        self.ops[eng].append((fn, deps, tok))
        return tok

    def op(self, eng, meth, rd=(), wr=(), **kw):
        return self.add(eng, lambda e: getattr(e, meth)(**kw), rd, wr)

    def dma(self, eng, out, in_, rd=(), wr=(), **kw):
        return self.add(eng, lambda e: e.dma_start(out=out, in_=in_, **kw), rd, wr, dma=True)

    def emit(self):
        g = self.g
        nc = g.nc

        def run(ename, e):
            known = g.known[ename]

            def wait(tok):
                if tok[0] == "c":
                    sem, val, key = g.sem[tok[1]], tok[2], tok[1]
                else:
                    sem, val, key = g.dsem[tok[1]], 16 * tok[2], "d%d" % tok[1]
                if known.get(key, 0) < val:
                    e.wait_ge(sem, val)
                    known[key] = val

            for o in ENGS:
                if o != ename and self.start_cnt[o] > 0:
                    wait(("c", o, self.start_cnt[o]))
            for j in range(NDS):
                if self.start_d[j] > 0:
                    wait(("d", j, self.start_d[j]))
            for fn, deps, tok in self.ops[ename]:
                for d in deps:
                    wait(d)
                ins = fn(e)
                if tok[0] == "c":
                    ins.then_inc(g.sem[tok[1]], 1)
                else:
                    ins.then_inc(g.dsem[tok[1]], 16)

        with nc.Block() as block:
            if self.ops["pe"]:
                block.tensor(lambda e: run("pe", e))
            if self.ops["act"]:
                block.scalar(lambda e: run("act", e))
            if self.ops["dve"]:
                block.vector(lambda e: run("dve", e))
            if self.ops["pool"]:
                block.gpsimd(lambda e: run("pool", e))
            if self.ops["sp"]:
                block.sync(lambda e: run("sp", e))
        self.es.close()


def final_wait(g):
    st = Stage(g, "final")
    nc = g.nc

    def run(e):
        for o in ENGS:
            if o != "sp" and g.cnt[o] > 0:
                e.wait_ge(g.sem[o], g.cnt[o])
        for j in range(NDS):
            if g.dcnt[j] > 0:
                e.wait_ge(g.dsem[j], 16 * g.dcnt[j])

    with nc.Block() as block:
        block.sync(run)
    st.es.close()


def bcast_row(ap_row, nparts=128):
    return ap_row.partition_broadcast(nparts)


def stage_mod(g, cc, w_ada_l, b_ada_l, norm_g_l, modv):
    st = Stage(g, "mod")
    ccT = st.sb([128, 2, KC], F32, "ccT")
    sil = st.sb([128, 2, KC], F32, "sil")
    sig = st.sb([128, 2, KC], F32, "sig")
    NCH = 6 * D // 512
    wst = [st.sb([128, KC, 512], F32, "wada") for _ in range(2)]
    bb = [st.sb([2, 512], F32, "bada") for _ in range(2)]
    ng = [st.sb([2, 512], F32, "ng") for _ in range(2)]
    res = [st.sb([2, 512], F32, "res") for _ in range(2)]
    pst = [st.ps([128, 512], F32, "pmod") for _ in range(2)]
    for r in range(2):
        st.dma("sp", ccT[:, r, :], cc[r, :].rearrange("(k p) -> p k", p=128), wr=["ccT"], allow_slow_non_contiguous=True)
    st.add("act", lambda e: e.activation(out=sig[:], in_=ccT[:], func=AF.Sigmoid), rd=["ccT"], wr=["sig"])
    st.add("dve", lambda e: e.tensor_tensor(out=sil[:], in0=ccT[:], in1=sig[:], op=ALU.mult), rd=["ccT", "sig"], wr=["sil"])
    segmap = {0: 1, 1: 0, 2: 2, 3: 4, 4: 3, 5: 5}
    for n in range(NCH):
        i = n % 2
        w = wst[i]
        seg, cs = n // 4, (n % 4) * 512
        st.dma("sp", w[:], w_ada_l[:, n * 512:(n + 1) * 512].rearrange("(k p) c -> p k c", p=128), wr=["w%d" % i])
        for r in range(2):
            st.dma("sp", bb[i][r:r + 1, :], b_ada_l[:, n * 512:(n + 1) * 512], wr=["bb%d" % i])
            if seg in (1, 4):
                j = 0 if seg == 1 else 1
                st.dma("sp", ng[i][r:r + 1, :], norm_g_l[:, j * D + cs:j * D + cs + 512], wr=["ng%d" % i])
        for k in range(KC):
            st.add("pe", lambda e, w=w, k=k, i=i: e.matmul(pst[i][0:2, :], lhsT=sil[:, :, k], rhs=w[:, k, :],
                                                           start=(k == 0), stop=(k == KC - 1)),
                   rd=["sil", "w%d" % i], wr=["p%d" % i])
        st.add("dve", lambda e, i=i: e.tensor_tensor(out=res[i][:], in0=pst[i][0:2, :], in1=bb[i][:], op=ALU.add),
               rd=["p%d" % i, "bb%d" % i], wr=["res%d" % i])
        if seg in (1, 4):
            st.add("dve", lambda e, i=i: e.scalar_tensor_tensor(out=res[i][:], in0=res[i][:], scalar=1.0, in1=ng[i][:],
                                                                op0=ALU.add, op1=ALU.mult),
                   rd=["res%d" % i, "ng%d" % i], wr=["res%d" % i])
        o0 = segmap[seg] * D + cs
        st.dma("sp", modv[:, o0:o0 + 512], res[i][:], rd=["res%d" % i])
    st.emit()


def stage_norm(g, src, modv, jn, xnT, tiles, ident_bf):
    st = Stage(g, "norm")
    A = [st.sb([128, D], F32, "A") for _ in range(2)]
    B = [st.sb([128, D], F32, "B") for _ in range(2)]
    ident = st.sb([128, 128], BF16, "ident")
    st.dma("sp", ident[:], ident_bf, wr=["ident"])
    for r in range(2):
        st.dma("sp", A[r][:], bcast_row(modv[r:r + 1, (3 * jn) * D:(3 * jn + 1) * D]), wr=["A%d" % r])
        st.dma("sp", B[r][:], bcast_row(modv[r:r + 1, (3 * jn + 1) * D:(3 * jn + 2) * D]), wr=["B%d" % r])
    NB = 2
    xt = [st.sb([128, D], F32, "x") for _ in range(NB)]
    junk = [st.sb([128, D], F32, "junk") for _ in range(NB)]
    tmp = [st.sb([128, D], F32, "tmp") for _ in range(NB)]
    xb = [st.sb([128, D], BF16, "xb") for _ in range(NB)]
    xT = [st.sb([128, D], BF16, "xT") for _ in range(NB)]
    ss = [st.sb([128, 1], F32, "ss") for _ in range(NB)]
    rs = [st.sb([128, 1], F32, "rs") for _ in range(NB)]
    pt = [st.ps([128, D], BF16, "pT") for _ in range(NB)]
    for i, t in enumerate(tiles):
        b = i % NB
        r = 1 if t < 2 else 0
        st.dma("sp", xt[b][:], src[t * 128:(t + 1) * 128, :], wr=["x%d" % b])
        st.add("act", lambda e, b=b: e.activation(out=junk[b][:], in_=xt[b][:], func=AF.Square, accum_out=ss[b][:]),
               rd=["x%d" % b], wr=["junk%d" % b, "ss%d" % b])
        st.add("act", lambda e, b=b: e.activation(out=rs[b][:], in_=ss[b][:], func=AF.Sqrt, scale=1.0 / D, bias=EPS),
               rd=["ss%d" % b], wr=["rs%d" % b])
        st.add("dve", lambda e, b=b: e.reciprocal(out=rs[b][:], in_=rs[b][:]),
               rd=["rs%d" % b], wr=["rs%d" % b])
        st.add("dve", lambda e, b=b, r=r: e.scalar_tensor_tensor(out=tmp[b][:], in0=xt[b][:], scalar=rs[b][:], in1=A[r][:],
                                                                 op0=ALU.mult, op1=ALU.mult),
               rd=["x%d" % b, "rs%d" % b, "A%d" % r], wr=["tmp%d" % b])
        st.add("dve", lambda e, b=b, r=r: e.tensor_tensor(out=xb[b][:], in0=tmp[b][:], in1=B[r][:], op=ALU.add),
               rd=["tmp%d" % b, "B%d" % r], wr=["xb%d" % b])
        for k in range(KC):
            st.add("pe", lambda e, b=b, k=k: e.transpose(out=pt[b][:, k * 128:(k + 1) * 128], in_=xb[b][:, k * 128:(k + 1) * 128], identity=ident[:]),
                   rd=["xb%d" % b, "ident"], wr=["pt%d" % b])
        st.add("act", lambda e, b=b: e.copy(out=xT[b][:, 0:1024], in_=pt[b][:, 0:1024]), rd=["pt%d" % b], wr=["xTa%d" % b])
        st.add("dve", lambda e, b=b: e.tensor_copy(out=xT[b][:, 1024:2048], in_=pt[b][:, 1024:2048]), rd=["pt%d" % b], wr=["xTb%d" % b])
        st.dma("pool", xnT[t], xT[b][:], rd=["xTa%d" % b, "xTb%d" % b])
    st.emit()


def stage_linear(g, xT, tiles, W, N, out, oc0=0, w_bf=False, slab=1024):
    st = Stage(g, "lin")
    wst = [st.sb([128, KC, 512], F32, "wst") for _ in range(2)] if not w_bf else None
    wsl = [st.sb([128, KC, slab], BF16, "wsl") for _ in range(2)]
    NX = 3
    xt = [st.sb([128, D], BF16, "xt") for _ in range(NX)]
    ob = [st.sb([128, slab], F32, "ob") for _ in range(2)]
    pp = [st.ps([128, 512], F32, "pl") for _ in range(4)]
    nsl = (N + slab - 1) // slab
    cnt = 0
    hcnt = 0
    pcnt = 0
    for s in range(nsl):
        c0 = s * slab
        ns = min(slab, N - c0)
        w = wsl[s % 2]
        wk = "wsl%d" % (s % 2)
        for h0 in range(0, ns, 512):
            hw = min(512, ns - h0)
            src = W[:, c0 + h0:c0 + h0 + hw].rearrange("(k p) c -> p k c", p=128)
            if w_bf:
                st.dma("sp", w[:, :, h0:h0 + hw], src, wr=[wk + "_%d" % h0])
            else:
                ws = wst[hcnt % 2]
                wsk = "wst%d" % (hcnt % 2)
                st.dma("sp", ws[:, :, 0:hw], src, wr=[wsk])
                if hcnt % 2 == 0:
                    st.add("act", lambda e, w=w, ws=ws, h0=h0, hw=hw: e.copy(out=w[:, :, h0:h0 + hw], in_=ws[:, :, 0:hw]),
                           rd=[wsk], wr=[wk + "_%d" % h0])
                else:
                    st.add("dve", lambda e, w=w, ws=ws, h0=h0, hw=hw: e.tensor_copy(out=w[:, :, h0:h0 + hw], in_=ws[:, :, 0:hw]),
                           rd=[wsk], wr=[wk + "_%d" % h0])
                hcnt += 1
        for t in tiles:
            xb = cnt % NX
            o = ob[cnt % 2]
            ok = "ob%d" % (cnt % 2)
            st.dma("sp", xt[xb][:], xT[t], wr=["xt%d" % xb])
            for h0 in range(0, ns, 512):
                hw = min(512, ns - h0)
                p = pp[pcnt % 4]
                pk = "pp%d" % (pcnt % 4)
                for k in range(KC):
                    st.add("pe", lambda e, p=p, xb=xb, k=k, w=w, h0=h0, hw=hw: e.matmul(
                        p[:, 0:hw], lhsT=xt[xb][:, k * 128:(k + 1) * 128], rhs=w[:, k, h0:h0 + hw], start=(k == 0), stop=(k == KC - 1)),
                        rd=["xt%d" % xb, wk + "_%d" % h0], wr=[pk])
                if pcnt % 2 == 0:
                    st.add("act", lambda e, o=o, p=p, h0=h0, hw=hw: e.copy(out=o[:, h0:h0 + hw], in_=p[:, 0:hw]), rd=[pk], wr=[ok + "_%d" % h0])
                else:
                    st.add("dve", lambda e, o=o, p=p, h0=h0, hw=hw: e.tensor_copy(out=o[:, h0:h0 + hw], in_=p[:, 0:hw]), rd=[pk], wr=[ok + "_%d" % h0])
                pcnt += 1
            st.dma("pool", out[t * 128:(t + 1) * 128, oc0 + c0:oc0 + c0 + ns], o[:, 0:ns],
                   rd=[ok + "_%d" % h0 for h0 in range(0, ns, 512)])
            cnt += 1
    st.emit()


def bc3(ap2d, n):
    return ap2d.unsqueeze(2).broadcast_to([ap2d.shape[0], ap2d.shape[1], n])


def bcmid(ap2d, n):
    return ap2d.unsqueeze(1).broadcast_to([ap2d.shape[0], n, ap2d.shape[1]])


class TrOut:
    def __init__(self, st, ident, nb=2):
        self.st = st
        self.ident = ident
        self.nb = nb
        self.pt = [st.ps([128, D], BF16, "trp") for _ in range(nb)]
        self.xT = [st.sb([128, D], BF16, "trx") for _ in range(nb)]
        self.i = 0

    def go(self, src, srckeys, dst):
        st = self.st
        b = self.i % self.nb
        self.i += 1
        pt, xT, ident = self.pt[b], self.xT[b], self.ident
        for k in range(KC):
            st.add("pe", lambda e, k=k: e.transpose(out=pt[:, k * 128:(k + 1) * 128], in_=src[:, k * 128:(k + 1) * 128], identity=ident[:]),
                   rd=list(srckeys) + ["ident"], wr=["trp%d" % b])
        st.add("act", lambda e: e.copy(out=xT[:, 0:1024], in_=pt[:, 0:1024]), rd=["trp%d" % b], wr=["trxa%d" % b])
        st.add("dve", lambda e: e.tensor_copy(out=xT[:, 1024:2048], in_=pt[:, 1024:2048]), rd=["trp%d" % b], wr=["trxb%d" % b])
        st.dma("pool", dst, xT[:], rd=["trxa%d" % b, "trxb%d" % b])


def stage_mlstm_prep(g, p, tiles, cst, ident_bf, gate_b_l, rope, qT_s, kT_s, kb_s, v2f_s, v2b_s, gpk_s):
    st = Stage(g, "mprep")
    SC = 128.0 ** -0.5
    cs = st.sb([128, 512], F32, "cst")
    ident = st.sb([128, 128], BF16, "ident")
    gbias = st.sb([128, 32], F32, "gbias")
    st.dma("sp", cs[:], cst, wr=["cst"])
    st.dma("sp", ident[:], ident_bf, wr=["ident"])
    st.dma("sp", gbias[:], bcast_row(gate_b_l), wr=["gbias"])
    MF, MB, ONES = cs[:, 0:128], cs[:, 128:256], cs[:, 256:384]
    NB = 2
    q = [st.sb([128, 1024], F32, "q") for _ in range(NB)]
    k = [st.sb([128, 1024], F32, "k") for _ in range(NB)]
    v = [st.sb([128, 2048], F32, "v") for _ in range(NB)]
    gp = [st.sb([128, 32], F32, "gp") for _ in range(NB)]
    cq = [st.sb([128, 128], F32, "cq") for _ in range(NB)]
    sq = [st.sb([128, 128], F32, "sq") for _ in range(NB)]
    ck = [st.sb([128, 128], F32, "ck") for _ in range(NB)]
    sk = [st.sb([128, 128], F32, "sk") for _ in range(NB)]
    t1d = {n: [st.sb([128, 1024], F32, "t1" + n) for _ in range(NB)] for n in ("q", "k")}
    t2d = {n: [st.sb([128, 1024], F32, "t2" + n) for _ in range(NB)] for n in ("q", "k")}
    qb = [st.sb([128, 1024], BF16, "qb") for _ in range(NB)]
    kb = [st.sb([128, 1024], BF16, "kb") for _ in range(NB)]
    qTs = [st.sb([128, 1024], BF16, "qTs") for _ in range(NB)]
    kTs = [st.sb([128, 1024], BF16, "kTs") for _ in range(NB)]
    v2f = [st.sb([128, 8, 257], BF16, "v2f") for _ in range(NB)]
    v2b = [st.sb([128, 8, 257], BF16, "v2b") for _ in range(NB)]
    lf = [st.sb([128, 16], F32, "lf") for _ in range(NB)]
    gk = [st.sb([128, 48], F32, "gk") for _ in range(NB)]
    tmp = [st.sb([128, 16], F32, "tmpg") for _ in range(NB)]
    pq = [st.ps([128, 1024], BF16, "pq") for _ in range(NB)]
    pk = [st.ps([128, 1024], BF16, "pk") for _ in range(NB)]
    pg = [st.ps([128, 32], F32, "pg") for _ in range(NB)]
    cosq, sinq, cosk, sink = rope
    for i, t in enumerate(tiles):
        b = i % NB
        B = "%d" % b
        rows = slice(t * 128, (t + 1) * 128)
        st.dma("sp", q[b][:], p[rows, O_AQ:O_AQ + 1024], wr=["q" + B])
        st.dma("sp", k[b][:], p[rows, O_AK:O_AK + 1024], wr=["k" + B])
        st.dma("sp", v[b][:], p[rows, O_AV:O_AV + 2048], wr=["v" + B])
        st.dma("sp", gp[b][:], p[rows, O_AG:O_AG + 32], wr=["gp" + B])
        if t >= 2:
            lr = slice((t - 2) * 128, (t - 1) * 128)
            st.dma("sp", cq[b][:], cosq[lr, :], wr=["cq" + B])
            st.dma("sp", sq[b][:], sinq[lr, :], wr=["sq" + B])
            st.dma("sp", ck[b][:], cosk[lr, :], wr=["ck" + B])
            st.dma("sp", sk[b][:], sink[lr, :], wr=["sk" + B])
            for (src, cc_, ss_, dst, nm, eng) in ((q, cq, sq, qb, "q", "dve"), (k, ck, sk, kb, "k", "dve")):
                s4 = src[b][:].rearrange("p (h f u j) -> p h f u j", h=8, f=2, u=2, j=32)
                t1v, t2v = t1d[nm][b], t2d[nm][b]
                d4 = t2v[:].rearrange("p (h f u j) -> p h f u j", h=8, f=2, u=2, j=32)
                nsn = ss_[b][:, 0:64].rearrange("p (f j) -> p f j", f=2).unsqueeze(1).broadcast_to([128, 8, 2, 32])
                psn = ss_[b][:, 64:128].rearrange("p (f j) -> p f j", f=2).unsqueeze(1).broadcast_to([128, 8, 2, 32])
                tk1, tk2 = "t1" + nm + B, "t2" + nm + B
                st.op(eng, "tensor_tensor", rd=[nm + B, "c" + nm + B], wr=[tk1],
                      out=t1v[:].rearrange("p (h d) -> p h d", h=8), in0=src[b][:].rearrange("p (h d) -> p h d", h=8),
                      in1=bcmid(cc_[b][:], 8), op=ALU.mult)
                st.op(eng, "tensor_tensor", rd=[nm + B, "s" + nm + B], wr=[tk2],
                      out=d4[:, :, :, 0, :], in0=s4[:, :, :, 1, :], in1=nsn, op=ALU.mult)
                st.op(eng, "tensor_tensor", rd=[nm + B, "s" + nm + B], wr=[tk2],
                      out=d4[:, :, :, 1, :], in0=s4[:, :, :, 0, :], in1=psn, op=ALU.mult)
                st.op(eng, "tensor_tensor", rd=[tk1, tk2], wr=[nm + "b" + B], out=dst[b][:], in0=t1v[:], in1=t2v[:], op=ALU.add)
        else:
            st.op("act", "mul", rd=["q" + B], wr=["qb" + B], out=qb[b][:], in_=q[b][:], mul=SC)
            st.op("dve", "tensor_copy", rd=["k" + B], wr=["kb" + B], out=kb[b][:], in_=k[b][:])
        for h in range(8):
            st.op("pe", "transpose", rd=["qb" + B, "ident"], wr=["pq" + B],
                  out=pq[b][:, h * 128:(h + 1) * 128], in_=qb[b][:, h * 128:(h + 1) * 128], identity=ident[:])
        st.op("act", "copy", rd=["pq" + B], wr=["qTs" + B], out=qTs[b][:], in_=pq[b][:])
        for h in range(8):
            st.op("pe", "transpose", rd=["kb" + B, "ident"], wr=["pk" + B],
                  out=pk[b][:, h * 128:(h + 1) * 128], in_=kb[b][:, h * 128:(h + 1) * 128], identity=ident[:])
        st.op("dve", "tensor_copy", rd=["pk" + B], wr=["kTs" + B], out=kTs[b][:], in_=pk[b][:])
        st.dma("pool", qT_s[t], qTs[b][:], rd=["qTs" + B])
        st.dma("pool", kT_s[t], kTs[b][:], rd=["kTs" + B])
        st.dma("pool", kb_s[rows, :], kb[b][:], rd=["kb" + B])
        st.op("dve", "tensor_tensor", rd=["gp" + B, "gbias"], wr=["gp" + B], out=gp[b][:], in0=gp[b][:], in1=gbias[:], op=ALU.add)
        st.op("act", "activation", rd=["gp" + B], wr=["lf" + B], out=lf[b][:, 0:8], in_=gp[b][:, 8:16], func=AF.Exp, scale=-1.0)
        st.op("act", "activation", rd=["gp" + B], wr=["lf" + B], out=lf[b][:, 8:16], in_=gp[b][:, 24:32], func=AF.Exp, scale=-1.0)
        st.op("act", "activation", rd=["lf" + B], wr=["lf" + B], out=lf[b][:], in_=lf[b][:], func=AF.Ln, bias=1.0)
        st.op("dve", "tensor_scalar", rd=["lf" + B], wr=["lf" + B], out=lf[b][:], in0=lf[b][:], scalar1=-1.0, scalar2=None, op0=ALU.mult)
        st.op("pe", "matmul", rd=["lf" + B, "cst"], wr=["pg" + B], out=pg[b][:, 0:8], lhsT=MF, rhs=lf[b][:, 0:8], start=True, stop=True)
        st.op("pe", "matmul", rd=["lf" + B, "cst"], wr=["pg" + B], out=pg[b][:, 8:16], lhsT=MB, rhs=lf[b][:, 8:16], start=True, stop=True)
        st.op("pe", "matmul", rd=["lf" + B, "cst"], wr=["pg" + B], out=pg[b][:, 16:32], lhsT=ONES, rhs=lf[b][:, 0:16], start=True, stop=True)
        st.op("dve", "tensor_tensor", rd=["gp" + B, "pg" + B], wr=["tmp" + B], out=tmp[b][:, 0:8], in0=gp[b][:, 0:8], in1=pg[b][:, 0:8], op=ALU.subtract)
        st.op("dve", "tensor_tensor", rd=["gp" + B, "pg" + B], wr=["tmp" + B], out=tmp[b][:, 8:16], in0=gp[b][:, 16:24], in1=pg[b][:, 8:16], op=ALU.subtract)
        st.op("act", "activation", rd=["tmp" + B], wr=["gk" + B], out=gk[b][:, 0:8], in_=tmp[b][:, 0:8], func=AF.Exp)
        st.op("act", "activation", rd=["tmp" + B], wr=["gk" + B], out=gk[b][:, 16:24], in_=tmp[b][:, 8:16], func=AF.Exp)
        st.op("act", "activation", rd=["pg" + B], wr=["gk" + B], out=gk[b][:, 8:16], in_=pg[b][:, 0:8], func=AF.Exp)
        st.op("act", "activation", rd=["pg" + B], wr=["gk" + B], out=gk[b][:, 24:32], in_=pg[b][:, 8:16], func=AF.Exp)
        st.op("act", "activation", rd=["pg" + B], wr=["gk" + B], out=gk[b][:, 32:48], in_=pg[b][:, 16:32], func=AF.Exp)
        st.dma("pool", gpk_s[rows, :], gk[b][:], rd=["gk" + B])
        v3 = v[b][:].rearrange("p (h d) -> p h d", h=8)
        for h in range(8):
            st.op("act", "activation", rd=["v" + B, "gk" + B], wr=["v2f" + B], out=v2f[b][:, h, 0:256], in_=v3[:, h, :], func=AF.Identity, scale=gk[b][:, h:h + 1])
            st.op("act", "activation", rd=["v" + B, "gk" + B], wr=["v2b" + B], out=v2b[b][:, h, 0:256], in_=v3[:, h, :], func=AF.Identity, scale=gk[b][:, 16 + h:17 + h])
        st.op("act", "copy", rd=["gk" + B], wr=["v2f" + B], out=v2f[b][:, :, 256], in_=gk[b][:, 0:8])
        st.op("act", "copy", rd=["gk" + B], wr=["v2b" + B], out=v2b[b][:, :, 256], in_=gk[b][:, 16:24])
        st.dma("pool", v2f_s[rows, :], v2f[b][:].rearrange("p h d -> p (h d)"), rd=["v2f" + B])
        st.dma("pool", v2b_s[rows, :], v2b[b][:].rearrange("p h d -> p (h d)"), rd=["v2b" + B])
    st.emit()


def stage_mlstm_scan(g, bwd, order, cst, ident_bf, qT_s, kT_s, kb_s, v2_s, gpk_s, hf_s, p=None, hnorm_l=None, yaT=None, skip_out=()):
    st = Stage(g, "mscan")
    cs = st.sb([128, 512], F32, "cst")
    st.dma("sp", cs[:], cst, wr=["cst"])
    MASK = cs[:, 128:256] if bwd else cs[:, 0:128]
    o_ebc = 24 if bwd else 8
    o_eB = 40 if bwd else 32
    C32 = st.sb([128, 8, 257], F32, "C32")
    Cb = st.sb([128, 8, 257], BF16, "Cb")
    st.op("dve", "memset", wr=["C32_%d" % h for h in range(8)], ap=C32[:], constant=0.0)
    st.op("pool", "memset", wr=["Cb_%d" % h for h in range(8)], ap=Cb[:], constant=0.0)
    NB = 2
    qT = [st.sb([128, 1024], BF16, "qT") for _ in range(NB)]
    kT = [st.sb([128, 1024], BF16, "kT") for _ in range(NB)]
    kb = [st.sb([128, 1024], BF16, "kb") for _ in range(NB)]
    v2 = [st.sb([128, 8, 257], BF16, "v2") for _ in range(NB)]
    gk = [st.sb([128, 48], F32, "gk") for _ in range(NB)]
    hb = [st.sb([128, 2048], F32, "hb") for _ in range(NB)]
    sm = [st.sb([128, 128], BF16, "sm") for _ in range(2)]
    dd = [st.sb([128, 4], F32, "dd") for _ in range(2)]
    ps_s = [st.ps([128, 512], F32, "ps_s") for _ in range(2)]
    ps_o = [st.ps([128, 512], F32, "ps_o") for _ in range(2)]
    ps_c = [st.ps([128, 512], F32, "ps_c") for _ in range(2)]
    if bwd:
        ident = st.sb([128, 128], BF16, "ident")
        st.dma("sp", ident[:], ident_bf, wr=["ident"])
        HN = st.sb([128, 2048], F32, "HN")
        st.dma("sp", HN[:], bcast_row(hnorm_l), wr=["HN"])
        hf = st.sb([128, 2048], F32, "hf")
        og = st.sb([128, 2048], F32, "og")
        sq = st.sb([128, 2048], F32, "sq")
        yab = st.sb([128, 2048], BF16, "yab")
        ms = st.sb([128, 8], F32, "ms")
        tr = TrOut(st, ident, nb=1)
    hc = 0
    for i, t in enumerate(order):
        b = i % NB
        B = "%d" % b
        rows = slice(t * 128, (t + 1) * 128)
        st.dma("sp", qT[b][:], qT_s[t], wr=["qT" + B])
        st.dma("sp", kT[b][:], kT_s[t], wr=["kT" + B])
        st.dma("sp", kb[b][:], kb_s[rows, :], wr=["kb" + B])
        st.dma("sp", v2[b][:].rearrange("p h d -> p (h d)"), v2_s[rows, :], wr=["v2" + B])
        st.dma("sp", gk[b][:], gpk_s[rows, :], wr=["gk" + B])
        def smat(h_, j_):
            hs_ = slice(h_ * 128, (h_ + 1) * 128)
            st.op("pe", "matmul", rd=["kT" + B, "qT" + B], wr=["ps_s%d" % j_], out=ps_s[j_][:, 0:128], lhsT=kT[b][:, hs_], rhs=qT[b][:, hs_], start=True, stop=True)

        smat(0, hc % 2)
        for h in range(8):
            j = hc % 2
            J = "%d" % j
            hc += 1
            hs = slice(h * 128, (h + 1) * 128)
            if h + 1 < 8:
                smat(h + 1, hc % 2)
            st.op("dve", "tensor_tensor", rd=["ps_s" + J, "cst"], wr=["sm" + J], out=sm[j][:], in0=ps_s[j][:, 0:128], in1=MASK, op=ALU.mult)
            st.op("pe", "matmul", rd=["qT" + B, "Cb_%d" % h], wr=["ps_o" + J], out=ps_o[j][:, 0:257], lhsT=qT[b][:, hs], rhs=Cb[:, h, :], start=True, stop=False)
            st.op("pe", "matmul", rd=["sm" + J, "v2" + B], wr=["ps_o" + J], out=ps_o[j][:, 0:257], lhsT=sm[j][:], rhs=v2[b][:, h, :], start=False, stop=True)
            ebc = gk[b][:, o_ebc + h:o_ebc + h + 1]
            st.op("dve", "tensor_scalar", rd=["ps_o" + J, "gk" + B], wr=["dd" + J], out=dd[j][:, 3:4], in0=ps_o[j][:, 256:257], scalar1=ebc, scalar2=-1.0,
                  op0=ALU.mult, op1=ALU.mult)
            st.op("dve", "tensor_scalar", rd=["ps_o" + J, "gk" + B], wr=["dd" + J], out=dd[j][:, 0:1], in0=ps_o[j][:, 256:257], scalar1=ebc, scalar2=1.0,
                  op0=ALU.mult, op1=ALU.max)
            st.op("dve", "tensor_tensor", rd=["dd" + J], wr=["dd" + J], out=dd[j][:, 0:1], in0=dd[j][:, 0:1], in1=dd[j][:, 3:4], op=ALU.max)
            st.op("dve", "reciprocal", rd=["dd" + J], wr=["dd" + J], out=dd[j][:, 1:2], in_=dd[j][:, 0:1])
            st.op("dve", "tensor_tensor", rd=["dd" + J, "gk" + B], wr=["dd" + J], out=dd[j][:, 2:3], in0=dd[j][:, 1:2], in1=ebc, op=ALU.mult)
            st.op("act", "activation", rd=["ps_o" + J, "dd" + J], wr=["hb" + B], out=hb[b][:, h * 256:(h + 1) * 256], in_=ps_o[j][:, 0:256],
                  func=AF.Identity, scale=dd[j][:, 2:3])
            st.op("pe", "matmul", rd=["kb" + B, "v2" + B], wr=["ps_c" + J], out=ps_c[j][:, 0:257], lhsT=kb[b][:, hs], rhs=v2[b][:, h, :], start=True, stop=True)
            st.op("dve", "tensor_tensor", rd=["ps_c" + J, "C32_%d" % h], wr=["C32_%d" % h], out=C32[:, h, :], in0=ps_c[j][:, 0:257], in1=C32[:, h, :], op=ALU.add)
            st.op("act", "activation", rd=["C32_%d" % h, "gk" + B], wr=["Cb_%d" % h], out=Cb[:, h, :], in_=C32[:, h, :], func=AF.Identity,
                  scale=gk[b][:, o_eB + h:o_eB + h + 1])
            st.op("act", "activation", rd=["C32_%d" % h, "gk" + B], wr=["C32_%d" % h], out=C32[:, h, :], in_=C32[:, h, :], func=AF.Identity,
                  scale=gk[b][:, o_eB + h:o_eB + h + 1])
        if not bwd:
            st.dma("pool", hf_s[rows, :], hb[b][:], rd=["hb" + B])
        elif t not in skip_out:
            st.dma("sp", hf[:], hf_s[rows, :], wr=["hf"])
            st.dma("sp", og[:], p[rows, O_AO:O_AO + 2048], wr=["og"])
            st.op("dve", "tensor_tensor", rd=["hb" + B, "hf"], wr=["hf"], out=hf[:], in0=hb[b][:], in1=hf[:], op=ALU.add)
            st.op("act", "activation", rd=["hf"], wr=["sq"], out=sq[:], in_=hf[:], func=AF.Square)
            st.op("dve", "tensor_reduce", rd=["sq"], wr=["ms"], out=ms[:], in_=sq[:].rearrange("p (h d) -> p h d", h=8), axis=AX.X, op=ALU.add)
            st.op("act", "activation", rd=["ms"], wr=["ms"], out=ms[:], in_=ms[:], func=AF.Sqrt, scale=1.0 / 256, bias=EPS)
            st.op("dve", "reciprocal", rd=["ms"], wr=["ms"], out=ms[:], in_=ms[:])
            st.op("dve", "tensor_tensor", rd=["hf", "ms"], wr=["hf"], out=hf[:].rearrange("p (h d) -> p h d", h=8),
                  in0=hf[:].rearrange("p (h d) -> p h d", h=8), in1=bc3(ms[:], 256), op=ALU.mult)
            st.op("act", "activation", rd=["og"], wr=["og"], out=og[:], in_=og[:], func=AF.Sigmoid)
            st.op("dve", "tensor_tensor", rd=["hf", "HN"], wr=["hf"], out=hf[:], in0=hf[:], in1=HN[:], op=ALU.mult)
            st.op("dve", "tensor_tensor", rd=["hf", "og"], wr=["yab"], out=yab[:], in0=hf[:], in1=og[:], op=ALU.mult)
            tr.go(yab, ["yab"], yaT[t])
    st.emit()


def host_consts(n_lat_tokens):
    import ml_dtypes
    s = np.arange(128)
    MF = (s[:, None] <= s[None, :]).astype(np.float32)
    MB = (s[:, None] >= s[None, :]).astype(np.float32)
    cst = np.concatenate([MF, MB, np.ones((128, 128), np.float32), np.eye(128, dtype=np.float32)], axis=1)
    ident_bf = np.eye(128).astype(ml_dtypes.bfloat16)
    nf = 32
    inv = (10000.0 ** (-np.arange(nf, dtype=np.float32) / nf)).astype(np.float32)
    pos = np.arange(n_lat_tokens)
    ang_r = (pos // 64).astype(np.float32)[:, None] * inv
    ang_c = (pos % 64).astype(np.float32)[:, None] * inv
    cr, sr, cc_, sc_ = np.cos(ang_r), np.sin(ang_r), np.cos(ang_c), np.sin(ang_c)
    cosf = np.concatenate([cr, cr, cc_, cc_], axis=1).astype(np.float32)
    sinf = np.concatenate([-sr, -sc_, sr, sc_], axis=1).astype(np.float32)
    SC = np.float32(128.0 ** -0.5)
    return dict(cst=cst, ident_bf=ident_bf, cosq=cosf * SC, sinq=sinf * SC, cosk=cosf, sink=sinf)


def stage_cast_T(g, src, tiles, ident_bf, dstT):
    st = Stage(g, "castT")
    ident = st.sb([128, 128], BF16, "ident")
    st.dma("sp", ident[:], ident_bf, wr=["ident"])
    x = [st.sb([128, D], F32, "x") for _ in range(2)]
    xb = [st.sb([128, D], BF16, "xb") for _ in range(2)]
    tr = TrOut(st, ident, nb=2)
    for i, t in enumerate(tiles):
        b = i % 2
        B = "%d" % b
        st.dma("sp", x[b][:], src[t * 128:(t + 1) * 128, :], wr=["x" + B])
        st.op("act" if b else "dve", "copy" if b else "tensor_copy", rd=["x" + B], wr=["xb" + B], out=xb[b][:], in_=x[b][:])
        tr.go(xb[b], ["xb" + B], dstT[t])
    st.emit()


def stage_conv(g, p, tiles, NT, conv_l, ident_bf, ybT):
    st = Stage(g, "conv")
    ident = st.sb([128, 128], BF16, "ident")
    st.dma("sp", ident[:], ident_bf, wr=["ident"])
    W = [st.sb([128, D], F32, "cw") for _ in range(3)]
    for j in range(3):
        st.dma("sp", W[j][:], bcast_row(conv_l[j:j + 1, :]), wr=["W%d" % j])
    NB = 2
    names = ["c0", "x0", "bb", "cm", "xm", "cp", "xp"]
    buf = {n: [st.sb([128, D], F32, n) for _ in range(NB)] for n in names}
    yb = [st.sb([128, D], BF16, "yb") for _ in range(NB)]
    tr = TrOut(st, ident, nb=2)
    for i, t in enumerate(tiles):
        b = i % NB
        B = "%d" % b
        r0 = t * 128
        first = t in (0, 2)
        last = t in (1, NT - 1)
        T = {n: buf[n][b] for n in names}
        K = {n: n + B for n in names}
        st.dma("sp", T["c0"][:], p[r0:r0 + 128, O_BC:O_BC + D], wr=[K["c0"]])
        st.dma("sp", T["x0"][:], p[r0:r0 + 128, O_BX:O_BX + D], wr=[K["x0"]])
        st.dma("sp", T["bb"][:], p[r0:r0 + 128, O_BB:O_BB + D], wr=[K["bb"]])
        for (cn, xn, off, edge) in (("cm", "xm", -1, first), ("cp", "xp", 1, last)):
            for (n, col) in ((cn, O_BC), (xn, O_BX)):
                if not edge:
                    st.dma("sp", T[n][:], p[r0 + off:r0 + off + 128, col:col + D], wr=[K[n]])
                else:
                    st.op("pool", "memset", wr=[K[n]], ap=T[n][:], constant=0.0)
                    if off < 0:
                        st.dma("sp", T[n][1:128, :], p[r0:r0 + 127, col:col + D], wr=[K[n]])
                    else:
                        st.dma("sp", T[n][0:127, :], p[r0 + 1:r0 + 128, col:col + D], wr=[K[n]])
        tt = lambda eng, o, a, bb_, op, rd, wr: st.op(eng, "tensor_tensor", rd=rd, wr=wr, out=o, in0=a, in1=bb_, op=op)
        tt("dve", T["x0"][:], T["c0"][:], T["x0"][:], ALU.mult, [K["c0"], K["x0"]], [K["x0"]])
        tt("dve", T["xm"][:], T["cm"][:], T["xm"][:], ALU.mult, [K["cm"], K["xm"]], [K["xm"]])
        tt("dve", T["xp"][:], T["cp"][:], T["xp"][:], ALU.mult, [K["cp"], K["xp"]], [K["xp"]])
        tt("dve", T["xm"][:], T["xm"][:], W[0][:], ALU.mult, [K["xm"], "W0"], [K["xm"]])
        tt("dve", T["x0"][:], T["x0"][:], W[1][:], ALU.mult, [K["x0"], "W1"], [K["x0"]])
        tt("dve", T["xp"][:], T["xp"][:], W[2][:], ALU.mult, [K["xp"], "W2"], [K["xp"]])
        tt("dve", T["x0"][:], T["x0"][:], T["xm"][:], ALU.add, [K["x0"], K["xm"]], [K["x0"]])
        tt("dve", T["x0"][:], T["x0"][:], T["xp"][:], ALU.add, [K["x0"], K["xp"]], [K["x0"]])
        tt("dve", yb[b][:], T["x0"][:], T["bb"][:], ALU.mult, [K["x0"], K["bb"]], ["yb" + B])
        tr.go(yb[b], ["yb" + B], ybT[t])
    st.emit()


def na_plan(ROWS):
    kbs, cls, keys = [], [], {}
    for i in range(ROWS // 2):
        r = 2 * i
        kb = min(max(r - 4, 0), ROWS - 9)
        r0a = min(max(r - 4, 0), ROWS - 8)
        r0b = min(max(r + 1 - 4, 0), ROWS - 8)
        key = (r - kb, r0a - kb, r0b - kb)
        if key not in keys:
            keys[key] = len(keys)
        kbs.append(kb)
        cls.append(keys[key])
    return kbs, cls, list(keys.keys())


def na_bias_host(rpb_l, ROWS):
    _, _, keys = na_plan(ROWS)
    qrow = np.arange(128) // 64
    qc = np.arange(128) % 64
    j = np.arange(576) // 64
    kc = np.arange(576) % 64
    col0 = np.clip(qc - 8, 0, 48)
    colok = (kc[None, :] >= col0[:, None]) & (kc[None, :] < col0[:, None] + 16)
    dc = np.clip(kc[None, :] - qc[:, None] + 15, 0, 30)
    out = np.full((16, len(keys), 128, 5, 128), -30000.0, np.float32)
    for ci, (rel, a, b) in enumerate(keys):
        lo = np.where(qrow == 0, a, b)
        rowok = (j[None, :] >= lo[:, None]) & (j[None, :] < lo[:, None] + 8)
        dr = np.clip(j[None, :] - rel - qrow[:, None] + 7, 0, 14)
        ok = rowok & colok
        Tm = np.where(ok[None], rpb_l[:, dr, dc], np.float32(-30000.0))
        TT = np.full((16, 640, 128), -30000.0, np.float32)
        TT[:, :576, :] = Tm.transpose(0, 2, 1)
        out[:, ci] = TT.reshape(16, 5, 128, 128).transpose(0, 2, 1, 3)
    return out.reshape(16, len(keys), 128, 640)


def stage_na_prep(g, p, tiles, qkg_l, ident_bf, cqT_s, ckT_s, cv1_s):
    st = Stage(g, "naprep")
    ident = st.sb([128, 128], BF16, "ident")
    st.dma("sp", ident[:], ident_bf, wr=["ident"])
    GQ = st.sb([128, D], F32, "GQ")
    GK = st.sb([128, D], F32, "GK")
    for h in range(16):
        st.dma("sp", GQ[:, h * 128:(h + 1) * 128], bcast_row(qkg_l[0:1, :]), wr=["GQ"])
        st.dma("sp", GK[:, h * 128:(h + 1) * 128], bcast_row(qkg_l[1:2, :]), wr=["GK"])
    st.op("dve", "tensor_scalar", rd=["GQ"], wr=["GQ"], out=GQ[:], in0=GQ[:], scalar1=128.0 ** -0.5, scalar2=None, op0=ALU.mult)
    NB = 2
    x = {n: [st.sb([128, D], F32, n) for _ in range(NB)] for n in ("q", "k", "v")}
    sq = [st.sb([128, D], F32, "sq") for _ in range(NB)]
    ss = {n: [st.sb([128, 16], F32, "ss" + n) for _ in range(NB)] for n in ("q", "k")}
    xb = {n: [st.sb([128, D], BF16, "xb" + n) for _ in range(NB)] for n in ("q", "k")}
    v1 = [st.sb([128, 16, 129], BF16, "v1") for _ in range(NB)]
    for b in range(NB):
        st.op("pool", "memset", wr=["v1%d" % b], ap=v1[b][:], constant=1.0)
    tr = TrOut(st, ident, nb=2)
    for i, t in enumerate(tiles):
        b = i % NB
        B = "%d" % b
        rows = slice(t * 128, (t + 1) * 128)
        st.dma("sp", x["q"][b][:], p[rows, O_CQ:O_CQ + D], wr=["q" + B])
        st.dma("sp", x["k"][b][:], p[rows, O_CK:O_CK + D], wr=["k" + B])
        st.dma("sp", x["v"][b][:], p[rows, O_CV:O_CV + D], wr=["v" + B])
        for n, Gt, gk_, dst in (("q", GQ, "GQ", cqT_s), ("k", GK, "GK", ckT_s)):
            xt = x[n][b]
            s_ = ss[n][b]
            st.op("act", "activation", rd=[n + B], wr=["sq" + B], out=sq[b][:], in_=xt[:], func=AF.Square)
            st.op("dve", "tensor_reduce", rd=["sq" + B], wr=["ss" + n + B], out=s_[:], in_=sq[b][:].rearrange("p (h d) -> p h d", h=16), axis=AX.X, op=ALU.add)
            st.op("act", "activation", rd=["ss" + n + B], wr=["ss" + n + B], out=s_[:], in_=s_[:], func=AF.Sqrt, scale=1.0 / 128, bias=EPS)
            st.op("dve", "reciprocal", rd=["ss" + n + B], wr=["ss" + n + B], out=s_[:], in_=s_[:])
            st.op("dve", "tensor_tensor", rd=[n + B, "ss" + n + B], wr=[n + B], out=xt[:].rearrange("p (h d) -> p h d", h=16),
                  in0=xt[:].rearrange("p (h d) -> p h d", h=16), in1=bc3(s_[:], 128), op=ALU.mult)
            st.op("dve", "tensor_tensor", rd=[n + B, gk_], wr=["xb" + n + B], out=xb[n][b][:], in0=xt[:], in1=Gt[:], op=ALU.mult)
            tr.go(xb[n][b], ["xb" + n + B], dst[t])
        st.op("act", "copy", rd=["v" + B], wr=["v1" + B], out=v1[b][:, :, 0:128], in_=x["v"][b][:].rearrange("p (h d) -> p h d", h=16))
        st.dma("pool", cv1_s[rows, :], v1[b][:].rearrange("p h d -> p (h d)"), rd=["v1" + B])
    st.emit()


def stage_na(g, NL, need_ctx, nab_l, ident_bf, cqT_s, ckT_s, cv1_s, yc_s):
    st = Stage(g, "na")
    NT = NL + 2
    ROWS = 2 * NL
    kbs, cls, keys = na_plan(ROWS)
    NC = len(keys)
    ident = st.sb([128, 128], BF16, "ident")
    st.dma("sp", ident[:], ident_bf, wr=["ident"])
    nm8 = st.sb([128, 1], F32, "nm8")
    st.op("dve", "memset", wr=["nm8"], ap=nm8[:], constant=-8.0)
    kTh = st.sb([128, NT * 128], BF16, "kTh")
    qTh = st.sb([128, NT * 128], BF16, "qTh")
    va = st.sb([128, NT, 129], BF16, "va")
    vb = st.sb([128, NL, 129], BF16, "vb")
    bst = st.sb([128, NC, 640], F32, "bst")
    bT = st.sb([128, NC, 640], BF16, "bT")
    E = [st.sb([128, 1024], BF16, "E") for _ in range(2)]
    ob = [st.sb([128, 128], F32, "ob") for _ in range(2)]
    rr = [st.sb([128, 1], F32, "rr") for _ in range(2)]
    ps = [st.ps([128, 1024], F32, "nps") for _ in range(2)]
    po = [st.ps([128, 512], F32, "npo") for _ in range(2)]
    cv3 = cv1_s.rearrange("(t p) (h c) -> p t h c", p=128, h=16)
    cnt = 0
    for h in range(16):
        hs = slice(h * 128, (h + 1) * 128)
        st.dma("sp", kTh[:].rearrange("p (t c) -> p t c", t=NT), ckT_s[:, :, hs].rearrange("t p c -> p t c"), wr=["kTh"])
        st.dma("sp", qTh[:].rearrange("p (t c) -> p t c", t=NT), cqT_s[:, :, hs].rearrange("t p c -> p t c"), wr=["qTh"])
        st.dma("sp", va[:], cv3[:, :, h, :], wr=["va"])
        st.dma("sp", vb[:, 0:NL - 1, :], cv1_s[256 + 64:256 + 64 + (NL - 1) * 128, h * 129:(h + 1) * 129].rearrange("(t p) c -> p t c", p=128), wr=["vb"])
        st.dma("sp", vb[0:64, NL - 1, :], cv1_s[256 + 64 + (NL - 1) * 128:256 + NL * 128, h * 129:(h + 1) * 129], wr=["vb"])
        st.dma("sp", bst[:], nab_l[h].rearrange("c p x -> p c x"), wr=["bst"])
        st.op("pool", "tensor_copy", rd=["bst"], wr=["bT"], out=bT[:], in_=bst[:])
        qtiles = ([0, 1] if need_ctx else []) + list(range(2, NT))

        def qk(t, j):
            J = "%d" % j
            q_ = qTh[:, t * 128:(t + 1) * 128]
            pj = ps[j]
            if t >= 2:
                i = t - 2
                kb, c_ = kbs[i], cls[i]
                tok0 = 256 + kb * 64
                for c in range(5):
                    kn = 128 if c < 4 else 64
                    st.op("pe", "matmul", rd=["kTh", "qTh"], wr=["ps" + J], out=pj[0:kn, c * 128:(c + 1) * 128],
                          lhsT=kTh[:, tok0 + c * 128:tok0 + c * 128 + kn], rhs=q_, start=True, stop=False)
                    st.op("pe", "matmul", rd=["ident", "bT"], wr=["ps" + J], out=pj[0:kn, c * 128:(c + 1) * 128],
                          lhsT=ident[0:kn, 0:kn], rhs=bT[0:kn, c_, c * 128:(c + 1) * 128], start=False, stop=True)
            for c in range(2):
                st.op("pe", "matmul", rd=["kTh", "qTh"], wr=["ps" + J], out=pj[:, (5 + c) * 128:(6 + c) * 128],
                      lhsT=kTh[:, c * 128:(c + 1) * 128], rhs=q_, start=True, stop=True)

        def rest(t, j):
            J = "%d" % j
            pj = ps[j]
            if t >= 2:
                kb = kbs[t - 2]
                st.op("act", "activation", rd=["ps" + J, "nm8"], wr=["E" + J], out=E[j][:, 0:512], in_=pj[:, 0:512], func=AF.Exp, bias=nm8[:])
                st.op("act", "activation", rd=["ps" + J, "nm8"], wr=["E" + J], out=E[j][0:64, 512:640], in_=pj[0:64, 512:640], func=AF.Exp, bias=nm8[0:64, :])
            st.op("act", "activation", rd=["ps" + J, "nm8"], wr=["E" + J], out=E[j][:, 640:896], in_=pj[:, 640:896], func=AF.Exp, bias=nm8[:])
            chunks = []
            if t >= 2:
                for c in range(5):
                    kn = 128 if c < 4 else 64
                    if kb % 2 == 0:
                        vv = va[0:kn, 2 + kb // 2 + c, :]
                    else:
                        vv = vb[0:kn, (kb - 1) // 2 + c, :]
                    chunks.append((E[j][0:kn, c * 128:(c + 1) * 128], vv))
            for c in range(2):
                chunks.append((E[j][:, (5 + c) * 128:(6 + c) * 128], va[:, c, :]))
            for ci, (l_, r_) in enumerate(chunks):
                st.op("pe", "matmul", rd=["E" + J, "va", "vb"], wr=["po" + J], out=po[j][:, 0:129], lhsT=l_, rhs=r_,
                      start=(ci == 0), stop=(ci == len(chunks) - 1))
            st.op("dve", "reciprocal", rd=["po" + J], wr=["rr" + J], out=rr[j][:], in_=po[j][:, 128:129])
            st.op("dve", "tensor_scalar", rd=["po" + J, "rr" + J], wr=["ob" + J], out=ob[j][:], in0=po[j][:, 0:128], scalar1=rr[j][:], scalar2=None, op0=ALU.mult)
            st.dma("pool", yc_s[t * 128:(t + 1) * 128, hs], ob[j][:], rd=["ob" + J])

        qk(qtiles[0], cnt % 2)
        for n, t in enumerate(qtiles):
            if n + 1 < len(qtiles):
                qk(qtiles[n + 1], (cnt + 1) % 2)
            rest(t, cnt % 2)
            cnt += 1
    st.emit()


def stage_gate(g, p, tiles, brs, ident_bf, yT):
    st = Stage(g, "gate")
    ident = st.sb([128, 128], BF16, "ident")
    st.dma("sp", ident[:], ident_bf, wr=["ident"])
    NB = 2
    gt = [[st.sb([128, D], F32, "gt") for _ in range(3)] for _ in range(NB)]
    br = [[st.sb([128, D], F32, "br") for _ in range(3)] for _ in range(NB)]
    yb = [st.sb([128, D], BF16, "yb") for _ in range(NB)]
    tr = TrOut(st, ident, nb=2)
    for i, t in enumerate(tiles):
        b = i % NB
        B = "%d" % b
        rows = slice(t * 128, (t + 1) * 128)
        for j in range(3):
            st.dma("sp", gt[b][j][:], p[rows, O_GT + j * D:O_GT + (j + 1) * D], wr=["gt%d" % j + B])
            st.dma("sp", br[b][j][:], brs[j][rows, :], wr=["br%d" % j + B])
            st.op("act", "activation", rd=["gt%d" % j + B], wr=["gt%d" % j + B], out=gt[b][j][:], in_=gt[b][j][:], func=AF.Sigmoid)
            st.op("dve", "tensor_tensor", rd=["gt%d" % j + B, "br%d" % j + B], wr=["br%d" % j + B],
                  out=br[b][j][:], in0=br[b][j][:], in1=gt[b][j][:], op=ALU.mult)
        st.op("dve", "tensor_tensor", rd=["br0" + B, "br1" + B], wr=["br0" + B], out=br[b][0][:], in0=br[b][0][:], in1=br[b][1][:], op=ALU.add)
        st.op("dve", "tensor_tensor", rd=["br0" + B, "br2" + B], wr=["yb" + B], out=yb[b][:], in0=br[b][0][:], in1=br[b][2][:], op=ALU.add)
        tr.go(yb[b], ["yb" + B], yT[t])
    st.emit()


def stage_resid(g, xsrc, ysrc, tiles, modv, seg, dst, dst_row0=0):
    st = Stage(g, "resid")
    Gv = [st.sb([128, D], F32, "Gv") for _ in range(2)]
    for r in range(2):
        st.dma("sp", Gv[r][:], bcast_row(modv[r:r + 1, seg * D:(seg + 1) * D]), wr=["G%d" % r])
    NB = 2
    x = [st.sb([128, D], F32, "x") for _ in range(NB)]
    y = [st.sb([128, D], F32, "y") for _ in range(NB)]
    for i, t in enumerate(tiles):
        b = i % NB
        B = "%d" % b
        r = 1 if t < 2 else 0
        rows = slice(t * 128, (t + 1) * 128)
        st.dma("sp", x[b][:], xsrc[rows, :], wr=["x" + B])
        st.dma("sp", y[b][:], ysrc[rows, :], wr=["y" + B])
        st.op("dve", "tensor_tensor", rd=["y" + B, "G%d" % r], wr=["y" + B], out=y[b][:], in0=y[b][:], in1=Gv[r][:], op=ALU.mult)
        st.op("dve", "tensor_tensor", rd=["x" + B, "y" + B], wr=["y" + B], out=y[b][:], in0=y[b][:], in1=x[b][:], op=ALU.add)
        st.dma("pool", dst[t * 128 - dst_row0:(t + 1) * 128 - dst_row0, :], y[b][:], rd=["y" + B])
    st.emit()


def stage_uT(g, u_l, cst, uT_s):
    st = Stage(g, "uT")
    cs = st.sb([128, 512], F32, "cst")
    st.dma("sp", cs[:], cst, wr=["cst"])
    IDF = cs[:, 384:512]
    ub = [st.sb([128, D], F32, "ub") for _ in range(2)]
    ob = [st.sb([128, KC, 128], BF16, "uo") for _ in range(2)]
    pu = [st.ps([128, D], F32, "pu") for _ in range(2)]
    for eb in range(128):
        b = eb % 2
        B = "%d" % b
        st.dma("sp", ub[b][:], u_l[eb * 128:(eb + 1) * 128, :], wr=["ub" + B])
        for k in range(KC):
            st.op("pe", "transpose", rd=["ub" + B, "cst"], wr=["pu" + B], out=pu[b][:, k * 128:(k + 1) * 128], in_=ub[b][:, k * 128:(k + 1) * 128], identity=IDF)
        for q4 in range(4):
            st.op("act" if q4 % 2 == 0 else "dve", "copy" if q4 % 2 == 0 else "tensor_copy", rd=["pu" + B], wr=["uo%d" % q4 + B],
                  out=ob[b][:, q4 * 4:(q4 + 1) * 4, :], in_=pu[b][:, q4 * 512:(q4 + 1) * 512].rearrange("p (k e) -> p k e", k=4))
        st.dma("pool", uT_s[:, eb * 128:(eb + 1) * 128].rearrange("(k p) e -> p k e", p=128), ob[b][:], rd=["uo%d" % q4 + B for q4 in range(4)])
    st.emit()


def stage_peer_scores(g, xT, tiles, wq_l, keys_l, cst, sc_s, selp_s):
    st = Stage(g, "pscore")
    cs = st.sb([128, 512], F32, "cst")
    st.dma("sp", cs[:], cst, wr=["cst"])
    IDF = cs[:, 384:512]
    wq = st.sb([128, KC, D], BF16, "wq")
    wst = [st.sb([128, KC, 512], F32, "wst") for _ in range(2)]
    for n in range(4):
        st.dma("sp", wst[n % 2][:], wq_l[:, n * 512:(n + 1) * 512].rearrange("(k p) c -> p k c", p=128), wr=["wst%d" % (n % 2)])
        st.op("pool" if n % 2 else "dve", "tensor_copy", rd=["wst%d" % (n % 2)], wr=["wq"], out=wq[:, :, n * 512:(n + 1) * 512], in_=wst[n % 2][:])
    keysT = st.sb([128, 16, 128], BF16, "keysT")
    kst = [st.sb([128, 128], F32, "kst") for _ in range(2)]
    pq = [st.ps([128, 512], F32, "pq") for _ in range(2)]
    kl = keys_l.rearrange("h p k d -> (h p) k d")
    for blk in range(16):
        j = blk % 2
        st.dma("sp", kst[j][:], kl[blk], wr=["kst%d" % j])
        st.op("pe", "transpose", rd=["kst%d" % j, "cst"], wr=["pq%d" % j], out=pq[j][:, 0:128], in_=kst[j][:], identity=IDF)
        st.op("dve", "tensor_copy", rd=["pq%d" % j], wr=["keysT"], out=keysT[:, blk, :], in_=pq[j][:, 0:128])
    pss = st.ps([128, D], F32, "pss")
    NB = 2
    xt = [st.sb([128, D], BF16, "xt") for _ in range(NB)]
    qTb = [st.sb([128, 128], BF16, "qTb") for _ in range(2)]
    sc = [st.sb([128, D], F32, "sc") for _ in range(NB)]
    wk = st.sb([128, 256], F32, "wk")
    t16 = st.sb([128, 16, 16], F32, "t16")
    cand = st.sb([128, 16, 16], F32, "cand")
    ex = st.sb([128, 256], F32, "ex")
    junk = st.sb([128, 256], F32, "junk")
    c8 = st.sb([128, 16], F32, "c8")
    sm_ = st.sb([128, 32], F32, "small")
    selp = [st.sb([128, 16], F32, "selp") for _ in range(NB)]
    qc = 0
    for i, t in enumerate(tiles):
        b = i % NB
        B = "%d" % b
        rows = slice(t * 128, (t + 1) * 128)
        st.dma("sp", xt[b][:], xT[t], wr=["xt" + B])
        for blk in range(16):
            j = qc % 2
            J = "%d" % j
            qc += 1
            for k in range(KC):
                st.op("pe", "matmul", rd=["wq", "xt" + B], wr=["pq" + J], out=pq[j][:, 0:128], lhsT=wq[:, k, blk * 128:(blk + 1) * 128],
                      rhs=xt[b][:, k * 128:(k + 1) * 128], start=(k == 0), stop=(k == KC - 1))
            if j == 0:
                st.op("act", "copy", rd=["pq" + J], wr=["qTb" + J], out=qTb[j][:], in_=pq[j][:, 0:128])
            else:
                st.op("dve", "tensor_copy", rd=["pq" + J], wr=["qTb" + J], out=qTb[j][:], in_=pq[j][:, 0:128])
            st.op("pe", "matmul", rd=["qTb" + J, "keysT"], wr=["pss"], out=pss[:, blk * 128:(blk + 1) * 128], lhsT=qTb[j][:], rhs=keysT[:, blk, :], start=True, stop=True)
        for q4 in range(4):
            st.op("act" if q4 % 2 == 0 else "dve", "copy" if q4 % 2 == 0 else "tensor_copy", rd=["pss"], wr=["sc%d" % q4 + B],
                  out=sc[b][:, q4 * 512:(q4 + 1) * 512], in_=pss[:, q4 * 512:(q4 + 1) * 512])
        SK = ["sc%d" % q4 + B for q4 in range(4)]
        st.dma("pool", sc_s[rows, :], sc[b][:], rd=SK)
        for blk in range(16):
            sl = sc[b][:, blk * 128:(blk + 1) * 128]
            st.op("dve", "max", rd=SK, wr=["t16"], out=t16[:, blk, 0:8], in_=sl)
            st.op("dve", "match_replace", rd=SK + ["t16"], wr=["wk"], out=wk[:, 0:128], in_to_replace=t16[:, blk, 0:8], in_values=sl, imm_value=-1e30)
            st.op("dve", "max", rd=["wk"], wr=["t16"], out=t16[:, blk, 8:16], in_=wk[:, 0:128])
        for h in range(8):
            st.op("dve", "tensor_tensor", rd=["t16"], wr=["cand"], out=cand[:], in0=bc3(t16[:, 2 * h, :], 16), in1=bcmid(t16[:, 2 * h + 1, :], 16), op=ALU.add)
            cf = cand[:].rearrange("p a b -> p (a b)")
            st.op("dve", "max", rd=["cand"], wr=["c8"], out=c8[:, 0:8], in_=cf)
            st.op("dve", "match_replace", rd=["cand", "c8"], wr=["wk"], out=wk[:], in_to_replace=c8[:, 0:8], in_values=cf, imm_value=-1e30)
            st.op("dve", "max", rd=["wk"], wr=["c8"], out=c8[:, 8:16], in_=wk[:])
            st.op("dve", "tensor_scalar", rd=["c8"], wr=["small"], out=sm_[:, 0:1], in0=c8[:, 0:1], scalar1=-1.0, scalar2=None, op0=ALU.mult)
            st.op("act", "activation", rd=["cand", "small"], wr=["ex"], out=ex[:], in_=cf, func=AF.Exp, bias=sm_[:, 0:1])
            st.op("dve", "scalar_tensor_tensor", rd=["cand", "c8", "ex"], wr=["junk", "small"], out=junk[:], in0=cf, scalar=c8[:, 15:16], in1=ex[:],
                  op0=ALU.is_ge, op1=ALU.mult, accum_out=sm_[:, 1:2])
            st.op("act", "activation", rd=["small"], wr=["small"], out=sm_[:, 2:3], in_=sm_[:, 1:2], func=AF.Ln)
            st.op("dve", "tensor_copy", rd=["c8"], wr=["selp" + B], out=selp[b][:, 2 * h:2 * h + 1], in_=c8[:, 15:16])
            st.op("dve", "tensor_tensor", rd=["small"], wr=["selp" + B], out=selp[b][:, 2 * h + 1:2 * h + 2], in0=sm_[:, 0:1], in1=sm_[:, 2:3], op=ALU.subtract)
        st.dma("pool", selp_s[rows, :], selp[b][:], rd=["selp" + B])
    st.emit()


def stage_peer_experts(g, xT, tiles, uT_s, v_l, ident_bf, sc_s, selp_s, po_s, NG=16):
    st = Stage(g, "pexp")
    ident = st.sb([128, 128], BF16, "ident")
    st.dma("sp", ident[:], ident_bf, wr=["ident"])
    uTg = st.sb([128, KC, 1024], BF16, "uTg")
    vg = st.sb([128, 8, D], BF16, "vg")
    vst = [st.sb([128, D], F32, "vst") for _ in range(2)]
    NB = 2
    xt = [st.sb([128, D], BF16, "xt") for _ in range(NB)]
    sc = [st.sb([128, D], F32, "sc") for _ in range(NB)]
    selp = [st.sb([128, 16], F32, "selp") for _ in range(NB)]
    sm = [st.sb([128, 1024], F32, "sm") for _ in range(2)]
    ex = [st.sb([128, 1024], F32, "ex") for _ in range(2)]
    mk = [[st.sb([128, 1024], BF16, "mk") for _ in range(8)] for _ in range(NB)]
    gl = [st.sb([128, 1024], F32, "gl") for _ in range(NB)]
    WT = [st.sb([128, 1024], BF16, "WT") for _ in range(NB)]
    ob = [st.sb([128, D], F32, "ob") for _ in range(2)]
    ps_h = st.ps([128, 1024], F32, "ps_h")
    ps_g = st.ps([128, 1024], F32, "ps_g")
    ps_o = [st.ps([128, 512], F32, "ps_o") for _ in range(4)]
    hc = [0]
    iters = [(gi, t) for gi in range(NG) for t in tiles]
    NI = len(iters)

    def loads(n):
        gi, t = iters[n]
        B = "%d" % (n % NB)
        b = n % NB
        rows = slice(t * 128, (t + 1) * 128)
        if t == tiles[0]:
            st.dma("sp", uTg[:], uT_s[:, gi * 1024:(gi + 1) * 1024].rearrange("(k p) e -> p k e", p=128), wr=["uTg"])
        st.dma("sp", xt[b][:], xT[t], wr=["xt" + B])
        st.dma("sp", sc[b][:], sc_s[rows, :], wr=["sc" + B])
        st.dma("sp", selp[b][:], selp_s[rows, :], wr=["selp" + B])

    def heads(n, h0, h1):
        gi, t = iters[n]
        b = n % NB
        B = "%d" % b

        def add(h, j):
            s1 = sc[b][:, (2 * h) * 128 + gi * 8:(2 * h) * 128 + gi * 8 + 8]
            s2 = sc[b][:, (2 * h + 1) * 128:(2 * h + 2) * 128]
            sm3 = sm[j][:].rearrange("p (a b) -> p a b", a=8)
            st.op("dve", "tensor_tensor", rd=["sc" + B], wr=["sm%d" % j], out=sm3, in0=bc3(s1, 128), in1=bcmid(s2, 8), op=ALU.add)

        add(h0, hc[0] % 2)
        for h in range(h0, h1):
            j = hc[0] % 2
            J = "%d" % j
            hc[0] += 1
            if h + 1 < h1:
                add(h + 1, hc[0] % 2)
            st.op("act", "activation", rd=["sm" + J, "selp" + B], wr=["ex" + J], out=ex[j][:], in_=sm[j][:], func=AF.Exp, bias=selp[b][:, 2 * h + 1:2 * h + 2])
            st.op("dve", "scalar_tensor_tensor", rd=["sm" + J, "ex" + J, "selp" + B], wr=["mk%d" % h + B], out=mk[b][h][:], in0=sm[j][:], scalar=selp[b][:, 2 * h:2 * h + 1],
                  in1=ex[j][:], op0=ALU.is_ge, op1=ALU.mult)

    def hT(n):
        b = n % NB
        B = "%d" % b
        for c in range(8):
            for k in range(KC):
                st.op("pe", "matmul", rd=["uTg", "xt" + B], wr=["ps_h"], out=ps_h[:, c * 128:(c + 1) * 128], lhsT=uTg[:, k, c * 128:(c + 1) * 128],
                      rhs=xt[b][:, k * 128:(k + 1) * 128], start=(k == 0), stop=(k == KC - 1))

    def gelu(n):
        b = n % NB
        B = "%d" % b
        for q2 in range(2):
            st.op("act", "activation", rd=["ps_h"], wr=["gl%d" % q2 + B], out=gl[b][:, q2 * 512:(q2 + 1) * 512], in_=ps_h[:, q2 * 512:(q2 + 1) * 512], func=AF.Gelu)

    def vload(n):
        gi, t = iters[n]
        if t == tiles[0]:
            for c in range(8):
                j = c % 2
                st.dma("sp", vst[j][:], v_l[gi * 1024 + c * 128:gi * 1024 + (c + 1) * 128, :], wr=["vst%d" % j])
                st.op("act", "copy", rd=["vst%d" % j], wr=["vg"], out=vg[:, c, :], in_=vst[j][:])

    def mask(n):
        b = n % NB
        B = "%d" % b
        for c in range(8):
            for h in range(8):
                st.op("pe", "matmul", rd=["mk%d" % h + B, "ident"], wr=["ps_g"], out=ps_g[:, c * 128:(c + 1) * 128], lhsT=mk[b][h][:, c * 128:(c + 1) * 128],
                      rhs=ident[:], start=(h == 0), stop=(h == 7))

    def wt(n):
        b = n % NB
        B = "%d" % b
        for q2 in range(2):
            st.op("dve", "tensor_tensor", rd=["ps_g", "gl%d" % q2 + B], wr=["WT" + B], out=WT[b][:, q2 * 512:(q2 + 1) * 512], in0=ps_g[:, q2 * 512:(q2 + 1) * 512],
                  in1=gl[b][:, q2 * 512:(q2 + 1) * 512], op=ALU.mult)

    def outmm(n):
        b = n % NB
        B = "%d" % b
        for cb in range(4):
            for c in range(8):
                st.op("pe", "matmul", rd=["WT" + B, "vg"], wr=["ps_o%d" % cb], out=ps_o[cb][:], lhsT=WT[b][:, c * 128:(c + 1) * 128], rhs=vg[:, c, cb * 512:(cb + 1) * 512],
                      start=(c == 0), stop=(c == 7))

    def evac_store(n):
        gi, t = iters[n]
        b = n % NB
        rows = slice(t * 128, (t + 1) * 128)
        o = ob[b]
        for cb in range(4):
            st.op("act", "copy", rd=["ps_o%d" % cb], wr=["ob%d_%d" % (b, cb)], out=o[:, cb * 512:(cb + 1) * 512], in_=ps_o[cb][:])
        okeys = ["ob%d_%d" % (b, cb) for cb in range(4)]
        if gi == 0:
            st.dma("pool", po_s[rows, :], o[:], rd=okeys, wr=[("po", t)])
        else:
            st.dma("pool", po_s[rows, :], o[:], rd=okeys, wr=[("po", t)], accum_op=ALU.add)

    loads(0)
    heads(0, 0, 8)
    hT(0)
    gelu(0)
    for n in range(NI):
        nx = n + 1 < NI
        if nx:
            loads(n + 1)
        vload(n)
        mask(n)
        if nx:
            heads(n + 1, 0, 4)
        wt(n)
        if nx:
            hT(n + 1)
        outmm(n)
        if nx:
            heads(n + 1, 4, 8)
            gelu(n + 1)
        evac_store(n)
    st.emit()


def build_program(NL, depth=2):
    NT = NL + 2
    ROWS = 2 * NL
    NCLS = len(na_plan(ROWS)[2])
    nc = bass.Bass("TRN2", target_bir_lowering=False)

    def inp(name, shape, dt=F32):
        return nc.dram_tensor(name, list(shape), dt, kind="ExternalInput").ap()

    xcat = inp("xcat", [NT * 128, D])
    cc = inp("cc", [2, D])
    w_ada = inp("w_ada", [depth, D, 6 * D])
    b_ada = inp("b_ada", [depth, 6 * D])
    norm_g = inp("norm_g", [depth, 2 * D])
    w_in = inp("w_in", [depth, D, PW])
    gate_b = inp("a_gate_b", [depth, 32])
    hnorm = inp("a_hnorm_g", [depth, D])
    b_conv = inp("b_conv", [depth, 3, D])
    qkg = inp("c_qk_g", [depth, 2, 128])
    nab = inp("nab", [depth, 16, NCLS, 128, 640])
    w_a = inp("w_a_out", [depth, D, D])
    w_b = inp("w_b_out", [depth, D, D])
    w_c = inp("w_c_out", [depth, D, D])
    w_o = inp("w_out", [depth, D, D])
    wq = inp("peer_wq", [depth, D, D])
    pkeys = inp("peer_keys", [depth, 8, 2, 128, 128])
    pu = inp("peer_u", [depth, 16384, D])
    pv = inp("peer_v", [depth, 16384, D])
    cst = inp("cst", [128, 512])
    ident = inp("ident_bf", [128, 128], BF16)
    rope = [inp(n, [NL * 128, 128]) for n in ("cosq", "sinq", "cosk", "sink")]
    out = nc.dram_tensor("out", [NL * 128, D], F32, kind="ExternalOutput").ap()

    with ExitStack() as es:
        es.enter_context(nc.allow_low_precision("bf16 matmul operands, fp32 accumulation"))
        g = G(nc, es)
        R = NT * 128

        def dr(name, shape, dt=F32):
            return g.dram(name, shape, dt).ap()

        modv = dr("modv", [2, 6 * D])
        xnT = dr("xnT", [NT, 128, D], BF16)
        pparts = [dr("p%d" % gi, [R, PSplit.BOUNDS[gi + 1] - PSplit.BOUNDS[gi]]) for gi in range(4)]
        p = PSplit(pparts)
        qT_s = dr("qT_s", [NT, 128, 1024], BF16)
        kT_s = dr("kT_s", [NT, 128, 1024], BF16)
        kb_s = dr("kb_s", [R, 1024], BF16)
        v2f_s = dr("v2f_s", [R, 8 * 257], BF16)
        v2b_s = dr("v2b_s", [R, 8 * 257], BF16)
        gpk_s = dr("gpk_s", [R, 48])
        hf_s = dr("hf_s", [R, D])
        yaT = dr("yaT", [NT, 128, D], BF16)
        ybT = dr("ybT", [NT, 128, D], BF16)
        ycT = dr("ycT", [NT, 128, D], BF16)
        yT = dr("yT", [NT, 128, D], BF16)
        cqT_s = dr("cqT_s", [NT, 128, D], BF16)
        ckT_s = dr("ckT_s", [NT, 128, D], BF16)
        cv1_s = dr("cv1_s", [R, 16 * 129], BF16)
        yc_s = dr("yc_s", [R, D])
        brs = [dr("br%d" % j, [R, D]) for j in range(3)]
        mix = dr("mix", [R, D])
        x1 = dr("x1", [R, D])
        xmid = dr("xmid", [R, D])
        uT_s = dr("uT_s", [D, 16384], BF16)
        sc_s = dr("sc_s", [R, D])
        selp_s = dr("selp_s", [R, 16])
        po_s = dr("po_s", [R, D])

        allt = list(range(NT))
        lat = list(range(2, NT))
        for l in range(depth):
            last = l == depth - 1
            xin = xcat if l == 0 else xmid
            T = lat if last else allt
            stage_mod(g, cc, w_ada[l], b_ada[l:l + 1, :], norm_g[l:l + 1, :], modv)
            stage_norm(g, xin, modv, 0, xnT, allt, ident)
            for gi in range(4):
                lo, hi = PSplit.BOUNDS[gi], PSplit.BOUNDS[gi + 1]
                stage_linear(g, xnT, allt, w_in[l][:, lo:hi], hi - lo, pparts[gi])
            stage_mlstm_prep(g, p, allt, cst, ident, gate_b[l:l + 1, :], rope, qT_s, kT_s, kb_s, v2f_s, v2b_s, gpk_s)
            stage_mlstm_scan(g, False, [0, 1] + lat, cst, ident, qT_s, kT_s, kb_s, v2f_s, gpk_s, hf_s)
            stage_mlstm_scan(g, True, [1, 0] + lat[::-1], cst, ident, qT_s, kT_s, kb_s, v2b_s, gpk_s, hf_s, p=p, hnorm_l=hnorm[l:l + 1, :], yaT=yaT,
                             skip_out=((0, 1) if last else ()))
            stage_conv(g, p, T, NT, b_conv[l], ident, ybT)
            stage_na_prep(g, p, allt, qkg[l], ident, cqT_s, ckT_s, cv1_s)
            stage_na(g, NL, not last, nab[l], ident, cqT_s, ckT_s, cv1_s, yc_s)
            stage_cast_T(g, yc_s, T, ident, ycT)
            stage_linear(g, yaT, T, w_a[l], D, brs[0])
            stage_linear(g, ybT, T, w_b[l], D, brs[1])
            stage_linear(g, ycT, T, w_c[l], D, brs[2])
            stage_gate(g, p, T, brs, ident, yT)
            stage_linear(g, yT, T, w_o[l], D, mix)
            stage_resid(g, xin, mix, T, modv, 2, x1)
            stage_norm(g, x1, modv, 1, xnT, T, ident)
            stage_uT(g, pu[l], cst, uT_s)
            stage_peer_scores(g, xnT, T, wq[l], pkeys[l], cst, sc_s, selp_s)
            stage_peer_experts(g, xnT, T, uT_s, pv[l], ident, sc_s, selp_s, po_s)
            if last:
                stage_resid(g, x1, po_s, T, modv, 5, out, dst_row0=256)
            else:
                stage_resid(g, x1, po_s, T, modv, 5, xmid)
        final_wait(g)
    return nc


_PROG = {}


def kernel(x, c, ctx, c_ctx, w_ada, b_ada, norm_g, w_in, a_gate_b, a_hnorm_g, b_conv, c_qk_g, c_rpb,
           w_a_out, w_b_out, w_c_out, w_out, peer_wq, peer_keys, peer_u, peer_v):
    f = lambda a: np.ascontiguousarray(np.asarray(a, dtype=np.float32))
    x, ctx = f(x), f(ctx)
    B, S, _ = x.shape
    NL = S // 128
    depth = w_ada.shape[0]
    hc = host_consts(S)
    nab = np.stack([na_bias_host(f(c_rpb)[l], 2 * NL) for l in range(depth)], 0)
    shared = dict(w_ada=f(w_ada), b_ada=f(b_ada), norm_g=f(norm_g).reshape(depth, 2 * D), w_in=f(w_in),
                  a_gate_b=f(a_gate_b).reshape(depth, 32), a_hnorm_g=f(a_hnorm_g), b_conv=f(b_conv), c_qk_g=f(c_qk_g), nab=nab,
                  w_a_out=f(w_a_out), w_b_out=f(w_b_out), w_c_out=f(w_c_out), w_out=f(w_out), peer_wq=f(peer_wq),
                  peer_keys=f(peer_keys), peer_u=f(peer_u), peer_v=f(peer_v), **hc)
    in_maps = []
    for b in range(B):
        m = dict(shared)
        m["xcat"] = np.concatenate([ctx[b], x[b]], axis=0)
        m["cc"] = np.stack([f(c)[b], f(c_ctx)], axis=0)
        in_maps.append(m)
    key = (NL, depth)
    if key not in _PROG:
        _PROG[key] = build_program(NL, depth)
    res = run_bass_kernel_spmd(_PROG[key], in_maps, core_ids=list(range(B)))
    return np.stack([res.results[b]["out"] for b in range(B)], axis=0)
```

```python
import numpy as np
from contextlib import ExitStack
import concourse.bass as bass
import concourse.mybir as mybir
from concourse.bass_utils import run_bass_kernel_spmd

F32 = mybir.dt.float32
BF16 = mybir.dt.bfloat16
ALU = mybir.AluOpType
AF = mybir.ActivationFunctionType
AX = mybir.AxisListType

D = 2048
KC = 16
PW = 24608
EPS = 1e-6
ENGS = ["pe", "act", "dve", "pool", "sp"]
NDS = 24

O_AK, O_AV, O_AG, O_CK, O_CV, O_AQ, O_AO, O_BB, O_BC, O_BX, O_CQ, O_GT = (
    0, 1024, 3072, 3104, 5152, 7200, 8224, 10272, 12320, 14368, 16416, 18464)


class PSplit:
    BOUNDS = [0, 7200, 14368, 18464, PW]

    def __init__(self, aps):
        self.aps = aps

    def __getitem__(self, key):
        rows, cols = key
        for gi in range(4):
            lo, hi = self.BOUNDS[gi], self.BOUNDS[gi + 1]
            if cols.start >= lo and cols.stop <= hi:
                return self.aps[gi][rows, cols.start - lo:cols.stop - lo]
        raise ValueError("column range straddles projection groups: %s" % (cols,))


class G:
    def __init__(self, nc, es):
        self.nc = nc
        self.sem = {e: es.enter_context(nc.semaphore("s_" + e)) for e in ENGS}
        self.dsem = [es.enter_context(nc.semaphore("d%d" % i)) for i in range(NDS)]
        self.cnt = {e: 0 for e in ENGS}
        self.dcnt = [0] * NDS
        self.dnext = 0
        self.known = {e: {} for e in ENGS}
        self.nstage = 0
        self.ntens = 0

    def dram(self, name, shape, dt):
        return self.nc.dram_tensor(name, list(shape), dt)


class Stage:
    def __init__(self, g, name=None):
        self.g = g
        g.nstage += 1
        self.name = name or ("st%d" % g.nstage)
        self.ops = {e: [] for e in ENGS}
        self.lastw = {}
        self.readers = {}
        self.es = ExitStack()
        self.start_cnt = dict(g.cnt)
        self.start_d = list(g.dcnt)

    def sb(self, shape, dt=F32, name=None):
        self.g.ntens += 1
        return self.es.enter_context(self.g.nc.sbuf_tensor("%s_t%d" % (name or "sb", self.g.ntens), list(shape), dt))

    def ps(self, shape, dt=F32, name=None):
        self.g.ntens += 1
        return self.es.enter_context(self.g.nc.psum_tensor("%s_p%d" % (name or "ps", self.g.ntens), list(shape), dt))

    def add(self, eng, fn, rd=(), wr=(), dma=False):
        g = self.g
        deps = []
        for k in rd:
            t = self.lastw.get(k)
            if t is not None:
                deps.append(t)
        for k in wr:
            t = self.lastw.get(k)
            cands = ([t] if t is not None else []) + self.readers.get(k, [])
            for c in cands:
                if (not dma) and c[0] == "c" and c[1] == eng:
                    continue
                deps.append(c)
        if dma:
            j = g.dnext
            g.dnext = (j + 1) % NDS
            g.dcnt[j] += 1
            tok = ("d", j, g.dcnt[j])
            if g.dcnt[j] > 1:
                deps.append(("d", j, g.dcnt[j] - 1))
        else:
            g.cnt[eng] += 1
            tok = ("c", eng, g.cnt[eng])
        for k in wr:
            self.lastw[k] = tok
            self.readers[k] = []
        for k in rd:
            lst = self.readers.setdefault(k, [])
            if tok[0] == "c":
                lst[:] = [x for x in lst if not (x[0] == "c" and x[1] == tok[1])]
            lst.append(tok)
        self.ops[eng].append((fn, deps, tok))
        return tok

    def op(self, eng, meth, rd=(), wr=(), **kw):
        return self.add(eng, lambda e: getattr(e, meth)(**kw), rd, wr)

    def dma(self, eng, out, in_, rd=(), wr=(), **kw):
        return self.add(eng, lambda e: e.dma_start(out=out, in_=in_, **kw), rd, wr, dma=True)

    def emit(self):
        g = self.g
        nc = g.nc

        def run(ename, e):
            known = g.known[ename]

            def wait(tok):
                if tok[0] == "c":
                    sem, val, key = g.sem[tok[1]], tok[2], tok[1]
                else:
                    sem, val, key = g.dsem[tok[1]], 16 * tok[2], "d%d" % tok[1]
                if known.get(key, 0) < val:
                    e.wait_ge(sem, val)
                    known[key] = val

            for o in ENGS:
                if o != ename and self.start_cnt[o] > 0:
                    wait(("c", o, self.start_cnt[o]))
            for j in range(NDS):
                if self.start_d[j] > 0:
                    wait(("d", j, self.start_d[j]))
            for fn, deps, tok in self.ops[ename]:
                for d in deps:
                    wait(d)
                ins = fn(e)
                if tok[0] == "c":
                    ins.then_inc(g.sem[tok[1]], 1)
                else:
                    ins.then_inc(g.dsem[tok[1]], 16)

        with nc.Block() as block:
            if self.ops["pe"]:
                block.tensor(lambda e: run("pe", e))
            if self.ops["act"]:
                block.scalar(lambda e: run("act", e))
            if self.ops["dve"]:
                block.vector(lambda e: run("dve", e))
            if self.ops["pool"]:
                block.gpsimd(lambda e: run("pool", e))
            if self.ops["sp"]:
                block.sync(lambda e: run("sp", e))
        self.es.close()


def final_wait(g):
    st = Stage(g, "final")
    nc = g.nc

    def run(e):
        for o in ENGS:
            if o != "sp" and g.cnt[o] > 0:
                e.wait_ge(g.sem[o], g.cnt[o])
        for j in range(NDS):
            if g.dcnt[j] > 0:
                e.wait_ge(g.dsem[j], 16 * g.dcnt[j])

    with nc.Block() as block:
        block.sync(run)
    st.es.close()


def bcast_row(ap_row, nparts=128):
    return ap_row.partition_broadcast(nparts)


def stage_mod(g, cc, w_ada_l, b_ada_l, norm_g_l, modv):
    st = Stage(g, "mod")
    ccT = st.sb([128, 2, KC], F32, "ccT")
    sil = st.sb([128, 2, KC], F32, "sil")
    sig = st.sb([128, 2, KC], F32, "sig")
    NCH = 6 * D // 512
    wst = [st.sb([128, KC, 512], F32, "wada") for _ in range(2)]
    bb = [st.sb([2, 512], F32, "bada") for _ in range(2)]
    ng = [st.sb([2, 512], F32, "ng") for _ in range(2)]
    res = [st.sb([2, 512], F32, "res") for _ in range(2)]
    pst = [st.ps([128, 512], F32, "pmod") for _ in range(2)]
    for r in range(2):
        st.dma("sp", ccT[:, r, :], cc[r, :].rearrange("(k p) -> p k", p=128), wr=["ccT"], allow_slow_non_contiguous=True)
    st.add("act", lambda e: e.activation(out=sig[:], in_=ccT[:], func=AF.Sigmoid), rd=["ccT"], wr=["sig"])
    st.add("dve", lambda e: e.tensor_tensor(out=sil[:], in0=ccT[:], in1=sig[:], op=ALU.mult), rd=["ccT", "sig"], wr=["sil"])
    segmap = {0: 1, 1: 0, 2: 2, 3: 4, 4: 3, 5: 5}
    for n in range(NCH):
        i = n % 2
        w = wst[i]
        seg, cs = n // 4, (n % 4) * 512
        st.dma("sp", w[:], w_ada_l[:, n * 512:(n + 1) * 512].rearrange("(k p) c -> p k c", p=128), wr=["w%d" % i])
        for r in range(2):
            st.dma("sp", bb[i][r:r + 1, :], b_ada_l[:, n * 512:(n + 1) * 512], wr=["bb%d" % i])
            if seg in (1, 4):
                j = 0 if seg == 1 else 1
                st.dma("sp", ng[i][r:r + 1, :], norm_g_l[:, j * D + cs:j * D + cs + 512], wr=["ng%d" % i])
        for k in range(KC):
            st.add("pe", lambda e, w=w, k=k, i=i: e.matmul(pst[i][0:2, :], lhsT=sil[:, :, k], rhs=w[:, k, :],
                                                           start=(k == 0), stop=(k == KC - 1)),
                   rd=["sil", "w%d" % i], wr=["p%d" % i])
        st.add("dve", lambda e, i=i: e.tensor_tensor(out=res[i][:], in0=pst[i][0:2, :], in1=bb[i][:], op=ALU.add),
               rd=["p%d" % i, "bb%d" % i], wr=["res%d" % i])
        if seg in (1, 4):
            st.add("dve", lambda e, i=i: e.scalar_tensor_tensor(out=res[i][:], in0=res[i][:], scalar=1.0, in1=ng[i][:],
                                                                op0=ALU.add, op1=ALU.mult),
                   rd=["res%d" % i, "ng%d" % i], wr=["res%d" % i])
        o0 = segmap[seg] * D + cs
        st.dma("sp", modv[:, o0:o0 + 512], res[i][:], rd=["res%d" % i])
    st.emit()


def stage_norm(g, src, modv, jn, xnT, tiles, ident_bf):
    st = Stage(g, "norm")
    A = [st.sb([128, D], F32, "A") for _ in range(2)]
    B = [st.sb([128, D], F32, "B") for _ in range(2)]
    ident = st.sb([128, 128], BF16, "ident")
    st.dma("sp", ident[:], ident_bf, wr=["ident"])
    for r in range(2):
        st.dma("sp", A[r][:], bcast_row(modv[r:r + 1, (3 * jn) * D:(3 * jn + 1) * D]), wr=["A%d" % r])
        st.dma("sp", B[r][:], bcast_row(modv[r:r + 1, (3 * jn + 1) * D:(3 * jn + 2) * D]), wr=["B%d" % r])
    NB = 2
    xt = [st.sb([128, D], F32, "x") for _ in range(NB)]
    junk = [st.sb([128, D], F32, "junk") for _ in range(NB)]
    tmp = [st.sb([128, D], F32, "tmp") for _ in range(NB)]
    xb = [st.sb([128, D], BF16, "xb") for _ in range(NB)]
    xT = [st.sb([128, D], BF16, "xT") for _ in range(NB)]
    ss = [st.sb([128, 1], F32, "ss") for _ in range(NB)]
    rs = [st.sb([128, 1], F32, "rs") for _ in range(NB)]
    pt = [st.ps([128, D], BF16, "pT") for _ in range(NB)]
    for i, t in enumerate(tiles):
        b = i % NB
        r = 1 if t < 2 else 0
        st.dma("sp", xt[b][:], src[t * 128:(t + 1) * 128, :], wr=["x%d" % b])
        st.add("act", lambda e, b=b: e.activation(out=junk[b][:], in_=xt[b][:], func=AF.Square, accum_out=ss[b][:]),
               rd=["x%d" % b], wr=["junk%d" % b, "ss%d" % b])
        st.add("act", lambda e, b=b: e.activation(out=rs[b][:], in_=ss[b][:], func=AF.Sqrt, scale=1.0 / D, bias=EPS),
               rd=["ss%d" % b], wr=["rs%d" % b])
        st.add("dve", lambda e, b=b: e.reciprocal(out=rs[b][:], in_=rs[b][:]),
               rd=["rs%d" % b], wr=["rs%d" % b])
        st.add("dve", lambda e, b=b, r=r: e.scalar_tensor_tensor(out=tmp[b][:], in0=xt[b][:], scalar=rs[b][:], in1=A[r][:],
                                                                 op0=ALU.mult, op1=ALU.mult),
               rd=["x%d" % b, "rs%d" % b, "A%d" % r], wr=["tmp%d" % b])
        st.add("dve", lambda e, b=b, r=r: e.tensor_tensor(out=xb[b][:], in0=tmp[b][:], in1=B[r][:], op=ALU.add),
               rd=["tmp%d" % b, "B%d" % r], wr=["xb%d" % b])
        for k in range(KC):
            st.add("pe", lambda e, b=b, k=k: e.transpose(out=pt[b][:, k * 128:(k + 1) * 128], in_=xb[b][:, k * 128:(k + 1) * 128], identity=ident[:]),
                   rd=["xb%d" % b, "ident"], wr=["pt%d" % b])
        st.add("act", lambda e, b=b: e.copy(out=xT[b][:, 0:1024], in_=pt[b][:, 0:1024]), rd=["pt%d" % b], wr=["xTa%d" % b])
        st.add("dve", lambda e, b=b: e.tensor_copy(out=xT[b][:, 1024:2048], in_=pt[b][:, 1024:2048]), rd=["pt%d" % b], wr=["xTb%d" % b])
        st.dma("pool", xnT[t], xT[b][:], rd=["xTa%d" % b, "xTb%d" % b])
    st.emit()


def stage_linear(g, xT, tiles, W, N, out, oc0=0, w_bf=False, slab=1024):
    st = Stage(g, "lin")
    wst = [st.sb([128, KC, 512], F32, "wst") for _ in range(2)] if not w_bf else None
    wsl = [st.sb([128, KC, slab], BF16, "wsl") for _ in range(2)]
    NX = 3
    xt = [st.sb([128, D], BF16, "xt") for _ in range(NX)]
    ob = [st.sb([128, slab], F32, "ob") for _ in range(2)]
    pp = [st.ps([128, 512], F32, "pl") for _ in range(4)]
    nsl = (N + slab - 1) // slab
    cnt = 0
    hcnt = 0
    pcnt = 0
    for s in range(nsl):
        c0 = s * slab
        ns = min(slab, N - c0)
        w = wsl[s % 2]
        wk = "wsl%d" % (s % 2)
        for h0 in range(0, ns, 512):
            hw = min(512, ns - h0)
            src = W[:, c0 + h0:c0 + h0 + hw].rearrange("(k p) c -> p k c", p=128)
            if w_bf:
                st.dma("sp", w[:, :, h0:h0 + hw], src, wr=[wk + "_%d" % h0])
            else:
                ws = wst[hcnt % 2]
                wsk = "wst%d" % (hcnt % 2)
                st.dma("sp", ws[:, :, 0:hw], src, wr=[wsk])
                if hcnt % 2 == 0:
                    st.add("act", lambda e, w=w, ws=ws, h0=h0, hw=hw: e.copy(out=w[:, :, h0:h0 + hw], in_=ws[:, :, 0:hw]),
                           rd=[wsk], wr=[wk + "_%d" % h0])
                else:
                    st.add("dve", lambda e, w=w, ws=ws, h0=h0, hw=hw: e.tensor_copy(out=w[:, :, h0:h0 + hw], in_=ws[:, :, 0:hw]),
                           rd=[wsk], wr=[wk + "_%d" % h0])
                hcnt += 1
        for t in tiles:
            xb = cnt % NX
            o = ob[cnt % 2]
            ok = "ob%d" % (cnt % 2)
            st.dma("sp", xt[xb][:], xT[t], wr=["xt%d" % xb])
            for h0 in range(0, ns, 512):
                hw = min(512, ns - h0)
                p = pp[pcnt % 4]
                pk = "pp%d" % (pcnt % 4)
                for k in range(KC):
                    st.add("pe", lambda e, p=p, xb=xb, k=k, w=w, h0=h0, hw=hw: e.matmul(
                        p[:, 0:hw], lhsT=xt[xb][:, k * 128:(k + 1) * 128], rhs=w[:, k, h0:h0 + hw], start=(k == 0), stop=(k == KC - 1)),
                        rd=["xt%d" % xb, wk + "_%d" % h0], wr=[pk])
                if pcnt % 2 == 0:
                    st.add("act", lambda e, o=o, p=p, h0=h0, hw=hw: e.copy(out=o[:, h0:h0 + hw], in_=p[:, 0:hw]), rd=[pk], wr=[ok + "_%d" % h0])
                else:
                    st.add("dve", lambda e, o=o, p=p, h0=h0, hw=hw: e.tensor_copy(out=o[:, h0:h0 + hw], in_=p[:, 0:hw]), rd=[pk], wr=[ok + "_%d" % h0])
                pcnt += 1
            st.dma("pool", out[t * 128:(t + 1) * 128, oc0 + c0:oc0 + c0 + ns], o[:, 0:ns],
                   rd=[ok + "_%d" % h0 for h0 in range(0, ns, 512)])
            cnt += 1
    st.emit()


def bc3(ap2d, n):
    return ap2d.unsqueeze(2).broadcast_to([ap2d.shape[0], ap2d.shape[1], n])


def bcmid(ap2d, n):
    return ap2d.unsqueeze(1).broadcast_to([ap2d.shape[0], n, ap2d.shape[1]])


class TrOut:
    def __init__(self, st, ident, nb=2):
        self.st = st
        self.ident = ident
        self.nb = nb
        self.pt = [st.ps([128, D], BF16, "trp") for _ in range(nb)]
        self.xT = [st.sb([128, D], BF16, "trx") for _ in range(nb)]
        self.i = 0

    def go(self, src, srckeys, dst):
        st = self.st
        b = self.i % self.nb
        self.i += 1
        pt, xT, ident = self.pt[b], self.xT[b], self.ident
        for k in range(KC):
            st.add("pe", lambda e, k=k: e.transpose(out=pt[:, k * 128:(k + 1) * 128], in_=src[:, k * 128:(k + 1) * 128], identity=ident[:]),
                   rd=list(srckeys) + ["ident"], wr=["trp%d" % b])
        st.add("act", lambda e: e.copy(out=xT[:, 0:1024], in_=pt[:, 0:1024]), rd=["trp%d" % b], wr=["trxa%d" % b])
        st.add("dve", lambda e: e.tensor_copy(out=xT[:, 1024:2048], in_=pt[:, 1024:2048]), rd=["trp%d" % b], wr=["trxb%d" % b])
        st.dma("pool", dst, xT[:], rd=["trxa%d" % b, "trxb%d" % b])


def stage_mlstm_prep(g, p, tiles, cst, ident_bf, gate_b_l, rope, qT_s, kT_s, kb_s, v2f_s, v2b_s, gpk_s):
    st = Stage(g, "mprep")
    SC = 128.0 ** -0.5
    cs = st.sb([128, 512], F32, "cst")
    ident = st.sb([128, 128], BF16, "ident")
    gbias = st.sb([128, 32], F32, "gbias")
    st.dma("sp", cs[:], cst, wr=["cst"])
    st.dma("sp", ident[:], ident_bf, wr=["ident"])
    st.dma("sp", gbias[:], bcast_row(gate_b_l), wr=["gbias"])
    MF, MB, ONES = cs[:, 0:128], cs[:, 128:256], cs[:, 256:384]
    NB = 2
    q = [st.sb([128, 1024], F32, "q") for _ in range(NB)]
    k = [st.sb([128, 1024], F32, "k") for _ in range(NB)]
    v = [st.sb([128, 2048], F32, "v") for _ in range(NB)]
    gp = [st.sb([128, 32], F32, "gp") for _ in range(NB)]
    cq = [st.sb([128, 128], F32, "cq") for _ in range(NB)]
    sq = [st.sb([128, 128], F32, "sq") for _ in range(NB)]
    ck = [st.sb([128, 128], F32, "ck") for _ in range(NB)]
    sk = [st.sb([128, 128], F32, "sk") for _ in range(NB)]
    t1d = {n: [st.sb([128, 1024], F32, "t1" + n) for _ in range(NB)] for n in ("q", "k")}
    t2d = {n: [st.sb([128, 1024], F32, "t2" + n) for _ in range(NB)] for n in ("q", "k")}
    qb = [st.sb([128, 1024], BF16, "qb") for _ in range(NB)]
    kb = [st.sb([128, 1024], BF16, "kb") for _ in range(NB)]
    qTs = [st.sb([128, 1024], BF16, "qTs") for _ in range(NB)]
    kTs = [st.sb([128, 1024], BF16, "kTs") for _ in range(NB)]
    v2f = [st.sb([128, 8, 257], BF16, "v2f") for _ in range(NB)]
    v2b = [st.sb([128, 8, 257], BF16, "v2b") for _ in range(NB)]
    lf = [st.sb([128, 16], F32, "lf") for _ in range(NB)]
    gk = [st.sb([128, 48], F32, "gk") for _ in range(NB)]
    tmp = [st.sb([128, 16], F32, "tmpg") for _ in range(NB)]
    pq = [st.ps([128, 1024], BF16, "pq") for _ in range(NB)]
    pk = [st.ps([128, 1024], BF16, "pk") for _ in range(NB)]
    pg = [st.ps([128, 32], F32, "pg") for _ in range(NB)]
    cosq, sinq, cosk, sink = rope
    for i, t in enumerate(tiles):
        b = i % NB
        B = "%d" % b
        rows = slice(t * 128, (t + 1) * 128)
        st.dma("sp", q[b][:], p[rows, O_AQ:O_AQ + 1024], wr=["q" + B])
        st.dma("sp", k[b][:], p[rows, O_AK:O_AK + 1024], wr=["k" + B])
        st.dma("sp", v[b][:], p[rows, O_AV:O_AV + 2048], wr=["v" + B])
        st.dma("sp", gp[b][:], p[rows, O_AG:O_AG + 32], wr=["gp" + B])
        if t >= 2:
            lr = slice((t - 2) * 128, (t - 1) * 128)
            st.dma("sp", cq[b][:], cosq[lr, :], wr=["cq" + B])
            st.dma("sp", sq[b][:], sinq[lr, :], wr=["sq" + B])
            st.dma("sp", ck[b][:], cosk[lr, :], wr=["ck" + B])
            st.dma("sp", sk[b][:], sink[lr, :], wr=["sk" + B])
            for (src, cc_, ss_, dst, nm, eng) in ((q, cq, sq, qb, "q", "dve"), (k, ck, sk, kb, "k", "dve")):
                s4 = src[b][:].rearrange("p (h f u j) -> p h f u j", h=8, f=2, u=2, j=32)
                t1v, t2v = t1d[nm][b], t2d[nm][b]
                d4 = t2v[:].rearrange("p (h f u j) -> p h f u j", h=8, f=2, u=2, j=32)
                nsn = ss_[b][:, 0:64].rearrange("p (f j) -> p f j", f=2).unsqueeze(1).broadcast_to([128, 8, 2, 32])
                psn = ss_[b][:, 64:128].rearrange("p (f j) -> p f j", f=2).unsqueeze(1).broadcast_to([128, 8, 2, 32])
                tk1, tk2 = "t1" + nm + B, "t2" + nm + B
                st.op(eng, "tensor_tensor", rd=[nm + B, "c" + nm + B], wr=[tk1],
                      out=t1v[:].rearrange("p (h d) -> p h d", h=8), in0=src[b][:].rearrange("p (h d) -> p h d", h=8),
                      in1=bcmid(cc_[b][:], 8), op=ALU.mult)
                st.op(eng, "tensor_tensor", rd=[nm + B, "s" + nm + B], wr=[tk2],
                      out=d4[:, :, :, 0, :], in0=s4[:, :, :, 1, :], in1=nsn, op=ALU.mult)
                st.op(eng, "tensor_tensor", rd=[nm + B, "s" + nm + B], wr=[tk2],
                      out=d4[:, :, :, 1, :], in0=s4[:, :, :, 0, :], in1=psn, op=ALU.mult)
                st.op(eng, "tensor_tensor", rd=[tk1, tk2], wr=[nm + "b" + B], out=dst[b][:], in0=t1v[:], in1=t2v[:], op=ALU.add)
        else:
            st.op("act", "mul", rd=["q" + B], wr=["qb" + B], out=qb[b][:], in_=q[b][:], mul=SC)
            st.op("dve", "tensor_copy", rd=["k" + B], wr=["kb" + B], out=kb[b][:], in_=k[b][:])
        for h in range(8):
            st.op("pe", "transpose", rd=["qb" + B, "ident"], wr=["pq" + B],
                  out=pq[b][:, h * 128:(h + 1) * 128], in_=qb[b][:, h * 128:(h + 1) * 128], identity=ident[:])
        st.op("act", "copy", rd=["pq" + B], wr=["qTs" + B], out=qTs[b][:], in_=pq[b][:])
        for h in range(8):
            st.op("pe", "transpose", rd=["kb" + B, "ident"], wr=["pk" + B],
                  out=pk[b][:, h * 128:(h + 1) * 128], in_=kb[b][:, h * 128:(h + 1) * 128], identity=ident[:])
        st.op("dve", "tensor_copy", rd=["pk" + B], wr=["kTs" + B], out=kTs[b][:], in_=pk[b][:])
        st.dma("pool", qT_s[t], qTs[b][:], rd=["qTs" + B])
        st.dma("pool", kT_s[t], kTs[b][:], rd=["kTs" + B])
        st.dma("pool", kb_s[rows, :], kb[b][:], rd=["kb" + B])
        st.op("dve", "tensor_tensor", rd=["gp" + B, "gbias"], wr=["gp" + B], out=gp[b][:], in0=gp[b][:], in1=gbias[:], op=ALU.add)
        st.op("act", "activation", rd=["gp" + B], wr=["lf" + B], out=lf[b][:, 0:8], in_=gp[b][:, 8:16], func=AF.Exp, scale=-1.0)
        st.op("act", "activation", rd=["gp" + B], wr=["lf" + B], out=lf[b][:, 8:16], in_=gp[b][:, 24:32], func=AF.Exp, scale=-1.0)
        st.op("act", "activation", rd=["lf" + B], wr=["lf" + B], out=lf[b][:], in_=lf[b][:], func=AF.Ln, bias=1.0)
        st.op("dve", "tensor_scalar", rd=["lf" + B], wr=["lf" + B], out=lf[b][:], in0=lf[b][:], scalar1=-1.0, scalar2=None, op0=ALU.mult)
        st.op("pe", "matmul", rd=["lf" + B, "cst"], wr=["pg" + B], out=pg[b][:, 0:8], lhsT=MF, rhs=lf[b][:, 0:8], start=True, stop=True)
        st.op("pe", "matmul", rd=["lf" + B, "cst"], wr=["pg" + B], out=pg[b][:, 8:16], lhsT=MB, rhs=lf[b][:, 8:16], start=True, stop=True)
        st.op("pe", "matmul", rd=["lf" + B, "cst"], wr=["pg" + B], out=pg[b][:, 16:32], lhsT=ONES, rhs=lf[b][:, 0:16], start=True, stop=True)
        st.op("dve", "tensor_tensor", rd=["gp" + B, "pg" + B], wr=["tmp" + B], out=tmp[b][:, 0:8], in0=gp[b][:, 0:8], in1=pg[b][:, 0:8], op=ALU.subtract)
        st.op("dve", "tensor_tensor", rd=["gp" + B, "pg" + B], wr=["tmp" + B], out=tmp[b][:, 8:16], in0=gp[b][:, 16:24], in1=pg[b][:, 8:16], op=ALU.subtract)
        st.op("act", "activation", rd=["tmp" + B], wr=["gk" + B], out=gk[b][:, 0:8], in_=tmp[b][:, 0:8], func=AF.Exp)
        st.op("act", "activation", rd=["tmp" + B], wr=["gk" + B], out=gk[b][:, 16:24], in_=tmp[b][:, 8:16], func=AF.Exp)
        st.op("act", "activation", rd=["pg" + B], wr=["gk" + B], out=gk[b][:, 8:16], in_=pg[b][:, 0:8], func=AF.Exp)
        st.op("act", "activation", rd=["pg" + B], wr=["gk" + B], out=gk[b][:, 24:32], in_=pg[b][:, 8:16], func=AF.Exp)
        st.op("act", "activation", rd=["pg" + B], wr=["gk" + B], out=gk[b][:, 32:48], in_=pg[b][:, 16:32], func=AF.Exp)
        st.dma("pool", gpk_s[rows, :], gk[b][:], rd=["gk" + B])
        v3 = v[b][:].rearrange("p (h d) -> p h d", h=8)
        for h in range(8):
            st.op("act", "activation", rd=["v" + B, "gk" + B], wr=["v2f" + B], out=v2f[b][:, h, 0:256], in_=v3[:, h, :], func=AF.Identity, scale=gk[b][:, h:h + 1])
            st.op("act", "activation", rd=["v" + B, "gk" + B], wr=["v2b" + B], out=v2b[b][:, h, 0:256], in_=v3[:, h, :], func=AF.Identity, scale=gk[b][:, 16 + h:17 + h])
        st.op("act", "copy", rd=["gk" + B], wr=["v2f" + B], out=v2f[b][:, :, 256], in_=gk[b][:, 0:8])
        st.op("act", "copy", rd=["gk" + B], wr=["v2b" + B], out=v2b[b][:, :, 256], in_=gk[b][:, 16:24])
        st.dma("pool", v2f_s[rows, :], v2f[b][:].rearrange("p h d -> p (h d)"), rd=["v2f" + B])
        st.dma("pool", v2b_s[rows, :], v2b[b][:].rearrange("p h d -> p (h d)"), rd=["v2b" + B])
    st.emit()


def stage_mlstm_scan(g, bwd, order, cst, ident_bf, qT_s, kT_s, kb_s, v2_s, gpk_s, hf_s, p=None, hnorm_l=None, yaT=None, skip_out=()):
    st = Stage(g, "mscan")
    cs = st.sb([128, 512], F32, "cst")
    st.dma("sp", cs[:], cst, wr=["cst"])
    MASK = cs[:, 128:256] if bwd else cs[:, 0:128]
    o_ebc = 24 if bwd else 8
    o_eB = 40 if bwd else 32
    C32 = st.sb([128, 8, 257], F32, "C32")
    Cb = st.sb([128, 8, 257], BF16, "Cb")
    st.op("dve", "memset", wr=["C32_%d" % h for h in range(8)], ap=C32[:], constant=0.0)
    st.op("pool", "memset", wr=["Cb_%d" % h for h in range(8)], ap=Cb[:], constant=0.0)
    NB = 2
    qT = [st.sb([128, 1024], BF16, "qT") for _ in range(NB)]
    kT = [st.sb([128, 1024], BF16, "kT") for _ in range(NB)]
    kb = [st.sb([128, 1024], BF16, "kb") for _ in range(NB)]
    v2 = [st.sb([128, 8, 257], BF16, "v2") for _ in range(NB)]
    gk = [st.sb([128, 48], F32, "gk") for _ in range(NB)]
    hb = [st.sb([128, 2048], F32, "hb") for _ in range(NB)]
    sm = [st.sb([128, 128], BF16, "sm") for _ in range(2)]
    dd = [st.sb([128, 4], F32, "dd") for _ in range(2)]
    ps_s = [st.ps([128, 512], F32, "ps_s") for _ in range(2)]
    ps_o = [st.ps([128, 512], F32, "ps_o") for _ in range(2)]
    ps_c = [st.ps([128, 512], F32, "ps_c") for _ in range(2)]
    if bwd:
        ident = st.sb([128, 128], BF16, "ident")
        st.dma("sp", ident[:], ident_bf, wr=["ident"])
        HN = st.sb([128, 2048], F32, "HN")
        st.dma("sp", HN[:], bcast_row(hnorm_l), wr=["HN"])
        hf = st.sb([128, 2048], F32, "hf")
        og = st.sb([128, 2048], F32, "og")
        sq = st.sb([128, 2048], F32, "sq")
        yab = st.sb([128, 2048], BF16, "yab")
        ms = st.sb([128, 8], F32, "ms")
        tr = TrOut(st, ident, nb=1)
    hc = 0
    for i, t in enumerate(order):
        b = i % NB
        B = "%d" % b
        rows = slice(t * 128, (t + 1) * 128)
        st.dma("sp", qT[b][:], qT_s[t], wr=["qT" + B])
        st.dma("sp", kT[b][:], kT_s[t], wr=["kT" + B])
        st.dma("sp", kb[b][:], kb_s[rows, :], wr=["kb" + B])
        st.dma("sp", v2[b][:].rearrange("p h d -> p (h d)"), v2_s[rows, :], wr=["v2" + B])
        st.dma("sp", gk[b][:], gpk_s[rows, :], wr=["gk" + B])
        def smat(h_, j_):
            hs_ = slice(h_ * 128, (h_ + 1) * 128)
            st.op("pe", "matmul", rd=["kT" + B, "qT" + B], wr=["ps_s%d" % j_], out=ps_s[j_][:, 0:128], lhsT=kT[b][:, hs_], rhs=qT[b][:, hs_], start=True, stop=True)

        smat(0, hc % 2)
        for h in range(8):
            j = hc % 2
            J = "%d" % j
            hc += 1
            hs = slice(h * 128, (h + 1) * 128)
            if h + 1 < 8:
                smat(h + 1, hc % 2)
            st.op("dve", "tensor_tensor", rd=["ps_s" + J, "cst"], wr=["sm" + J], out=sm[j][:], in0=ps_s[j][:, 0:128], in1=MASK, op=ALU.mult)
            st.op("pe", "matmul", rd=["qT" + B, "Cb_%d" % h], wr=["ps_o" + J], out=ps_o[j][:, 0:257], lhsT=qT[b][:, hs], rhs=Cb[:, h, :], start=True, stop=False)
            st.op("pe", "matmul", rd=["sm" + J, "v2" + B], wr=["ps_o" + J], out=ps_o[j][:, 0:257], lhsT=sm[j][:], rhs=v2[b][:, h, :], start=False, stop=True)
            ebc = gk[b][:, o_ebc + h:o_ebc + h + 1]
            st.op("dve", "tensor_scalar", rd=["ps_o" + J, "gk" + B], wr=["dd" + J], out=dd[j][:, 3:4], in0=ps_o[j][:, 256:257], scalar1=ebc, scalar2=-1.0,
                  op0=ALU.mult, op1=ALU.mult)
            st.op("dve", "tensor_scalar", rd=["ps_o" + J, "gk" + B], wr=["dd" + J], out=dd[j][:, 0:1], in0=ps_o[j][:, 256:257], scalar1=ebc, scalar2=1.0,
                  op0=ALU.mult, op1=ALU.max)
            st.op("dve", "tensor_tensor", rd=["dd" + J], wr=["dd" + J], out=dd[j][:, 0:1], in0=dd[j][:, 0:1], in1=dd[j][:, 3:4], op=ALU.max)
            st.op("dve", "reciprocal", rd=["dd" + J], wr=["dd" + J], out=dd[j][:, 1:2], in_=dd[j][:, 0:1])
            st.op("dve", "tensor_tensor", rd=["dd" + J, "gk" + B], wr=["dd" + J], out=dd[j][:, 2:3], in0=dd[j][:, 1:2], in1=ebc, op=ALU.mult)
            st.op("act", "activation", rd=["ps_o" + J, "dd" + J], wr=["hb" + B], out=hb[b][:, h * 256:(h + 1) * 256], in_=ps_o[j][:, 0:256],
                  func=AF.Identity, scale=dd[j][:, 2:3])
            st.op("pe", "matmul", rd=["kb" + B, "v2" + B], wr=["ps_c" + J], out=ps_c[j][:, 0:257], lhsT=kb[b][:, hs], rhs=v2[b][:, h, :], start=True, stop=True)
            st.op("dve", "tensor_tensor", rd=["ps_c" + J, "C32_%d" % h], wr=["C32_%d" % h], out=C32[:, h, :], in0=ps_c[j][:, 0:257], in1=C32[:, h, :], op=ALU.add)
            st.op("act", "activation", rd=["C32_%d" % h, "gk" + B], wr=["Cb_%d" % h], out=Cb[:, h, :], in_=C32[:, h, :], func=AF.Identity,
                  scale=gk[b][:, o_eB + h:o_eB + h + 1])
            st.op("act", "activation", rd=["C32_%d" % h, "gk" + B], wr=["C32_%d" % h], out=C32[:, h, :], in_=C32[:, h, :], func=AF.Identity,
                  scale=gk[b][:, o_eB + h:o_eB + h + 1])
        if not bwd:
            st.dma("pool", hf_s[rows, :], hb[b][:], rd=["hb" + B])
        elif t not in skip_out:
            st.dma("sp", hf[:], hf_s[rows, :], wr=["hf"])
            st.dma("sp", og[:], p[rows, O_AO:O_AO + 2048], wr=["og"])
            st.op("dve", "tensor_tensor", rd=["hb" + B, "hf"], wr=["hf"], out=hf[:], in0=hb[b][:], in1=hf[:], op=ALU.add)
            st.op("act", "activation", rd=["hf"], wr=["sq"], out=sq[:], in_=hf[:], func=AF.Square)
            st.op("dve", "tensor_reduce", rd=["sq"], wr=["ms"], out=ms[:], in_=sq[:].rearrange("p (h d) -> p h d", h=8), axis=AX.X, op=ALU.add)
            st.op("act", "activation", rd=["ms"], wr=["ms"], out=ms[:], in_=ms[:], func=AF.Sqrt, scale=1.0 / 256, bias=EPS)
            st.op("dve", "reciprocal", rd=["ms"], wr=["ms"], out=ms[:], in_=ms[:])
            st.op("dve", "tensor_tensor", rd=["hf", "ms"], wr=["hf"], out=hf[:].rearrange("p (h d) -> p h d", h=8),
                  in0=hf[:].rearrange("p (h d) -> p h d", h=8), in1=bc3(ms[:], 256), op=ALU.mult)
            st.op("act", "activation", rd=["og"], wr=["og"], out=og[:], in_=og[:], func=AF.Sigmoid)
            st.op("dve", "tensor_tensor", rd=["hf", "HN"], wr=["hf"], out=hf[:], in0=hf[:], in1=HN[:], op=ALU.mult)
            st.op("dve", "tensor_tensor", rd=["hf", "og"], wr=["yab"], out=yab[:], in0=hf[:], in1=og[:], op=ALU.mult)
            tr.go(yab, ["yab"], yaT[t])
    st.emit()


def host_consts(n_lat_tokens):
    import ml_dtypes
    s = np.arange(128)
    MF = (s[:, None] <= s[None, :]).astype(np.float32)
    MB = (s[:, None] >= s[None, :]).astype(np.float32)
    cst = np.concatenate([MF, MB, np.ones((128, 128), np.float32), np.eye(128, dtype=np.float32)], axis=1)
    ident_bf = np.eye(128).astype(ml_dtypes.bfloat16)
    nf = 32
    inv = (10000.0 ** (-np.arange(nf, dtype=np.float32) / nf)).astype(np.float32)
    pos = np.arange(n_lat_tokens)
    ang_r = (pos // 64).astype(np.float32)[:, None] * inv
    ang_c = (pos % 64).astype(np.float32)[:, None] * inv
    cr, sr, cc_, sc_ = np.cos(ang_r), np.sin(ang_r), np.cos(ang_c), np.sin(ang_c)
    cosf = np.concatenate([cr, cr, cc_, cc_], axis=1).astype(np.float32)
    sinf = np.concatenate([-sr, -sc_, sr, sc_], axis=1).astype(np.float32)
    SC = np.float32(128.0 ** -0.5)
    return dict(cst=cst, ident_bf=ident_bf, cosq=cosf * SC, sinq=sinf * SC, cosk=cosf, sink=sinf)


def stage_cast_T(g, src, tiles, ident_bf, dstT):
    st = Stage(g, "castT")
    ident = st.sb([128, 128], BF16, "ident")
    st.dma("sp", ident[:], ident_bf, wr=["ident"])
    x = [st.sb([128, D], F32, "x") for _ in range(2)]
    xb = [st.sb([128, D], BF16, "xb") for _ in range(2)]
    tr = TrOut(st, ident, nb=2)
    for i, t in enumerate(tiles):
        b = i % 2
        B = "%d" % b
        st.dma("sp", x[b][:], src[t * 128:(t + 1) * 128, :], wr=["x" + B])
        st.op("act" if b else "dve", "copy" if b else "tensor_copy", rd=["x" + B], wr=["xb" + B], out=xb[b][:], in_=x[b][:])
        tr.go(xb[b], ["xb" + B], dstT[t])
    st.emit()


def stage_conv(g, p, tiles, NT, conv_l, ident_bf, ybT):
    st = Stage(g, "conv")
    ident = st.sb([128, 128], BF16, "ident")
    st.dma("sp", ident[:], ident_bf, wr=["ident"])
    W = [st.sb([128, D], F32, "cw") for _ in range(3)]
    for j in range(3):
        st.dma("sp", W[j][:], bcast_row(conv_l[j:j + 1, :]), wr=["W%d" % j])
    NB = 2
    names = ["c0", "x0", "bb", "cm", "xm", "cp", "xp"]
    buf = {n: [st.sb([128, D], F32, n) for _ in range(NB)] for n in names}
    yb = [st.sb([128, D], BF16, "yb") for _ in range(NB)]
    tr = TrOut(st, ident, nb=2)
    for i, t in enumerate(tiles):
        b = i % NB
        B = "%d" % b
        r0 = t * 128
        first = t in (0, 2)
        last = t in (1, NT - 1)
        T = {n: buf[n][b] for n in names}
        K = {n: n + B for n in names}
        st.dma("sp", T["c0"][:], p[r0:r0 + 128, O_BC:O_BC + D], wr=[K["c0"]])
        st.dma("sp", T["x0"][:], p[r0:r0 + 128, O_BX:O_BX + D], wr=[K["x0"]])
        st.dma("sp", T["bb"][:], p[r0:r0 + 128, O_BB:O_BB + D], wr=[K["bb"]])
        for (cn, xn, off, edge) in (("cm", "xm", -1, first), ("cp", "xp", 1, last)):
            for (n, col) in ((cn, O_BC), (xn, O_BX)):
                if not edge:
                    st.dma("sp", T[n][:], p[r0 + off:r0 + off + 128, col:col + D], wr=[K[n]])
                else:
                    st.op("pool", "memset", wr=[K[n]], ap=T[n][:], constant=0.0)
                    if off < 0:
                        st.dma("sp", T[n][1:128, :], p[r0:r0 + 127, col:col + D], wr=[K[n]])
                    else:
                        st.dma("sp", T[n][0:127, :], p[r0 + 1:r0 + 128, col:col + D], wr=[K[n]])
        tt = lambda eng, o, a, bb_, op, rd, wr: st.op(eng, "tensor_tensor", rd=rd, wr=wr, out=o, in0=a, in1=bb_, op=op)
        tt("dve", T["x0"][:], T["c0"][:], T["x0"][:], ALU.mult, [K["c0"], K["x0"]], [K["x0"]])
        tt("dve", T["xm"][:], T["cm"][:], T["xm"][:], ALU.mult, [K["cm"], K["xm"]], [K["xm"]])
        tt("dve", T["xp"][:], T["cp"][:], T["xp"][:], ALU.mult, [K["cp"], K["xp"]], [K["xp"]])
        tt("dve", T["xm"][:], T["xm"][:], W[0][:], ALU.mult, [K["xm"], "W0"], [K["xm"]])
        tt("dve", T["x0"][:], T["x0"][:], W[1][:], ALU.mult, [K["x0"], "W1"], [K["x0"]])
        tt("dve", T["xp"][:], T["xp"][:], W[2][:], ALU.mult, [K["xp"], "W2"], [K["xp"]])
        tt("dve", T["x0"][:], T["x0"][:], T["xm"][:], ALU.add, [K["x0"], K["xm"]], [K["x0"]])
        tt("dve", T["x0"][:], T["x0"][:], T["xp"][:], ALU.add, [K["x0"], K["xp"]], [K["x0"]])
        tt("dve", yb[b][:], T["x0"][:], T["bb"][:], ALU.mult, [K["x0"], K["bb"]], ["yb" + B])
        tr.go(yb[b], ["yb" + B], ybT[t])
    st.emit()


def na_plan(ROWS):
    kbs, cls, keys = [], [], {}
    for i in range(ROWS // 2):
        r = 2 * i
        kb = min(max(r - 4, 0), ROWS - 9)
        r0a = min(max(r - 4, 0), ROWS - 8)
        r0b = min(max(r + 1 - 4, 0), ROWS - 8)
        key = (r - kb, r0a - kb, r0b - kb)
        if key not in keys:
            keys[key] = len(keys)
        kbs.append(kb)
        cls.append(keys[key])
    return kbs, cls, list(keys.keys())


def na_bias_host(rpb_l, ROWS):
    _, _, keys = na_plan(ROWS)
    qrow = np.arange(128) // 64
    qc = np.arange(128) % 64
    j = np.arange(576) // 64
    kc = np.arange(576) % 64
    col0 = np.clip(qc - 8, 0, 48)
    colok = (kc[None, :] >= col0[:, None]) & (kc[None, :] < col0[:, None] + 16)
    dc = np.clip(kc[None, :] - qc[:, None] + 15, 0, 30)
    out = np.full((16, len(keys), 128, 5, 128), -30000.0, np.float32)
    for ci, (rel, a, b) in enumerate(keys):
        lo = np.where(qrow == 0, a, b)
        rowok = (j[None, :] >= lo[:, None]) & (j[None, :] < lo[:, None] + 8)
        dr = np.clip(j[None, :] - rel - qrow[:, None] + 7, 0, 14)
        ok = rowok & colok
        Tm = np.where(ok[None], rpb_l[:, dr, dc], np.float32(-30000.0))
        TT = np.full((16, 640, 128), -30000.0, np.float32)
        TT[:, :576, :] = Tm.transpose(0, 2, 1)
        out[:, ci] = TT.reshape(16, 5, 128, 128).transpose(0, 2, 1, 3)
    return out.reshape(16, len(keys), 128, 640)


def stage_na_prep(g, p, tiles, qkg_l, ident_bf, cqT_s, ckT_s, cv1_s):
    st = Stage(g, "naprep")
    ident = st.sb([128, 128], BF16, "ident")
    st.dma("sp", ident[:], ident_bf, wr=["ident"])
    GQ = st.sb([128, D], F32, "GQ")
    GK = st.sb([128, D], F32, "GK")
    for h in range(16):
        st.dma("sp", GQ[:, h * 128:(h + 1) * 128], bcast_row(qkg_l[0:1, :]), wr=["GQ"])
        st.dma("sp", GK[:, h * 128:(h + 1) * 128], bcast_row(qkg_l[1:2, :]), wr=["GK"])
    st.op("dve", "tensor_scalar", rd=["GQ"], wr=["GQ"], out=GQ[:], in0=GQ[:], scalar1=128.0 ** -0.5, scalar2=None, op0=ALU.mult)
    NB = 2
    x = {n: [st.sb([128, D], F32, n) for _ in range(NB)] for n in ("q", "k", "v")}
    sq = [st.sb([128, D], F32, "sq") for _ in range(NB)]
    ss = {n: [st.sb([128, 16], F32, "ss" + n) for _ in range(NB)] for n in ("q", "k")}
    xb = {n: [st.sb([128, D], BF16, "xb" + n) for _ in range(NB)] for n in ("q", "k")}
    v1 = [st.sb([128, 16, 129], BF16, "v1") for _ in range(NB)]
    for b in range(NB):
        st.op("pool", "memset", wr=["v1%d" % b], ap=v1[b][:], constant=1.0)
    tr = TrOut(st, ident, nb=2)
    for i, t in enumerate(tiles):
        b = i % NB
        B = "%d" % b
        rows = slice(t * 128, (t + 1) * 128)
        st.dma("sp", x["q"][b][:], p[rows, O_CQ:O_CQ + D], wr=["q" + B])
        st.dma("sp", x["k"][b][:], p[rows, O_CK:O_CK + D], wr=["k" + B])
        st.dma("sp", x["v"][b][:], p[rows, O_CV:O_CV + D], wr=["v" + B])
        for n, Gt, gk_, dst in (("q", GQ, "GQ", cqT_s), ("k", GK, "GK", ckT_s)):
            xt = x[n][b]
            s_ = ss[n][b]
            st.op("act", "activation", rd=[n + B], wr=["sq" + B], out=sq[b][:], in_=xt[:], func=AF.Square)
            st.op("dve", "tensor_reduce", rd=["sq" + B], wr=["ss" + n + B], out=s_[:], in_=sq[b][:].rearrange("p (h d) -> p h d", h=16), axis=AX.X, op=ALU.add)
            st.op("act", "activation", rd=["ss" + n + B], wr=["ss" + n + B], out=s_[:], in_=s_[:], func=AF.Sqrt, scale=1.0 / 128, bias=EPS)
            st.op("dve", "reciprocal", rd=["ss" + n + B], wr=["ss" + n + B], out=s_[:], in_=s_[:])
            st.op("dve", "tensor_tensor", rd=[n + B, "ss" + n + B], wr=[n + B], out=xt[:].rearrange("p (h d) -> p h d", h=16),
                  in0=xt[:].rearrange("p (h d) -> p h d", h=16), in1=bc3(s_[:], 128), op=ALU.mult)
            st.op("dve", "tensor_tensor", rd=[n + B, gk_], wr=["xb" + n + B], out=xb[n][b][:], in0=xt[:], in1=Gt[:], op=ALU.mult)
            tr.go(xb[n][b], ["xb" + n + B], dst[t])
        st.op("act", "copy", rd=["v" + B], wr=["v1" + B], out=v1[b][:, :, 0:128], in_=x["v"][b][:].rearrange("p (h d) -> p h d", h=16))
        st.dma("pool", cv1_s[rows, :], v1[b][:].rearrange("p h d -> p (h d)"), rd=["v1" + B])
    st.emit()


def stage_na(g, NL, need_ctx, nab_l, ident_bf, cqT_s, ckT_s, cv1_s, yc_s):
    st = Stage(g, "na")
    NT = NL + 2
    ROWS = 2 * NL
    kbs, cls, keys = na_plan(ROWS)
    NC = len(keys)
    ident = st.sb([128, 128], BF16, "ident")
    st.dma("sp", ident[:], ident_bf, wr=["ident"])
    nm8 = st.sb([128, 1], F32, "nm8")
    st.op("dve", "memset", wr=["nm8"], ap=nm8[:], constant=-8.0)
    kTh = st.sb([128, NT * 128], BF16, "kTh")
    qTh = st.sb([128, NT * 128], BF16, "qTh")
    va = st.sb([128, NT, 129], BF16, "va")
    vb = st.sb([128, NL, 129], BF16, "vb")
    bst = st.sb([128, NC, 640], F32, "bst")
    bT = st.sb([128, NC, 640], BF16, "bT")
    E = [st.sb([128, 1024], BF16, "E") for _ in range(2)]
    ob = [st.sb([128, 128], F32, "ob") for _ in range(2)]
    rr = [st.sb([128, 1], F32, "rr") for _ in range(2)]
    ps = [st.ps([128, 1024], F32, "nps") for _ in range(2)]
    po = [st.ps([128, 512], F32, "npo") for _ in range(2)]
    cv3 = cv1_s.rearrange("(t p) (h c) -> p t h c", p=128, h=16)
    cnt = 0
    for h in range(16):
        hs = slice(h * 128, (h + 1) * 128)
        st.dma("sp", kTh[:].rearrange("p (t c) -> p t c", t=NT), ckT_s[:, :, hs].rearrange("t p c -> p t c"), wr=["kTh"])
        st.dma("sp", qTh[:].rearrange("p (t c) -> p t c", t=NT), cqT_s[:, :, hs].rearrange("t p c -> p t c"), wr=["qTh"])
        st.dma("sp", va[:], cv3[:, :, h, :], wr=["va"])
        st.dma("sp", vb[:, 0:NL - 1, :], cv1_s[256 + 64:256 + 64 + (NL - 1) * 128, h * 129:(h + 1) * 129].rearrange("(t p) c -> p t c", p=128), wr=["vb"])
        st.dma("sp", vb[0:64, NL - 1, :], cv1_s[256 + 64 + (NL - 1) * 128:256 + NL * 128, h * 129:(h + 1) * 129], wr=["vb"])
        st.dma("sp", bst[:], nab_l[h].rearrange("c p x -> p c x"), wr=["bst"])
        st.op("pool", "tensor_copy", rd=["bst"], wr=["bT"], out=bT[:], in_=bst[:])
        qtiles = ([0, 1] if need_ctx else []) + list(range(2, NT))

        def qk(t, j):
            J = "%d" % j
            q_ = qTh[:, t * 128:(t + 1) * 128]
            pj = ps[j]
            if t >= 2:
                i = t - 2
                kb, c_ = kbs[i], cls[i]
                tok0 = 256 + kb * 64
                for c in range(5):
                    kn = 128 if c < 4 else 64
                    st.op("pe", "matmul", rd=["kTh", "qTh"], wr=["ps" + J], out=pj[0:kn, c * 128:(c + 1) * 128],
                          lhsT=kTh[:, tok0 + c * 128:tok0 + c * 128 + kn], rhs=q_, start=True, stop=False)
                    st.op("pe", "matmul", rd=["ident", "bT"], wr=["ps" + J], out=pj[0:kn, c * 128:(c + 1) * 128],
                          lhsT=ident[0:kn, 0:kn], rhs=bT[0:kn, c_, c * 128:(c + 1) * 128], start=False, stop=True)
            for c in range(2):
                st.op("pe", "matmul", rd=["kTh", "qTh"], wr=["ps" + J], out=pj[:, (5 + c) * 128:(6 + c) * 128],
                      lhsT=kTh[:, c * 128:(c + 1) * 128], rhs=q_, start=True, stop=True)

        def rest(t, j):
            J = "%d" % j
            pj = ps[j]
            if t >= 2:
                kb = kbs[t - 2]
                st.op("act", "activation", rd=["ps" + J, "nm8"], wr=["E" + J], out=E[j][:, 0:512], in_=pj[:, 0:512], func=AF.Exp, bias=nm8[:])
                st.op("act", "activation", rd=["ps" + J, "nm8"], wr=["E" + J], out=E[j][0:64, 512:640], in_=pj[0:64, 512:640], func=AF.Exp, bias=nm8[0:64, :])
            st.op("act", "activation", rd=["ps" + J, "nm8"], wr=["E" + J], out=E[j][:, 640:896], in_=pj[:, 640:896], func=AF.Exp, bias=nm8[:])
            chunks = []
            if t >= 2:
                for c in range(5):
                    kn = 128 if c < 4 else 64
                    if kb % 2 == 0:
                        vv = va[0:kn, 2 + kb // 2 + c, :]
                    else:
                        vv = vb[0:kn, (kb - 1) // 2 + c, :]
                    chunks.append((E[j][0:kn, c * 128:(c + 1) * 128], vv))
            for c in range(2):
                chunks.append((E[j][:, (5 + c) * 128:(6 + c) * 128], va[:, c, :]))
            for ci, (l_, r_) in enumerate(chunks):
                st.op("pe", "matmul", rd=["E" + J, "va", "vb"], wr=["po" + J], out=po[j][:, 0:129], lhsT=l_, rhs=r_,
                      start=(ci == 0), stop=(ci == len(chunks) - 1))
            st.op("dve", "reciprocal", rd=["po" + J], wr=["rr" + J], out=rr[j][:], in_=po[j][:, 128:129])
            st.op("dve", "tensor_scalar", rd=["po" + J, "rr" + J], wr=["ob" + J], out=ob[j][:], in0=po[j][:, 0:128], scalar1=rr[j][:], scalar2=None, op0=ALU.mult)
            st.dma("pool", yc_s[t * 128:(t + 1) * 128, hs], ob[j][:], rd=["ob" + J])

        qk(qtiles[0], cnt % 2)
        for n, t in enumerate(qtiles):
            if n + 1 < len(qtiles):
                qk(qtiles[n + 1], (cnt + 1) % 2)
            rest(t, cnt % 2)
            cnt += 1
    st.emit()


def stage_gate(g, p, tiles, brs, ident_bf, yT):
    st = Stage(g, "gate")
    ident = st.sb([128, 128], BF16, "ident")
    st.dma("sp", ident[:], ident_bf, wr=["ident"])
    NB = 2
    gt = [[st.sb([128, D], F32, "gt") for _ in range(3)] for _ in range(NB)]
    br = [[st.sb([128, D], F32, "br") for _ in range(3)] for _ in range(NB)]
    yb = [st.sb([128, D], BF16, "yb") for _ in range(NB)]
    tr = TrOut(st, ident, nb=2)
    for i, t in enumerate(tiles):
        b = i % NB
        B = "%d" % b
        rows = slice(t * 128, (t + 1) * 128)
        for j in range(3):
            st.dma("sp", gt[b][j][:], p[rows, O_GT + j * D:O_GT + (j + 1) * D], wr=["gt%d" % j + B])
            st.dma("sp", br[b][j][:], brs[j][rows, :], wr=["br%d" % j + B])
            st.op("act", "activation", rd=["gt%d" % j + B], wr=["gt%d" % j + B], out=gt[b][j][:], in_=gt[b][j][:], func=AF.Sigmoid)
            st.op("dve", "tensor_tensor", rd=["gt%d" % j + B, "br%d" % j + B], wr=["br%d" % j + B],
                  out=br[b][j][:], in0=br[b][j][:], in1=gt[b][j][:], op=ALU.mult)
        st.op("dve", "tensor_tensor", rd=["br0" + B, "br1" + B], wr=["br0" + B], out=br[b][0][:], in0=br[b][0][:], in1=br[b][1][:], op=ALU.add)
        st.op("dve", "tensor_tensor", rd=["br0" + B, "br2" + B], wr=["yb" + B], out=yb[b][:], in0=br[b][0][:], in1=br[b][2][:], op=ALU.add)
        tr.go(yb[b], ["yb" + B], yT[t])
    st.emit()


def stage_resid(g, xsrc, ysrc, tiles, modv, seg, dst, dst_row0=0):
    st = Stage(g, "resid")
    Gv = [st.sb([128, D], F32, "Gv") for _ in range(2)]
    for r in range(2):
        st.dma("sp", Gv[r][:], bcast_row(modv[r:r + 1, seg * D:(seg + 1) * D]), wr=["G%d" % r])
    NB = 2
    x = [st.sb([128, D], F32, "x") for _ in range(NB)]
    y = [st.sb([128, D], F32, "y") for _ in range(NB)]
    for i, t in enumerate(tiles):
        b = i % NB
        B = "%d" % b
        r = 1 if t < 2 else 0
        rows = slice(t * 128, (t + 1) * 128)
        st.dma("sp", x[b][:], xsrc[rows, :], wr=["x" + B])
        st.dma("sp", y[b][:], ysrc[rows, :], wr=["y" + B])
        st.op("dve", "tensor_tensor", rd=["y" + B, "G%d" % r], wr=["y" + B], out=y[b][:], in0=y[b][:], in1=Gv[r][:], op=ALU.mult)
        st.op("dve", "tensor_tensor", rd=["x" + B, "y" + B], wr=["y" + B], out=y[b][:], in0=y[b][:], in1=x[b][:], op=ALU.add)
        st.dma("pool", dst[t * 128 - dst_row0:(t + 1) * 128 - dst_row0, :], y[b][:], rd=["y" + B])
    st.emit()


def stage_uT(g, u_l, cst, uT_s):
    st = Stage(g, "uT")
    cs = st.sb([128, 512], F32, "cst")
    st.dma("sp", cs[:], cst, wr=["cst"])
    IDF = cs[:, 384:512]
    ub = [st.sb([128, D], F32, "ub") for _ in range(2)]
    ob = [st.sb([128, KC, 128], BF16, "uo") for _ in range(2)]
    pu = [st.ps([128, D], F32, "pu") for _ in range(2)]
    for eb in range(128):
        b = eb % 2
        B = "%d" % b
        st.dma("sp", ub[b][:], u_l[eb * 128:(eb + 1) * 128, :], wr=["ub" + B])
        for k in range(KC):
            st.op("pe", "transpose", rd=["ub" + B, "cst"], wr=["pu" + B], out=pu[b][:, k * 128:(k + 1) * 128], in_=ub[b][:, k * 128:(k + 1) * 128], identity=IDF)
        for q4 in range(4):
            st.op("act" if q4 % 2 == 0 else "dve", "copy" if q4 % 2 == 0 else "tensor_copy", rd=["pu" + B], wr=["uo%d" % q4 + B],
                  out=ob[b][:, q4 * 4:(q4 + 1) * 4, :], in_=pu[b][:, q4 * 512:(q4 + 1) * 512].rearrange("p (k e) -> p k e", k=4))
        st.dma("pool", uT_s[:, eb * 128:(eb + 1) * 128].rearrange("(k p) e -> p k e", p=128), ob[b][:], rd=["uo%d" % q4 + B for q4 in range(4)])
    st.emit()


def stage_peer_scores(g, xT, tiles, wq_l, keys_l, cst, sc_s, selp_s):
    st = Stage(g, "pscore")
    cs = st.sb([128, 512], F32, "cst")
    st.dma("sp", cs[:], cst, wr=["cst"])
    IDF = cs[:, 384:512]
    wq = st.sb([128, KC, D], BF16, "wq")
    wst = [st.sb([128, KC, 512], F32, "wst") for _ in range(2)]
    for n in range(4):
        st.dma("sp", wst[n % 2][:], wq_l[:, n * 512:(n + 1) * 512].rearrange("(k p) c -> p k c", p=128), wr=["wst%d" % (n % 2)])
        st.op("pool" if n % 2 else "dve", "tensor_copy", rd=["wst%d" % (n % 2)], wr=["wq"], out=wq[:, :, n * 512:(n + 1) * 512], in_=wst[n % 2][:])
    keysT = st.sb([128, 16, 128], BF16, "keysT")
    kst = [st.sb([128, 128], F32, "kst") for _ in range(2)]
    pq = [st.ps([128, 512], F32, "pq") for _ in range(2)]
    kl = keys_l.rearrange("h p k d -> (h p) k d")
    for blk in range(16):
        j = blk % 2
        st.dma("sp", kst[j][:], kl[blk], wr=["kst%d" % j])
        st.op("pe", "transpose", rd=["kst%d" % j, "cst"], wr=["pq%d" % j], out=pq[j][:, 0:128], in_=kst[j][:], identity=IDF)
        st.op("dve", "tensor_copy", rd=["pq%d" % j], wr=["keysT"], out=keysT[:, blk, :], in_=pq[j][:, 0:128])
    pss = st.ps([128, D], F32, "pss")
    NB = 2
    xt = [st.sb([128, D], BF16, "xt") for _ in range(NB)]
    qTb = [st.sb([128, 128], BF16, "qTb") for _ in range(2)]
    sc = [st.sb([128, D], F32, "sc") for _ in range(NB)]
    wk = st.sb([128, 256], F32, "wk")
    t16 = st.sb([128, 16, 16], F32, "t16")
    cand = st.sb([128, 16, 16], F32, "cand")
    ex = st.sb([128, 256], F32, "ex")
    junk = st.sb([128, 256], F32, "junk")
    c8 = st.sb([128, 16], F32, "c8")
    sm_ = st.sb([128, 32], F32, "small")
    selp = [st.sb([128, 16], F32, "selp") for _ in range(NB)]
    qc = 0
    for i, t in enumerate(tiles):
        b = i % NB
        B = "%d" % b
        rows = slice(t * 128, (t + 1) * 128)
        st.dma("sp", xt[b][:], xT[t], wr=["xt" + B])
        for blk in range(16):
            j = qc % 2
            J = "%d" % j
            qc += 1
            for k in range(KC):
                st.op("pe", "matmul", rd=["wq", "xt" + B], wr=["pq" + J], out=pq[j][:, 0:128], lhsT=wq[:, k, blk * 128:(blk + 1) * 128],
                      rhs=xt[b][:, k * 128:(k + 1) * 128], start=(k == 0), stop=(k == KC - 1))
            if j == 0:
                st.op("act", "copy", rd=["pq" + J], wr=["qTb" + J], out=qTb[j][:], in_=pq[j][:, 0:128])
            else:
                st.op("dve", "tensor_copy", rd=["pq" + J], wr=["qTb" + J], out=qTb[j][:], in_=pq[j][:, 0:128])
            st.op("pe", "matmul", rd=["qTb" + J, "keysT"], wr=["pss"], out=pss[:, blk * 128:(blk + 1) * 128], lhsT=qTb[j][:], rhs=keysT[:, blk, :], start=True, stop=True)
        for q4 in range(4):
            st.op("act" if q4 % 2 == 0 else "dve", "copy" if q4 % 2 == 0 else "tensor_copy", rd=["pss"], wr=["sc%d" % q4 + B],
                  out=sc[b][:, q4 * 512:(q4 + 1) * 512], in_=pss[:, q4 * 512:(q4 + 1) * 512])
        SK = ["sc%d" % q4 + B for q4 in range(4)]
        st.dma("pool", sc_s[rows, :], sc[b][:], rd=SK)
        for blk in range(16):
            sl = sc[b][:, blk * 128:(blk + 1) * 128]
            st.op("dve", "max", rd=SK, wr=["t16"], out=t16[:, blk, 0:8], in_=sl)
            st.op("dve", "match_replace", rd=SK + ["t16"], wr=["wk"], out=wk[:, 0:128], in_to_replace=t16[:, blk, 0:8], in_values=sl, imm_value=-1e30)
            st.op("dve", "max", rd=["wk"], wr=["t16"], out=t16[:, blk, 8:16], in_=wk[:, 0:128])
        for h in range(8):
            st.op("dve", "tensor_tensor", rd=["t16"], wr=["cand"], out=cand[:], in0=bc3(t16[:, 2 * h, :], 16), in1=bcmid(t16[:, 2 * h + 1, :], 16), op=ALU.add)
            cf = cand[:].rearrange("p a b -> p (a b)")
            st.op("dve", "max", rd=["cand"], wr=["c8"], out=c8[:, 0:8], in_=cf)
            st.op("dve", "match_replace", rd=["cand", "c8"], wr=["wk"], out=wk[:], in_to_replace=c8[:, 0:8], in_values=cf, imm_value=-1e30)
            st.op("dve", "max", rd=["wk"], wr=["c8"], out=c8[:, 8:16], in_=wk[:])
            st.op("dve", "tensor_scalar", rd=["c8"], wr=["small"], out=sm_[:, 0:1], in0=c8[:, 0:1], scalar1=-1.0, scalar2=None, op0=ALU.mult)
            st.op("act", "activation", rd=["cand", "small"], wr=["ex"], out=ex[:], in_=cf, func=AF.Exp, bias=sm_[:, 0:1])
            st.op("dve", "scalar_tensor_tensor", rd=["cand", "c8", "ex"], wr=["junk", "small"], out=junk[:], in0=cf, scalar=c8[:, 15:16], in1=ex[:],
                  op0=ALU.is_ge, op1=ALU.mult, accum_out=sm_[:, 1:2])
            st.op("act", "activation", rd=["small"], wr=["small"], out=sm_[:, 2:3], in_=sm_[:, 1:2], func=AF.Ln)
            st.op("dve", "tensor_copy", rd=["c8"], wr=["selp" + B], out=selp[b][:, 2 * h:2 * h + 1], in_=c8[:, 15:16])
            st.op("dve", "tensor_tensor", rd=["small"], wr=["selp" + B], out=selp[b][:, 2 * h + 1:2 * h + 2], in0=sm_[:, 0:1], in1=sm_[:, 2:3], op=ALU.subtract)
        st.dma("pool", selp_s[rows, :], selp[b][:], rd=["selp" + B])
    st.emit()


def stage_peer_experts(g, xT, tiles, uT_s, v_l, ident_bf, sc_s, selp_s, po_s, NG=16):
    st = Stage(g, "pexp")
    ident = st.sb([128, 128], BF16, "ident")
    st.dma("sp", ident[:], ident_bf, wr=["ident"])
    uTg = st.sb([128, KC, 1024], BF16, "uTg")
    vg = st.sb([128, 8, D], BF16, "vg")
    vst = [st.sb([128, D], F32, "vst") for _ in range(2)]
    NB = 2
    xt = [st.sb([128, D], BF16, "xt") for _ in range(NB)]
    sc = [st.sb([128, D], F32, "sc") for _ in range(NB)]
    selp = [st.sb([128, 16], F32, "selp") for _ in range(NB)]
    smp = [st.sb([128, 2, 1024], F32, "smp") for _ in range(2)]
    ex = [st.sb([128, 1024], F32, "ex") for _ in range(2)]
    mk = [[st.sb([128, 1024], BF16, "mk") for _ in range(8)] for _ in range(NB)]
    gl = [st.sb([128, 1024], F32, "gl") for _ in range(NB)]
    WT = [st.sb([128, 1024], BF16, "WT") for _ in range(NB)]
    ob = [st.sb([128, D], F32, "ob") for _ in range(2)]
    ps_h = st.ps([128, 1024], F32, "ps_h")
    ps_g = st.ps([128, 1024], F32, "ps_g")
    ps_o = [st.ps([128, 512], F32, "ps_o") for _ in range(4)]
    hc = [0]
    iters = [(gi, t) for gi in range(NG) for t in tiles]
    NI = len(iters)

    def loads(n):
        gi, t = iters[n]
        B = "%d" % (n % NB)
        b = n % NB
        rows = slice(t * 128, (t + 1) * 128)
        if t == tiles[0]:
            st.dma("sp", uTg[:], uT_s[:, gi * 1024:(gi + 1) * 1024].rearrange("(k p) e -> p k e", p=128), wr=["uTg"])
        st.dma("sp", xt[b][:], xT[t], wr=["xt" + B])
        st.dma("sp", sc[b][:], sc_s[rows, :], wr=["sc" + B])
        st.dma("sp", selp[b][:], selp_s[rows, :], wr=["selp" + B])

    def heads(n, h0, h1):
        gi, t = iters[n]
        b = n % NB
        B = "%d" % b
        sc4 = sc[b][:].rearrange("p (h two k) -> p h two k", h=8, two=2)

        def addpair(hp, j):
            s1 = sc4[:, 2 * hp:2 * hp + 2, 0, gi * 8:gi * 8 + 8]
            s2 = sc4[:, 2 * hp:2 * hp + 2, 1, :]
            out4 = smp[j][:].rearrange("p q (a c) -> p q a c", a=8)
            in0 = s1.unsqueeze(3).broadcast_to([128, 2, 8, 128])
            in1 = s2.unsqueeze(2).broadcast_to([128, 2, 8, 128])
            st.op("dve", "tensor_tensor", rd=["sc" + B], wr=["smp%d" % j], out=out4, in0=in0, in1=in1, op=ALU.add)

        hp0, hp1 = h0 // 2, h1 // 2
        addpair(hp0, hc[0] % 2)
        for hp in range(hp0, hp1):
            j = hc[0] % 2
            hc[0] += 1
            if hp + 1 < hp1:
                addpair(hp + 1, hc[0] % 2)
            for q in range(2):
                h = 2 * hp + q
                e_ = ex[q]
                st.op("act", "activation", rd=["smp%d" % j, "selp" + B], wr=["ex%d" % q], out=e_[:], in_=smp[j][:, q, :], func=AF.Exp, bias=selp[b][:, 2 * h + 1:2 * h + 2])
                st.op("dve", "scalar_tensor_tensor", rd=["smp%d" % j, "ex%d" % q, "selp" + B], wr=["mk%d" % h + B], out=mk[b][h][:], in0=smp[j][:, q, :],
                      scalar=selp[b][:, 2 * h:2 * h + 1], in1=e_[:], op0=ALU.is_ge, op1=ALU.mult)

    def hT(n):
        b = n % NB
        B = "%d" % b
        for c in range(8):
            for k in range(KC):
                st.op("pe", "matmul", rd=["uTg", "xt" + B], wr=["ps_h"], out=ps_h[:, c * 128:(c + 1) * 128], lhsT=uTg[:, k, c * 128:(c + 1) * 128],
                      rhs=xt[b][:, k * 128:(k + 1) * 128], start=(k == 0), stop=(k == KC - 1))

    def gelu(n):
        b = n % NB
        B = "%d" % b
        for q2 in range(2):
            st.op("act", "activation", rd=["ps_h"], wr=["gl%d" % q2 + B], out=gl[b][:, q2 * 512:(q2 + 1) * 512], in_=ps_h[:, q2 * 512:(q2 + 1) * 512], func=AF.Gelu)

    def vload(n):
        gi, t = iters[n]
        if t == tiles[0]:
            for c in range(8):
                j = c % 2
                st.dma("sp", vst[j][:], v_l[gi * 1024 + c * 128:gi * 1024 + (c + 1) * 128, :], wr=["vst%d" % j])
                st.op("act", "copy", rd=["vst%d" % j], wr=["vg"], out=vg[:, c, :], in_=vst[j][:])

    def mask(n):
        b = n % NB
        B = "%d" % b
        for c in range(8):
            for h in range(8):
                st.op("pe", "matmul", rd=["mk%d" % h + B, "ident"], wr=["ps_g"], out=ps_g[:, c * 128:(c + 1) * 128], lhsT=mk[b][h][:, c * 128:(c + 1) * 128],
                      rhs=ident[:], start=(h == 0), stop=(h == 7))

    def wt(n):
        b = n % NB
        B = "%d" % b
        for q2 in range(2):
            st.op("dve", "tensor_tensor", rd=["ps_g", "gl%d" % q2 + B], wr=["WT" + B], out=WT[b][:, q2 * 512:(q2 + 1) * 512], in0=ps_g[:, q2 * 512:(q2 + 1) * 512],
                  in1=gl[b][:, q2 * 512:(q2 + 1) * 512], op=ALU.mult)

    def outmm(n):
        b = n % NB
        B = "%d" % b
        for cb in range(4):
            for c in range(8):
                st.op("pe", "matmul", rd=["WT" + B, "vg"], wr=["ps_o%d" % cb], out=ps_o[cb][:], lhsT=WT[b][:, c * 128:(c + 1) * 128], rhs=vg[:, c, cb * 512:(cb + 1) * 512],
                      start=(c == 0), stop=(c == 7))

    def evac_store(n):
        gi, t = iters[n]
        b = n % NB
        rows = slice(t * 128, (t + 1) * 128)
        o = ob[b]
        for cb in range(4):
            st.op("act", "copy", rd=["ps_o%d" % cb], wr=["ob%d_%d" % (b, cb)], out=o[:, cb * 512:(cb + 1) * 512], in_=ps_o[cb][:])
        okeys = ["ob%d_%d" % (b, cb) for cb in range(4)]
        if gi == 0:
            st.dma("pool", po_s[rows, :], o[:], rd=okeys, wr=[("po", t)])
        else:
            st.dma("pool", po_s[rows, :], o[:], rd=okeys, wr=[("po", t)], accum_op=ALU.add)

    loads(0)
    heads(0, 0, 8)
    hT(0)
    gelu(0)
    for n in range(NI):
        nx = n + 1 < NI
        if nx:
            loads(n + 1)
        vload(n)
        mask(n)
        if nx:
            heads(n + 1, 0, 4)
        wt(n)
        if nx:
            hT(n + 1)
        outmm(n)
        if nx:
            heads(n + 1, 4, 8)
            gelu(n + 1)
        evac_store(n)
    st.emit()


def build_program(NL, depth=2):
    NT = NL + 2
    ROWS = 2 * NL
    NCLS = len(na_plan(ROWS)[2])
    nc = bass.Bass("TRN2", target_bir_lowering=False)

    def inp(name, shape, dt=F32):
        return nc.dram_tensor(name, list(shape), dt, kind="ExternalInput").ap()

    xcat = inp("xcat", [NT * 128, D])
    cc = inp("cc", [2, D])
    w_ada = inp("w_ada", [depth, D, 6 * D])
    b_ada = inp("b_ada", [depth, 6 * D])
    norm_g = inp("norm_g", [depth, 2 * D])
    w_in = inp("w_in", [depth, D, PW])
    gate_b = inp("a_gate_b", [depth, 32])
    hnorm = inp("a_hnorm_g", [depth, D])
    b_conv = inp("b_conv", [depth, 3, D])
    qkg = inp("c_qk_g", [depth, 2, 128])
    nab = inp("nab", [depth, 16, NCLS, 128, 640])
    w_a = inp("w_a_out", [depth, D, D])
    w_b = inp("w_b_out", [depth, D, D])
    w_c = inp("w_c_out", [depth, D, D])
    w_o = inp("w_out", [depth, D, D])
    wq = inp("peer_wq", [depth, D, D])
    pkeys = inp("peer_keys", [depth, 8, 2, 128, 128])
    pu = inp("peer_u", [depth, 16384, D])
    pv = inp("peer_v", [depth, 16384, D])
    cst = inp("cst", [128, 512])
    ident = inp("ident_bf", [128, 128], BF16)
    rope = [inp(n, [NL * 128, 128]) for n in ("cosq", "sinq", "cosk", "sink")]
    out = nc.dram_tensor("out", [NL * 128, D], F32, kind="ExternalOutput").ap()

    with ExitStack() as es:
        es.enter_context(nc.allow_low_precision("bf16 matmul operands, fp32 accumulation"))
        g = G(nc, es)
        R = NT * 128

        def dr(name, shape, dt=F32):
            return g.dram(name, shape, dt).ap()

        modv = dr("modv", [2, 6 * D])
        xnT = dr("xnT", [NT, 128, D], BF16)
        pparts = [dr("p%d" % gi, [R, PSplit.BOUNDS[gi + 1] - PSplit.BOUNDS[gi]]) for gi in range(4)]
        p = PSplit(pparts)
        qT_s = dr("qT_s", [NT, 128, 1024], BF16)
        kT_s = dr("kT_s", [NT, 128, 1024], BF16)
        kb_s = dr("kb_s", [R, 1024], BF16)
        v2f_s = dr("v2f_s", [R, 8 * 257], BF16)
        v2b_s = dr("v2b_s", [R, 8 * 257], BF16)
        gpk_s = dr("gpk_s", [R, 48])
        hf_s = dr("hf_s", [R, D])
        yaT = dr("yaT", [NT, 128, D], BF16)
        ybT = dr("ybT", [NT, 128, D], BF16)
        ycT = dr("ycT", [NT, 128, D], BF16)
        yT = dr("yT", [NT, 128, D], BF16)
        cqT_s = dr("cqT_s", [NT, 128, D], BF16)
        ckT_s = dr("ckT_s", [NT, 128, D], BF16)
        cv1_s = dr("cv1_s", [R, 16 * 129], BF16)
        yc_s = dr("yc_s", [R, D])
        brs = [dr("br%d" % j, [R, D]) for j in range(3)]
        mix = dr("mix", [R, D])
        x1 = dr("x1", [R, D])
        xmid = dr("xmid", [R, D])
        uT_s = dr("uT_s", [D, 16384], BF16)
        sc_s = dr("sc_s", [R, D])
        selp_s = dr("selp_s", [R, 16])
        po_s = dr("po_s", [R, D])

        allt = list(range(NT))
        lat = list(range(2, NT))
        for l in range(depth):
            last = l == depth - 1
            xin = xcat if l == 0 else xmid
            T = lat if last else allt
            stage_mod(g, cc, w_ada[l], b_ada[l:l + 1, :], norm_g[l:l + 1, :], modv)
            stage_norm(g, xin, modv, 0, xnT, allt, ident)
            for gi in range(4):
                lo, hi = PSplit.BOUNDS[gi], PSplit.BOUNDS[gi + 1]
                stage_linear(g, xnT, allt, w_in[l][:, lo:hi], hi - lo, pparts[gi])
            stage_mlstm_prep(g, p, allt, cst, ident, gate_b[l:l + 1, :], rope, qT_s, kT_s, kb_s, v2f_s, v2b_s, gpk_s)
            stage_mlstm_scan(g, False, [0, 1] + lat, cst, ident, qT_s, kT_s, kb_s, v2f_s, gpk_s, hf_s)
            stage_mlstm_scan(g, True, [1, 0] + lat[::-1], cst, ident, qT_s, kT_s, kb_s, v2b_s, gpk_s, hf_s, p=p, hnorm_l=hnorm[l:l + 1, :], yaT=yaT,
                             skip_out=((0, 1) if last else ()))
            stage_conv(g, p, T, NT, b_conv[l], ident, ybT)
            stage_na_prep(g, p, allt, qkg[l], ident, cqT_s, ckT_s, cv1_s)
            stage_na(g, NL, not last, nab[l], ident, cqT_s, ckT_s, cv1_s, yc_s)
            stage_cast_T(g, yc_s, T, ident, ycT)
            stage_linear(g, yaT, T, w_a[l], D, brs[0])
            stage_linear(g, ybT, T, w_b[l], D, brs[1])
            stage_linear(g, ycT, T, w_c[l], D, brs[2])
            stage_gate(g, p, T, brs, ident, yT)
            stage_linear(g, yT, T, w_o[l], D, mix)
            stage_resid(g, xin, mix, T, modv, 2, x1)
            stage_norm(g, x1, modv, 1, xnT, T, ident)
            stage_uT(g, pu[l], cst, uT_s)
            stage_peer_scores(g, xnT, T, wq[l], pkeys[l], cst, sc_s, selp_s)
            stage_peer_experts(g, xnT, T, uT_s, pv[l], ident, sc_s, selp_s, po_s)
            if last:
                stage_resid(g, x1, po_s, T, modv, 5, out, dst_row0=256)
            else:
                stage_resid(g, x1, po_s, T, modv, 5, xmid)
        final_wait(g)
    return nc


_PROG = {}


def kernel(x, c, ctx, c_ctx, w_ada, b_ada, norm_g, w_in, a_gate_b, a_hnorm_g, b_conv, c_qk_g, c_rpb,
           w_a_out, w_b_out, w_c_out, w_out, peer_wq, peer_keys, peer_u, peer_v):
    f = lambda a: np.ascontiguousarray(np.asarray(a, dtype=np.float32))
    x, ctx = f(x), f(ctx)
    B, S, _ = x.shape
    NL = S // 128
    depth = w_ada.shape[0]
    hc = host_consts(S)
    nab = np.stack([na_bias_host(f(c_rpb)[l], 2 * NL) for l in range(depth)], 0)
    shared = dict(w_ada=f(w_ada), b_ada=f(b_ada), norm_g=f(norm_g).reshape(depth, 2 * D), w_in=f(w_in),
                  a_gate_b=f(a_gate_b).reshape(depth, 32), a_hnorm_g=f(a_hnorm_g), b_conv=f(b_conv), c_qk_g=f(c_qk_g), nab=nab,
                  w_a_out=f(w_a_out), w_b_out=f(w_b_out), w_c_out=f(w_c_out), w_out=f(w_out), peer_wq=f(peer_wq),
                  peer_keys=f(peer_keys), peer_u=f(peer_u), peer_v=f(peer_v), **hc)
    in_maps = []
    for b in range(B):
        m = dict(shared)
        m["xcat"] = np.concatenate([ctx[b], x[b]], axis=0)
        m["cc"] = np.stack([f(c)[b], f(c_ctx)], axis=0)
        in_maps.append(m)
    key = (NL, depth)
    if key not in _PROG:
        _PROG[key] = build_program(NL, depth)
    res = run_bass_kernel_spmd(_PROG[key], in_maps, core_ids=list(range(B)))
    return np.stack([res.results[b]["out"] for b in range(B)], axis=0)
```
